# Optimizing a Trainium2 kernel written in Bass

```python
import math
import jax
import jax.numpy as jnp
from jax import lax
import numpy as np

D_MODEL = 1024
BATCH = 2
SEQ = 16384
DEPTH = 2

MLA_HEADS = 8
MLA_NOPE = 64
MLA_ROPE = 32
MLA_V = 64
MLA_Q_RANK = 256
MLA_KV_RANK = 128
ROPE_THETA = 10000.0
Q_BLOCK = 128
GLA_HEADS = 4
GLA_DK = 64
GLA_DV = 128
GLA_GATE_RANK = 16
GLA_TAU = 16.0
GDN_HEADS = 4
GDN_DK = 128
GDN_DV = 128
GDN_CONV = 4
RWKV_HEADS = 8
RWKV_N = 64
RWKV_W_RANK = 64
RWKV_A_RANK = 64
RWKV_V_RANK = 32
RWKV_G_RANK = 128
RWKV_LN_EPS = 64e-5
CHUNK = 64
N_BRANCH = 4
BRANCH_WIDTH = 512
N_EXPERTS = 16
N_GROUPS = 4
TOP_K = 2
D_EXPERT = 512
DEEPNORM_ALPHA = (2.0 * DEPTH) ** 0.25
DEEPNORM_BETA = (8.0 * DEPTH) ** -0.25
LN_EPS = 1e-5
NORM_EPS = 1e-6

RWKV_WIDTH = RWKV_HEADS * RWKV_N
RWKV_FEAT = 3 * RWKV_WIDTH + RWKV_W_RANK + RWKV_A_RANK + RWKV_G_RANK
IN_WIDTHS = (
    MLA_Q_RANK, MLA_KV_RANK + MLA_ROPE,
    GLA_HEADS * GLA_DK, GLA_HEADS * GLA_DK, GLA_HEADS * GLA_DV,
    GLA_GATE_RANK, GLA_HEADS * GLA_DV,
    GDN_HEADS * (2 * GDN_DK + GDN_DV), GDN_HEADS, GDN_HEADS, GDN_HEADS * GDN_DV,
    RWKV_FEAT,
    N_BRANCH * D_MODEL,
)
IN_COLS = sum(IN_WIDTHS)

kernel_name = 'hybrid_mla_gla_gdn_rwkv7_grouped_moe_deepnorm'


def _split(t, widths):
    return jnp.split(t, np.cumsum(widths)[:-1].tolist(), axis=-1)


def _rmsnorm(x, g, eps=NORM_EPS):
    xf = x.astype(jnp.float32)
    y = xf * lax.rsqrt(jnp.mean(xf * xf, axis=-1, keepdims=True) + eps)
    return (y * g.astype(jnp.float32)).astype(x.dtype)


def _layernorm(x, g, b, eps=LN_EPS):
    xf = x.astype(jnp.float32)
    mu = jnp.mean(xf, axis=-1, keepdims=True)
    var = jnp.mean(jnp.square(xf - mu), axis=-1, keepdims=True)
    y = (xf - mu) * lax.rsqrt(var + eps)
    return (y * g.astype(jnp.float32) + b.astype(jnp.float32)).astype(x.dtype)


def _l2norm(x, eps=NORM_EPS):
    xf = x.astype(jnp.float32)
    return xf * lax.rsqrt(jnp.sum(xf * xf, axis=-1, keepdims=True) + eps)


def _rope(x, positions):
    half = x.shape[-1] // 2
    inv_freq = ROPE_THETA ** (-jnp.arange(half, dtype=jnp.float32) / half)
    ang = positions.astype(jnp.float32)[:, :, None] * inv_freq
    cos = jnp.cos(ang)[:, :, None, :]
    sin = jnp.sin(ang)[:, :, None, :]
    xf = x.astype(jnp.float32)
    x1, x2 = xf[..., :half], xf[..., half:]
    return jnp.concatenate([x1 * cos - x2 * sin, x1 * sin + x2 * cos], axis=-1).astype(x.dtype)


def _chunks(t):
    B, S, H, d = t.shape
    return t.reshape(B, S // CHUNK, CHUNK, H, d).transpose(1, 0, 3, 2, 4)


def _unchunk(t):
    nc, B, H, C, d = t.shape
    return t.transpose(1, 0, 3, 2, 4).reshape(B, nc * C, H, d)


def _causal_conv(x, w):
    K, C = w.shape
    return lax.conv_general_dilated(x, w[:, None, :].astype(x.dtype), window_strides=(1,),
                                    padding=((K - 1, 0),), dimension_numbers=('NWC', 'WIO', 'NWC'),
                                    feature_group_count=C)


def _token_shift_mix(p, mu):
    prev = jnp.pad(p, ((0, 0), (1, 0), (0, 0)))[:, :-1]
    return p + (prev - p) * mu


def _causal_attention(q, k, v, scale):
    B, S, H, Dh = q.shape
    nb = S // Q_BLOCK
    qb = q.reshape(B, nb, Q_BLOCK, H, Dh).transpose(1, 0, 2, 3, 4)
    key_pos = jnp.arange(S)

    def one_block(args):
        q_i, idx = args
        s = jnp.einsum('bqhd,bkhd->bhqk', q_i, k, preferred_element_type=jnp.float32) * scale
        q_pos = idx * Q_BLOCK + jnp.arange(Q_BLOCK)
        s = jnp.where(key_pos[None, :] <= q_pos[:, None], s, -jnp.inf)
        p = jax.nn.softmax(s, axis=-1).astype(v.dtype)
        return jnp.einsum('bhqk,bkhd->bqhd', p, v)

    out = lax.map(one_block, (qb, jnp.arange(nb)))
    return out.transpose(1, 0, 2, 3, 4).reshape(B, S, H, v.shape[-1])


def _mla_branch(q_lat, kv_lat, positions, q_norm, w_uq, kv_norm, w_ukv):
    B, S, _ = q_lat.shape
    q = (_rmsnorm(q_lat, q_norm) @ w_uq).reshape(B, S, MLA_HEADS, MLA_NOPE + MLA_ROPE)
    c_kv, k_rope = kv_lat[..., :MLA_KV_RANK], kv_lat[..., MLA_KV_RANK:]
    kv = (_rmsnorm(c_kv, kv_norm) @ w_ukv).reshape(B, S, MLA_HEADS, MLA_NOPE + MLA_V)
    k_nope, v = kv[..., :MLA_NOPE], kv[..., MLA_NOPE:]
    q = jnp.concatenate([q[..., :MLA_NOPE], _rope(q[..., MLA_NOPE:], positions)], axis=-1)
    k_rope = jnp.broadcast_to(_rope(k_rope[:, :, None, :], positions), (B, S, MLA_HEADS, MLA_ROPE))
    k = jnp.concatenate([k_nope, k_rope], axis=-1)
    o = _causal_attention(q, k, v, (MLA_NOPE + MLA_ROPE) ** -0.5)
    return o.reshape(B, S, MLA_HEADS * MLA_V)


def _gla_branch(q, k, v, gate_lat, out_gate, w_gate_up, b_gate, norm_g):
    B, S, _ = q.shape
    f32 = jnp.float32
    q = q.reshape(B, S, GLA_HEADS, GLA_DK).astype(f32) * GLA_DK ** -0.5
    k = k.reshape(B, S, GLA_HEADS, GLA_DK).astype(f32)
    v = v.reshape(B, S, GLA_HEADS, GLA_DV).astype(f32)
    log_a = jax.nn.log_sigmoid((gate_lat @ w_gate_up + b_gate).astype(f32)) / GLA_TAU
    log_a = log_a.reshape(B, S, GLA_HEADS, GLA_DK)
    qc, kc, vc, gc = _chunks(q), _chunks(k), _chunks(v), _chunks(log_a)
    b = jnp.cumsum(gc, axis=3)
    b_last = b[:, :, :, -1:, :]
    q_dec = qc * jnp.exp(b)
    k_inv = kc * jnp.exp(-b)
    k_end = kc * jnp.exp(b_last - b)
    causal = jnp.tril(jnp.ones((CHUNK, CHUNK), dtype=bool))
    att = jnp.where(causal, jnp.einsum('nbhid,nbhjd->nbhij', q_dec, k_inv), 0.0)
    o_intra = jnp.einsum('nbhij,nbhjv->nbhiv', att, vc)

    def step(state, xs):
        q_n, k_n, v_n, a_n = xs
        o = jnp.einsum('bhid,bhdv->bhiv', q_n, state)
        state = state * a_n[:, :, 0, :, None] + jnp.einsum('bhjd,bhjv->bhdv', k_n, v_n)
        return state, o

    s0 = jnp.zeros((B, GLA_HEADS, GLA_DK, GLA_DV), f32)
    _, o_inter = lax.scan(step, s0, (q_dec, k_end, vc, jnp.exp(b_last)))
    o = _unchunk(o_intra + o_inter)
    o = _rmsnorm(o, norm_g) * jax.nn.silu(out_gate.astype(f32)).reshape(B, S, GLA_HEADS, GLA_DV)
    return o.reshape(B, S, GLA_HEADS * GLA_DV).astype(out_gate.dtype)


def _gdn_branch(qkv, beta_logit, a_logit, z, conv_w, a_log, dt_bias, norm_g):
    B, S, _ = qkv.shape
    f32 = jnp.float32
    qkv = jax.nn.silu(_causal_conv(qkv, conv_w)).astype(f32)
    q, k, v = _split(qkv, (GDN_HEADS * GDN_DK, GDN_HEADS * GDN_DK, GDN_HEADS * GDN_DV))
    q = _l2norm(q.reshape(B, S, GDN_HEADS, GDN_DK)) * GDN_DK ** -0.5
    k = _l2norm(k.reshape(B, S, GDN_HEADS, GDN_DK))
    v = v.reshape(B, S, GDN_HEADS, GDN_DV)
    beta = jax.nn.sigmoid(beta_logit.astype(f32))
    g = -jnp.exp(a_log.astype(f32)) * jax.nn.softplus(a_logit.astype(f32) + dt_bias)
    qc, kc, vc = _chunks(q), _chunks(k), _chunks(v)
    bc = _chunks(beta[..., None])[..., 0]
    gc = jnp.cumsum(_chunks(g[..., None])[..., 0], axis=-1)
    causal = jnp.tril(jnp.ones((CHUNK, CHUNK), dtype=bool))
    strict = jnp.tril(jnp.ones((CHUNK, CHUNK), dtype=bool), k=-1)
    decay = jnp.exp(jnp.where(causal, gc[..., :, None] - gc[..., None, :], -jnp.inf))
    k_beta = kc * bc[..., None]
    v_beta = vc * bc[..., None]
    l_mat = jnp.where(strict, jnp.einsum('nbhid,nbhjd->nbhij', k_beta, kc) * decay, 0.0)
    rhs = jnp.concatenate([v_beta, k_beta * jnp.exp(gc)[..., None]], axis=-1)
    sol = lax.linalg.triangular_solve(l_mat + jnp.eye(CHUNK, dtype=f32), rhs, left_side=True,
                                      lower=True, unit_diagonal=True)
    u, w = sol[..., :GDN_DV], sol[..., GDN_DV:]
    att = jnp.einsum('nbhid,nbhjd->nbhij', qc, kc) * decay
    q_dec = qc * jnp.exp(gc)[..., None]
    g_last = gc[..., -1:]
    k_end = kc * jnp.exp(g_last - gc)[..., None]

    def step(state, xs):
        u_n, w_n, att_n, q_n, k_n, gl_n = xs
        v_new = u_n - jnp.einsum('bhik,bhkv->bhiv', w_n, state)
        o = jnp.einsum('bhik,bhkv->bhiv', q_n, state) + jnp.einsum('bhij,bhjv->bhiv', att_n, v_new)
        state = state * jnp.exp(gl_n)[..., None] + jnp.einsum('bhjk,bhjv->bhkv', k_n, v_new)
        return state, o

    s0 = jnp.zeros((B, GDN_HEADS, GDN_DK, GDN_DV), f32)
    _, o = lax.scan(step, s0, (u, w, att, q_dec, k_end, g_last))
    o = _unchunk(o)
    o = _rmsnorm(o, norm_g) * jax.nn.silu(z.astype(f32)).reshape(B, S, GDN_HEADS, GDN_DV)
    return o.reshape(B, S, GDN_HEADS * GDN_DV).astype(z.dtype)


def _rwkv7_branch(feat, mu, w0, w_up, a0, a_up, g_up, k_k, k_a, r_k, ln_g, ln_b, v_first, v_gate):
    B, S, _ = feat.shape
    f32 = jnp.float32
    feat = _token_shift_mix(feat, mu)
    r, k, v, w_lat, a_lat, g_lat = _split(feat, (RWKV_WIDTH, RWKV_WIDTH, RWKV_WIDTH,
                                                 RWKV_W_RANK, RWKV_A_RANK, RWKV_G_RANK))
    w = -jax.nn.softplus(-(w0 + jnp.tanh(w_lat) @ w_up).astype(f32)) - 0.5
    decay = jnp.exp(-jnp.exp(w))
    a = jax.nn.sigmoid(a0 + a_lat @ a_up)
    g = jax.nn.sigmoid(g_lat) @ g_up
    if v_first is not None:
        v = v + (v_first - v) * v_gate
    kk = _l2norm((k * k_k).reshape(B, S, RWKV_HEADS, RWKV_N))
    k = k * (1.0 + (a - 1.0) * k_a)

    def heads(t):
        return t.reshape(B, S, RWKV_HEADS, RWKV_N).astype(f32)

    r_h, k_h, v_h, a_h, w_h = heads(r), heads(k), heads(v), heads(a), heads(decay)
    b_h = kk * a_h

    def step(state, xs):
        r_t, w_t, k_t, v_t, kk_t, b_t = xs
        sa = jnp.einsum('bhij,bhj->bhi', state, -kk_t)
        state = (state * w_t[:, :, None, :] + sa[..., None] * b_t[:, :, None, :]
                 + v_t[..., None] * k_t[:, :, None, :])
        return state, jnp.einsum('bhij,bhj->bhi', state, r_t)

    def time_major(t):
        return jnp.moveaxis(t, 1, 0)

    s0 = jnp.zeros((B, RWKV_HEADS, RWKV_N, RWKV_N), f32)
    _, y = lax.scan(step, s0, (time_major(r_h), time_major(w_h), time_major(k_h),
                               time_major(v_h), time_major(kk), time_major(b_h)))
    y = jnp.moveaxis(y, 0, 1)
    y = _layernorm(y, ln_g.reshape(RWKV_HEADS, RWKV_N), ln_b.reshape(RWKV_HEADS, RWKV_N), eps=RWKV_LN_EPS)
    bonus = jnp.sum(r_h * k_h * r_k, axis=-1, keepdims=True) * v_h
    out = (y + bonus).reshape(B, S, RWKV_WIDTH) * g.astype(f32)
    return out.astype(feat.dtype), v


def _merge_branches(branches, gate_logits, w_branch, w_out):
    B, S, _ = gate_logits.shape
    gates = jax.nn.sigmoid(gate_logits.astype(jnp.float32)).astype(gate_logits.dtype)
    gates = gates.reshape(B, S, N_BRANCH, D_MODEL)
    merged = gates[:, :, 0] * (branches[0] @ w_branch[0])
    for n in range(1, N_BRANCH):
        merged = merged + gates[:, :, n] * (branches[n] @ w_branch[n])
    return merged @ w_out


def _grouped_moe(h, router_w, router_bias, w_gate, w_up, w_down):
    B, S, D = h.shape
    t = h.reshape(B * S, D)
    scores = jax.nn.sigmoid(jnp.matmul(t, router_w, preferred_element_type=jnp.float32))
    biased = (scores + router_bias.astype(jnp.float32)).reshape(-1, N_GROUPS, N_EXPERTS // N_GROUPS)
    group_score = jnp.sum(lax.top_k(biased, TOP_K)[0], axis=-1)
    best_group = jnp.argmax(group_score, axis=-1)
    in_group = jnp.arange(N_GROUPS)[None, :] == best_group[:, None]
    masked = jnp.where(in_group[:, :, None], biased, -jnp.inf).reshape(-1, N_EXPERTS)
    _, top_idx = lax.top_k(masked, TOP_K)
    top_scores = jnp.take_along_axis(scores, top_idx, axis=-1)
    weights = top_scores / jnp.sum(top_scores, axis=-1, keepdims=True)
    combine = jnp.sum(jax.nn.one_hot(top_idx, N_EXPERTS, dtype=jnp.float32) * weights[..., None],
                      axis=1).astype(h.dtype)
    out = jnp.zeros_like(t)
    for e in range(N_EXPERTS):
        hid = jax.nn.silu(t @ w_gate[e]) * (t @ w_up[e])
        out = out + combine[:, e:e + 1] * (hid @ w_down[e])
    return out.reshape(B, S, D)


def setup_inputs(seed: int = 0) -> dict:
    key = jax.random.key(seed)
    ks = jax.random.split(key, 40)
    f32 = jnp.float32
    L, LR = DEPTH, DEPTH - 1

    def nrm(k, shape, scale):
        return jax.random.normal(k, shape, f32) * scale

    def gain(k, shape):
        return 1.0 + 0.02 * jax.random.normal(k, shape, f32)

    x = jax.random.normal(ks[0], (BATCH, SEQ, D_MODEL), f32)
    start = jax.random.randint(ks[1], (BATCH, 1), 0, 4096, dtype=jnp.int32)
    positions = start + jnp.arange(SEQ, dtype=jnp.int32)[None, :]
    dt = jnp.exp(jax.random.uniform(ks[14], (L, GDN_HEADS), f32, math.log(1e-3), math.log(1e-1)))
    return {
        'x': x,
        'positions': positions,
        'router_w': nrm(ks[2], (D_MODEL, N_EXPERTS), D_MODEL ** -0.5),
        'w_in': nrm(ks[3], (L, D_MODEL, IN_COLS), D_MODEL ** -0.5),
        'w_in_vres': nrm(ks[4], (LR, D_MODEL, RWKV_V_RANK), D_MODEL ** -0.5),
        'mla_q_norm': gain(ks[5], (L, MLA_Q_RANK)),
        'mla_w_uq': nrm(ks[6], (L, MLA_Q_RANK, MLA_HEADS * (MLA_NOPE + MLA_ROPE)), MLA_Q_RANK ** -0.5),
        'mla_kv_norm': gain(ks[7], (L, MLA_KV_RANK)),
        'mla_w_ukv': nrm(ks[8], (L, MLA_KV_RANK, MLA_HEADS * (MLA_NOPE + MLA_V)), MLA_KV_RANK ** -0.5),
        'gla_w_gate_up': nrm(ks[9], (L, GLA_GATE_RANK, GLA_HEADS * GLA_DK), GLA_GATE_RANK ** -0.5),
        'gla_b_gate': nrm(ks[10], (L, GLA_HEADS * GLA_DK), 0.1),
        'gla_norm': gain(ks[11], (L, GLA_DV)),
        'gdn_conv': nrm(ks[12], (L, GDN_CONV, GDN_HEADS * (2 * GDN_DK + GDN_DV)), GDN_CONV ** -0.5),
        'gdn_a_log': jnp.log(jax.random.uniform(ks[13], (L, GDN_HEADS), f32, 1.0, 16.0)),
        'gdn_dt_bias': dt + jnp.log(-jnp.expm1(-dt)),
        'gdn_norm': gain(ks[15], (L, GDN_DV)),
        'rwkv_mu': jax.random.uniform(ks[16], (L, RWKV_FEAT), f32),
        'rwkv_w0': jnp.linspace(-6.0, -1.0, RWKV_WIDTH, dtype=f32)[None, :] + nrm(ks[17], (L, RWKV_WIDTH), 0.1),
        'rwkv_w_up': nrm(ks[18], (L, RWKV_W_RANK, RWKV_WIDTH), 0.1),
        'rwkv_a0': nrm(ks[19], (L, RWKV_WIDTH), 0.1),
        'rwkv_a_up': nrm(ks[20], (L, RWKV_A_RANK, RWKV_WIDTH), RWKV_A_RANK ** -0.5),
        'rwkv_g_up': nrm(ks[21], (L, RWKV_G_RANK, RWKV_WIDTH), RWKV_G_RANK ** -0.5),
        'rwkv_k_k': 0.85 + nrm(ks[22], (L, RWKV_WIDTH), 0.05),
        'rwkv_k_a': 1.0 + nrm(ks[23], (L, RWKV_WIDTH), 0.05),
        'rwkv_r_k': nrm(ks[24], (L, RWKV_HEADS, RWKV_N), 0.1),
        'rwkv_ln_g': gain(ks[25], (L, RWKV_WIDTH)),
        'rwkv_ln_b': nrm(ks[26], (L, RWKV_WIDTH), 0.02),
        'rwkv_vres_mu': jax.random.uniform(ks[27], (LR, RWKV_V_RANK), f32),
        'rwkv_v0': nrm(ks[28], (LR, RWKV_WIDTH), 0.1),
        'rwkv_v_up': nrm(ks[29], (LR, RWKV_V_RANK, RWKV_WIDTH), RWKV_V_RANK ** -0.5),
        'w_branch': nrm(ks[30], (L, N_BRANCH, BRANCH_WIDTH, D_MODEL), DEEPNORM_BETA * BRANCH_WIDTH ** -0.5),
        'w_out': nrm(ks[31], (L, D_MODEL, D_MODEL), DEEPNORM_BETA * D_MODEL ** -0.5),
        'ln1_g': gain(ks[32], (L, D_MODEL)),
        'ln1_b': nrm(ks[33], (L, D_MODEL), 0.02),
        'ln2_g': gain(ks[34], (L, D_MODEL)),
        'ln2_b': nrm(ks[35], (L, D_MODEL), 0.02),
        'router_bias': nrm(ks[36], (L, N_EXPERTS), 0.01),
        'moe_w_gate': nrm(ks[37], (L, N_EXPERTS, D_MODEL, D_EXPERT), D_MODEL ** -0.5),
        'moe_w_up': nrm(ks[38], (L, N_EXPERTS, D_MODEL, D_EXPERT), D_MODEL ** -0.5),
        'moe_w_down': nrm(ks[39], (L, N_EXPERTS, D_EXPERT, D_MODEL), DEEPNORM_BETA * D_EXPERT ** -0.5),
    }


def reference(x, positions, router_w, w_in, w_in_vres, mla_q_norm, mla_w_uq, mla_kv_norm, mla_w_ukv,
              gla_w_gate_up, gla_b_gate, gla_norm, gdn_conv, gdn_a_log, gdn_dt_bias, gdn_norm,
              rwkv_mu, rwkv_w0, rwkv_w_up, rwkv_a0, rwkv_a_up, rwkv_g_up, rwkv_k_k, rwkv_k_a, rwkv_r_k,
              rwkv_ln_g, rwkv_ln_b, rwkv_vres_mu, rwkv_v0, rwkv_v_up, w_branch, w_out,
              ln1_g, ln1_b, ln2_g, ln2_b, router_bias, moe_w_gate, moe_w_up, moe_w_down):
    v_first = None
    for l in range(DEPTH):
        if l == 0:
            w_proj = w_in[l]
        else:
            w_proj = jnp.concatenate([w_in[l], w_in_vres[l - 1]], axis=1)
        proj = x @ w_proj
        (mla_q_lat, mla_kv_lat, gla_q, gla_k, gla_v, gla_gate_lat, gla_out_gate,
         gdn_qkv, gdn_beta, gdn_a, gdn_z, rwkv_feat, gate_logits) = _split(proj[..., :IN_COLS], IN_WIDTHS)

        y_a = _mla_branch(mla_q_lat, mla_kv_lat, positions, mla_q_norm[l], mla_w_uq[l],
                          mla_kv_norm[l], mla_w_ukv[l])
        y_b = _gla_branch(gla_q, gla_k, gla_v, gla_gate_lat, gla_out_gate, gla_w_gate_up[l],
                          gla_b_gate[l], gla_norm[l])
        y_c = _gdn_branch(gdn_qkv, gdn_beta, gdn_a, gdn_z, gdn_conv[l], gdn_a_log[l],
                          gdn_dt_bias[l], gdn_norm[l])
        if l == 0:
            v_gate = None
        else:
            vres_lat = _token_shift_mix(proj[..., IN_COLS:], rwkv_vres_mu[l - 1])
            v_gate = jax.nn.sigmoid(rwkv_v0[l - 1] + vres_lat @ rwkv_v_up[l - 1])
        y_d, v_d = _rwkv7_branch(rwkv_feat, rwkv_mu[l], rwkv_w0[l], rwkv_w_up[l], rwkv_a0[l],
                                 rwkv_a_up[l], rwkv_g_up[l], rwkv_k_k[l], rwkv_k_a[l], rwkv_r_k[l],
                                 rwkv_ln_g[l], rwkv_ln_b[l], v_first, v_gate)
        if l == 0:
            v_first = v_d

        mix = _merge_branches((y_a, y_b, y_c, y_d), gate_logits, w_branch[l], w_out[l])
        x = _layernorm(DEEPNORM_ALPHA * x + mix, ln1_g[l], ln1_b[l])
        moe = _grouped_moe(x, router_w, router_bias[l], moe_w_gate[l], moe_w_up[l], moe_w_down[l])
        x = _layernorm(DEEPNORM_ALPHA * x + moe, ln2_g[l], ln2_b[l])
    return x
```

```python
import contextlib
import math
import numpy as np
import ml_dtypes
import concourse.bass as bass
import concourse.mybir as mybir
from concourse.bass_utils import run_bass_kernel_spmd

F32 = mybir.dt.float32
BF16 = mybir.dt.bfloat16
I32 = mybir.dt.int32
ALU = mybir.AluOpType
AF = mybir.ActivationFunctionType
AX = mybir.AxisListType

D_MODEL = 1024
IN_COLS = 9912
TT = 512
NKC = 8


class Dep:
    __slots__ = ("w", "r", "wd")

    def __init__(self):
        self.w = None
        self.r = []
        self.wd = []


class V:
    __slots__ = ("ap", "deps")

    def __init__(self, ap, deps):
        self.ap = ap
        self.deps = deps

    def __getitem__(self, k):
        return V(self.ap[k], self.deps)

    def v(self, ap):
        return V(ap, self.deps)


class Eng:
    def __init__(self, name):
        self.name = name
        self.count = 0
        self.ops = []
        self.waited = {}


class KB:
    def __init__(self, nc):
        self.nc = nc
        self.stack = contextlib.ExitStack()
        self.E = {n: Eng(n) for n in ("pe", "act", "dve", "pool", "sp")}
        import os as _os2
        self.ndma = int(_os2.environ.get("DBG_NDMA", "32"))
        self.dma_state = [0] * self.ndma
        self.dma_next = 0
        self.n_ops = 0
        self.out_tokens = []
        import os as _os
        self.limit = int(_os.environ["DBG_LIMIT"]) if _os.environ.get("DBG_LIMIT") else None
        self.dump = int(_os.environ["DBG_DUMP"]) if _os.environ.get("DBG_DUMP") else None

    def _nm(self, name):
        self._uid = getattr(self, "_uid", 0) + 1
        return "%s_u%d" % (name, self._uid)

    def sbuf(self, name, shape, dt, stack=None):
        t = (stack or self.stack).enter_context(self.nc.sbuf_tensor(self._nm(name), list(shape), dt))
        return V(t[:] if False else t, [Dep()])

    def psum(self, name, shape, dt=F32, stack=None):
        t = (stack or self.stack).enter_context(self.nc.psum_tensor(self._nm(name), list(shape), dt))
        return V(t, [Dep()])

    def dram(self, name, shape, dt, kind="Internal"):
        t = self.nc.dram_tensor(name, list(shape), dt, kind=kind)
        return V(t.ap(), [Dep()])

    @staticmethod
    def _deps(reads, writes):
        out = []
        for d in reads:
            if d.w is not None:
                out.append(d.w)
            out.extend(d.wd)
        for d in writes:
            if d.w is not None:
                out.append(d.w)
            out.extend(d.wd)
            out.extend(d.r)
        return out

    def _waits(self, e, reads, writes, skip):
        need = {}
        for (pn, pv) in self._deps(reads, writes):
            if pn == skip:
                continue
            if need.get(pn, 0) < pv:
                need[pn] = pv
        waits = []
        for pn, pv in need.items():
            if e.waited.get(pn, 0) < pv:
                e.waited[pn] = pv
                waits.append((pn, pv))
        return waits

    @staticmethod
    def _mark(tok, reads, writes):
        for d in reads:
            d.r.append(tok)
            if len(d.r) > 64:
                last = {}
                for (n, v) in d.r:
                    if last.get(n, 0) < v:
                        last[n] = v
                d.r = list(last.items())
        is_async = tok[0].startswith("dma") or tok[0] == "coll"
        for d in writes:
            if is_async:
                d.wd.append(tok)
                if len(d.wd) > 48:
                    last = {}
                    for (n, v) in d.wd:
                        if last.get(n, 0) < v:
                            last[n] = v
                    d.wd = list(last.items())
            else:
                d.w = tok
                d.wd = []
            d.r = []

    def op(self, eng, fn, reads=(), writes=()):
        if self.limit is not None and self.n_ops >= self.limit:
            return None
        if self.dump is not None and abs(self.n_ops - self.dump) <= 3:
            import sys as _s
            f = _s._getframe(1)
            chain = []
            while f is not None and len(chain) < 4:
                chain.append("%s:%d" % (f.f_code.co_name, f.f_lineno))
                f = f.f_back
            print("OP", self.n_ops, eng, chain)
        rd = [d for v in reads for d in v.deps]
        wd = [d for v in writes for d in v.deps]
        e = self.E[eng]
        skip = "pe" if eng == "pe" else None
        waits = self._waits(e, rd, wd, skip)
        e.count += 1
        tok = (eng, e.count)
        e.ops.append((waits, fn, None))
        self._mark(tok, rd, wd)
        self.n_ops += 1
        return tok

    def dma(self, q, out, in_, **kw):
        if self.limit is not None and self.n_ops >= self.limit:
            return None
        rd = list(in_.deps)
        wd = list(out.deps)
        e = self.E[q]
        waits = self._waits(e, rd, wd, None)
        s = self.dma_next
        self.dma_next = (s + 1) % self.ndma
        prev = self.dma_state[s]
        name = "dma%d" % s
        if prev > 0 and e.waited.get(name, 0) < prev:
            e.waited[name] = prev
            waits.append((name, prev))
        self.dma_state[s] = prev + 16
        tok = (name, prev + 16)
        oa, ia = out.ap, in_.ap
        e.ops.append((waits, lambda h: h.dma_start(out=oa, in_=ia, **kw), name))
        self._mark(tok, rd, wd)
        self.n_ops += 1
        return tok

    def coll(self, kind, out, in_, groups):
        import os as _os
        if _os.environ.get("DBG_NOCOLL"):
            return None
        self._cidx = getattr(self, "_cidx", -1) + 1
        if _os.environ.get("DBG_COLLMASK") and not (int(_os.environ["DBG_COLLMASK"]) >> self._cidx) & 1:
            return None
        rd = list(in_.deps)
        wd = list(out.deps)
        e = self.E["pool"]
        waits = self._waits(e, rd, wd, None)
        self.ncoll = getattr(self, "ncoll", 0) + 1
        tok = ("coll", self.ncoll)
        oa, ia = out.ap, in_.ap
        e.ops.append((waits, lambda h: h.collective_compute(kind, ALU.bypass, replica_groups=groups, ins=[ia], outs=[oa]), "coll"))
        self._mark(tok, rd, wd)
        self.n_ops += 1
        return tok

    def _ensure_sems(self):
        if getattr(self, "sem", None) is None:
            self.sem = {}
            for n in ("pe", "act", "dve", "pool"):
                self.sem[n] = self.stack.enter_context(self.nc.semaphore("s_" + n))
            for i in range(self.ndma):
                self.sem["dma%d" % i] = self.stack.enter_context(self.nc.semaphore("s_dma%d" % i))
            self.sem["coll"] = self.stack.enter_context(self.nc.semaphore("s_coll"))

    def flush(self):
        self._ensure_sems()
        sem = self.sem
        toks = [(n, self.E[n].count) for n in ("pe", "act", "dve", "pool") if self.E[n].count > 0]
        toks += [("dma%d" % i, v) for i, v in enumerate(self.dma_state) if v > 0]
        if getattr(self, "ncoll", 0) > 0:
            toks.append(("coll", self.ncoll))
        for n, e in self.E.items():
            waits = []
            for (pn, pv) in toks:
                if pn == n:
                    continue
                if e.waited.get(pn, 0) < pv:
                    e.waited[pn] = pv
                    waits.append((pn, pv))
            e.ops.append((waits, None, None))
        with self.nc.Block() as block:
            def replay(e, own):
                ops = e.ops

                def body(h):
                    for waits, fn, dsem in ops:
                        for (pn, pv) in waits:
                            h.wait_ge(sem[pn], pv)
                        if fn is None:
                            continue
                        ins = fn(h)
                        if dsem == "coll":
                            ins.then_inc(sem[dsem], 1)
                        elif dsem is not None:
                            ins.then_inc(sem[dsem], 16)
                        elif own is not None:
                            ins.then_inc(sem[own], 1)
                return body
            block.sync(replay(self.E["sp"], None))
            block.tensor(replay(self.E["pe"], "pe"))
            block.scalar(replay(self.E["act"], "act"))
            block.vector(replay(self.E["dve"], "dve"))
            block.gpsimd(replay(self.E["pool"], "pool"))
        for e in self.E.values():
            e.ops = []

    def emit(self):
        self.flush()

    def close(self):
        self.stack.close()

    def mm(self, out, lhsT, rhs, start=True, stop=True):
        oa, la, ra = out.ap, lhsT.ap, rhs.ap
        return self.op("pe", lambda h: h.matmul(oa, la, ra, start=start, stop=stop),
                       reads=[lhsT, rhs], writes=[out])

    def act(self, out, in_, func, scale=1.0, bias=None, eng="act"):
        oa, ia = out.ap, in_.ap
        reads = [in_]
        kw = {}
        if isinstance(bias, V):
            reads.append(bias)
            kw["bias"] = bias.ap
        elif bias is not None:
            kw["bias"] = bias
        if isinstance(scale, V):
            reads.append(scale)
            kw["scale"] = scale.ap
        else:
            kw["scale"] = scale
        return self.op("act", lambda h: h.activation(oa, ia, func, **kw), reads=reads, writes=[out])

    def tt(self, eng, out, in0, in1, op):
        oa, a, b = out.ap, in0.ap, in1.ap
        return self.op(eng, lambda h: h.tensor_tensor(oa, a, b, op), reads=[in0, in1], writes=[out])

    def ts(self, eng, out, in0, s1, op0, s2=None, op1=None):
        oa, a = out.ap, in0.ap
        reads = [in0]
        if isinstance(s1, V):
            reads.append(s1)
            s1 = s1.ap
        if isinstance(s2, V):
            reads.append(s2)
            s2 = s2.ap
        if op1 is None:
            return self.op(eng, lambda h: h.tensor_scalar(oa, a, s1, None, op0), reads=reads, writes=[out])
        return self.op(eng, lambda h: h.tensor_scalar(oa, a, s1, s2, op0, op1), reads=reads, writes=[out])

    def stt(self, out, in0, scalar, in1, op0, op1):
        oa, a, b = out.ap, in0.ap, in1.ap
        reads = [in0, in1]
        if isinstance(scalar, V):
            reads.append(scalar)
            scalar = scalar.ap
        return self.op("dve", lambda h: h.scalar_tensor_tensor(oa, a, scalar, b, op0, op1),
                       reads=reads, writes=[out])

    def copy(self, eng, out, in_):
        oa, ia = out.ap, in_.ap
        if eng == "act":
            return self.op("act", lambda h: h.activation(oa, ia, AF.Copy), reads=[in_], writes=[out])
        return self.op(eng, lambda h: h.tensor_copy(oa, ia), reads=[in_], writes=[out])

    def memset(self, eng, out, val):
        oa = out.ap
        return self.op(eng, lambda h: h.memset(oa, val), writes=[out])

    def scan(self, out, d0, d1, init, op0, op1):
        oa, a, b = out.ap, d0.ap, d1.ap
        return self.op("dve", lambda h: h.tensor_tensor_scan(oa, a, b, init, op0, op1),
                       reads=[d0, d1], writes=[out])

    def recip(self, out, in_):
        oa, ia = out.ap, in_.ap
        return self.op("dve", lambda h: h.reciprocal(oa, ia), reads=[in_], writes=[out])


class Pool:
    def __init__(self, kb, name, n, shape, dt, space="sbuf", stack=None):
        mk = kb.sbuf if space == "sbuf" else kb.psum
        self.t = [mk("%s%d" % (name, i), shape, dt, stack=stack) for i in range(n)]
        self.i = 0

    def get(self):
        t = self.t[self.i]
        self.i = (self.i + 1) % len(self.t)
        return t


C_W = math.exp(-0.5)
MLA_SCALE = 96 ** -0.5
LN_EPS = 1e-5
NORM_EPS = 1e-6
RWKV_LN_EPS = 64e-5
ALPHA = (2.0 * 2) ** 0.25

_IW = (256, 160, 256, 256, 512, 16, 512, 1536, 4, 4, 512, 1792, 4096)
_IO = np.concatenate([[0], np.cumsum(_IW)]).tolist()
(O_QLAT, O_KVLAT, O_GQ, O_GK, O_GV, O_GGATE, O_GOG, O_DQKV, O_DBETA, O_DA, O_DZ, O_RW, O_GATES) = _IO[:13]


class Packer:
    def __init__(self, rows):
        self.rows = rows
        self.blocks = []
        self.off = {}
        self.n = 0

    def add(self, name, arr):
        arr = np.asarray(arr, dtype=np.float32)
        if arr.ndim == 1:
            arr = arr[:, None]
        if arr.shape[0] < self.rows:
            pad = np.zeros((self.rows - arr.shape[0], arr.shape[1]), np.float32)
            arr = np.concatenate([arr, pad], 0)
        self.off[name] = (self.n, arr.shape[1])
        self.blocks.append(arr)
        self.n += arr.shape[1]

    def build(self):
        return np.ascontiguousarray(np.concatenate(self.blocks, 1))


def const_masks():
    i = np.arange(128)
    same = (i[:, None] // 64) == (i[None, :] // 64)
    m = {}
    m["U_incl"] = (same & (i[:, None] <= i[None, :])).astype(np.float32)
    m["U_strict"] = (same & (i[:, None] < i[None, :])).astype(np.float32)
    m["L_incl"] = m["U_incl"].T.copy()
    m["L_strict"] = m["U_strict"].T.copy()
    m["ident"] = np.eye(128, dtype=np.float32)
    m["ones"] = np.ones((128, 128), np.float32)
    m["ones_blk"] = same.astype(np.float32)
    m["istack"] = np.concatenate([np.eye(64), np.eye(64)], 0).astype(np.float32)
    return m


def pack_p1a(inp, l, hg):
    w_in = inp["w_in"][l]
    W = Packer(1024)
    W.add("qlat", w_in[:, O_QLAT:O_QLAT + 256])
    W.add("ckv", w_in[:, O_KVLAT:O_KVLAT + 128])
    kr = w_in[:, O_KVLAT + 128:O_KVLAT + 160]
    W.add("krope", kr)
    W.add("krope_sw", np.concatenate([kr[:, 16:32], kr[:, 0:16]], 1))
    W.add("gq", w_in[:, O_GQ + 64 * hg:O_GQ + 64 * hg + 64])
    W.add("gk", w_in[:, O_GK + 64 * hg:O_GK + 64 * hg + 64])
    W.add("gv", w_in[:, O_GV + 128 * hg:O_GV + 128 * hg + 128])
    W.add("ggate", w_in[:, O_GGATE:O_GGATE + 16])
    W.add("gog", w_in[:, O_GOG + 128 * hg:O_GOG + 128 * hg + 128])
    W.add("dq", w_in[:, O_DQKV + 128 * hg:O_DQKV + 128 * hg + 128])
    W.add("dk", w_in[:, O_DQKV + 512 + 128 * hg:O_DQKV + 512 + 128 * hg + 128])
    W.add("dv", w_in[:, O_DQKV + 1024 + 128 * hg:O_DQKV + 1024 + 128 * hg + 128])
    W.add("dbeta", np.repeat(w_in[:, O_DBETA + hg:O_DBETA + hg + 1], 128, 1))
    W.add("da", np.repeat(w_in[:, O_DA + hg:O_DA + hg + 1], 128, 1))
    W.add("dz", w_in[:, O_DZ + 128 * hg:O_DZ + 128 * hg + 128])
    for i, nm in enumerate(("rr", "rk", "rv")):
        W.add(nm, w_in[:, O_RW + 512 * i + 128 * hg:O_RW + 512 * i + 128 * hg + 128])
    W.add("rwl", w_in[:, O_RW + 1536:O_RW + 1600])
    W.add("ral", w_in[:, O_RW + 1600:O_RW + 1664])
    W.add("rgl", w_in[:, O_RW + 1664:O_RW + 1792])
    if l > 0:
        W.add("rvr", inp["w_in_vres"][l - 1])
    P = Packer(128)
    ch = slice(128 * hg, 128 * hg + 128)
    mu = inp["rwkv_mu"][l]
    P.add("mu_r", mu[0:512][ch]); P.add("mu_k", mu[512:1024][ch]); P.add("mu_v", mu[1024:1536][ch])
    P.add("mu_wl", mu[1536:1600]); P.add("mu_al", mu[1600:1664]); P.add("mu_gl", mu[1664:1792])
    P.add("w0", inp["rwkv_w0"][l][ch]); P.add("a0", inp["rwkv_a0"][l][ch])
    P.add("k_k", inp["rwkv_k_k"][l][ch]); P.add("k_a", inp["rwkv_k_a"][l][ch])
    P.add("r_k", inp["rwkv_r_k"][l].reshape(-1)[ch])
    P.add("ln_g", inp["rwkv_ln_g"][l][ch]); P.add("ln_b", inp["rwkv_ln_b"][l][ch])
    if l > 0:
        P.add("mu_vr", inp["rwkv_vres_mu"][l - 1]); P.add("v0", inp["rwkv_v0"][l - 1][ch])
    P.add("gla_norm", inp["gla_norm"][l]); P.add("gdn_norm", inp["gdn_norm"][l])
    cw = inp["gdn_conv"][l]
    for nm, o in (("cq", 0), ("ck", 512), ("cv", 1024)):
        P.add("conv_" + nm, cw[:, o + 128 * hg:o + 128 * hg + 128].T)
    P.add("a_log", np.full(128, inp["gdn_a_log"][l][hg])); P.add("dt_bias", np.full(128, inp["gdn_dt_bias"][l][hg]))
    P.add("q_norm", inp["mla_q_norm"][l].reshape(2, 128).T)
    P.add("kv_norm", inp["mla_kv_norm"][l])
    invf = (10000.0 ** (-np.arange(16, dtype=np.float32) / 16)).astype(np.float32)
    P.add("invf", np.concatenate([np.zeros(64, np.float32), invf, invf]))
    P.add("rsign", np.concatenate([np.zeros(64, np.float32), -np.ones(16, np.float32), np.ones(16, np.float32)]))
    M = Packer(128)
    M.add("gup", np.concatenate([inp["gla_w_gate_up"][l][:, 64 * hg:64 * hg + 64],
                                 inp["gla_b_gate"][l][None, 64 * hg:64 * hg + 64]], 0))
    M.add("w_up", inp["rwkv_w_up"][l][:, ch]); M.add("a_up", inp["rwkv_a_up"][l][:, ch])
    M.add("g_up", inp["rwkv_g_up"][l][:, ch])
    if l > 0:
        M.add("v_up", inp["rwkv_v_up"][l - 1][:, ch])
    uq = inp["mla_w_uq"][l]
    ukv = inp["mla_w_ukv"][l]
    for hh in range(2):
        h = 2 * hg + hh
        q = uq[:, 96 * h:96 * h + 96]
        for kc in range(2):
            M.add("uq%d_%d" % (hh, kc), q[128 * kc:128 * kc + 128])
            sw = np.concatenate([np.zeros((128, 64), np.float32), q[128 * kc:128 * kc + 128, 80:96],
                                 q[128 * kc:128 * kc + 128, 64:80]], 1)
            M.add("uqs%d_%d" % (hh, kc), sw)
        M.add("ukn%d" % hh, ukv[:, 128 * h:128 * h + 64])
    M.add("ukv_v", np.concatenate([ukv[:, 128 * (2 * hg) + 64:128 * (2 * hg) + 128],
                                   ukv[:, 128 * (2 * hg + 1) + 64:128 * (2 * hg + 1) + 128]], 1))
    return W, P, M


class Ctx:
    pass


def setup_common(kb, cx, masks_d, stack):
    mk = const_masks()
    names = ["U_strict", "U_incl", "L_incl", "L_strict", "ident", "ones", "ones_blk"]
    cx.mf = kb.sbuf("maskf", [128, 7 * 128 + 64], F32, stack)
    cx.mb = kb.sbuf("maskb", [128, 7 * 128 + 64], BF16, stack)
    kb.dma("sp", cx.mf[:, :], masks_d)
    kb.copy("pool", cx.mb[:, :], cx.mf[:, :])
    cx.M = {}
    cx.Mf = {}
    for i, n in enumerate(names):
        cx.M[n] = cx.mb[:, 128 * i:128 * i + 128]
        cx.Mf[n] = cx.mf[:, 128 * i:128 * i + 128]
    cx.M["istack"] = cx.mb[:, 896:960]
    cx.Mf["istack"] = cx.mf[:, 896:960]
    cx.M["pairU"] = cx.mb[:, 0:256]
    banks = [kb.psum("pb%d" % i, [128, 512], F32, stack) for i in range(8)]
    cx.banks = banks

    class RR:
        def __init__(self, items):
            self.items = items
            self.i = 0

        def get(self):
            x = self.items[self.i]
            self.i = (self.i + 1) % len(self.items)
            return x
    cx.RR = RR
    cx.PB = RR([banks[i][:, :] for i in range(2)])
    cx.PH = RR([banks[2][:, 256 * j:256 * j + 256] for j in range(2)])
    cx.PQ = RR([banks[3 + i][:, 128 * j:128 * j + 128] for j in range(4) for i in range(3)])
    cx.PD = [banks[6 + i][:, 128 * j:128 * j + 128] for i in range(2) for j in range(4)]


def masks_host():
    mk = const_masks()
    names = ["U_strict", "U_incl", "L_incl", "L_strict", "ident", "ones", "ones_blk"]
    return np.ascontiguousarray(np.concatenate([mk[n] for n in names] + [mk["istack"]], 1).astype(np.float32))


def scanmask_host():
    m = np.ones((128, TT), np.float32)
    m[:, ::64] = 0.0
    return m


def rsqrt_act(kb, out, in_, scale, eps, tmp):
    kb.act(tmp, in_, AF.Ln, scale=scale, bias=eps)
    kb.act(out, tmp, AF.Exp, scale=-0.5)


def sigmoid_into(kb, out, in_, tmp, scale=1.0, bias=None, eng="dve"):
    kb.act(tmp, in_, AF.Exp, scale=-scale, bias=bias)
    kb.ts(eng, tmp, tmp, 1.0, ALU.add)
    kb.recip(out, tmp)


def neumann(kb, cx, PA, PB_, pool_bf, ident_b):
    TT_ = pool_bf.get()
    kb.tt("dve", TT_[:, :], PB_, ident_b, ALU.add)
    A, B = PA, PB_
    for k in range(1, 6):
        psA = cx.PQ.get()
        kb.mm(psA, B, A)
        nA = pool_bf.get()
        kb.copy("act", nA[:, :], psA)
        if k < 5:
            psB = cx.PQ.get()
            kb.mm(psB, A, B)
            nB = pool_bf.get()
            kb.copy("pool" if False else "dve", nB[:, :], psB)
        psT = cx.PQ.get()
        kb.mm(psT, nA[:, :], TT_[:, :])
        nT = pool_bf.get()
        kb.tt("dve", nT[:, :], TT_[:, :], psT, ALU.add)
        TT_ = nT
        A = nA[:, :]
        if k < 5:
            B = nB[:, :]
    return TT_[:, :]


def build_p1a(kb, S, l, d, branches=("gla", "gdn", "rwkv", "mla")):
    import os as _os
    if "build_p1a" in _os.environ.get("DBG_SKIP", "").split(","):
        return None

    st = contextlib.ExitStack()
    cx = Ctx()
    setup_common(kb, cx, d["masks"], st)
    Wo, Po, Mo = d["Woff"], d["Poff"], d["Moff"]
    NW, NP, NM = d["NW"], d["NP"], d["NM"]
    nt = S // TT
    M, Mf = cx.M, cx.Mf

    PC = kb.sbuf("PC", [128, NP], F32, st)
    kb.dma("sp", PC[:, :], d["PC"])
    SMf = kb.sbuf("SMf", [128, NM], F32, st)
    kb.dma("sp", SMf[:, :], d["SM"])
    SMb = kb.sbuf("SMb", [128, NM], BF16, st)
    kb.copy("pool", SMb[:, :], SMf[:, :])
    scm = kb.sbuf("scm", [128, TT], F32, st)
    kb.dma("sp", scm[:, :], d["scanmask"])
    Wbf = kb.sbuf("Wbf", [128, NKC, NW], BF16, st)
    wst = Pool(kb, "wst", 2, [128, 512], F32, stack=st)
    for k in range(NKC):
        for c0 in range(0, NW, 512):
            cw_ = min(512, NW - c0)
            s_ = wst.get()
            kb.dma("sp", s_[:, 0:cw_], d["W"][128 * k:128 * k + 128, c0:c0 + cw_])
            kb.copy("pool", Wbf[:, k, c0:c0 + cw_], s_[:, 0:cw_])

    def pc(name, rows=128, j=0):
        o, w = Po[name]
        return PC[0:rows, o + j:o + j + 1]

    def sm(name, rows, cols=None):
        o, w = Mo[name]
        return SMb[0:rows, o:o + (cols or w)]

    def wcol(name, k, c0=0, cn=None):
        o, w = Wo[name]
        return Wbf[:, k, o + c0:o + c0 + (cn or w)]

    cst = kb.sbuf("cst", [128, 8], F32, st)
    kb.memset("pool", cst[:, 0:1], NORM_EPS)
    kb.memset("pool", cst[:, 1:2], RWKV_LN_EPS)
    kb.memset("pool", cst[:, 2:3], 1.0)
    kb.memset("pool", cst[:, 3:4], 0.0)
    EPS_N, EPS_R, ONE_C, ZERO_C = cst[:, 0:1], cst[:, 1:2], cst[:, 2:3], cst[:, 3:4]
    ncol = kb.sbuf("ncol", [128, 8], F32, st)
    kb.ts("pool", ncol[:, 0:1], pc("w0"), -1.0, ALU.mult)
    kb.ts("pool", ncol[:, 1:2], pc("a0"), -1.0, ALU.mult)
    if l > 0:
        kb.ts("pool", ncol[:, 2:3], pc("v0"), -1.0, ALU.mult)
    kb.act(ncol[:, 3:4], pc("a_log"), AF.Exp)

    F5 = Pool(kb, "f5_", 10, [128, TT], F32, stack=st)
    L5 = Pool(kb, "l5_", 18, [128, TT], F32, stack=st)
    B5 = Pool(kb, "b5_", 10, [128, TT], BF16, stack=st)
    BQ = Pool(kb, "bq_", 28, [128, 128], BF16, stack=st)
    FQ = Pool(kb, "fq_", 16, [128, 128], F32, stack=st)
    kb_rhs = Pool(kb, "rhs_", 4, [128, 256], BF16, stack=st)
    BL = Pool(kb, "bl_", 8, [128, 128], BF16, stack=st)
    xst = Pool(kb, "xst", 3, [128, TT], F32, stack=st)
    xbfs = [kb.sbuf("xbf%d" % i, [128, NKC, TT], BF16, st) for i in range(2)]

    def proj_f(name, xb, rows=None, c0=0, out=None):
        o, w = Wo[name]
        rows = rows or w
        ps = out if out is not None else cx.PB.get()[0:rows, :]
        for k in range(NKC):
            kb.mm(ps, wcol(name, k, c0, rows), xb[:, k, :], start=(k == 0), stop=(k == NKC - 1))
        return ps

    def shift_mix(ps, rows, mu, halo, t):
        raw = halo[t % 2]
        kb.copy("act", raw[0:rows, 1:TT + 1], ps)
        if t == 0:
            kb.memset("pool", raw[0:rows, 0:1], 0.0)
        kb.copy("pool", halo[(t + 1) % 2][0:rows, 0:1], raw[0:rows, TT:TT + 1])
        dd = F5.get()
        kb.tt("dve", dd[0:rows, :], raw[0:rows, 0:TT], raw[0:rows, 1:TT + 1], ALU.subtract)
        out = F5.get()
        kb.stt(out[0:rows, :], dd[0:rows, :], mu, raw[0:rows, 1:TT + 1], ALU.mult, ALU.add)
        return out[0:rows, :]

    if "gla" in branches:
        gl_H32 = kb.sbuf("gl_H32", [64, 128], F32, st)
        gl_Hbf = kb.sbuf("gl_Hbf", [64, 128], BF16, st)
        kb.memset("pool", gl_H32[:, :], 0.0)
        kb.memset("pool", gl_Hbf[:, :], 0.0)
        gl_ext = kb.sbuf("gl_ext", [32, TT], BF16, st)
        kb.memset("pool", gl_ext[:, :], 1.0)
    if "gdn" in branches:
        gd_H32 = kb.sbuf("gd_H32", [128, 128], F32, st)
        gd_Hbf = kb.sbuf("gd_Hbf", [128, 128], BF16, st)
        kb.memset("pool", gd_H32[:, :], 0.0)
        kb.memset("pool", gd_Hbf[:, :], 0.0)
        gd_halo = {n: [kb.sbuf("gdh_%s" % n, [128, TT + 3], F32, st), kb.sbuf("gdc_%s" % n, [128, 4], F32, st)] for n in ("q", "k", "v")}
    if "rwkv" in branches:
        rw_H32 = kb.sbuf("rw_H32", [128, 64], F32, st)
        rw_Hbf = kb.sbuf("rw_Hbf", [128, 64], BF16, st)
        kb.memset("pool", rw_H32[:, :], 0.0)
        kb.memset("pool", rw_Hbf[:, :], 0.0)
        rw_halo = {n: [kb.sbuf("rwh_%s" % n, [128, TT + 1], F32, st), kb.sbuf("rwc_%s" % n, [128, 2], F32, st)]
                   for n in ("r", "k", "v", "wl", "al", "gl", "vr")}
        rw_AR = kb.sbuf("rw_AR", [128, 2 * TT], BF16, st)
        rw_MT = kb.sbuf("rw_MT", [128, 128], F32, st)
        kb.memset("pool", rw_MT[:, :], 0.0)
    if "mla" in branches:
        mla_posi = kb.sbuf("mla_posi", [96, TT], I32, st)
        mla_vt = [kb.sbuf("mla_vt%d" % i, [128, 2, 65], BF16, st) for i in range(2)]
        for i in range(2):
            kb.memset("pool", mla_vt[i][:, :, :], 1.0)
        mla_ni = kb.sbuf("mla_ni", [96, TT], I32, st)
        uqg = kb.sbuf("uqg", [128, 2, 2, 2, 96], BF16, st)
        for hh in range(2):
            for kc in range(2):
                for sw, nm in enumerate(("uq", "uqs")):
                    o, w = Mo["%s%d_%d" % (nm, hh, kc)]
                    kb.ts("pool", uqg[:, hh, kc, sw, :], SMf[:, o:o + 96], pc("q_norm", 128, kc), ALU.mult,
                          MLA_SCALE, ALU.mult)
        ukg = kb.sbuf("ukg", [128, 2, 64], BF16, st)
        for hh in range(2):
            o, w = Mo["ukn%d" % hh]
            kb.ts("pool", ukg[:, hh, :], SMf[:, o:o + 64], pc("kv_norm"), ALU.mult)
        uvg = kb.sbuf("uvg", [128, 128], BF16, st)
        o, w = Mo["ukv_v"]
        kb.ts("pool", uvg[:, :], SMf[:, o:o + 128], pc("kv_norm"), ALU.mult)

    def silu_of(src):
        sg = L5.get()
        tmp2 = F5.get()
        sigmoid_into(kb, sg[:, :], src, tmp2[:, :])
        kb.tt("dve", sg[:, :], sg[:, :], src, ALU.mult)
        return sg

    def post_norm_gate(oT, gate_sb, gcol, row0):
        osq = B5.get()
        kb.act(osq[:, :], oT[:, :], AF.Square)
        ps_s = cx.PB.get()
        kb.mm(ps_s, M["ones"], osq[:, :])
        tmp = F5.get()
        rstd = F5.get()
        rsqrt_act(kb, rstd[:, :], ps_s, 1.0 / 128, EPS_N, tmp[:, :])
        sg = silu_of(gate_sb)
        kb.stt(oT[:, :], oT[:, :], gcol, rstd[:, :], ALU.mult, ALU.mult)
        yb = B5.get()
        kb.tt("dve", yb[:, :], oT[:, :], sg[:, :], ALU.mult)
        kb.dma("sp", d["ydst"](row0, 128, tsl_box[0].start // TT), yb[:, :])

    def l2norm_f(x, ones_m, scale):
        sq = B5.get()
        kb.act(sq[:, :], x, AF.Square)
        ps_s = cx.PB.get()
        kb.mm(ps_s, ones_m, sq[:, :])
        tmp = F5.get()
        rs = F5.get()
        rsqrt_act(kb, rs[:, :], ps_s, 1.0, EPS_N, tmp[:, :])
        out = L5.get()
        kb.stt(out[:, :], x, scale, rs[:, :], ALU.mult, ALU.mult)
        return out

    def transpose_bf(src, dst_pool=None):
        ps_t = cx.PQ.get()
        kb.mm(ps_t, src, M["ident"])
        o = BL.get()
        kb.copy("act", o[:, :], ps_t)
        return o

    tsl_box = [None]
    youts = {}

    for t in range(nt):
        tsl = slice(t * TT, (t + 1) * TT)
        tsl_box[0] = tsl
        xb = xbfs[t % 2]
        for k in range(NKC):
            s_ = xst.get()
            kb.dma("sp", s_[:, :], d["xsrc"](k, t))
            kb.copy("pool", xb[:, k, :], s_[:, :])

        if "gla" in branches:
            L5.i = 0
            ps_q = proj_f("gq", xb)
            qraw = L5.get()
            kb.copy("act", qraw[0:64, :], ps_q)
            ps_k = proj_f("gk", xb)
            kraw = L5.get()
            kb.copy("act", kraw[0:64, :], ps_k)
            ps_g = proj_f("ggate", xb)
            kb.copy("act", gl_ext[0:16, :], ps_g)
            ps_og = proj_f("gog", xb)
            og = L5.get()
            kb.copy("act", og[:, :], ps_og)
            ps_z = cx.PB.get()[0:64, :]
            kb.mm(ps_z, sm("gup", 17), gl_ext[0:17, :])
            e1 = F5.get()
            kb.act(e1[0:64, :], ps_z, AF.Exp, scale=-1.0)
            sp = F5.get()
            kb.act(sp[0:64, :], e1[0:64, :], AF.Ln, bias=ONE_C[0:64, :])
            bsp = L5.get()
            kb.scan(bsp[0:64, :], scm[0:64, :], sp[0:64, :], 0.0, ALU.mult, ALU.add)
            epos = L5.get()
            kb.act(epos[0:64, :], bsp[0:64, :], AF.Exp, scale=-1.0 / 16)
            eneg = F5.get()
            kb.act(eneg[0:64, :], bsp[0:64, :], AF.Exp, scale=1.0 / 16)
            b3 = bsp.ap[0:64, :].rearrange("p (c t) -> p c t", t=64)
            dl = F5.get()
            dl3 = dl.ap[0:64, :].rearrange("p (c t) -> p c t", t=64)
            bl_bc = b3[:, :, 63:64].broadcast_to([64, TT // 64, 64])
            kb.tt("dve", V(dl3, dl.deps), V(b3, bsp.deps), V(bl_bc, bsp.deps), ALU.subtract)
            eend = F5.get()
            kb.act(eend[0:64, :], dl[0:64, :], AF.Exp, scale=1.0 / 16)
            qd = B5.get()
            kb.stt(qd[0:64, :], qraw[0:64, :], 0.125, epos[0:64, :], ALU.mult, ALU.mult)
            ki = B5.get()
            kb.tt("dve", ki[0:64, :], kraw[0:64, :], eneg[0:64, :], ALU.mult)
            ke = B5.get()
            kb.tt("dve", ke[0:64, :], kraw[0:64, :], eend[0:64, :], ALU.mult)
            oT = L5.get()
            for blk in range(4):
                bs = slice(blk * 128, blk * 128 + 128)
                ps_v = cx.PQ.get()
                for k in range(NKC):
                    kb.mm(ps_v, xb[:, k, bs], wcol("gv", k), start=(k == 0), stop=(k == NKC - 1))
                vt = BQ.get()
                kb.copy("act", vt[:, :], ps_v)
                ps_t = cx.PQ.get()[:, 0:64]
                kb.mm(ps_t, ke[0:64, bs], M["ident"][0:64, 0:64])
                ket = BQ.get()
                kb.copy("dve", ket[:, 0:64], ps_t)
                ps_a = cx.PQ.get()
                kb.mm(ps_a, ki[0:64, bs], qd[0:64, bs])
                am = BQ.get()
                kb.tt("dve", am[:, :], ps_a, Mf["U_incl"], ALU.mult)
                ps_o = cx.PD[0]
                kb.mm(ps_o, vt[:, :], am[:, :], start=True, stop=False)
                for c in range(2):
                    cs = slice(blk * 128 + c * 64, blk * 128 + c * 64 + 64)
                    rs = slice(c * 64, c * 64 + 64)
                    kb.mm(ps_o[:, rs], gl_Hbf[:, :], qd[0:64, cs], start=False, stop=(c == 1))
                    ps_n = cx.PD[4][0:64, :]
                    kb.mm(ps_n, ket[rs, 0:64], vt[rs, :])
                    gc = blk * 2 + c
                    acol = epos[0:64, 64 * (t * 0 + gc) + 63:64 * gc + 64]
                    kb.stt(gl_H32[:, :], gl_H32[:, :], acol, ps_n, ALU.mult, ALU.add)
                    kb.copy("act", gl_Hbf[:, :], gl_H32[:, :])
                kb.copy("act", oT[:, bs], ps_o)
            post_norm_gate(oT, og[:, :], pc("gla_norm"), 128)

        if "gdn" in branches:
            L5.i = 0
            conv = {}
            for nm, cn in (("q", "conv_cq"), ("k", "conv_ck"), ("v", "conv_cv")):
                ps = proj_f("d" + nm, xb)
                raw = gd_halo[nm][0]
                hcol = gd_halo[nm][1]
                if t == 0:
                    kb.memset("pool", raw[:, 0:3], 0.0)
                else:
                    kb.copy("pool", raw[:, 0:3], hcol[:, 0:3])
                kb.copy("act", raw[:, 3:TT + 3], ps)
                kb.copy("pool", hcol[:, 0:3], raw[:, TT:TT + 3])
                acc = F5.get()
                kb.ts("dve", acc[:, :], raw[:, 0:TT], pc(cn, 128, 0), ALU.mult)
                for j in range(1, 4):
                    kb.stt(acc[:, :], raw[:, j:j + TT], pc(cn, 128, j), acc[:, :], ALU.mult, ALU.add)
                conv[nm] = silu_of(acc[:, :])
            qn = l2norm_f(conv["q"][:, :], M["ones"], 128 ** -0.5)
            kn32 = l2norm_f(conv["k"][:, :], M["ones"], 1.0)
            kn = B5.get()
            kb.copy("pool", kn[:, :], kn32[:, :])
            qnb = B5.get()
            kb.copy("pool", qnb[:, :], qn[:, :])
            ps_b = proj_f("dbeta", xb)
            beta = L5.get()
            tb = F5.get()
            sigmoid_into(kb, beta[:, :], ps_b, tb[:, :])
            nbeta = L5.get()
            kb.ts("pool", nbeta[:, :], beta[:, :], -1.0, ALU.mult)
            ps_a = proj_f("da", xb)
            e1 = F5.get()
            kb.act(e1[:, :], ps_a, AF.Exp, bias=pc("dt_bias"))
            spl = F5.get()
            kb.act(spl[:, :], e1[:, :], AF.Ln, bias=ONE_C)
            g = F5.get()
            kb.ts("dve", g[:, :], spl[:, :], ncol[:, 3:4], ALU.mult, -1.0, ALU.mult)
            gc = L5.get()
            kb.scan(gc[:, :], scm[:, :], g[:, :], 0.0, ALU.mult, ALU.add)
            egc = L5.get()
            kb.act(egc[:, :], gc[:, :], AF.Exp)
            g3 = gc.ap[:, :].rearrange("p (c t) -> p c t", t=64)
            dl = F5.get()
            dl3 = dl.ap[:, :].rearrange("p (c t) -> p c t", t=64)
            gl_bc = g3[:, :, 63:64].broadcast_to([128, TT // 64, 64])
            kb.tt("dve", V(dl3, dl.deps), V(gl_bc, gc.deps), V(g3, gc.deps), ALU.subtract)
            eend = F5.get()
            kb.act(eend[:, :], dl[:, :], AF.Exp)
            qdec = L5.get()
            kb.tt("dve", qdec[:, :], qn[:, :], egc[:, :], ALU.mult)
            kbg = B5.get()
            tk = F5.get()
            kb.tt("dve", tk[:, :], kn32[:, :], beta[:, :], ALU.mult)
            kb.tt("dve", kbg[:, :], tk[:, :], egc[:, :], ALU.mult)
            vb = B5.get()
            kb.tt("dve", vb[:, :], conv["v"][:, :], beta[:, :], ALU.mult)
            kend = B5.get()
            kb.tt("dve", kend[:, :], kn32[:, :], eend[:, :], ALU.mult)
            ps_z = proj_f("dz", xb)
            zs = L5.get()
            kb.copy("act", zs[:, :], ps_z)
            oT = L5.get()
            for blk in range(4):
                bs = slice(blk * 128, blk * 128 + 128)
                BL.i = 0
                Rm = gc[:, bs]
                t1 = FQ.get()
                kb.tt("dve", t1[:, :], Rm, Mf["ident"], ALU.mult)
                cc = FQ.get()
                kb.op("dve", (lambda o_, i_: (lambda h: h.tensor_reduce(o_, i_, AX.X, ALU.add)))(cc.ap[:, 0:1], t1.ap[:, :]),
                      reads=[t1], writes=[cc])
                kb.tt("dve", t1[:, :], nbeta[:, bs], Mf["ident"], ALU.mult)
                kb.op("dve", (lambda o_, i_: (lambda h: h.tensor_reduce(o_, i_, AX.X, ALU.add)))(cc.ap[:, 1:2], t1.ap[:, :]),
                      reads=[t1], writes=[cc])
                ccol, nbcol = cc[:, 0:1], cc[:, 1:2]
                tB = FQ.get()
                kb.ts("dve", tB[:, :], Rm, ccol, ALU.subtract, 0.0, ALU.min)
                eB = FQ.get()
                kb.act(eB[:, :], tB[:, :], AF.Exp)
                DBi = FQ.get()
                kb.tt("dve", DBi[:, :], eB[:, :], Mf["U_incl"], ALU.mult)
                DBs = FQ.get()
                kb.tt("pool", DBs[:, :], eB[:, :], Mf["U_strict"], ALU.mult)
                kb.tt("pool", DBs[:, :], DBs[:, :], nbeta[:, bs], ALU.mult)
                tA = FQ.get()
                kb.ts("dve", tA[:, :], Rm, ccol, ALU.subtract, 0.0, ALU.max)
                eA = FQ.get()
                kb.act(eA[:, :], tA[:, :], AF.Exp, scale=-1.0)
                kb.tt("pool", eA[:, :], eA[:, :], Mf["L_strict"], ALU.mult)
                ps_kk = cx.PQ.get()
                kb.mm(ps_kk, kn[:, bs], kn[:, bs])
                PA = BQ.get()
                kb.stt(PA[:, :], eA[:, :], nbcol, ps_kk, ALU.mult, ALU.mult)
                PB_ = BQ.get()
                kb.tt("dve", PB_[:, :], ps_kk, DBs[:, :], ALU.mult)
                TTm = neumann(kb, cx, PA[:, :], PB_[:, :], BQ, M["ident"])
                rhs = kb_rhs.get()
                ps_t = cx.PQ.get()
                kb.mm(ps_t, vb[:, bs], M["ident"])
                kb.copy("act", rhs[:, 0:128], ps_t)
                ps_t = cx.PQ.get()
                kb.mm(ps_t, kbg[:, bs], M["ident"])
                kb.copy("act", rhs[:, 128:256], ps_t)
                kendt = transpose_bf(kend[:, bs])
                ps_uw = cx.PH.get()
                kb.mm(ps_uw, TTm, rhs[:, :])
                UW = kb_rhs.get()
                kb.copy("act", UW[:, :], ps_uw)
                ps_qk = cx.PQ.get()
                kb.mm(ps_qk, kn[:, bs], qnb[:, bs])
                attB = BL.get()
                kb.tt("dve", attB[:, :], ps_qk, DBi[:, :], ALU.mult)
                ps_aw = cx.PQ.get()
                kb.mm(ps_aw, UW[:, 128:256], attB[:, :])
                qeff = BL.get()
                kb.tt("dve", qeff[:, :], qdec[:, bs], ps_aw, ALU.subtract)
                ps_o = cx.PD[1]
                kb.mm(ps_o, UW[:, 0:128], attB[:, :], start=True, stop=False)
                for c in range(2):
                    rs = slice(c * 64, c * 64 + 64)
                    gcn = blk * 2 + c
                    kb.mm(ps_o[:, rs], gd_Hbf[:, :], qeff[:, rs], start=False, stop=(c == 1))
                    ps_m = cx.PD[5]
                    kb.mm(ps_m, UW[rs, 128:256], kendt[rs, :])
                    MT = FQ.get()
                    kb.stt(MT[:, :], Mf["ident"], egc[:, 64 * gcn + 63:64 * gcn + 64], ps_m, ALU.mult, ALU.subtract)
                    ps_h = cx.PD[6]
                    kb.mm(ps_h, MT[:, :], gd_H32[:, :], start=True, stop=False)
                    kb.mm(ps_h, kendt[rs, :], UW[rs, 0:128], start=False, stop=True)
                    kb.copy("act", gd_Hbf[:, :], ps_h)
                    kb.copy("act", gd_H32[:, :], ps_h)
                kb.copy("act", oT[:, bs], ps_o)
            post_norm_gate(oT, zs[:, :], pc("gdn_norm"), 256)

        if "rwkv" in branches:
            L5.i = 0

            def shiftmix(pname, rows, mu_name, hname):
                ps = proj_f(pname, xb)
                raw, hcol = rw_halo[hname]
                if t == 0:
                    kb.memset("pool", raw[0:rows, 0:1], 0.0)
                else:
                    kb.copy("pool", raw[0:rows, 0:1], hcol[0:rows, 0:1])
                kb.copy("act", raw[0:rows, 1:TT + 1], ps)
                kb.copy("pool", hcol[0:rows, 0:1], raw[0:rows, TT:TT + 1])
                dd = F5.get()
                kb.tt("dve", dd[0:rows, :], raw[0:rows, 0:TT], raw[0:rows, 1:TT + 1], ALU.subtract)
                out = L5.get()
                kb.stt(out[0:rows, :], dd[0:rows, :], pc(mu_name, rows), raw[0:rows, 1:TT + 1], ALU.mult, ALU.add)
                return out
            r_ = shiftmix("rr", 128, "mu_r", "r")
            k_ = shiftmix("rk", 128, "mu_k", "k")
            v_ = shiftmix("rv", 128, "mu_v", "v")
            wl = shiftmix("rwl", 64, "mu_wl", "wl")
            al = shiftmix("ral", 64, "mu_al", "al")
            gl = shiftmix("rgl", 128, "mu_gl", "gl")
            tmp = F5.get()
            sgt = F5.get()
            sigmoid_into(kb, sgt[0:64, :], wl[0:64, :], tmp[0:64, :], scale=2.0)
            th = B5.get()
            kb.ts("dve", th[0:64, :], sgt[0:64, :], 2.0, ALU.mult, -1.0, ALU.add)
            ps_z = cx.PB.get()
            kb.mm(ps_z, sm("w_up", 64), th[0:64, :])
            sgw = L5.get()
            tmp = F5.get()
            sigmoid_into(kb, sgw[:, :], ps_z, tmp[:, :], bias=ncol[:, 0:1])
            cs = L5.get()
            kb.scan(cs[:, :], scm[:, :], sgw[:, :], 0.0, ALU.mult, ALU.add)
            albf = B5.get()
            kb.copy("pool", albf[0:64, :], al[0:64, :])
            ps_a = cx.PB.get()
            kb.mm(ps_a, sm("a_up", 64), albf[0:64, :])
            a_ = L5.get()
            tmp = F5.get()
            sigmoid_into(kb, a_[:, :], ps_a, tmp[:, :], bias=ncol[:, 1:2])
            sgg = F5.get()
            tmp = F5.get()
            sigmoid_into(kb, sgg[:, :], gl[:, :], tmp[:, :])
            sggb = B5.get()
            kb.copy("pool", sggb[:, :], sgg[:, :])
            ps_g = cx.PB.get()
            kb.mm(ps_g, sm("g_up", 128), sggb[:, :])
            gte = L5.get()
            kb.copy("act", gte[:, :], ps_g)
            if l > 0:
                ps = proj_f("rvr", xb)
                raw, hcol = rw_halo["vr"]
                if t == 0:
                    kb.memset("pool", raw[0:32, 0:1], 0.0)
                else:
                    kb.copy("pool", raw[0:32, 0:1], hcol[0:32, 0:1])
                kb.copy("act", raw[0:32, 1:TT + 1], ps)
                kb.copy("pool", hcol[0:32, 0:1], raw[0:32, TT:TT + 1])
                dd = F5.get()
                kb.tt("dve", dd[0:32, :], raw[0:32, 0:TT], raw[0:32, 1:TT + 1], ALU.subtract)
                vrl = B5.get()
                kb.stt(vrl[0:32, :], dd[0:32, :], pc("mu_vr", 32), raw[0:32, 1:TT + 1], ALU.mult, ALU.add)
                ps_v = cx.PB.get()
                kb.mm(ps_v, sm("v_up", 32), vrl[0:32, :])
                vg = F5.get()
                tmp = F5.get()
                sigmoid_into(kb, vg[:, :], ps_v, tmp[:, :], bias=ncol[:, 2:3])
                vf = F5.get()
                kb.dma("sp", vf[:, :], d["vfirst"][:, tsl])
                kb.tt("dve", vf[:, :], vf[:, :], v_[:, :], ALU.subtract)
                kb.tt("dve", vf[:, :], vf[:, :], vg[:, :], ALU.mult)
                kb.tt("dve", v_[:, :], v_[:, :], vf[:, :], ALU.add)
            else:
                kb.dma("sp", d["vfirst"][:, tsl], v_[:, :])
            kx = F5.get()
            kb.ts("dve", kx[:, :], k_[:, :], pc("k_k"), ALU.mult)
            kk = l2norm_f(kx[:, :], M["ones_blk"], 1.0)
            tk = F5.get()
            kb.ts("dve", tk[:, :], a_[:, :], -1.0, ALU.add, pc("k_a"), ALU.mult)
            kmod = L5.get()
            kb.stt(kmod[:, :], tk[:, :], 1.0, k_[:, :], ALU.add, ALU.mult)
            bb = L5.get()
            kb.tt("dve", bb[:, :], kk[:, :], a_[:, :], ALU.mult)
            rk = B5.get()
            kb.stt(rk[:, :], r_[:, :], pc("r_k"), kmod[:, :], ALU.mult, ALU.mult)
            ps_bn = cx.PB.get()
            kb.mm(ps_bn, M["ones_blk"], rk[:, :])
            bonus = L5.get()
            kb.tt("dve", bonus[:, :], ps_bn, v_[:, :], ALU.mult)
            ecw = F5.get()
            kb.act(ecw[:, :], cs[:, :], AF.Exp, scale=-C_W)
            encw = L5.get()
            kb.act(encw[:, :], cs[:, :], AF.Exp, scale=C_W)
            dx = F5.get()
            kb.tt("dve", dx[:, :], cs[:, :], sgw[:, :], ALU.subtract)
            eprev = F5.get()
            kb.act(eprev[:, :], dx[:, :], AF.Exp, scale=-C_W)
            c3 = cs.ap[:, :].rearrange("p (c t) -> p c t", t=64)
            dl = F5.get()
            dl3 = dl.ap[:, :].rearrange("p (c t) -> p c t", t=64)
            cl_bc = c3[:, :, 63:64].broadcast_to([128, TT // 64, 64])
            kb.tt("dve", V(dl3, dl.deps), V(cl_bc, cs.deps), V(c3, cs.deps), ALU.subtract)
            eend = F5.get()
            kb.act(eend[:, :], dl[:, :], AF.Exp, scale=-C_W)
            gam = L5.get()
            kb.copy("pool", gam[:, :], ecw[:, :])
            AR = rw_AR
            AR4 = AR.ap[:, :].rearrange("p (b two i) -> p b two i", two=2, i=128)
            e4 = eprev.ap[:, :].rearrange("p (b i) -> p b i", i=128)
            k4 = kk.ap[:, :].rearrange("p (b i) -> p b i", i=128)
            kb.stt(V(AR4[:, :, 0, :], AR.deps), V(k4, kk.deps), -1.0, V(e4, eprev.deps), ALU.mult, ALU.mult)
            r4 = r_.ap[:, :].rearrange("p (b i) -> p b i", i=128)
            g4 = ecw.ap[:, :].rearrange("p (b i) -> p b i", i=128)
            kb.tt("dve", V(AR4[:, :, 1, :], AR.deps), V(r4, r_.deps), V(g4, ecw.deps), ALU.mult)
            bt = B5.get()
            kb.tt("dve", bt[:, :], bb[:, :], encw[:, :], ALU.mult)
            kt = B5.get()
            kb.tt("dve", kt[:, :], kmod[:, :], encw[:, :], ALU.mult)
            bendT = B5.get()
            kb.tt("dve", bendT[:, :], bb[:, :], eend[:, :], ALU.mult)
            kendT = B5.get()
            kb.tt("dve", kendT[:, :], kmod[:, :], eend[:, :], ALU.mult)
            vbf = B5.get()
            kb.copy("pool", vbf[:, :], v_[:, :])
            yT_ = L5.get()
            for blk in range(4):
                bs = slice(blk * 128, blk * 128 + 128)
                ARb = AR[:, blk * 256:blk * 256 + 256]
                BL.i = 0
                At_tok = transpose_bf(ARb[:, 0:128])
                V_tok = transpose_bf(vbf[:, bs])
                Bend = transpose_bf(bendT[:, bs])
                Kend = transpose_bf(kendT[:, bs])
                psQ = cx.PD[2]
                psY = cx.PD[3]
                WUs = []
                for h in range(2):
                    hp = slice(64 * h, 64 * h + 64)
                    ps1 = cx.PH.get()
                    kb.mm(ps1, bt[hp, bs], ARb[hp, :])
                    SB1 = kb_rhs.get()
                    kb.tt("dve", SB1[:, :], ps1, cx.mf[:, 0:256], ALU.mult)
                    ps2 = cx.PH.get()
                    kb.mm(ps2, kt[hp, bs], ARb[hp, :])
                    SB2 = kb_rhs.get()
                    kb.tt("dve", SB2[:, :], ps2, cx.mf[:, 0:256], ALU.mult)
                    psA = cx.PQ.get()
                    kb.mm(psA, ARb[hp, 0:128], bt[hp, bs])
                    PA = BQ.get()
                    kb.tt("dve", PA[:, :], psA, Mf["L_strict"], ALU.mult)
                    TTm = neumann(kb, cx, PA[:, :], SB1[:, 0:128], BQ, M["ident"])
                    psX = cx.PQ.get()
                    kb.mm(psX[:, 0:64], SB2[:, 0:128], V_tok[:, hp])
                    Xb = BQ.get()
                    kb.copy("act", Xb[:, 0:64], psX[:, 0:64])
                    psWU = cx.PQ.get()
                    kb.mm(psWU[:, 0:64], TTm, At_tok[:, hp])
                    kb.mm(psWU[:, 64:128], TTm, Xb[:, 0:64])
                    WU = BL.get()
                    kb.copy("act", WU[:, :], psWU)
                    WUs.append(WU)
                    kb.mm(psQ[hp, :], WU[:, 0:64], SB1[:, 128:256])
                    kb.mm(psY[hp, :], WU[:, 64:128], SB1[:, 128:256], start=True, stop=False)
                    kb.mm(psY[hp, :], V_tok[:, hp], SB2[:, 128:256], start=False, stop=False)
                qeff = BL.get()
                kb.tt("dve", qeff[:, :], ARb[:, 128:256], psQ, ALU.add)
                for c in range(2):
                    rs = slice(c * 64, c * 64 + 64)
                    gcn = blk * 2 + c
                    for h in range(2):
                        hp = slice(64 * h, 64 * h + 64)
                        kb.mm(psY[hp, rs], rw_Hbf[hp, :], qeff[hp, rs], start=False, stop=(c == 1))
                    psM = cx.PD[5]
                    for h in range(2):
                        hp = slice(64 * h, 64 * h + 64)
                        kb.mm(psM[hp, 0:64], WUs[h][rs, 0:64], Bend[rs, hp])
                    MT = rw_MT
                    for h in range(2):
                        hp = slice(64 * h, 64 * h + 64)
                        kb.stt(MT[hp, 64 * h:64 * h + 64], Mf["istack"][hp, :], gam[hp, 64 * gcn + 63:64 * gcn + 64],
                               psM[hp, 0:64], ALU.mult, ALU.add)
                    psH = cx.PD[6]
                    kb.mm(psH[:, 0:64], MT[:, :], rw_H32[:, :], start=True, stop=False)
                    for h in range(2):
                        hp = slice(64 * h, 64 * h + 64)
                        kb.mm(psH[hp, 0:64], Bend[rs, hp], WUs[h][rs, 64:128], start=False, stop=False)
                        kb.mm(psH[hp, 0:64], Kend[rs, hp], V_tok[rs, hp], start=False, stop=True)
                    kb.copy("act", rw_Hbf[:, :], psH[:, 0:64])
                    kb.copy("act", rw_H32[:, :], psH[:, 0:64])
                kb.copy("act", yT_[:, bs], psY)
            ps_m = cx.PB.get()
            kb.mm(ps_m, Mf["ones_blk"], yT_[:, :])
            yc = F5.get()
            kb.stt(yc[:, :], ps_m, -1.0 / 64, yT_[:, :], ALU.mult, ALU.add)
            ysq = F5.get()
            kb.act(ysq[:, :], yc[:, :], AF.Square)
            ps_v2 = cx.PB.get()
            kb.mm(ps_v2, Mf["ones_blk"], ysq[:, :])
            tmp = F5.get()
            rstd = F5.get()
            rsqrt_act(kb, rstd[:, :], ps_v2, 1.0 / 64, EPS_R, tmp[:, :])
            kb.stt(yc[:, :], yc[:, :], pc("ln_g"), rstd[:, :], ALU.mult, ALU.mult)
            kb.stt(yc[:, :], yc[:, :], pc("ln_b"), bonus[:, :], ALU.add, ALU.add)
            yo = B5.get()
            kb.tt("dve", yo[:, :], yc[:, :], gte[:, :], ALU.mult)
            kb.dma("sp", d["ydst"](384, 128, t), yo[:, :])

        if "mla" in branches:
            L5.i = 0
            posi = mla_posi
            kb.dma("sp", posi[:, :], d["pos"][:, tsl])
            posf = L5.get()
            kb.copy("dve", posf[0:96, :], posi[:, :])
            ang = L5.get()
            kb.ts("dve", ang[0:96, :], posf[0:96, :], pc("invf", 96), ALU.mult)
            tabs = []
            for phase in (math.pi / 2, 0.0):
                y = F5.get()
                kb.ts("dve", y[0:96, :], ang[0:96, :], phase, ALU.add, 1.0 / (2 * math.pi), ALU.mult)
                ni = mla_ni
                kb.copy("dve", ni[:, :], y[0:96, :])
                nf = F5.get()
                kb.copy("dve", nf[0:96, :], ni[:, :])
                r = F5.get()
                kb.stt(r[0:96, :], nf[0:96, :], -2 * math.pi, ang[0:96, :], ALU.mult, ALU.add)
                if phase != 0.0:
                    kb.ts("dve", r[0:96, :], r[0:96, :], phase, ALU.add)
                m1 = F5.get()
                kb.ts("dve", m1[0:96, :], r[0:96, :], math.pi, ALU.is_gt, -2 * math.pi, ALU.mult)
                kb.tt("dve", r[0:96, :], r[0:96, :], m1[0:96, :], ALU.add)
                kb.ts("dve", m1[0:96, :], r[0:96, :], -math.pi, ALU.is_lt, 2 * math.pi, ALU.mult)
                kb.tt("dve", r[0:96, :], r[0:96, :], m1[0:96, :], ALU.add)
                tab = L5.get()
                kb.act(tab[0:96, :], r[0:96, :], AF.Sin)
                tabs.append(tab)
            C96, S96 = tabs
            kb.ts("dve", S96[0:96, :], S96[0:96, :], pc("rsign", 96), ALU.mult)
            qn = []
            qr = []
            sqs = []
            for kc in range(2):
                ps = proj_f("qlat", xb, rows=128, c0=128 * kc)
                rr = L5.get()
                kb.copy("act", rr[:, :], ps)
                sq = B5.get()
                kb.act(sq[:, :], rr[:, :], AF.Square)
                qr.append(rr)
                sqs.append(sq)
            ps_s = cx.PB.get()
            kb.mm(ps_s, M["ones"], sqs[0][:, :], start=True, stop=False)
            kb.mm(ps_s, M["ones"], sqs[1][:, :], start=False, stop=True)
            tmp = F5.get()
            rstd = F5.get()
            rsqrt_act(kb, rstd[:, :], ps_s, 1.0 / 256, EPS_N, tmp[:, :])
            for kc in range(2):
                q_ = B5.get()
                kb.tt("dve", q_[:, :], qr[kc][:, :], rstd[:, :], ALU.mult)
                qn.append(q_)
            ps = proj_f("ckv", xb)
            cr = L5.get()
            kb.copy("act", cr[:, :], ps)
            sq = B5.get()
            kb.act(sq[:, :], cr[:, :], AF.Square)
            ps_s = cx.PB.get()
            kb.mm(ps_s, M["ones"], sq[:, :])
            tmp = F5.get()
            rstd = F5.get()
            rsqrt_act(kb, rstd[:, :], ps_s, 1.0 / 128, EPS_N, tmp[:, :])
            cn = B5.get()
            kb.tt("dve", cn[:, :], cr[:, :], rstd[:, :], ALU.mult)
            psK = cx.PB.get()
            proj_f("krope", xb, rows=32, out=psK[64:96, :])
            t1 = F5.get()
            kb.tt("dve", t1[64:96, :], psK[64:96, :], C96[64:96, :], ALU.mult)
            psK2 = cx.PB.get()
            proj_f("krope_sw", xb, rows=32, out=psK2[64:96, :])
            t2 = F5.get()
            kb.tt("dve", t2[64:96, :], psK2[64:96, :], S96[64:96, :], ALU.mult)
            krot = B5.get()
            kb.tt("dve", krot[64:96, :], t1[64:96, :], t2[64:96, :], ALU.add)
            for hh in range(2):
                kb.dma("sp", d["KT"][hh, 64:96, tsl], krot[64:96, :])
                psn = cx.PB.get()
                kb.mm(psn[0:64, :], ukg[:, hh, :], cn[:, :])
                kn_ = B5.get()
                kb.copy("act", kn_[0:64, :], psn[0:64, :])
                kb.dma("sp", d["KT"][hh, 0:64, tsl], kn_[0:64, :])
                psP = cx.PB.get()
                kb.mm(psP[0:96, :], uqg[:, hh, 0, 0, :], qn[0][:, :], start=True, stop=False)
                kb.mm(psP[0:96, :], uqg[:, hh, 1, 0, :], qn[1][:, :], start=False, stop=True)
                t1 = F5.get()
                kb.tt("dve", t1[0:96, :], psP[0:96, :], C96[0:96, :], ALU.mult)
                psS = cx.PB.get()
                kb.mm(psS[0:96, :], uqg[:, hh, 0, 1, :], qn[0][:, :], start=True, stop=False)
                kb.mm(psS[0:96, :], uqg[:, hh, 1, 1, :], qn[1][:, :], start=False, stop=True)
                t2 = F5.get()
                kb.tt("dve", t2[0:96, :], psS[0:96, :], S96[0:96, :], ALU.mult)
                qb_ = B5.get()
                kb.tt("dve", qb_[0:96, :], t1[0:96, :], t2[0:96, :], ALU.add)
                kb.dma("sp", d["QT"][hh, :, tsl], qb_[0:96, :])
            for blk in range(4):
                bs = slice(blk * 128, blk * 128 + 128)
                psV = cx.PQ.get()
                kb.mm(psV, cn[:, bs], uvg[:, :])
                vt = mla_vt[blk % 2]
                kb.op("act", (lambda o_, i_: (lambda h: h.activation(o_, i_, AF.Copy)))(
                    vt.ap[:, :, 0:64], psV.ap.rearrange("p (h e) -> p h e", e=64)), reads=[psV], writes=[vt])
                kb.dma("sp", d["VT"][:, :, t * 4 + blk, :], vt[:, :, :])
    kb.flush()
    st.close()
    return cx


def build_p1b(kb, S, d):
    import os as _os
    if "build_p1b" in _os.environ.get("DBG_SKIP", "").split(","):
        return None

    st = contextlib.ExitStack()
    nt = S // TT
    nb = S // 128
    banks = [kb.psum("ab%d" % i, [128, 512], F32, st) for i in range(8)]
    KTs = kb.sbuf("a_KT", [96, S], BF16, st)
    Vx = kb.sbuf("a_Vx", [128, nb, 65], BF16, st)
    onesf = kb.sbuf("a_ones", [128, 64], F32, st)
    kb.memset("pool", onesf[:, :], 1.0)
    dms = []
    for dd in range(4):
        m = kb.sbuf("a_dm%d" % dd, [128, TT], BF16, st)
        kb.memset("pool", m[:, :], 1.0)
        ma = m.ap[:, :]
        kb.op("pool", (lambda a, b: (lambda h: h.affine_select(a, a, pattern=[[1, TT]], compare_op=ALU.is_ge,
                                                               fill=0.0, base=b, channel_multiplier=-1)))(ma, -128 * dd),
              reads=[m], writes=[m])
        dms.append(m)
    Qts = [kb.sbuf("a_Q%d" % i, [96, TT], BF16, st) for i in range(2)]
    Ps = Pool(kb, "a_P", 4, [128, TT], BF16, stack=st)
    rsb = kb.sbuf("a_rs", [65, TT], F32, st)
    rbs = Pool(kb, "a_rb", 2, [64, TT], F32, stack=st)
    yos = Pool(kb, "a_yo", 2, [64, TT], BF16, stack=st)
    si = 0
    for hh in range(2):
        for c0 in range(0, S, 2048):
            c1 = min(S, c0 + 2048)
            kb.dma("sp", KTs[:, c0:c1], d["KT"][hh, :, c0:c1])
        for b0 in range(0, nb, 32):
            b1 = min(nb, b0 + 32)
            kb.dma("sp", Vx[:, b0:b1, :], d["VT"][:, hh, b0:b1, :])
        for t in range(nt):
            tsl = slice(t * TT, (t + 1) * TT)
            Qt = Qts[t % 2]
            kb.dma("sp", Qt[:, :], d["QT"][hh, :, tsl])
            psO = banks[4 + (t % 2)]
            nkb = 4 * (t + 1)
            for kb_ in range(nkb):
                dd = kb_ - 4 * t
                c0 = 128 * dd if dd > 0 else 0
                psS = banks[si % 3]
                si += 1
                kb.mm(psS[:, c0:TT], KTs[:, kb_ * 128:kb_ * 128 + 128], Qt[:, c0:TT])
                P = Ps.get()
                kb.act(P[:, c0:TT], psS[:, c0:TT], AF.Exp)
                if dd >= 0:
                    kb.tt("pool", P[:, c0:TT], P[:, c0:TT], dms[dd][:, c0:TT], ALU.mult)
                kb.mm(psO[0:65, c0:TT], Vx[:, kb_, :], P[:, c0:TT], start=(kb_ == 0), stop=(kb_ == nkb - 1))
            kb.copy("act", rsb[64:65, :], psO[64:65, :])
            psB = banks[6]
            kb.mm(psB[0:64, :], onesf[64:65, 0:64], rsb[64:65, :])
            rb = rbs.get()
            kb.recip(rb[:, :], psB[0:64, :])
            yo = yos.get()
            kb.tt("dve", yo[:, :], psO[0:64, :], rb[:, :], ALU.mult)
            kb.dma("sp", d["ydst"](hh * 64, 64, t), yo[:, :])
    kb.flush()
    st.close()


def pack_p2(inp, l):
    P = Packer(128)
    for nm in ("ln1_g", "ln1_b", "ln2_g", "ln2_b"):
        P.add(nm, inp[nm][l].reshape(8, 128).T)
    P.add("rbias", np.repeat(inp["router_bias"][l][None, :], 128, 0))
    P.add("rw", inp["router_w"].reshape(8, 128, 16).transpose(1, 0, 2).reshape(128, 128))
    sel = np.zeros((16, 16, 128), np.float32)
    for e in range(16):
        sel[e, e, :] = 1.0
    P.add("sel", np.concatenate([sel.reshape(16, 2048), np.zeros((112, 2048), np.float32)], 0))
    return P


def ln_fmajor(kb, cx, h32, gname, bname, pcs, F5, EPS, out_fn):
    Mf = cx.Mf
    ps_m = cx.PB.get()
    for j in range(8):
        kb.mm(ps_m, Mf["ones"], h32[:, j, :], start=(j == 0), stop=(j == 7))
    mean = F5.get()
    kb.act(mean[:, :], ps_m, AF.Copy, scale=1.0 / 1024)
    for j in range(8):
        kb.tt("pool", h32[:, j, :], h32[:, j, :], mean[:, :], ALU.subtract)
    ps_v = cx.PB.get()
    for j in range(8):
        sq = F5.get()
        kb.act(sq[:, :], h32[:, j, :], AF.Square)
        kb.mm(ps_v, Mf["ones"], sq[:, :], start=(j == 0), stop=(j == 7))
    tmp = F5.get()
    rstd = F5.get()
    rsqrt_act(kb, rstd[:, :], ps_v, 1.0 / 1024, EPS, tmp[:, :])
    for j in range(8):
        o = F5.get()
        kb.tt("dve", o[:, :], h32[:, j, :], rstd[:, :], ALU.mult)
        kb.ts("dve", o[:, :], o[:, :], pcs(gname, j), ALU.mult, pcs(bname, j), ALU.add)
        out_fn(j, o)


def build_p2(kb, T, l, d):
    import os as _os
    if "build_p2" in _os.environ.get("DBG_SKIP", "").split(","):
        return None

    nt = T // TT
    Po = d["P2off"]

    st = contextlib.ExitStack()
    cx = Ctx()
    setup_common(kb, cx, d["masks"], st)
    wst = Pool(kb, "wst2", 3, [128, 512], F32, stack=st)
    Wg = kb.sbuf("Wg", [128, 8, 4096], BF16, st)
    for k in range(8):
        for c0 in range(0, 4096, 512):
            s_ = wst.get()
            kb.dma("sp", s_[:, :], d["Wgate"][128 * k:128 * k + 128, c0:c0 + 512])
            kb.copy("pool", Wg[:, k, c0:c0 + 512], s_[:, :])
    Wb = kb.sbuf("Wb", [128, 4, 4, 1024], BF16, st)
    for n in range(4):
        for kc in range(4):
            for c0 in range(0, 1024, 512):
                s_ = wst.get()
                kb.dma("sp", s_[:, :], d["Wb"][n, 128 * kc:128 * kc + 128, c0:c0 + 512])
                kb.copy("pool", Wb[:, n, kc, c0:c0 + 512], s_[:, :])
    F5 = Pool(kb, "p2f", 8, [128, TT], F32, stack=st)
    yst = Pool(kb, "p2ys", 6, [128, TT], BF16, stack=st)
    OH = kb.sbuf("p2oh", [128, 4], F32, st)
    kb.dma("sp", OH[:, :], d["onehot"])
    xbf = kb.sbuf("p2x", [128, 8, TT], BF16, st)
    ybf = kb.sbuf("p2y", [128, 16, TT], BF16, st)
    m32 = kb.sbuf("p2m", [128, 8, TT], F32, st)
    mbf = kb.sbuf("p2mb", [128, 8, TT], BF16, st)
    for t in range(nt):
        tsl = slice(t * TT, (t + 1) * TT)
        for k in range(8):
            s_ = wst.get()
            kb.dma("sp", s_[:, :], d["xsrc2"](k, t))
            kb.copy("pool", xbf[:, k, :], s_[:, :])
        for c in range(16):
            n_, hg_ = c // 4, c % 4
            r0 = hg_ * 512 + n_ * 128
            for jq in range(4):
                yq = yst.get()
                kb.dma("sp", yq[:, :], d["ysrc"](r0, jq, t))
                if jq == 0:
                    kb.ts("pool", ybf[:, c, :], yq[:, :], OH[:, 0:1], ALU.mult)
                else:
                    kb.stt(ybf[:, c, :], yq[:, :], OH[:, jq:jq + 1], ybf[:, c, :], ALU.mult, ALU.add)
        for n in range(4):
            for j in range(8):
                ps_g = cx.PB.get()
                for k in range(8):
                    kb.mm(ps_g, Wg[:, k, n * 1024 + 128 * j:n * 1024 + 128 * j + 128], xbf[:, k, :],
                          start=(k == 0), stop=(k == 7))
                e = F5.get()
                kb.act(e[:, :], ps_g, AF.Exp, scale=-1.0)
                kb.ts("pool", e[:, :], e[:, :], 1.0, ALU.add)
                sg = F5.get()
                kb.recip(sg[:, :], e[:, :])
                ps_p = cx.banks[2 + (j % 2)][:, :]
                for kc in range(4):
                    kb.mm(ps_p, Wb[:, n, kc, 128 * j:128 * j + 128], ybf[:, 4 * n + kc, :],
                          start=(kc == 0), stop=(kc == 3))
                if n == 0:
                    kb.tt("dve", m32[:, j, :], sg[:, :], ps_p, ALU.mult)
                else:
                    tq = F5.get()
                    kb.tt("dve", tq[:, :], sg[:, :], ps_p, ALU.mult)
                    kb.tt("pool", m32[:, j, :], m32[:, j, :], tq[:, :], ALU.add)
        for j in range(8):
            kb.copy("act", mbf[:, j, :], m32[:, j, :])
            kb.dma("sp", d["mT"][128 * j:128 * j + 128, tsl], mbf[:, j, :])
    kb.flush()
    st.close()

    st = contextlib.ExitStack()
    cx = Ctx()
    setup_common(kb, cx, d["masks"], st)
    PC = kb.sbuf("PC2", [128, d["NP2"]], F32, st)
    kb.dma("sp", PC[:, :], d["PC2"])

    def pcs(name, j):
        o, w = Po[name]
        return PC[:, o + j:o + j + 1]
    cst = kb.sbuf("cst2", [128, 2], F32, st)
    kb.memset("pool", cst[:, 0:1], LN_EPS)
    EPS = cst[:, 0:1]
    wst = Pool(kb, "wst3", 3, [128, 512], F32, stack=st)
    Wo = kb.sbuf("Wo", [128, 8, 1024], BF16, st)
    for k in range(8):
        for c0 in range(0, 1024, 512):
            s_ = wst.get()
            kb.dma("sp", s_[:, :], d["Wout"][128 * k:128 * k + 128, c0:c0 + 512])
            kb.copy("pool", Wo[:, k, c0:c0 + 512], s_[:, :])
    F5 = Pool(kb, "p2g", 10, [128, TT], F32, stack=st)
    mbf = kb.sbuf("p2mb2", [128, 8, TT], BF16, st)
    x32 = kb.sbuf("p2x32", [128, 8, TT], F32, st)
    h32 = kb.sbuf("p2h32", [128, 8, TT], F32, st)
    for t in range(nt):
        tsl = slice(t * TT, (t + 1) * TT)
        for k in range(8):
            kb.dma("sp", mbf[:, k, :], d["mT"][128 * k:128 * k + 128, tsl])
            kb.dma("sp", x32[:, k, :], d["xsrc2"](k, t))
        for j in range(8):
            ps = cx.PB.get()
            for k in range(8):
                kb.mm(ps, Wo[:, k, 128 * j:128 * j + 128], mbf[:, k, :], start=(k == 0), stop=(k == 7))
            kb.stt(h32[:, j, :], x32[:, j, :], ALPHA, ps, ALU.mult, ALU.add)

        def out1(j, o, tsl=tsl):
            kb.dma("sp", d["x1T"][128 * j:128 * j + 128, tsl], o[:, :])
        ln_fmajor(kb, cx, h32, "ln1_g", "ln1_b", pcs, F5, EPS, out1)
    kb.flush()
    st.close()

    HT = min(T, 2048)
    for half in range(T // HT):
        st = contextlib.ExitStack()
        cx = Ctx()
        setup_common(kb, cx, d["masks"], st)
        Mf = cx.Mf
        PC = kb.sbuf("PC2b", [128, d["NP2"]], F32, st)
        kb.dma("sp", PC[:, :], d["PC2"])
        PCb = kb.sbuf("PC2bb", [16, 2048], BF16, st)
        o_sel = Po["sel"][0]
        kb.copy("pool", PCb[:, :], PC[0:16, o_sel:o_sel + 2048])

        def pcs(name, j):
            o, w = Po[name]
            return PC[:, o + j:o + j + 1]
        cst = kb.sbuf("cst3", [128, 2], F32, st)
        kb.memset("pool", cst[:, 0:1], LN_EPS)
        EPS = cst[:, 0:1]
        o_rw = Po["rw"][0]
        o_rb = Po["rbias"][0]
        wst = Pool(kb, "wst4", 4, [128, 512], F32, stack=st)
        F5 = Pool(kb, "p2h", 8, [128, TT], F32, stack=st)
        S16 = Pool(kb, "s16", 24, [128, 16], F32, stack=st)
        x1bf = kb.sbuf("x1bf", [128, 8, HT], BF16, st)
        acc = kb.sbuf("acc", [128, 8, HT], F32, st)
        cTb = kb.sbuf("cTb", [16, HT], BF16, st)
        nth = HT // TT
        for t in range(nth):
            g0 = half * HT + t * TT
            rps = [cx.banks[b][:, 0:16] for b in range(4)]
            for k in range(8):
                s_ = wst.get()
                kb.dma("sp", s_[:, :], d["x1T"][128 * k:128 * k + 128, g0:g0 + TT])
                kb.copy("pool", x1bf[:, k, t * TT:(t + 1) * TT], s_[:, :])
                for blk in range(4):
                    kb.mm(rps[blk], s_[:, blk * 128:blk * 128 + 128], PC[:, o_rw + 16 * k:o_rw + 16 * k + 16],
                          start=(k == 0), stop=(k == 7))
            for blk in range(4):
                e = S16.get()
                kb.act(e[:, :], rps[blk], AF.Exp, scale=-1.0)
                kb.ts("dve", e[:, :], e[:, :], 1.0, ALU.add)
                sc = S16.get()
                kb.recip(sc[:, :], e[:, :])
                bi = S16.get()
                kb.tt("dve", bi[:, :], sc[:, :], PC[:, o_rb:o_rb + 16], ALU.add)
                b4 = bi.ap[:, :].rearrange("p (g e) -> p g e", e=4)
                p6 = S16.get()
                p6v = p6.ap[:, :].rearrange("p (g s) -> p g s", s=4)
                p6b = S16.get()
                p6bv = p6b.ap[:, :].rearrange("p (g s) -> p g s", s=4)
                pairs = [(0, 1), (0, 2), (0, 3), (1, 2), (1, 3), (2, 3)]
                for pi, (i_, j_) in enumerate(pairs):
                    dst = p6v[:, :, pi:pi + 1] if pi < 4 else p6bv[:, :, pi - 4:pi - 3]
                    dd = p6 if pi < 4 else p6b
                    kb.tt("dve", V(dst, dd.deps), V(b4[:, :, i_:i_ + 1], bi.deps), V(b4[:, :, j_:j_ + 1], bi.deps), ALU.add)
                gs = S16.get()
                gsv = gs.ap[:, 0:4].rearrange("p (g o) -> p g o", o=1)
                kb.tt("dve", V(gsv, gs.deps), V(p6v[:, :, 0:1], p6.deps), V(p6v[:, :, 1:2], p6.deps), ALU.max)
                kb.tt("dve", V(gsv, gs.deps), V(gsv, gs.deps), V(p6v[:, :, 2:3], p6.deps), ALU.max)
                kb.tt("dve", V(gsv, gs.deps), V(gsv, gs.deps), V(p6v[:, :, 3:4], p6.deps), ALU.max)
                kb.tt("dve", V(gsv, gs.deps), V(gsv, gs.deps), V(p6bv[:, :, 0:1], p6b.deps), ALU.max)
                kb.tt("dve", V(gsv, gs.deps), V(gsv, gs.deps), V(p6bv[:, :, 1:2], p6b.deps), ALU.max)
                gm = S16.get()
                kb.op("dve", (lambda o_, i_: (lambda h: h.tensor_reduce(o_, i_, AX.X, ALU.max)))(gm.ap[:, 0:1], gs.ap[:, 0:4]),
                      reads=[gs], writes=[gm])
                ing = S16.get()
                kb.ts("dve", ing[:, 0:4], gs[:, 0:4], gm[:, 0:1], ALU.is_ge)
                off = S16.get()
                kb.ts("dve", off[:, 0:4], ing[:, 0:4], 1e30, ALU.mult, -1e30, ALU.add)
                mk = S16.get()
                mk4 = mk.ap[:, :].rearrange("p (g e) -> p g e", e=4)
                ing_bc = ing.ap[:, 0:4].rearrange("p (g o) -> p g o", o=1).broadcast_to([128, 4, 4])
                off_bc = off.ap[:, 0:4].rearrange("p (g o) -> p g o", o=1).broadcast_to([128, 4, 4])
                kb.tt("dve", V(mk4, mk.deps), V(b4, bi.deps), V(ing_bc, ing.deps), ALU.mult)
                kb.tt("dve", V(mk4, mk.deps), V(mk4, mk.deps), V(off_bc, off.deps), ALU.add)
                t8 = S16.get()
                kb.op("dve", (lambda o_, i_: (lambda h: h.max(o_, i_)))(t8.ap[:, 0:8], mk.ap[:, :]), reads=[mk], writes=[t8])
                sel = S16.get()
                kb.ts("dve", sel[:, :], mk[:, :], t8[:, 1:2], ALU.is_ge)
                ws = S16.get()
                kb.tt("dve", ws[:, :], sel[:, :], sc[:, :], ALU.mult)
                den = S16.get()
                kb.op("dve", (lambda o_, i_: (lambda h: h.tensor_reduce(o_, i_, AX.X, ALU.add)))(den.ap[:, 0:1], ws.ap[:, :]),
                      reads=[ws], writes=[den])
                kb.recip(den[:, 1:2], den[:, 0:1])
                cmb = S16.get()
                kb.ts("dve", cmb[:, :], ws[:, :], den[:, 1:2], ALU.mult)
                ps_t = cx.banks[4][0:16, 0:128]
                kb.mm(ps_t, cmb[:, :], Mf["ident"])
                kb.copy("act", cTb[:, t * TT + blk * 128:t * TT + blk * 128 + 128], ps_t)
        st2 = contextlib.ExitStack()
        Wgs = [kb.sbuf("Wge%d" % i, [128, 8, 512], BF16, st2) for i in range(2)]
        Wus = [kb.sbuf("Wue%d" % i, [128, 8, 512], BF16, st2) for i in range(2)]
        Wds = [kb.sbuf("Wde%d" % i, [128, 4, 1024], BF16, st2) for i in range(2)]
        cbcs = Pool(kb, "cbc", 2, [128, TT], BF16, stack=st2)
        acts = Pool(kb, "actb", 8, [128, TT], BF16, stack=st2)
        gi = 0
        for e in range(16):
            Wg_, Wu_, Wd_ = Wgs[e % 2], Wus[e % 2], Wds[e % 2]
            for k in range(8):
                for (dst, src) in ((Wg_, d["moe_g"]), (Wu_, d["moe_u"])):
                    s_ = wst.get()
                    kb.dma("sp", s_[:, :], src[e, 128 * k:128 * k + 128, :])
                    kb.copy("pool", dst[:, k, :], s_[:, :])
            for fc in range(4):
                for c0 in (0, 512):
                    s_ = wst.get()
                    kb.dma("sp", s_[:, :], d["moe_d"][e, 128 * fc:128 * fc + 128, c0:c0 + 512])
                    kb.copy("pool", Wd_[:, fc, c0:c0 + 512], s_[:, :])
            for t in range(nth):
                tl = slice(t * TT, (t + 1) * TT)
                ps_c = cx.banks[6][:, :]
                kb.mm(ps_c, PCb[0:16, 128 * e:128 * e + 128], cTb[:, tl])
                cbc = cbcs.get()
                kb.copy("act", cbc[:, :], ps_c)
                at = []
                for fc in range(4):
                    ps_g = cx.banks[gi % 2][:, :]
                    ps_u = cx.banks[2 + gi % 2][:, :]
                    gi += 1
                    for k in range(8):
                        kb.mm(ps_g, Wg_[:, k, 128 * fc:128 * fc + 128], x1bf[:, k, tl], start=(k == 0), stop=(k == 7))
                    for k in range(8):
                        kb.mm(ps_u, Wu_[:, k, 128 * fc:128 * fc + 128], x1bf[:, k, tl], start=(k == 0), stop=(k == 7))
                    e_ = F5.get()
                    kb.act(e_[:, :], ps_g, AF.Exp, scale=-1.0)
                    kb.ts("pool", e_[:, :], e_[:, :], 1.0, ALU.add)
                    r_ = F5.get()
                    kb.recip(r_[:, :], e_[:, :])
                    kb.tt("dve", r_[:, :], r_[:, :], ps_g, ALU.mult)
                    kb.tt("dve", r_[:, :], r_[:, :], ps_u, ALU.mult)
                    a_ = acts.get()
                    kb.tt("pool", a_[:, :], r_[:, :], cbc[:, :], ALU.mult)
                    at.append(a_)
                for j in range(8):
                    ps_d = cx.banks[4 + j % 2][:, :]
                    for fc in range(4):
                        kb.mm(ps_d, Wd_[:, fc, 128 * j:128 * j + 128], at[fc][:, :], start=(fc == 0), stop=(fc == 3))
                    if e == 0:
                        kb.copy("dve", acc[:, j, tl], ps_d)
                    else:
                        kb.tt("dve", acc[:, j, tl], acc[:, j, tl], ps_d, ALU.add)
        kb.flush()
        st2.close()
        x32 = kb.sbuf("b_x32", [128, 8, TT], F32, st)
        for t in range(nth):
            g0 = half * HT + t * TT
            tl = slice(t * TT, (t + 1) * TT)
            for k in range(8):
                kb.dma("sp", x32[:, k, :], d["x1T"][128 * k:128 * k + 128, g0:g0 + TT])
                kb.stt(x32[:, k, :], x32[:, k, :], ALPHA, acc[:, k, tl], ALU.mult, ALU.add)

            def out2(j, o, g0=g0):
                kb.dma("sp", d["odst"](j, g0 // TT), o[:, :])
            ln_fmajor(kb, cx, x32, "ln2_g", "ln2_b", pcs, F5, EPS, out2)
        kb.flush()
        st.close()


def _din(nc, name, arr):
    dt = I32 if arr.dtype == np.int32 else F32
    t = nc.dram_tensor(name, list(arr.shape), dt, kind="ExternalInput")
    return V(t.ap(), [Dep()])


def _dout(nc, name, shape, dt=F32):
    t = nc.dram_tensor(name, list(shape), dt, kind="ExternalOutput")
    return V(t.ap(), [Dep()])


def _dint(nc, name, shape, dt):
    t = nc.dram_tensor(name, list(shape), dt)
    return V(t.ap(), [Dep()])


GROUPS = [[0, 1, 2, 3], [4, 5, 6, 7]]


def kernel(**inputs):
    inp = {k: np.asarray(v) for k, v in inputs.items()}
    x = inp["x"]
    B, S = x.shape[0], x.shape[1]
    T = S // 4
    xT = [np.ascontiguousarray(x[b].T) for b in range(B)]
    masks = masks_host()
    scm = scanmask_host()
    packs = [[pack_p1a(inp, l, hg) for hg in range(4)] for l in range(2)]
    arrs = [[(W.build(), P.build(), M.build()) for (W, P, M) in packs[l]] for l in range(2)]
    p2 = [pack_p2(inp, l) for l in range(2)]
    p2a = [p.build() for p in p2]
    posrep = [np.ascontiguousarray(np.repeat(inp["positions"][b][None, :], 96, 0)).astype(np.int32) for b in range(B)]
    common = {"Wgate0": np.ascontiguousarray(inp["w_in"][0][:, O_GATES:O_GATES + 4096]),
              "Wgate1": np.ascontiguousarray(inp["w_in"][1][:, O_GATES:O_GATES + 4096]),
              "Wb": np.ascontiguousarray(inp["w_branch"]), "Wout": np.ascontiguousarray(inp["w_out"]),
              "PC2_0": p2a[0], "PC2_1": p2a[1], "moe_g": np.ascontiguousarray(inp["moe_w_gate"]),
              "moe_u": np.ascontiguousarray(inp["moe_w_up"]), "moe_d": np.ascontiguousarray(inp["moe_w_down"]),
              "masks": masks, "scanmask": scm}
    in_maps = []
    for b in range(B):
        for r in range(4):
            oh = np.zeros((128, 4), np.float32)
            oh[:, r] = 1.0
            m = {"xT": xT[b], "xq": np.ascontiguousarray(xT[b][:, r * T:(r + 1) * T]), "pos": posrep[b], "onehot": oh}
            for l in range(2):
                m["W%d" % l], m["PC%d" % l], m["SM%d" % l] = arrs[l][r]
            m.update(common)
            in_maps.append({"in_" + k: v for k, v in m.items()})

    nc = bass.Bass("TRN2", target_bir_lowering=False)
    kb = KB(nc)
    di = {k[3:]: _din(nc, k, v) for k, v in in_maps[0].items()}
    CW = 1024
    nyc = S // CW
    yTc = _dint(nc, "yTc", [nyc, 512, CW], BF16)
    ygc = _dint(nc, "ygc", [nyc, 2048, CW], BF16)
    ntq = T // TT
    x2c = _dint(nc, "x2c", [ntq, 2, 512, TT], F32)
    xgc = _dint(nc, "xgc", [ntq, 2, 2048, TT], F32)
    vfirst = _dint(nc, "vfirst_s", [128, S], F32)
    oT = _dout(nc, "oT", [1024, T])
    scratch = {"QT": _dint(nc, "QT", [2, 96, S], BF16), "KT": _dint(nc, "KT", [2, 96, S], BF16),
               "VT": _dint(nc, "VT", [128, 2, S // 128, 65], BF16),
               "x1T": _dint(nc, "x1T", [1024, T], F32), "mT": _dint(nc, "mT", [1024, T], BF16)}

    def ydst(row0, nrows, t):
        c, o = divmod(t * TT, CW)
        return yTc[c][row0:row0 + nrows, o:o + TT]

    def ysrc(r0, jq, t):
        c, o = divmod(jq * T + t * TT, CW)
        return ygc[c][r0:r0 + 128, o:o + TT]

    def x2_local(k, t):
        return x2c[t][k // 4][(k % 4) * 128:(k % 4) * 128 + 128, :]

    def x_gathered(k, tt):
        q, tl = divmod(tt, ntq)
        return xgc[tl][k // 4][q * 512 + (k % 4) * 128:q * 512 + (k % 4) * 128 + 128, :]

    for l in range(2):
        W0, P0, M0 = packs[l][0]
        d = {"pos": di["pos"], "W": di["W%d" % l], "PC": di["PC%d" % l], "SM": di["SM%d" % l],
             "masks": di["masks"], "scanmask": di["scanmask"], "ydst": ydst, "vfirst": vfirst}
        d.update(scratch)
        d.update(Woff=W0.off, Poff=P0.off, Moff=M0.off, NW=arrs[l][0][0].shape[1], NP=arrs[l][0][1].shape[1],
                 NM=arrs[l][0][2].shape[1])
        if l == 0:
            d["xsrc"] = lambda k, t: di["xT"][128 * k:128 * k + 128, t * TT:(t + 1) * TT]
        else:
            d["xsrc"] = x_gathered
        build_p1a(kb, S, l, d)
        build_p1b(kb, S, d)
        for c in range(nyc):
            kb.coll("AllGather", ygc[c], yTc[c], GROUPS)
        d2 = {"ysrc": ysrc, "onehot": di["onehot"], "Wgate": di["Wgate%d" % l], "Wb": di["Wb"][l], "Wout": di["Wout"][l],
              "PC2": di["PC2_%d" % l], "moe_g": di["moe_g"][l], "moe_u": di["moe_u"][l], "moe_d": di["moe_d"][l],
              "masks": di["masks"], "x1T": scratch["x1T"], "mT": scratch["mT"],
              "P2off": p2[l].off, "NP2": p2a[l].shape[1]}
        if l == 0:
            d2["xsrc2"] = lambda k, t: di["xq"][128 * k:128 * k + 128, t * TT:(t + 1) * TT]
            d2["odst"] = lambda j, t: x2_local(j, t)
        else:
            d2["xsrc2"] = x2_local
            d2["odst"] = lambda j, t: oT[128 * j:128 * j + 128, t * TT:(t + 1) * TT]
        build_p2(kb, T, l, d2)
        if l == 0:
            for t_ in range(ntq):
                for h_ in range(2):
                    kb.coll("AllGather", xgc[t_][h_], x2c[t_][h_], GROUPS)
    kb.flush()
    kb.close()
    res = run_bass_kernel_spmd(nc, in_maps, core_ids=list(range(len(in_maps))))
    out = np.empty((B, S, 1024), np.float32)
    for b in range(B):
        for q in range(4):
            out[b, q * T:(q + 1) * T, :] = res.results[b * 4 + q]["oT"].T
    return out
```

```python
import contextlib
import math
import numpy as np
import ml_dtypes
import concourse.bass as bass
import concourse.mybir as mybir
from concourse.bass_utils import run_bass_kernel_spmd

F32 = mybir.dt.float32
BF16 = mybir.dt.bfloat16
I32 = mybir.dt.int32
ALU = mybir.AluOpType
AF = mybir.ActivationFunctionType
AX = mybir.AxisListType

D_MODEL = 1024
IN_COLS = 9912
TT = 512
NKC = 8


class Dep:
    __slots__ = ("w", "r", "wd")

    def __init__(self):
        self.w = None
        self.r = []
        self.wd = []


class V:
    __slots__ = ("ap", "deps")

    def __init__(self, ap, deps):
        self.ap = ap
        self.deps = deps

    def __getitem__(self, k):
        return V(self.ap[k], self.deps)

    def v(self, ap):
        return V(ap, self.deps)


class Eng:
    def __init__(self, name):
        self.name = name
        self.count = 0
        self.ops = []
        self.waited = {}


class KB:
    def __init__(self, nc):
        self.nc = nc
        self.stack = contextlib.ExitStack()
        self.E = {n: Eng(n) for n in ("pe", "act", "dve", "pool", "sp")}
        import os as _os2
        self.ndma = int(_os2.environ.get("DBG_NDMA", "32"))
        self.dma_state = [0] * self.ndma
        self.dma_next = 0
        self.n_ops = 0
        self.out_tokens = []
        import os as _os
        self.limit = int(_os.environ["DBG_LIMIT"]) if _os.environ.get("DBG_LIMIT") else None
        self.dump = int(_os.environ["DBG_DUMP"]) if _os.environ.get("DBG_DUMP") else None

    def _nm(self, name):
        self._uid = getattr(self, "_uid", 0) + 1
        return "%s_u%d" % (name, self._uid)

    def sbuf(self, name, shape, dt, stack=None):
        t = (stack or self.stack).enter_context(self.nc.sbuf_tensor(self._nm(name), list(shape), dt))
        return V(t[:] if False else t, [Dep()])

    def psum(self, name, shape, dt=F32, stack=None):
        t = (stack or self.stack).enter_context(self.nc.psum_tensor(self._nm(name), list(shape), dt))
        return V(t, [Dep()])

    def dram(self, name, shape, dt, kind="Internal"):
        t = self.nc.dram_tensor(name, list(shape), dt, kind=kind)
        return V(t.ap(), [Dep()])

    @staticmethod
    def _deps(reads, writes):
        out = []
        for d in reads:
            if d.w is not None:
                out.append(d.w)
            out.extend(d.wd)
        for d in writes:
            if d.w is not None:
                out.append(d.w)
            out.extend(d.wd)
            out.extend(d.r)
        return out

    def _waits(self, e, reads, writes, skip):
        need = {}
        for (pn, pv) in self._deps(reads, writes):
            if pn == skip:
                continue
            if need.get(pn, 0) < pv:
                need[pn] = pv
        waits = []
        for pn, pv in need.items():
            if e.waited.get(pn, 0) < pv:
                e.waited[pn] = pv
                waits.append((pn, pv))
        return waits

    @staticmethod
    def _mark(tok, reads, writes):
        for d in reads:
            d.r.append(tok)
            if len(d.r) > 64:
                last = {}
                for (n, v) in d.r:
                    if last.get(n, 0) < v:
                        last[n] = v
                d.r = list(last.items())
        is_async = tok[0].startswith("dma") or tok[0] == "coll"
        for d in writes:
            if is_async:
                d.wd.append(tok)
                if len(d.wd) > 48:
                    last = {}
                    for (n, v) in d.wd:
                        if last.get(n, 0) < v:
                            last[n] = v
                    d.wd = list(last.items())
            else:
                d.w = tok
                d.wd = []
            d.r = []

    def op(self, eng, fn, reads=(), writes=()):
        if self.limit is not None and self.n_ops >= self.limit:
            return None
        if self.dump is not None and abs(self.n_ops - self.dump) <= 3:
            import sys as _s
            f = _s._getframe(1)
            chain = []
            while f is not None and len(chain) < 4:
                chain.append("%s:%d" % (f.f_code.co_name, f.f_lineno))
                f = f.f_back
            print("OP", self.n_ops, eng, chain)
        rd = [d for v in reads for d in v.deps]
        wd = [d for v in writes for d in v.deps]
        e = self.E[eng]
        skip = "pe" if eng == "pe" else None
        waits = self._waits(e, rd, wd, skip)
        e.count += 1
        tok = (eng, e.count)
        e.ops.append((waits, fn, None))
        self._mark(tok, rd, wd)
        self.n_ops += 1
        return tok

    def dma(self, q, out, in_, **kw):
        if self.limit is not None and self.n_ops >= self.limit:
            return None
        rd = list(in_.deps)
        wd = list(out.deps)
        e = self.E[q]
        waits = self._waits(e, rd, wd, None)
        s = self.dma_next
        self.dma_next = (s + 1) % self.ndma
        prev = self.dma_state[s]
        name = "dma%d" % s
        if prev > 0 and e.waited.get(name, 0) < prev:
            e.waited[name] = prev
            waits.append((name, prev))
        self.dma_state[s] = prev + 16
        tok = (name, prev + 16)
        oa, ia = out.ap, in_.ap
        e.ops.append((waits, lambda h: h.dma_start(out=oa, in_=ia, **kw), name))
        self._mark(tok, rd, wd)
        self.n_ops += 1
        return tok

    def coll(self, kind, out, in_, groups):
        import os as _os
        if _os.environ.get("DBG_NOCOLL"):
            return None
        self._cidx = getattr(self, "_cidx", -1) + 1
        if _os.environ.get("DBG_COLLMASK") and not (int(_os.environ["DBG_COLLMASK"]) >> self._cidx) & 1:
            return None
        rd = list(in_.deps)
        wd = list(out.deps)
        e = self.E["pool"]
        waits = self._waits(e, rd, wd, None)
        self.ncoll = getattr(self, "ncoll", 0) + 1
        tok = ("coll", self.ncoll)
        oa, ia = out.ap, in_.ap
        e.ops.append((waits, lambda h: h.collective_compute(kind, ALU.bypass, replica_groups=groups, ins=[ia], outs=[oa]), "coll"))
        self._mark(tok, rd, wd)
        self.n_ops += 1
        return tok

    def _ensure_sems(self):
        if getattr(self, "sem", None) is None:
            self.sem = {}
            for n in ("pe", "act", "dve", "pool"):
                self.sem[n] = self.stack.enter_context(self.nc.semaphore("s_" + n))
            for i in range(self.ndma):
                self.sem["dma%d" % i] = self.stack.enter_context(self.nc.semaphore("s_dma%d" % i))
            self.sem["coll"] = self.stack.enter_context(self.nc.semaphore("s_coll"))

    def flush(self):
        self._ensure_sems()
        sem = self.sem
        toks = [(n, self.E[n].count) for n in ("pe", "act", "dve", "pool") if self.E[n].count > 0]
        toks += [("dma%d" % i, v) for i, v in enumerate(self.dma_state) if v > 0]
        if getattr(self, "ncoll", 0) > 0:
            toks.append(("coll", self.ncoll))
        for n, e in self.E.items():
            waits = []
            for (pn, pv) in toks:
                if pn == n:
                    continue
                if e.waited.get(pn, 0) < pv:
                    e.waited[pn] = pv
                    waits.append((pn, pv))
            e.ops.append((waits, None, None))
        with self.nc.Block() as block:
            def replay(e, own):
                ops = e.ops

                def body(h):
                    for waits, fn, dsem in ops:
                        for (pn, pv) in waits:
                            h.wait_ge(sem[pn], pv)
                        if fn is None:
                            continue
                        ins = fn(h)
                        if dsem == "coll":
                            ins.then_inc(sem[dsem], 1)
                        elif dsem is not None:
                            ins.then_inc(sem[dsem], 16)
                        elif own is not None:
                            ins.then_inc(sem[own], 1)
                return body
            block.sync(replay(self.E["sp"], None))
            block.tensor(replay(self.E["pe"], "pe"))
            block.scalar(replay(self.E["act"], "act"))
            block.vector(replay(self.E["dve"], "dve"))
            block.gpsimd(replay(self.E["pool"], "pool"))
        for e in self.E.values():
            e.ops = []

    def emit(self):
        self.flush()

    def close(self):
        self.stack.close()

    def mm(self, out, lhsT, rhs, start=True, stop=True):
        oa, la, ra = out.ap, lhsT.ap, rhs.ap
        return self.op("pe", lambda h: h.matmul(oa, la, ra, start=start, stop=stop),
                       reads=[lhsT, rhs], writes=[out])

    def act(self, out, in_, func, scale=1.0, bias=None, eng="act"):
        oa, ia = out.ap, in_.ap
        reads = [in_]
        kw = {}
        if isinstance(bias, V):
            reads.append(bias)
            kw["bias"] = bias.ap
        elif bias is not None:
            kw["bias"] = bias
        if isinstance(scale, V):
            reads.append(scale)
            kw["scale"] = scale.ap
        else:
            kw["scale"] = scale
        return self.op("act", lambda h: h.activation(oa, ia, func, **kw), reads=reads, writes=[out])

    def tt(self, eng, out, in0, in1, op):
        oa, a, b = out.ap, in0.ap, in1.ap
        return self.op(eng, lambda h: h.tensor_tensor(oa, a, b, op), reads=[in0, in1], writes=[out])

    def ts(self, eng, out, in0, s1, op0, s2=None, op1=None):
        oa, a = out.ap, in0.ap
        reads = [in0]
        if isinstance(s1, V):
            reads.append(s1)
            s1 = s1.ap
        if isinstance(s2, V):
            reads.append(s2)
            s2 = s2.ap
        if op1 is None:
            return self.op(eng, lambda h: h.tensor_scalar(oa, a, s1, None, op0), reads=reads, writes=[out])
        return self.op(eng, lambda h: h.tensor_scalar(oa, a, s1, s2, op0, op1), reads=reads, writes=[out])

    def stt(self, out, in0, scalar, in1, op0, op1):
        oa, a, b = out.ap, in0.ap, in1.ap
        reads = [in0, in1]
        if isinstance(scalar, V):
            reads.append(scalar)
            scalar = scalar.ap
        return self.op("dve", lambda h: h.scalar_tensor_tensor(oa, a, scalar, b, op0, op1),
                       reads=reads, writes=[out])

    def copy(self, eng, out, in_):
        oa, ia = out.ap, in_.ap
        if eng == "act":
            return self.op("act", lambda h: h.activation(oa, ia, AF.Copy), reads=[in_], writes=[out])
        return self.op(eng, lambda h: h.tensor_copy(oa, ia), reads=[in_], writes=[out])

    def memset(self, eng, out, val):
        oa = out.ap
        return self.op(eng, lambda h: h.memset(oa, val), writes=[out])

    def scan(self, out, d0, d1, init, op0, op1):
        oa, a, b = out.ap, d0.ap, d1.ap
        return self.op("dve", lambda h: h.tensor_tensor_scan(oa, a, b, init, op0, op1),
                       reads=[d0, d1], writes=[out])

    def recip(self, out, in_):
        oa, ia = out.ap, in_.ap
        return self.op("dve", lambda h: h.reciprocal(oa, ia), reads=[in_], writes=[out])


class Pool:
    def __init__(self, kb, name, n, shape, dt, space="sbuf", stack=None):
        mk = kb.sbuf if space == "sbuf" else kb.psum
        self.t = [mk("%s%d" % (name, i), shape, dt, stack=stack) for i in range(n)]
        self.i = 0

    def get(self):
        t = self.t[self.i]
        self.i = (self.i + 1) % len(self.t)
        return t


C_W = math.exp(-0.5)
MLA_SCALE = 96 ** -0.5
LN_EPS = 1e-5
NORM_EPS = 1e-6
RWKV_LN_EPS = 64e-5
ALPHA = (2.0 * 2) ** 0.25

_IW = (256, 160, 256, 256, 512, 16, 512, 1536, 4, 4, 512, 1792, 4096)
_IO = np.concatenate([[0], np.cumsum(_IW)]).tolist()
(O_QLAT, O_KVLAT, O_GQ, O_GK, O_GV, O_GGATE, O_GOG, O_DQKV, O_DBETA, O_DA, O_DZ, O_RW, O_GATES) = _IO[:13]


class Packer:
    def __init__(self, rows):
        self.rows = rows
        self.blocks = []
        self.off = {}
        self.n = 0

    def add(self, name, arr):
        arr = np.asarray(arr, dtype=np.float32)
        if arr.ndim == 1:
            arr = arr[:, None]
        if arr.shape[0] < self.rows:
            pad = np.zeros((self.rows - arr.shape[0], arr.shape[1]), np.float32)
            arr = np.concatenate([arr, pad], 0)
        self.off[name] = (self.n, arr.shape[1])
        self.blocks.append(arr)
        self.n += arr.shape[1]

    def build(self):
        return np.ascontiguousarray(np.concatenate(self.blocks, 1))


def const_masks():
    i = np.arange(128)
    same = (i[:, None] // 64) == (i[None, :] // 64)
    m = {}
    m["U_incl"] = (same & (i[:, None] <= i[None, :])).astype(np.float32)
    m["U_strict"] = (same & (i[:, None] < i[None, :])).astype(np.float32)
    m["L_incl"] = m["U_incl"].T.copy()
    m["L_strict"] = m["U_strict"].T.copy()
    m["ident"] = np.eye(128, dtype=np.float32)
    m["ones"] = np.ones((128, 128), np.float32)
    m["ones_blk"] = same.astype(np.float32)
    m["istack"] = np.concatenate([np.eye(64), np.eye(64)], 0).astype(np.float32)
    return m


def pack_p1a(inp, l, hg):
    w_in = inp["w_in"][l]
    W = Packer(1024)
    W.add("qlat", w_in[:, O_QLAT:O_QLAT + 256])
    W.add("ckv", w_in[:, O_KVLAT:O_KVLAT + 128])
    kr = w_in[:, O_KVLAT + 128:O_KVLAT + 160]
    W.add("krope", kr)
    W.add("krope_sw", np.concatenate([kr[:, 16:32], kr[:, 0:16]], 1))
    W.add("gq", w_in[:, O_GQ + 64 * hg:O_GQ + 64 * hg + 64])
    W.add("gk", w_in[:, O_GK + 64 * hg:O_GK + 64 * hg + 64])
    W.add("gv", w_in[:, O_GV + 128 * hg:O_GV + 128 * hg + 128])
    W.add("ggate", w_in[:, O_GGATE:O_GGATE + 16])
    W.add("gog", w_in[:, O_GOG + 128 * hg:O_GOG + 128 * hg + 128])
    W.add("dq", w_in[:, O_DQKV + 128 * hg:O_DQKV + 128 * hg + 128])
    W.add("dk", w_in[:, O_DQKV + 512 + 128 * hg:O_DQKV + 512 + 128 * hg + 128])
    W.add("dv", w_in[:, O_DQKV + 1024 + 128 * hg:O_DQKV + 1024 + 128 * hg + 128])
    W.add("dbeta", np.repeat(w_in[:, O_DBETA + hg:O_DBETA + hg + 1], 128, 1))
    W.add("da", np.repeat(w_in[:, O_DA + hg:O_DA + hg + 1], 128, 1))
    W.add("dz", w_in[:, O_DZ + 128 * hg:O_DZ + 128 * hg + 128])
    for i, nm in enumerate(("rr", "rk", "rv")):
        W.add(nm, w_in[:, O_RW + 512 * i + 128 * hg:O_RW + 512 * i + 128 * hg + 128])
    W.add("rwl", w_in[:, O_RW + 1536:O_RW + 1600])
    W.add("ral", w_in[:, O_RW + 1600:O_RW + 1664])
    W.add("rgl", w_in[:, O_RW + 1664:O_RW + 1792])
    if l > 0:
        W.add("rvr", inp["w_in_vres"][l - 1])
    P = Packer(128)
    ch = slice(128 * hg, 128 * hg + 128)
    mu = inp["rwkv_mu"][l]
    P.add("mu_r", mu[0:512][ch]); P.add("mu_k", mu[512:1024][ch]); P.add("mu_v", mu[1024:1536][ch])
    P.add("mu_wl", mu[1536:1600]); P.add("mu_al", mu[1600:1664]); P.add("mu_gl", mu[1664:1792])
    P.add("w0", inp["rwkv_w0"][l][ch]); P.add("a0", inp["rwkv_a0"][l][ch])
    P.add("k_k", inp["rwkv_k_k"][l][ch]); P.add("k_a", inp["rwkv_k_a"][l][ch])
    P.add("r_k", inp["rwkv_r_k"][l].reshape(-1)[ch])
    P.add("ln_g", inp["rwkv_ln_g"][l][ch]); P.add("ln_b", inp["rwkv_ln_b"][l][ch])
    if l > 0:
        P.add("mu_vr", inp["rwkv_vres_mu"][l - 1]); P.add("v0", inp["rwkv_v0"][l - 1][ch])
    P.add("gla_norm", inp["gla_norm"][l]); P.add("gdn_norm", inp["gdn_norm"][l])
    cw = inp["gdn_conv"][l]
    for nm, o in (("cq", 0), ("ck", 512), ("cv", 1024)):
        P.add("conv_" + nm, cw[:, o + 128 * hg:o + 128 * hg + 128].T)
    P.add("a_log", np.full(128, inp["gdn_a_log"][l][hg])); P.add("dt_bias", np.full(128, inp["gdn_dt_bias"][l][hg]))
    P.add("q_norm", inp["mla_q_norm"][l].reshape(2, 128).T)
    P.add("kv_norm", inp["mla_kv_norm"][l])
    invf = (10000.0 ** (-np.arange(16, dtype=np.float32) / 16)).astype(np.float32)
    P.add("invf", np.concatenate([np.zeros(64, np.float32), invf, invf]))
    P.add("rsign", np.concatenate([np.zeros(64, np.float32), -np.ones(16, np.float32), np.ones(16, np.float32)]))
    M = Packer(128)
    M.add("gup", np.concatenate([inp["gla_w_gate_up"][l][:, 64 * hg:64 * hg + 64],
                                 inp["gla_b_gate"][l][None, 64 * hg:64 * hg + 64]], 0))
    M.add("w_up", inp["rwkv_w_up"][l][:, ch]); M.add("a_up", inp["rwkv_a_up"][l][:, ch])
    M.add("g_up", inp["rwkv_g_up"][l][:, ch])
    if l > 0:
        M.add("v_up", inp["rwkv_v_up"][l - 1][:, ch])
    uq = inp["mla_w_uq"][l]
    ukv = inp["mla_w_ukv"][l]
    for hh in range(2):
        h = 2 * hg + hh
        q = uq[:, 96 * h:96 * h + 96]
        for kc in range(2):
            M.add("uq%d_%d" % (hh, kc), q[128 * kc:128 * kc + 128])
            sw = np.concatenate([np.zeros((128, 64), np.float32), q[128 * kc:128 * kc + 128, 80:96],
                                 q[128 * kc:128 * kc + 128, 64:80]], 1)
            M.add("uqs%d_%d" % (hh, kc), sw)
        M.add("ukn%d" % hh, ukv[:, 128 * h:128 * h + 64])
    M.add("ukv_v", np.concatenate([ukv[:, 128 * (2 * hg) + 64:128 * (2 * hg) + 128],
                                   ukv[:, 128 * (2 * hg + 1) + 64:128 * (2 * hg + 1) + 128]], 1))
    return W, P, M


class Ctx:
    pass


def setup_common(kb, cx, masks_d, stack):
    mk = const_masks()
    names = ["U_strict", "U_incl", "L_incl", "L_strict", "ident", "ones", "ones_blk"]
    cx.mf = kb.sbuf("maskf", [128, 7 * 128 + 64], F32, stack)
    cx.mb = kb.sbuf("maskb", [128, 7 * 128 + 64], BF16, stack)
    kb.dma("sp", cx.mf[:, :], masks_d)
    kb.copy("pool", cx.mb[:, :], cx.mf[:, :])
    cx.M = {}
    cx.Mf = {}
    for i, n in enumerate(names):
        cx.M[n] = cx.mb[:, 128 * i:128 * i + 128]
        cx.Mf[n] = cx.mf[:, 128 * i:128 * i + 128]
    cx.M["istack"] = cx.mb[:, 896:960]
    cx.Mf["istack"] = cx.mf[:, 896:960]
    cx.M["pairU"] = cx.mb[:, 0:256]
    banks = [kb.psum("pb%d" % i, [128, 512], F32, stack) for i in range(8)]
    cx.banks = banks

    class RR:
        def __init__(self, items):
            self.items = items
            self.i = 0

        def get(self):
            x = self.items[self.i]
            self.i = (self.i + 1) % len(self.items)
            return x
    cx.RR = RR
    cx.PB = RR([banks[i][:, :] for i in range(2)])
    cx.PH = RR([banks[2][:, 256 * j:256 * j + 256] for j in range(2)])
    cx.PQ = RR([banks[3 + i][:, 128 * j:128 * j + 128] for j in range(4) for i in range(3)])
    cx.PD = [banks[6 + i][:, 128 * j:128 * j + 128] for i in range(2) for j in range(4)]


def masks_host():
    mk = const_masks()
    names = ["U_strict", "U_incl", "L_incl", "L_strict", "ident", "ones", "ones_blk"]
    return np.ascontiguousarray(np.concatenate([mk[n] for n in names] + [mk["istack"]], 1).astype(np.float32))


def scanmask_host():
    m = np.ones((128, TT), np.float32)
    m[:, ::64] = 0.0
    return m


def rsqrt_act(kb, out, in_, scale, eps, tmp):
    kb.act(tmp, in_, AF.Ln, scale=scale, bias=eps)
    kb.act(out, tmp, AF.Exp, scale=-0.5)


def sigmoid_into(kb, out, in_, tmp, scale=1.0, bias=None, eng="dve"):
    kb.act(tmp, in_, AF.Exp, scale=-scale, bias=bias)
    kb.ts(eng, tmp, tmp, 1.0, ALU.add)
    kb.recip(out, tmp)


def neumann(kb, cx, PA, PB_, pool_bf, ident_b):
    TT_ = pool_bf.get()
    kb.tt("dve", TT_[:, :], PB_, ident_b, ALU.add)
    A, B = PA, PB_
    for k in range(1, 6):
        psA = cx.PQ.get()
        kb.mm(psA, B, A)
        nA = pool_bf.get()
        kb.copy("act", nA[:, :], psA)
        if k < 5:
            psB = cx.PQ.get()
            kb.mm(psB, A, B)
            nB = pool_bf.get()
            kb.copy("pool" if False else "dve", nB[:, :], psB)
        psT = cx.PQ.get()
        kb.mm(psT, nA[:, :], TT_[:, :])
        nT = pool_bf.get()
        kb.tt("dve", nT[:, :], TT_[:, :], psT, ALU.add)
        TT_ = nT
        A = nA[:, :]
        if k < 5:
            B = nB[:, :]
    return TT_[:, :]


def build_p1a(kb, S, l, d, branches=("gla", "gdn", "rwkv", "mla")):
    import os as _os
    if "build_p1a" in _os.environ.get("DBG_SKIP", "").split(","):
        return None

    st = contextlib.ExitStack()
    cx = Ctx()
    setup_common(kb, cx, d["masks"], st)
    Wo, Po, Mo = d["Woff"], d["Poff"], d["Moff"]
    NW, NP, NM = d["NW"], d["NP"], d["NM"]
    nt = S // TT
    M, Mf = cx.M, cx.Mf

    PC = kb.sbuf("PC", [128, NP], F32, st)
    kb.dma("sp", PC[:, :], d["PC"])
    SMf = kb.sbuf("SMf", [128, NM], F32, st)
    kb.dma("sp", SMf[:, :], d["SM"])
    SMb = kb.sbuf("SMb", [128, NM], BF16, st)
    kb.copy("pool", SMb[:, :], SMf[:, :])
    scm = kb.sbuf("scm", [128, TT], F32, st)
    kb.dma("sp", scm[:, :], d["scanmask"])
    Wbf = kb.sbuf("Wbf", [128, NKC, NW], BF16, st)
    wst = Pool(kb, "wst", 2, [128, 512], F32, stack=st)
    for k in range(NKC):
        for c0 in range(0, NW, 512):
            cw_ = min(512, NW - c0)
            s_ = wst.get()
            kb.dma("sp", s_[:, 0:cw_], d["W"][128 * k:128 * k + 128, c0:c0 + cw_])
            kb.copy("pool", Wbf[:, k, c0:c0 + cw_], s_[:, 0:cw_])

    def pc(name, rows=128, j=0):
        o, w = Po[name]
        return PC[0:rows, o + j:o + j + 1]

    def sm(name, rows, cols=None):
        o, w = Mo[name]
        return SMb[0:rows, o:o + (cols or w)]

    def wcol(name, k, c0=0, cn=None):
        o, w = Wo[name]
        return Wbf[:, k, o + c0:o + c0 + (cn or w)]

    cst = kb.sbuf("cst", [128, 8], F32, st)
    kb.memset("pool", cst[:, 0:1], NORM_EPS)
    kb.memset("pool", cst[:, 1:2], RWKV_LN_EPS)
    kb.memset("pool", cst[:, 2:3], 1.0)
    kb.memset("pool", cst[:, 3:4], 0.0)
    EPS_N, EPS_R, ONE_C, ZERO_C = cst[:, 0:1], cst[:, 1:2], cst[:, 2:3], cst[:, 3:4]
    ncol = kb.sbuf("ncol", [128, 8], F32, st)
    kb.ts("pool", ncol[:, 0:1], pc("w0"), -1.0, ALU.mult)
    kb.ts("pool", ncol[:, 1:2], pc("a0"), -1.0, ALU.mult)
    if l > 0:
        kb.ts("pool", ncol[:, 2:3], pc("v0"), -1.0, ALU.mult)
    kb.act(ncol[:, 3:4], pc("a_log"), AF.Exp)

    F5 = Pool(kb, "f5_", 10, [128, TT], F32, stack=st)
    L5 = Pool(kb, "l5_", 18, [128, TT], F32, stack=st)
    B5 = Pool(kb, "b5_", 10, [128, TT], BF16, stack=st)
    BQ = Pool(kb, "bq_", 28, [128, 128], BF16, stack=st)
    FQ = Pool(kb, "fq_", 16, [128, 128], F32, stack=st)
    kb_rhs = Pool(kb, "rhs_", 4, [128, 256], BF16, stack=st)
    BL = Pool(kb, "bl_", 8, [128, 128], BF16, stack=st)
    xst = Pool(kb, "xst", 3, [128, TT], F32, stack=st)
    xbfs = [kb.sbuf("xbf%d" % i, [128, NKC, TT], BF16, st) for i in range(2)]

    def proj_f(name, xb, rows=None, c0=0, out=None):
        o, w = Wo[name]
        rows = rows or w
        ps = out if out is not None else cx.PB.get()[0:rows, :]
        for k in range(NKC):
            kb.mm(ps, wcol(name, k, c0, rows), xb[:, k, :], start=(k == 0), stop=(k == NKC - 1))
        return ps

    def shift_mix(ps, rows, mu, halo, t):
        raw = halo[t % 2]
        kb.copy("act", raw[0:rows, 1:TT + 1], ps)
        if t == 0:
            kb.memset("pool", raw[0:rows, 0:1], 0.0)
        kb.copy("pool", halo[(t + 1) % 2][0:rows, 0:1], raw[0:rows, TT:TT + 1])
        dd = F5.get()
        kb.tt("dve", dd[0:rows, :], raw[0:rows, 0:TT], raw[0:rows, 1:TT + 1], ALU.subtract)
        out = F5.get()
        kb.stt(out[0:rows, :], dd[0:rows, :], mu, raw[0:rows, 1:TT + 1], ALU.mult, ALU.add)
        return out[0:rows, :]

    if "gla" in branches:
        gl_H32 = kb.sbuf("gl_H32", [64, 128], F32, st)
        gl_Hbf = kb.sbuf("gl_Hbf", [64, 128], BF16, st)
        kb.memset("pool", gl_H32[:, :], 0.0)
        kb.memset("pool", gl_Hbf[:, :], 0.0)
        gl_ext = kb.sbuf("gl_ext", [32, TT], BF16, st)
        kb.memset("pool", gl_ext[:, :], 1.0)
    if "gdn" in branches:
        gd_H32 = kb.sbuf("gd_H32", [128, 128], F32, st)
        gd_Hbf = kb.sbuf("gd_Hbf", [128, 128], BF16, st)
        kb.memset("pool", gd_H32[:, :], 0.0)
        kb.memset("pool", gd_Hbf[:, :], 0.0)
        gd_halo = {n: [kb.sbuf("gdh_%s" % n, [128, TT + 3], F32, st), kb.sbuf("gdc_%s" % n, [128, 4], F32, st)] for n in ("q", "k", "v")}
    if "rwkv" in branches:
        rw_H32 = kb.sbuf("rw_H32", [128, 64], F32, st)
        rw_Hbf = kb.sbuf("rw_Hbf", [128, 64], BF16, st)
        kb.memset("pool", rw_H32[:, :], 0.0)
        kb.memset("pool", rw_Hbf[:, :], 0.0)
        rw_halo = {n: [kb.sbuf("rwh_%s" % n, [128, TT + 1], F32, st), kb.sbuf("rwc_%s" % n, [128, 2], F32, st)]
                   for n in ("r", "k", "v", "wl", "al", "gl", "vr")}
        rw_AR = kb.sbuf("rw_AR", [128, 2 * TT], BF16, st)
        rw_MT = kb.sbuf("rw_MT", [128, 128], F32, st)
        kb.memset("pool", rw_MT[:, :], 0.0)
    if "mla" in branches:
        mla_posi = kb.sbuf("mla_posi", [96, TT], I32, st)
        mla_vt = [kb.sbuf("mla_vt%d" % i, [128, 2, 65], BF16, st) for i in range(2)]
        for i in range(2):
            kb.memset("pool", mla_vt[i][:, :, :], 1.0)
        mla_ni = kb.sbuf("mla_ni", [96, TT], I32, st)
        uqg = kb.sbuf("uqg", [128, 2, 2, 2, 96], BF16, st)
        for hh in range(2):
            for kc in range(2):
                for sw, nm in enumerate(("uq", "uqs")):
                    o, w = Mo["%s%d_%d" % (nm, hh, kc)]
                    kb.ts("pool", uqg[:, hh, kc, sw, :], SMf[:, o:o + 96], pc("q_norm", 128, kc), ALU.mult,
                          MLA_SCALE, ALU.mult)
        ukg = kb.sbuf("ukg", [128, 2, 64], BF16, st)
        for hh in range(2):
            o, w = Mo["ukn%d" % hh]
            kb.ts("pool", ukg[:, hh, :], SMf[:, o:o + 64], pc("kv_norm"), ALU.mult)
        uvg = kb.sbuf("uvg", [128, 128], BF16, st)
        o, w = Mo["ukv_v"]
        kb.ts("pool", uvg[:, :], SMf[:, o:o + 128], pc("kv_norm"), ALU.mult)

    def silu_of(src):
        sg = L5.get()
        tmp2 = F5.get()
        sigmoid_into(kb, sg[:, :], src, tmp2[:, :])
        kb.tt("dve", sg[:, :], sg[:, :], src, ALU.mult)
        return sg

    def post_norm_gate(oT, gate_sb, gcol, row0):
        osq = B5.get()
        kb.act(osq[:, :], oT[:, :], AF.Square)
        ps_s = cx.PB.get()
        kb.mm(ps_s, M["ones"], osq[:, :])
        tmp = F5.get()
        rstd = F5.get()
        rsqrt_act(kb, rstd[:, :], ps_s, 1.0 / 128, EPS_N, tmp[:, :])
        sg = silu_of(gate_sb)
        kb.stt(oT[:, :], oT[:, :], gcol, rstd[:, :], ALU.mult, ALU.mult)
        yb = B5.get()
        kb.tt("dve", yb[:, :], oT[:, :], sg[:, :], ALU.mult)
        kb.dma("sp", d["ydst"](row0, 128, tsl_box[0].start // TT), yb[:, :])

    def l2norm_f(x, ones_m, scale):
        sq = B5.get()
        kb.act(sq[:, :], x, AF.Square)
        ps_s = cx.PB.get()
        kb.mm(ps_s, ones_m, sq[:, :])
        tmp = F5.get()
        rs = F5.get()
        rsqrt_act(kb, rs[:, :], ps_s, 1.0, EPS_N, tmp[:, :])
        out = L5.get()
        kb.stt(out[:, :], x, scale, rs[:, :], ALU.mult, ALU.mult)
        return out

    def transpose_bf(src, dst_pool=None):
        ps_t = cx.PQ.get()
        kb.mm(ps_t, src, M["ident"])
        o = BL.get()
        kb.copy("act", o[:, :], ps_t)
        return o

    tsl_box = [None]
    youts = {}

    for t in range(nt):
        tsl = slice(t * TT, (t + 1) * TT)
        tsl_box[0] = tsl
        xb = xbfs[t % 2]
        for k in range(NKC):
            s_ = xst.get()
            kb.dma("sp", s_[:, :], d["xsrc"](k, t))
            kb.copy("pool", xb[:, k, :], s_[:, :])

        if "gla" in branches:
            L5.i = 0
            ps_q = proj_f("gq", xb)
            qraw = L5.get()
            kb.copy("act", qraw[0:64, :], ps_q)
            ps_k = proj_f("gk", xb)
            kraw = L5.get()
            kb.copy("act", kraw[0:64, :], ps_k)
            ps_g = proj_f("ggate", xb)
            kb.copy("act", gl_ext[0:16, :], ps_g)
            ps_og = proj_f("gog", xb)
            og = L5.get()
            kb.copy("act", og[:, :], ps_og)
            ps_z = cx.PB.get()[0:64, :]
            kb.mm(ps_z, sm("gup", 17), gl_ext[0:17, :])
            e1 = F5.get()
            kb.act(e1[0:64, :], ps_z, AF.Exp, scale=-1.0)
            sp = F5.get()
            kb.act(sp[0:64, :], e1[0:64, :], AF.Ln, bias=ONE_C[0:64, :])
            bsp = L5.get()
            kb.scan(bsp[0:64, :], scm[0:64, :], sp[0:64, :], 0.0, ALU.mult, ALU.add)
            epos = L5.get()
            kb.act(epos[0:64, :], bsp[0:64, :], AF.Exp, scale=-1.0 / 16)
            eneg = F5.get()
            kb.act(eneg[0:64, :], bsp[0:64, :], AF.Exp, scale=1.0 / 16)
            b3 = bsp.ap[0:64, :].rearrange("p (c t) -> p c t", t=64)
            dl = F5.get()
            dl3 = dl.ap[0:64, :].rearrange("p (c t) -> p c t", t=64)
            bl_bc = b3[:, :, 63:64].broadcast_to([64, TT // 64, 64])
            kb.tt("dve", V(dl3, dl.deps), V(b3, bsp.deps), V(bl_bc, bsp.deps), ALU.subtract)
            eend = F5.get()
            kb.act(eend[0:64, :], dl[0:64, :], AF.Exp, scale=1.0 / 16)
            qd = B5.get()
            kb.stt(qd[0:64, :], qraw[0:64, :], 0.125, epos[0:64, :], ALU.mult, ALU.mult)
            ki = B5.get()
            kb.tt("dve", ki[0:64, :], kraw[0:64, :], eneg[0:64, :], ALU.mult)
            ke = B5.get()
            kb.tt("dve", ke[0:64, :], kraw[0:64, :], eend[0:64, :], ALU.mult)
            oT = L5.get()
            for blk in range(4):
                bs = slice(blk * 128, blk * 128 + 128)
                ps_v = cx.PQ.get()
                for k in range(NKC):
                    kb.mm(ps_v, xb[:, k, bs], wcol("gv", k), start=(k == 0), stop=(k == NKC - 1))
                vt = BQ.get()
                kb.copy("act", vt[:, :], ps_v)
                ps_t = cx.PQ.get()[:, 0:64]
                kb.mm(ps_t, ke[0:64, bs], M["ident"][0:64, 0:64])
                ket = BQ.get()
                kb.copy("dve", ket[:, 0:64], ps_t)
                ps_a = cx.PQ.get()
                kb.mm(ps_a, ki[0:64, bs], qd[0:64, bs])
                am = BQ.get()
                kb.tt("dve", am[:, :], ps_a, Mf["U_incl"], ALU.mult)
                ps_o = cx.PD[0]
                kb.mm(ps_o, vt[:, :], am[:, :], start=True, stop=False)
                for c in range(2):
                    cs = slice(blk * 128 + c * 64, blk * 128 + c * 64 + 64)
                    rs = slice(c * 64, c * 64 + 64)
                    kb.mm(ps_o[:, rs], gl_Hbf[:, :], qd[0:64, cs], start=False, stop=(c == 1))
                    ps_n = cx.PD[4][0:64, :]
                    kb.mm(ps_n, ket[rs, 0:64], vt[rs, :])
                    gc = blk * 2 + c
                    acol = epos[0:64, 64 * (t * 0 + gc) + 63:64 * gc + 64]
                    kb.stt(gl_H32[:, :], gl_H32[:, :], acol, ps_n, ALU.mult, ALU.add)
                    kb.copy("act", gl_Hbf[:, :], gl_H32[:, :])
                kb.copy("act", oT[:, bs], ps_o)
            post_norm_gate(oT, og[:, :], pc("gla_norm"), 128)

        if "gdn" in branches:
            L5.i = 0
            conv = {}
            for nm, cn in (("q", "conv_cq"), ("k", "conv_ck"), ("v", "conv_cv")):
                ps = proj_f("d" + nm, xb)
                raw = gd_halo[nm][0]
                hcol = gd_halo[nm][1]
                if t == 0:
                    kb.memset("pool", raw[:, 0:3], 0.0)
                else:
                    kb.copy("pool", raw[:, 0:3], hcol[:, 0:3])
                kb.copy("act", raw[:, 3:TT + 3], ps)
                kb.copy("pool", hcol[:, 0:3], raw[:, TT:TT + 3])
                acc = F5.get()
                kb.ts("dve", acc[:, :], raw[:, 0:TT], pc(cn, 128, 0), ALU.mult)
                for j in range(1, 4):
                    kb.stt(acc[:, :], raw[:, j:j + TT], pc(cn, 128, j), acc[:, :], ALU.mult, ALU.add)
                conv[nm] = silu_of(acc[:, :])
            qn = l2norm_f(conv["q"][:, :], M["ones"], 128 ** -0.5)
            kn32 = l2norm_f(conv["k"][:, :], M["ones"], 1.0)
            kn = B5.get()
            kb.copy("pool", kn[:, :], kn32[:, :])
            qnb = B5.get()
            kb.copy("pool", qnb[:, :], qn[:, :])
            ps_b = proj_f("dbeta", xb)
            beta = L5.get()
            tb = F5.get()
            sigmoid_into(kb, beta[:, :], ps_b, tb[:, :])
            nbeta = L5.get()
            kb.ts("pool", nbeta[:, :], beta[:, :], -1.0, ALU.mult)
            ps_a = proj_f("da", xb)
            e1 = F5.get()
            kb.act(e1[:, :], ps_a, AF.Exp, bias=pc("dt_bias"))
            spl = F5.get()
            kb.act(spl[:, :], e1[:, :], AF.Ln, bias=ONE_C)
            g = F5.get()
            kb.ts("dve", g[:, :], spl[:, :], ncol[:, 3:4], ALU.mult, -1.0, ALU.mult)
            gc = L5.get()
            kb.scan(gc[:, :], scm[:, :], g[:, :], 0.0, ALU.mult, ALU.add)
            egc = L5.get()
            kb.act(egc[:, :], gc[:, :], AF.Exp)
            g3 = gc.ap[:, :].rearrange("p (c t) -> p c t", t=64)
            dl = F5.get()
            dl3 = dl.ap[:, :].rearrange("p (c t) -> p c t", t=64)
            gl_bc = g3[:, :, 63:64].broadcast_to([128, TT // 64, 64])
            kb.tt("dve", V(dl3, dl.deps), V(gl_bc, gc.deps), V(g3, gc.deps), ALU.subtract)
            eend = F5.get()
            kb.act(eend[:, :], dl[:, :], AF.Exp)
            qdec = L5.get()
            kb.tt("dve", qdec[:, :], qn[:, :], egc[:, :], ALU.mult)
            kbg = B5.get()
            tk = F5.get()
            kb.tt("dve", tk[:, :], kn32[:, :], beta[:, :], ALU.mult)
            kb.tt("dve", kbg[:, :], tk[:, :], egc[:, :], ALU.mult)
            vb = B5.get()
            kb.tt("dve", vb[:, :], conv["v"][:, :], beta[:, :], ALU.mult)
            kend = B5.get()
            kb.tt("dve", kend[:, :], kn32[:, :], eend[:, :], ALU.mult)
            ps_z = proj_f("dz", xb)
            zs = L5.get()
            kb.copy("act", zs[:, :], ps_z)
            oT = L5.get()
            for blk in range(4):
                bs = slice(blk * 128, blk * 128 + 128)
                BL.i = 0
                Rm = gc[:, bs]
                t1 = FQ.get()
                kb.tt("dve", t1[:, :], Rm, Mf["ident"], ALU.mult)
                cc = FQ.get()
                kb.op("dve", (lambda o_, i_: (lambda h: h.tensor_reduce(o_, i_, AX.X, ALU.add)))(cc.ap[:, 0:1], t1.ap[:, :]),
                      reads=[t1], writes=[cc])
                kb.tt("dve", t1[:, :], nbeta[:, bs], Mf["ident"], ALU.mult)
                kb.op("dve", (lambda o_, i_: (lambda h: h.tensor_reduce(o_, i_, AX.X, ALU.add)))(cc.ap[:, 1:2], t1.ap[:, :]),
                      reads=[t1], writes=[cc])
                ccol, nbcol = cc[:, 0:1], cc[:, 1:2]
                tB = FQ.get()
                kb.ts("dve", tB[:, :], Rm, ccol, ALU.subtract, 0.0, ALU.min)
                eB = FQ.get()
                kb.act(eB[:, :], tB[:, :], AF.Exp)
                DBi = FQ.get()
                kb.tt("dve", DBi[:, :], eB[:, :], Mf["U_incl"], ALU.mult)
                DBs = FQ.get()
                kb.tt("pool", DBs[:, :], eB[:, :], Mf["U_strict"], ALU.mult)
                kb.tt("pool", DBs[:, :], DBs[:, :], nbeta[:, bs], ALU.mult)
                tA = FQ.get()
                kb.ts("dve", tA[:, :], Rm, ccol, ALU.subtract, 0.0, ALU.max)
                eA = FQ.get()
                kb.act(eA[:, :], tA[:, :], AF.Exp, scale=-1.0)
                kb.tt("pool", eA[:, :], eA[:, :], Mf["L_strict"], ALU.mult)
                ps_kk = cx.PQ.get()
                kb.mm(ps_kk, kn[:, bs], kn[:, bs])
                PA = BQ.get()
                kb.stt(PA[:, :], eA[:, :], nbcol, ps_kk, ALU.mult, ALU.mult)
                PB_ = BQ.get()
                kb.tt("dve", PB_[:, :], ps_kk, DBs[:, :], ALU.mult)
                TTm = neumann(kb, cx, PA[:, :], PB_[:, :], BQ, M["ident"])
                rhs = kb_rhs.get()
                ps_t = cx.PQ.get()
                kb.mm(ps_t, vb[:, bs], M["ident"])
                kb.copy("act", rhs[:, 0:128], ps_t)
                ps_t = cx.PQ.get()
                kb.mm(ps_t, kbg[:, bs], M["ident"])
                kb.copy("act", rhs[:, 128:256], ps_t)
                kendt = transpose_bf(kend[:, bs])
                ps_uw = cx.PH.get()
                kb.mm(ps_uw, TTm, rhs[:, :])
                UW = kb_rhs.get()
                kb.copy("act", UW[:, :], ps_uw)
                ps_qk = cx.PQ.get()
                kb.mm(ps_qk, kn[:, bs], qnb[:, bs])
                attB = BL.get()
                kb.tt("dve", attB[:, :], ps_qk, DBi[:, :], ALU.mult)
                ps_aw = cx.PQ.get()
                kb.mm(ps_aw, UW[:, 128:256], attB[:, :])
                qeff = BL.get()
                kb.tt("dve", qeff[:, :], qdec[:, bs], ps_aw, ALU.subtract)
                ps_o = cx.PD[1]
                kb.mm(ps_o, UW[:, 0:128], attB[:, :], start=True, stop=False)
                for c in range(2):
                    rs = slice(c * 64, c * 64 + 64)
                    gcn = blk * 2 + c
                    kb.mm(ps_o[:, rs], gd_Hbf[:, :], qeff[:, rs], start=False, stop=(c == 1))
                    ps_m = cx.PD[5]
                    kb.mm(ps_m, UW[rs, 128:256], kendt[rs, :])
                    MT = FQ.get()
                    kb.stt(MT[:, :], Mf["ident"], egc[:, 64 * gcn + 63:64 * gcn + 64], ps_m, ALU.mult, ALU.subtract)
                    ps_h = cx.PD[6]
                    kb.mm(ps_h, MT[:, :], gd_H32[:, :], start=True, stop=False)
                    kb.mm(ps_h, kendt[rs, :], UW[rs, 0:128], start=False, stop=True)
                    kb.copy("act", gd_Hbf[:, :], ps_h)
                    kb.copy("act", gd_H32[:, :], ps_h)
                kb.copy("act", oT[:, bs], ps_o)
            post_norm_gate(oT, zs[:, :], pc("gdn_norm"), 256)

        if "rwkv" in branches:
            L5.i = 0

            def shiftmix(pname, rows, mu_name, hname):
                ps = proj_f(pname, xb)
                raw, hcol = rw_halo[hname]
                if t == 0:
                    kb.memset("pool", raw[0:rows, 0:1], 0.0)
                else:
                    kb.copy("pool", raw[0:rows, 0:1], hcol[0:rows, 0:1])
                kb.copy("act", raw[0:rows, 1:TT + 1], ps)
                kb.copy("pool", hcol[0:rows, 0:1], raw[0:rows, TT:TT + 1])
                dd = F5.get()
                kb.tt("dve", dd[0:rows, :], raw[0:rows, 0:TT], raw[0:rows, 1:TT + 1], ALU.subtract)
                out = L5.get()
                kb.stt(out[0:rows, :], dd[0:rows, :], pc(mu_name, rows), raw[0:rows, 1:TT + 1], ALU.mult, ALU.add)
                return out
            r_ = shiftmix("rr", 128, "mu_r", "r")
            k_ = shiftmix("rk", 128, "mu_k", "k")
            v_ = shiftmix("rv", 128, "mu_v", "v")
            wl = shiftmix("rwl", 64, "mu_wl", "wl")
            al = shiftmix("ral", 64, "mu_al", "al")
            gl = shiftmix("rgl", 128, "mu_gl", "gl")
            tmp = F5.get()
            sgt = F5.get()
            sigmoid_into(kb, sgt[0:64, :], wl[0:64, :], tmp[0:64, :], scale=2.0)
            th = B5.get()
            kb.ts("dve", th[0:64, :], sgt[0:64, :], 2.0, ALU.mult, -1.0, ALU.add)
            ps_z = cx.PB.get()
            kb.mm(ps_z, sm("w_up", 64), th[0:64, :])
            sgw = L5.get()
            tmp = F5.get()
            sigmoid_into(kb, sgw[:, :], ps_z, tmp[:, :], bias=ncol[:, 0:1])
            cs = L5.get()
            kb.scan(cs[:, :], scm[:, :], sgw[:, :], 0.0, ALU.mult, ALU.add)
            albf = B5.get()
            kb.copy("pool", albf[0:64, :], al[0:64, :])
            ps_a = cx.PB.get()
            kb.mm(ps_a, sm("a_up", 64), albf[0:64, :])
            a_ = L5.get()
            tmp = F5.get()
            sigmoid_into(kb, a_[:, :], ps_a, tmp[:, :], bias=ncol[:, 1:2])
            sgg = F5.get()
            tmp = F5.get()
            sigmoid_into(kb, sgg[:, :], gl[:, :], tmp[:, :])
            sggb = B5.get()
            kb.copy("pool", sggb[:, :], sgg[:, :])
            ps_g = cx.PB.get()
            kb.mm(ps_g, sm("g_up", 128), sggb[:, :])
            gte = L5.get()
            kb.copy("act", gte[:, :], ps_g)
            if l > 0:
                ps = proj_f("rvr", xb)
                raw, hcol = rw_halo["vr"]
                if t == 0:
                    kb.memset("pool", raw[0:32, 0:1], 0.0)
                else:
                    kb.copy("pool", raw[0:32, 0:1], hcol[0:32, 0:1])
                kb.copy("act", raw[0:32, 1:TT + 1], ps)
                kb.copy("pool", hcol[0:32, 0:1], raw[0:32, TT:TT + 1])
                dd = F5.get()
                kb.tt("dve", dd[0:32, :], raw[0:32, 0:TT], raw[0:32, 1:TT + 1], ALU.subtract)
                vrl = B5.get()
                kb.stt(vrl[0:32, :], dd[0:32, :], pc("mu_vr", 32), raw[0:32, 1:TT + 1], ALU.mult, ALU.add)
                ps_v = cx.PB.get()
                kb.mm(ps_v, sm("v_up", 32), vrl[0:32, :])
                vg = F5.get()
                tmp = F5.get()
                sigmoid_into(kb, vg[:, :], ps_v, tmp[:, :], bias=ncol[:, 2:3])
                vf = F5.get()
                kb.dma("sp", vf[:, :], d["vfirst"][:, tsl])
                kb.tt("dve", vf[:, :], vf[:, :], v_[:, :], ALU.subtract)
                kb.tt("dve", vf[:, :], vf[:, :], vg[:, :], ALU.mult)
                kb.tt("dve", v_[:, :], v_[:, :], vf[:, :], ALU.add)
            else:
                kb.dma("sp", d["vfirst"][:, tsl], v_[:, :])
            kx = F5.get()
            kb.ts("dve", kx[:, :], k_[:, :], pc("k_k"), ALU.mult)
            kk = l2norm_f(kx[:, :], M["ones_blk"], 1.0)
            tk = F5.get()
            kb.ts("dve", tk[:, :], a_[:, :], -1.0, ALU.add, pc("k_a"), ALU.mult)
            kmod = L5.get()
            kb.stt(kmod[:, :], tk[:, :], 1.0, k_[:, :], ALU.add, ALU.mult)
            bb = L5.get()
            kb.tt("dve", bb[:, :], kk[:, :], a_[:, :], ALU.mult)
            rk = B5.get()
            kb.stt(rk[:, :], r_[:, :], pc("r_k"), kmod[:, :], ALU.mult, ALU.mult)
            ps_bn = cx.PB.get()
            kb.mm(ps_bn, M["ones_blk"], rk[:, :])
            bonus = L5.get()
            kb.tt("dve", bonus[:, :], ps_bn, v_[:, :], ALU.mult)
            ecw = F5.get()
            kb.act(ecw[:, :], cs[:, :], AF.Exp, scale=-C_W)
            encw = L5.get()
            kb.act(encw[:, :], cs[:, :], AF.Exp, scale=C_W)
            dx = F5.get()
            kb.tt("dve", dx[:, :], cs[:, :], sgw[:, :], ALU.subtract)
            eprev = F5.get()
            kb.act(eprev[:, :], dx[:, :], AF.Exp, scale=-C_W)
            c3 = cs.ap[:, :].rearrange("p (c t) -> p c t", t=64)
            dl = F5.get()
            dl3 = dl.ap[:, :].rearrange("p (c t) -> p c t", t=64)
            cl_bc = c3[:, :, 63:64].broadcast_to([128, TT // 64, 64])
            kb.tt("dve", V(dl3, dl.deps), V(cl_bc, cs.deps), V(c3, cs.deps), ALU.subtract)
            eend = F5.get()
            kb.act(eend[:, :], dl[:, :], AF.Exp, scale=-C_W)
            gam = L5.get()
            kb.copy("pool", gam[:, :], ecw[:, :])
            AR = rw_AR
            AR4 = AR.ap[:, :].rearrange("p (b two i) -> p b two i", two=2, i=128)
            e4 = eprev.ap[:, :].rearrange("p (b i) -> p b i", i=128)
            k4 = kk.ap[:, :].rearrange("p (b i) -> p b i", i=128)
            kb.stt(V(AR4[:, :, 0, :], AR.deps), V(k4, kk.deps), -1.0, V(e4, eprev.deps), ALU.mult, ALU.mult)
            r4 = r_.ap[:, :].rearrange("p (b i) -> p b i", i=128)
            g4 = ecw.ap[:, :].rearrange("p (b i) -> p b i", i=128)
            kb.tt("dve", V(AR4[:, :, 1, :], AR.deps), V(r4, r_.deps), V(g4, ecw.deps), ALU.mult)
            bt = B5.get()
            kb.tt("dve", bt[:, :], bb[:, :], encw[:, :], ALU.mult)
            kt = B5.get()
            kb.tt("dve", kt[:, :], kmod[:, :], encw[:, :], ALU.mult)
            bendT = B5.get()
            kb.tt("dve", bendT[:, :], bb[:, :], eend[:, :], ALU.mult)
            kendT = B5.get()
            kb.tt("dve", kendT[:, :], kmod[:, :], eend[:, :], ALU.mult)
            vbf = B5.get()
            kb.copy("pool", vbf[:, :], v_[:, :])
            yT_ = L5.get()
            for blk in range(4):
                bs = slice(blk * 128, blk * 128 + 128)
                ARb = AR[:, blk * 256:blk * 256 + 256]
                BL.i = 0
                At_tok = transpose_bf(ARb[:, 0:128])
                V_tok = transpose_bf(vbf[:, bs])
                Bend = transpose_bf(bendT[:, bs])
                Kend = transpose_bf(kendT[:, bs])
                psQ = cx.PD[2]
                psY = cx.PD[3]
                WUs = []
                for h in range(2):
                    hp = slice(64 * h, 64 * h + 64)
                    ps1 = cx.PH.get()
                    kb.mm(ps1, bt[hp, bs], ARb[hp, :])
                    SB1 = kb_rhs.get()
                    kb.tt("dve", SB1[:, :], ps1, cx.mf[:, 0:256], ALU.mult)
                    ps2 = cx.PH.get()
                    kb.mm(ps2, kt[hp, bs], ARb[hp, :])
                    SB2 = kb_rhs.get()
                    kb.tt("dve", SB2[:, :], ps2, cx.mf[:, 0:256], ALU.mult)
                    psA = cx.PQ.get()
                    kb.mm(psA, ARb[hp, 0:128], bt[hp, bs])
                    PA = BQ.get()
                    kb.tt("dve", PA[:, :], psA, Mf["L_strict"], ALU.mult)
                    TTm = neumann(kb, cx, PA[:, :], SB1[:, 0:128], BQ, M["ident"])
                    psX = cx.PQ.get()
                    kb.mm(psX[:, 0:64], SB2[:, 0:128], V_tok[:, hp])
                    Xb = BQ.get()
                    kb.copy("act", Xb[:, 0:64], psX[:, 0:64])
                    psWU = cx.PQ.get()
                    kb.mm(psWU[:, 0:64], TTm, At_tok[:, hp])
                    kb.mm(psWU[:, 64:128], TTm, Xb[:, 0:64])
                    WU = BL.get()
                    kb.copy("act", WU[:, :], psWU)
                    WUs.append(WU)
                    kb.mm(psQ[hp, :], WU[:, 0:64], SB1[:, 128:256])
                    kb.mm(psY[hp, :], WU[:, 64:128], SB1[:, 128:256], start=True, stop=False)
                    kb.mm(psY[hp, :], V_tok[:, hp], SB2[:, 128:256], start=False, stop=False)
                qeff = BL.get()
                kb.tt("dve", qeff[:, :], ARb[:, 128:256], psQ, ALU.add)
                for c in range(2):
                    rs = slice(c * 64, c * 64 + 64)
                    gcn = blk * 2 + c
                    for h in range(2):
                        hp = slice(64 * h, 64 * h + 64)
                        kb.mm(psY[hp, rs], rw_Hbf[hp, :], qeff[hp, rs], start=False, stop=(c == 1))
                    psM = cx.PD[5]
                    for h in range(2):
                        hp = slice(64 * h, 64 * h + 64)
                        kb.mm(psM[hp, 0:64], WUs[h][rs, 0:64], Bend[rs, hp])
                    MT = rw_MT
                    for h in range(2):
                        hp = slice(64 * h, 64 * h + 64)
                        kb.stt(MT[hp, 64 * h:64 * h + 64], Mf["istack"][hp, :], gam[hp, 64 * gcn + 63:64 * gcn + 64],
                               psM[hp, 0:64], ALU.mult, ALU.add)
                    psH = cx.PD[6]
                    kb.mm(psH[:, 0:64], MT[:, :], rw_H32[:, :], start=True, stop=False)
                    for h in range(2):
                        hp = slice(64 * h, 64 * h + 64)
                        kb.mm(psH[hp, 0:64], Bend[rs, hp], WUs[h][rs, 64:128], start=False, stop=False)
                        kb.mm(psH[hp, 0:64], Kend[rs, hp], V_tok[rs, hp], start=False, stop=True)
                    kb.copy("act", rw_Hbf[:, :], psH[:, 0:64])
                    kb.copy("act", rw_H32[:, :], psH[:, 0:64])
                kb.copy("act", yT_[:, bs], psY)
            ps_m = cx.PB.get()
            kb.mm(ps_m, Mf["ones_blk"], yT_[:, :])
            yc = F5.get()
            kb.stt(yc[:, :], ps_m, -1.0 / 64, yT_[:, :], ALU.mult, ALU.add)
            ysq = F5.get()
            kb.act(ysq[:, :], yc[:, :], AF.Square)
            ps_v2 = cx.PB.get()
            kb.mm(ps_v2, Mf["ones_blk"], ysq[:, :])
            tmp = F5.get()
            rstd = F5.get()
            rsqrt_act(kb, rstd[:, :], ps_v2, 1.0 / 64, EPS_R, tmp[:, :])
            kb.stt(yc[:, :], yc[:, :], pc("ln_g"), rstd[:, :], ALU.mult, ALU.mult)
            kb.stt(yc[:, :], yc[:, :], pc("ln_b"), bonus[:, :], ALU.add, ALU.add)
            yo = B5.get()
            kb.tt("dve", yo[:, :], yc[:, :], gte[:, :], ALU.mult)
            kb.dma("sp", d["ydst"](384, 128, t), yo[:, :])

        if "mla" in branches:
            L5.i = 0
            posi = mla_posi
            kb.dma("sp", posi[:, :], d["pos"][:, tsl])
            posf = L5.get()
            kb.copy("dve", posf[0:96, :], posi[:, :])
            ang = L5.get()
            kb.ts("dve", ang[0:96, :], posf[0:96, :], pc("invf", 96), ALU.mult)
            tabs = []
            for phase in (math.pi / 2, 0.0):
                y = F5.get()
                kb.ts("dve", y[0:96, :], ang[0:96, :], phase, ALU.add, 1.0 / (2 * math.pi), ALU.mult)
                ni = mla_ni
                kb.copy("dve", ni[:, :], y[0:96, :])
                nf = F5.get()
                kb.copy("dve", nf[0:96, :], ni[:, :])
                r = F5.get()
                kb.stt(r[0:96, :], nf[0:96, :], -2 * math.pi, ang[0:96, :], ALU.mult, ALU.add)
                if phase != 0.0:
                    kb.ts("dve", r[0:96, :], r[0:96, :], phase, ALU.add)
                m1 = F5.get()
                kb.ts("dve", m1[0:96, :], r[0:96, :], math.pi, ALU.is_gt, -2 * math.pi, ALU.mult)
                kb.tt("dve", r[0:96, :], r[0:96, :], m1[0:96, :], ALU.add)
                kb.ts("dve", m1[0:96, :], r[0:96, :], -math.pi, ALU.is_lt, 2 * math.pi, ALU.mult)
                kb.tt("dve", r[0:96, :], r[0:96, :], m1[0:96, :], ALU.add)
                tab = L5.get()
                kb.act(tab[0:96, :], r[0:96, :], AF.Sin)
                tabs.append(tab)
            C96, S96 = tabs
            kb.ts("dve", S96[0:96, :], S96[0:96, :], pc("rsign", 96), ALU.mult)
            qn = []
            qr = []
            sqs = []
            for kc in range(2):
                ps = proj_f("qlat", xb, rows=128, c0=128 * kc)
                rr = L5.get()
                kb.copy("act", rr[:, :], ps)
                sq = B5.get()
                kb.act(sq[:, :], rr[:, :], AF.Square)
                qr.append(rr)
                sqs.append(sq)
            ps_s = cx.PB.get()
            kb.mm(ps_s, M["ones"], sqs[0][:, :], start=True, stop=False)
            kb.mm(ps_s, M["ones"], sqs[1][:, :], start=False, stop=True)
            tmp = F5.get()
            rstd = F5.get()
            rsqrt_act(kb, rstd[:, :], ps_s, 1.0 / 256, EPS_N, tmp[:, :])
            for kc in range(2):
                q_ = B5.get()
                kb.tt("dve", q_[:, :], qr[kc][:, :], rstd[:, :], ALU.mult)
                qn.append(q_)
            ps = proj_f("ckv", xb)
            cr = L5.get()
            kb.copy("act", cr[:, :], ps)
            sq = B5.get()
            kb.act(sq[:, :], cr[:, :], AF.Square)
            ps_s = cx.PB.get()
            kb.mm(ps_s, M["ones"], sq[:, :])
            tmp = F5.get()
            rstd = F5.get()
            rsqrt_act(kb, rstd[:, :], ps_s, 1.0 / 128, EPS_N, tmp[:, :])
            cn = B5.get()
            kb.tt("dve", cn[:, :], cr[:, :], rstd[:, :], ALU.mult)
            psK = cx.PB.get()
            proj_f("krope", xb, rows=32, out=psK[64:96, :])
            t1 = F5.get()
            kb.tt("dve", t1[64:96, :], psK[64:96, :], C96[64:96, :], ALU.mult)
            psK2 = cx.PB.get()
            proj_f("krope_sw", xb, rows=32, out=psK2[64:96, :])
            t2 = F5.get()
            kb.tt("dve", t2[64:96, :], psK2[64:96, :], S96[64:96, :], ALU.mult)
            krot = B5.get()
            kb.tt("dve", krot[64:96, :], t1[64:96, :], t2[64:96, :], ALU.add)
            for hh in range(2):
                kb.dma("sp", d["KT"][hh, 64:96, tsl], krot[64:96, :])
                psn = cx.PB.get()
                kb.mm(psn[0:64, :], ukg[:, hh, :], cn[:, :])
                kn_ = B5.get()
                kb.copy("act", kn_[0:64, :], psn[0:64, :])
                kb.dma("sp", d["KT"][hh, 0:64, tsl], kn_[0:64, :])
                psP = cx.PB.get()
                kb.mm(psP[0:96, :], uqg[:, hh, 0, 0, :], qn[0][:, :], start=True, stop=False)
                kb.mm(psP[0:96, :], uqg[:, hh, 1, 0, :], qn[1][:, :], start=False, stop=True)
                t1 = F5.get()
                kb.tt("dve", t1[0:96, :], psP[0:96, :], C96[0:96, :], ALU.mult)
                psS = cx.PB.get()
                kb.mm(psS[0:96, :], uqg[:, hh, 0, 1, :], qn[0][:, :], start=True, stop=False)
                kb.mm(psS[0:96, :], uqg[:, hh, 1, 1, :], qn[1][:, :], start=False, stop=True)
                t2 = F5.get()
                kb.tt("dve", t2[0:96, :], psS[0:96, :], S96[0:96, :], ALU.mult)
                qb_ = B5.get()
                kb.tt("dve", qb_[0:96, :], t1[0:96, :], t2[0:96, :], ALU.add)
                kb.dma("sp", d["QT"][hh, :, tsl], qb_[0:96, :])
            for blk in range(4):
                bs = slice(blk * 128, blk * 128 + 128)
                psV = cx.PQ.get()
                kb.mm(psV, cn[:, bs], uvg[:, :])
                vt = mla_vt[blk % 2]
                kb.op("act", (lambda o_, i_: (lambda h: h.activation(o_, i_, AF.Copy)))(
                    vt.ap[:, :, 0:64], psV.ap.rearrange("p (h e) -> p h e", e=64)), reads=[psV], writes=[vt])
                kb.dma("sp", d["VT"][:, :, t * 4 + blk, :], vt[:, :, :])
    kb.flush()
    st.close()
    return cx


def build_p1b(kb, S, d):
    import os as _os
    if "build_p1b" in _os.environ.get("DBG_SKIP", "").split(","):
        return None

    st = contextlib.ExitStack()
    nt = S // TT
    nb = S // 128
    banks = [kb.psum("ab%d" % i, [128, 512], F32, st) for i in range(8)]
    KTs = kb.sbuf("a_KT", [96, S], BF16, st)
    Vx = kb.sbuf("a_Vx", [128, nb, 65], BF16, st)
    onesf = kb.sbuf("a_ones", [128, 64], F32, st)
    kb.memset("pool", onesf[:, :], 1.0)
    dms = []
    for dd in range(4):
        m = kb.sbuf("a_dm%d" % dd, [128, TT], BF16, st)
        kb.memset("pool", m[:, :], 1.0)
        ma = m.ap[:, :]
        kb.op("pool", (lambda a, b: (lambda h: h.affine_select(a, a, pattern=[[1, TT]], compare_op=ALU.is_ge,
                                                               fill=0.0, base=b, channel_multiplier=-1)))(ma, -128 * dd),
              reads=[m], writes=[m])
        dms.append(m)
    Qts = [kb.sbuf("a_Q%d" % i, [96, TT], BF16, st) for i in range(2)]
    Ps = Pool(kb, "a_P", 4, [128, TT], BF16, stack=st)
    rsb = kb.sbuf("a_rs", [65, TT], F32, st)
    rbs = Pool(kb, "a_rb", 2, [64, TT], F32, stack=st)
    yos = Pool(kb, "a_yo", 2, [64, TT], BF16, stack=st)
    si = 0
    for hh in range(2):
        for c0 in range(0, S, 2048):
            c1 = min(S, c0 + 2048)
            kb.dma("sp", KTs[:, c0:c1], d["KT"][hh, :, c0:c1])
        for b0 in range(0, nb, 32):
            b1 = min(nb, b0 + 32)
            kb.dma("sp", Vx[:, b0:b1, :], d["VT"][:, hh, b0:b1, :])
        for t in range(nt):
            tsl = slice(t * TT, (t + 1) * TT)
            Qt = Qts[t % 2]
            kb.dma("sp", Qt[:, :], d["QT"][hh, :, tsl])
            psO = banks[4 + (t % 2)]
            nkb = 4 * (t + 1)

            def scores(kb_):
                nonlocal si
                dd = kb_ - 4 * t
                c0 = 128 * dd if dd > 0 else 0
                psS = banks[si % 3]
                si += 1
                kb.mm(psS[:, c0:TT], KTs[:, kb_ * 128:kb_ * 128 + 128], Qt[:, c0:TT])
                return psS, c0, dd
            nxt = scores(0)
            for kb_ in range(nkb):
                psS, c0, dd = nxt
                if kb_ + 1 < nkb:
                    nxt = scores(kb_ + 1)
                P = Ps.get()
                kb.act(P[:, c0:TT], psS[:, c0:TT], AF.Exp)
                if dd >= 0:
                    kb.tt("pool", P[:, c0:TT], P[:, c0:TT], dms[dd][:, c0:TT], ALU.mult)
                kb.mm(psO[0:65, c0:TT], Vx[:, kb_, :], P[:, c0:TT], start=(kb_ == 0), stop=(kb_ == nkb - 1))
            kb.copy("act", rsb[64:65, :], psO[64:65, :])
            psB = banks[6]
            kb.mm(psB[0:64, :], onesf[64:65, 0:64], rsb[64:65, :])
            rb = rbs.get()
            kb.recip(rb[:, :], psB[0:64, :])
            yo = yos.get()
            kb.tt("dve", yo[:, :], psO[0:64, :], rb[:, :], ALU.mult)
            kb.dma("sp", d["ydst"](hh * 64, 64, t), yo[:, :])
    kb.flush()
    st.close()


def pack_p2(inp, l):
    P = Packer(128)
    for nm in ("ln1_g", "ln1_b", "ln2_g", "ln2_b"):
        P.add(nm, inp[nm][l].reshape(8, 128).T)
    P.add("rbias", np.repeat(inp["router_bias"][l][None, :], 128, 0))
    P.add("rw", inp["router_w"].reshape(8, 128, 16).transpose(1, 0, 2).reshape(128, 128))
    sel = np.zeros((16, 16, 128), np.float32)
    for e in range(16):
        sel[e, e, :] = 1.0
    P.add("sel", np.concatenate([sel.reshape(16, 2048), np.zeros((112, 2048), np.float32)], 0))
    return P


def ln_fmajor(kb, cx, h32, gname, bname, pcs, F5, EPS, out_fn):
    Mf = cx.Mf
    ps_m = cx.PB.get()
    for j in range(8):
        kb.mm(ps_m, Mf["ones"], h32[:, j, :], start=(j == 0), stop=(j == 7))
    mean = F5.get()
    kb.act(mean[:, :], ps_m, AF.Copy, scale=1.0 / 1024)
    for j in range(8):
        kb.tt("pool", h32[:, j, :], h32[:, j, :], mean[:, :], ALU.subtract)
    ps_v = cx.PB.get()
    for j in range(8):
        sq = F5.get()
        kb.act(sq[:, :], h32[:, j, :], AF.Square)
        kb.mm(ps_v, Mf["ones"], sq[:, :], start=(j == 0), stop=(j == 7))
    tmp = F5.get()
    rstd = F5.get()
    rsqrt_act(kb, rstd[:, :], ps_v, 1.0 / 1024, EPS, tmp[:, :])
    for j in range(8):
        o = F5.get()
        kb.tt("dve", o[:, :], h32[:, j, :], rstd[:, :], ALU.mult)
        kb.ts("dve", o[:, :], o[:, :], pcs(gname, j), ALU.mult, pcs(bname, j), ALU.add)
        out_fn(j, o)


def build_p2(kb, T, l, d):
    import os as _os
    if "build_p2" in _os.environ.get("DBG_SKIP", "").split(","):
        return None

    nt = T // TT
    Po = d["P2off"]

    st = contextlib.ExitStack()
    cx = Ctx()
    setup_common(kb, cx, d["masks"], st)
    wst = Pool(kb, "wst2", 3, [128, 512], F32, stack=st)
    Wg = kb.sbuf("Wg", [128, 8, 4096], BF16, st)
    for k in range(8):
        for c0 in range(0, 4096, 512):
            s_ = wst.get()
            kb.dma("sp", s_[:, :], d["Wgate"][128 * k:128 * k + 128, c0:c0 + 512])
            kb.copy("pool", Wg[:, k, c0:c0 + 512], s_[:, :])
    Wb = kb.sbuf("Wb", [128, 4, 4, 1024], BF16, st)
    for n in range(4):
        for kc in range(4):
            for c0 in range(0, 1024, 512):
                s_ = wst.get()
                kb.dma("sp", s_[:, :], d["Wb"][n, 128 * kc:128 * kc + 128, c0:c0 + 512])
                kb.copy("pool", Wb[:, n, kc, c0:c0 + 512], s_[:, :])
    F5 = Pool(kb, "p2f", 8, [128, TT], F32, stack=st)
    yst = Pool(kb, "p2ys", 6, [128, TT], BF16, stack=st)
    OH = kb.sbuf("p2oh", [128, 4], F32, st)
    kb.dma("sp", OH[:, :], d["onehot"])
    xbf = kb.sbuf("p2x", [128, 8, TT], BF16, st)
    ybf = kb.sbuf("p2y", [128, 16, TT], BF16, st)
    m32 = kb.sbuf("p2m", [128, 8, TT], F32, st)
    mbf = kb.sbuf("p2mb", [128, 8, TT], BF16, st)
    for t in range(nt):
        tsl = slice(t * TT, (t + 1) * TT)
        for k in range(8):
            s_ = wst.get()
            kb.dma("sp", s_[:, :], d["xsrc2"](k, t))
            kb.copy("pool", xbf[:, k, :], s_[:, :])
        for c in range(16):
            n_, hg_ = c // 4, c % 4
            r0 = hg_ * 512 + n_ * 128
            for jq in range(4):
                yq = yst.get()
                kb.dma("sp", yq[:, :], d["ysrc"](r0, jq, t))
                if jq == 0:
                    kb.ts("pool", ybf[:, c, :], yq[:, :], OH[:, 0:1], ALU.mult)
                else:
                    kb.stt(ybf[:, c, :], yq[:, :], OH[:, jq:jq + 1], ybf[:, c, :], ALU.mult, ALU.add)
        for n in range(4):
            for j in range(8):
                ps_g = cx.PB.get()
                for k in range(8):
                    kb.mm(ps_g, Wg[:, k, n * 1024 + 128 * j:n * 1024 + 128 * j + 128], xbf[:, k, :],
                          start=(k == 0), stop=(k == 7))
                e = F5.get()
                kb.act(e[:, :], ps_g, AF.Exp, scale=-1.0)
                kb.ts("pool", e[:, :], e[:, :], 1.0, ALU.add)
                sg = F5.get()
                kb.recip(sg[:, :], e[:, :])
                ps_p = cx.banks[2 + (j % 2)][:, :]
                for kc in range(4):
                    kb.mm(ps_p, Wb[:, n, kc, 128 * j:128 * j + 128], ybf[:, 4 * n + kc, :],
                          start=(kc == 0), stop=(kc == 3))
                if n == 0:
                    kb.tt("dve", m32[:, j, :], sg[:, :], ps_p, ALU.mult)
                else:
                    tq = F5.get()
                    kb.tt("dve", tq[:, :], sg[:, :], ps_p, ALU.mult)
                    kb.tt("pool", m32[:, j, :], m32[:, j, :], tq[:, :], ALU.add)
        for j in range(8):
            kb.copy("act", mbf[:, j, :], m32[:, j, :])
            kb.dma("sp", d["mT"][128 * j:128 * j + 128, tsl], mbf[:, j, :])
    kb.flush()
    st.close()

    st = contextlib.ExitStack()
    cx = Ctx()
    setup_common(kb, cx, d["masks"], st)
    PC = kb.sbuf("PC2", [128, d["NP2"]], F32, st)
    kb.dma("sp", PC[:, :], d["PC2"])

    def pcs(name, j):
        o, w = Po[name]
        return PC[:, o + j:o + j + 1]
    cst = kb.sbuf("cst2", [128, 2], F32, st)
    kb.memset("pool", cst[:, 0:1], LN_EPS)
    EPS = cst[:, 0:1]
    wst = Pool(kb, "wst3", 3, [128, 512], F32, stack=st)
    Wo = kb.sbuf("Wo", [128, 8, 1024], BF16, st)
    for k in range(8):
        for c0 in range(0, 1024, 512):
            s_ = wst.get()
            kb.dma("sp", s_[:, :], d["Wout"][128 * k:128 * k + 128, c0:c0 + 512])
            kb.copy("pool", Wo[:, k, c0:c0 + 512], s_[:, :])
    F5 = Pool(kb, "p2g", 10, [128, TT], F32, stack=st)
    mbf = kb.sbuf("p2mb2", [128, 8, TT], BF16, st)
    x32 = kb.sbuf("p2x32", [128, 8, TT], F32, st)
    h32 = kb.sbuf("p2h32", [128, 8, TT], F32, st)
    for t in range(nt):
        tsl = slice(t * TT, (t + 1) * TT)
        for k in range(8):
            kb.dma("sp", mbf[:, k, :], d["mT"][128 * k:128 * k + 128, tsl])
            kb.dma("sp", x32[:, k, :], d["xsrc2"](k, t))
        for j in range(8):
            ps = cx.PB.get()
            for k in range(8):
                kb.mm(ps, Wo[:, k, 128 * j:128 * j + 128], mbf[:, k, :], start=(k == 0), stop=(k == 7))
            kb.stt(h32[:, j, :], x32[:, j, :], ALPHA, ps, ALU.mult, ALU.add)

        def out1(j, o, tsl=tsl):
            kb.dma("sp", d["x1T"][128 * j:128 * j + 128, tsl], o[:, :])
        ln_fmajor(kb, cx, h32, "ln1_g", "ln1_b", pcs, F5, EPS, out1)
    kb.flush()
    st.close()

    HT = min(T, 2048)
    for half in range(T // HT):
        st = contextlib.ExitStack()
        cx = Ctx()
        setup_common(kb, cx, d["masks"], st)
        Mf = cx.Mf
        PC = kb.sbuf("PC2b", [128, d["NP2"]], F32, st)
        kb.dma("sp", PC[:, :], d["PC2"])
        PCb = kb.sbuf("PC2bb", [16, 2048], BF16, st)
        o_sel = Po["sel"][0]
        kb.copy("pool", PCb[:, :], PC[0:16, o_sel:o_sel + 2048])

        def pcs(name, j):
            o, w = Po[name]
            return PC[:, o + j:o + j + 1]
        cst = kb.sbuf("cst3", [128, 2], F32, st)
        kb.memset("pool", cst[:, 0:1], LN_EPS)
        EPS = cst[:, 0:1]
        o_rw = Po["rw"][0]
        o_rb = Po["rbias"][0]
        wst = Pool(kb, "wst4", 4, [128, 512], F32, stack=st)
        F5 = Pool(kb, "p2h", 8, [128, TT], F32, stack=st)
        S16 = Pool(kb, "s16", 24, [128, 16], F32, stack=st)
        x1bf = kb.sbuf("x1bf", [128, 8, HT], BF16, st)
        acc = kb.sbuf("acc", [128, 8, HT], F32, st)
        cTb = kb.sbuf("cTb", [16, HT], BF16, st)
        nth = HT // TT
        for t in range(nth):
            g0 = half * HT + t * TT
            rps = [cx.banks[b][:, 0:16] for b in range(4)]
            for k in range(8):
                s_ = wst.get()
                kb.dma("sp", s_[:, :], d["x1T"][128 * k:128 * k + 128, g0:g0 + TT])
                kb.copy("pool", x1bf[:, k, t * TT:(t + 1) * TT], s_[:, :])
                for blk in range(4):
                    kb.mm(rps[blk], s_[:, blk * 128:blk * 128 + 128], PC[:, o_rw + 16 * k:o_rw + 16 * k + 16],
                          start=(k == 0), stop=(k == 7))
            for blk in range(4):
                e = S16.get()
                kb.act(e[:, :], rps[blk], AF.Exp, scale=-1.0)
                kb.ts("dve", e[:, :], e[:, :], 1.0, ALU.add)
                sc = S16.get()
                kb.recip(sc[:, :], e[:, :])
                bi = S16.get()
                kb.tt("dve", bi[:, :], sc[:, :], PC[:, o_rb:o_rb + 16], ALU.add)
                b4 = bi.ap[:, :].rearrange("p (g e) -> p g e", e=4)
                p6 = S16.get()
                p6v = p6.ap[:, :].rearrange("p (g s) -> p g s", s=4)
                p6b = S16.get()
                p6bv = p6b.ap[:, :].rearrange("p (g s) -> p g s", s=4)
                pairs = [(0, 1), (0, 2), (0, 3), (1, 2), (1, 3), (2, 3)]
                for pi, (i_, j_) in enumerate(pairs):
                    dst = p6v[:, :, pi:pi + 1] if pi < 4 else p6bv[:, :, pi - 4:pi - 3]
                    dd = p6 if pi < 4 else p6b
                    kb.tt("dve", V(dst, dd.deps), V(b4[:, :, i_:i_ + 1], bi.deps), V(b4[:, :, j_:j_ + 1], bi.deps), ALU.add)
                gs = S16.get()
                gsv = gs.ap[:, 0:4].rearrange("p (g o) -> p g o", o=1)
                kb.tt("dve", V(gsv, gs.deps), V(p6v[:, :, 0:1], p6.deps), V(p6v[:, :, 1:2], p6.deps), ALU.max)
                kb.tt("dve", V(gsv, gs.deps), V(gsv, gs.deps), V(p6v[:, :, 2:3], p6.deps), ALU.max)
                kb.tt("dve", V(gsv, gs.deps), V(gsv, gs.deps), V(p6v[:, :, 3:4], p6.deps), ALU.max)
                kb.tt("dve", V(gsv, gs.deps), V(gsv, gs.deps), V(p6bv[:, :, 0:1], p6b.deps), ALU.max)
                kb.tt("dve", V(gsv, gs.deps), V(gsv, gs.deps), V(p6bv[:, :, 1:2], p6b.deps), ALU.max)
                gm = S16.get()
                kb.op("dve", (lambda o_, i_: (lambda h: h.tensor_reduce(o_, i_, AX.X, ALU.max)))(gm.ap[:, 0:1], gs.ap[:, 0:4]),
                      reads=[gs], writes=[gm])
                ing = S16.get()
                kb.ts("dve", ing[:, 0:4], gs[:, 0:4], gm[:, 0:1], ALU.is_ge)
                off = S16.get()
                kb.ts("dve", off[:, 0:4], ing[:, 0:4], 1e30, ALU.mult, -1e30, ALU.add)
                mk = S16.get()
                mk4 = mk.ap[:, :].rearrange("p (g e) -> p g e", e=4)
                ing_bc = ing.ap[:, 0:4].rearrange("p (g o) -> p g o", o=1).broadcast_to([128, 4, 4])
                off_bc = off.ap[:, 0:4].rearrange("p (g o) -> p g o", o=1).broadcast_to([128, 4, 4])
                kb.tt("dve", V(mk4, mk.deps), V(b4, bi.deps), V(ing_bc, ing.deps), ALU.mult)
                kb.tt("dve", V(mk4, mk.deps), V(mk4, mk.deps), V(off_bc, off.deps), ALU.add)
                t8 = S16.get()
                kb.op("dve", (lambda o_, i_: (lambda h: h.max(o_, i_)))(t8.ap[:, 0:8], mk.ap[:, :]), reads=[mk], writes=[t8])
                sel = S16.get()
                kb.ts("dve", sel[:, :], mk[:, :], t8[:, 1:2], ALU.is_ge)
                ws = S16.get()
                kb.tt("dve", ws[:, :], sel[:, :], sc[:, :], ALU.mult)
                den = S16.get()
                kb.op("dve", (lambda o_, i_: (lambda h: h.tensor_reduce(o_, i_, AX.X, ALU.add)))(den.ap[:, 0:1], ws.ap[:, :]),
                      reads=[ws], writes=[den])
                kb.recip(den[:, 1:2], den[:, 0:1])
                cmb = S16.get()
                kb.ts("dve", cmb[:, :], ws[:, :], den[:, 1:2], ALU.mult)
                ps_t = cx.banks[4][0:16, 0:128]
                kb.mm(ps_t, cmb[:, :], Mf["ident"])
                kb.copy("act", cTb[:, t * TT + blk * 128:t * TT + blk * 128 + 128], ps_t)
        st2 = contextlib.ExitStack()
        Wgs = [kb.sbuf("Wge%d" % i, [128, 8, 512], BF16, st2) for i in range(2)]
        Wus = [kb.sbuf("Wue%d" % i, [128, 8, 512], BF16, st2) for i in range(2)]
        Wds = [kb.sbuf("Wde%d" % i, [128, 4, 1024], BF16, st2) for i in range(2)]
        cbcs = Pool(kb, "cbc", 2, [128, TT], BF16, stack=st2)
        acts = Pool(kb, "actb", 8, [128, TT], BF16, stack=st2)
        gi = 0

        def load_expert(e):
            Wg_, Wu_, Wd_ = Wgs[e % 2], Wus[e % 2], Wds[e % 2]
            for k in range(8):
                for (dst, src) in ((Wg_, d["moe_g"]), (Wu_, d["moe_u"])):
                    s_ = wst.get()
                    kb.dma("sp", s_[:, :], src[e, 128 * k:128 * k + 128, :])
                    kb.copy("act", dst[:, k, :], s_[:, :])
            for fc in range(4):
                for c0 in (0, 512):
                    s_ = wst.get()
                    kb.dma("sp", s_[:, :], d["moe_d"][e, 128 * fc:128 * fc + 128, c0:c0 + 512])
                    kb.copy("act", Wd_[:, fc, c0:c0 + 512], s_[:, :])
        load_expert(0)
        for e in range(16):
            Wg_, Wu_, Wd_ = Wgs[e % 2], Wus[e % 2], Wds[e % 2]
            if e + 1 < 16:
                load_expert(e + 1)
            for t in range(nth):
                tl = slice(t * TT, (t + 1) * TT)
                ps_c = cx.banks[6][:, :]
                kb.mm(ps_c, PCb[0:16, 128 * e:128 * e + 128], cTb[:, tl])
                cbc = cbcs.get()
                kb.copy("act", cbc[:, :], ps_c)
                at = []
                for fc in range(4):
                    ps_g = cx.banks[gi % 2][:, :]
                    ps_u = cx.banks[2 + gi % 2][:, :]
                    gi += 1
                    for k in range(8):
                        kb.mm(ps_g, Wg_[:, k, 128 * fc:128 * fc + 128], x1bf[:, k, tl], start=(k == 0), stop=(k == 7))
                    for k in range(8):
                        kb.mm(ps_u, Wu_[:, k, 128 * fc:128 * fc + 128], x1bf[:, k, tl], start=(k == 0), stop=(k == 7))
                    e_ = F5.get()
                    kb.act(e_[:, :], ps_g, AF.Exp, scale=-1.0)
                    kb.ts("pool", e_[:, :], e_[:, :], 1.0, ALU.add)
                    r_ = F5.get()
                    kb.recip(r_[:, :], e_[:, :])
                    kb.tt("dve", r_[:, :], r_[:, :], ps_g, ALU.mult)
                    kb.tt("dve", r_[:, :], r_[:, :], ps_u, ALU.mult)
                    a_ = acts.get()
                    kb.tt("pool", a_[:, :], r_[:, :], cbc[:, :], ALU.mult)
                    at.append(a_)
                for j in range(8):
                    ps_d = cx.banks[4 + j % 2][:, :]
                    for fc in range(4):
                        kb.mm(ps_d, Wd_[:, fc, 128 * j:128 * j + 128], at[fc][:, :], start=(fc == 0), stop=(fc == 3))
                    if e == 0:
                        kb.copy("dve", acc[:, j, tl], ps_d)
                    else:
                        kb.tt("dve", acc[:, j, tl], acc[:, j, tl], ps_d, ALU.add)
        kb.flush()
        st2.close()
        x32 = kb.sbuf("b_x32", [128, 8, TT], F32, st)
        for t in range(nth):
            g0 = half * HT + t * TT
            tl = slice(t * TT, (t + 1) * TT)
            for k in range(8):
                kb.dma("sp", x32[:, k, :], d["x1T"][128 * k:128 * k + 128, g0:g0 + TT])
                kb.stt(x32[:, k, :], x32[:, k, :], ALPHA, acc[:, k, tl], ALU.mult, ALU.add)

            def out2(j, o, g0=g0):
                kb.dma("sp", d["odst"](j, g0 // TT), o[:, :])
            ln_fmajor(kb, cx, x32, "ln2_g", "ln2_b", pcs, F5, EPS, out2)
        kb.flush()
        st.close()


def _din(nc, name, arr):
    dt = I32 if arr.dtype == np.int32 else F32
    t = nc.dram_tensor(name, list(arr.shape), dt, kind="ExternalInput")
    return V(t.ap(), [Dep()])


def _dout(nc, name, shape, dt=F32):
    t = nc.dram_tensor(name, list(shape), dt, kind="ExternalOutput")
    return V(t.ap(), [Dep()])


def _dint(nc, name, shape, dt):
    t = nc.dram_tensor(name, list(shape), dt)
    return V(t.ap(), [Dep()])


GROUPS = [[0, 1, 2, 3], [4, 5, 6, 7]]


def kernel(**inputs):
    inp = {k: np.asarray(v) for k, v in inputs.items()}
    x = inp["x"]
    B, S = x.shape[0], x.shape[1]
    T = S // 4
    xT = [np.ascontiguousarray(x[b].T) for b in range(B)]
    masks = masks_host()
    scm = scanmask_host()
    packs = [[pack_p1a(inp, l, hg) for hg in range(4)] for l in range(2)]
    arrs = [[(W.build(), P.build(), M.build()) for (W, P, M) in packs[l]] for l in range(2)]
    p2 = [pack_p2(inp, l) for l in range(2)]
    p2a = [p.build() for p in p2]
    posrep = [np.ascontiguousarray(np.repeat(inp["positions"][b][None, :], 96, 0)).astype(np.int32) for b in range(B)]
    common = {"Wgate0": np.ascontiguousarray(inp["w_in"][0][:, O_GATES:O_GATES + 4096]),
              "Wgate1": np.ascontiguousarray(inp["w_in"][1][:, O_GATES:O_GATES + 4096]),
              "Wb": np.ascontiguousarray(inp["w_branch"]), "Wout": np.ascontiguousarray(inp["w_out"]),
              "PC2_0": p2a[0], "PC2_1": p2a[1], "moe_g": np.ascontiguousarray(inp["moe_w_gate"]),
              "moe_u": np.ascontiguousarray(inp["moe_w_up"]), "moe_d": np.ascontiguousarray(inp["moe_w_down"]),
              "masks": masks, "scanmask": scm}
    in_maps = []
    for b in range(B):
        for r in range(4):
            oh = np.zeros((128, 4), np.float32)
            oh[:, r] = 1.0
            m = {"xT": xT[b], "xq": np.ascontiguousarray(xT[b][:, r * T:(r + 1) * T]), "pos": posrep[b], "onehot": oh}
            for l in range(2):
                m["W%d" % l], m["PC%d" % l], m["SM%d" % l] = arrs[l][r]
            m.update(common)
            in_maps.append({"in_" + k: v for k, v in m.items()})

    nc = bass.Bass("TRN2", target_bir_lowering=False)
    kb = KB(nc)
    di = {k[3:]: _din(nc, k, v) for k, v in in_maps[0].items()}
    CW = 1024
    nyc = S // CW
    yTc = _dint(nc, "yTc", [nyc, 512, CW], BF16)
    ygc = _dint(nc, "ygc", [nyc, 2048, CW], BF16)
    ntq = T // TT
    x2c = _dint(nc, "x2c", [ntq, 2, 512, TT], F32)
    xgc = _dint(nc, "xgc", [ntq, 2, 2048, TT], F32)
    vfirst = _dint(nc, "vfirst_s", [128, S], F32)
    oT = _dout(nc, "oT", [1024, T])
    scratch = {"QT": _dint(nc, "QT", [2, 96, S], BF16), "KT": _dint(nc, "KT", [2, 96, S], BF16),
               "VT": _dint(nc, "VT", [128, 2, S // 128, 65], BF16),
               "x1T": _dint(nc, "x1T", [1024, T], F32), "mT": _dint(nc, "mT", [1024, T], BF16)}

    def ydst(row0, nrows, t):
        c, o = divmod(t * TT, CW)
        return yTc[c][row0:row0 + nrows, o:o + TT]

    def ysrc(r0, jq, t):
        c, o = divmod(jq * T + t * TT, CW)
        return ygc[c][r0:r0 + 128, o:o + TT]

    def x2_local(k, t):
        return x2c[t][k // 4][(k % 4) * 128:(k % 4) * 128 + 128, :]

    def x_gathered(k, tt):
        q, tl = divmod(tt, ntq)
        return xgc[tl][k // 4][q * 512 + (k % 4) * 128:q * 512 + (k % 4) * 128 + 128, :]

    for l in range(2):
        W0, P0, M0 = packs[l][0]
        d = {"pos": di["pos"], "W": di["W%d" % l], "PC": di["PC%d" % l], "SM": di["SM%d" % l],
             "masks": di["masks"], "scanmask": di["scanmask"], "ydst": ydst, "vfirst": vfirst}
        d.update(scratch)
        d.update(Woff=W0.off, Poff=P0.off, Moff=M0.off, NW=arrs[l][0][0].shape[1], NP=arrs[l][0][1].shape[1],
                 NM=arrs[l][0][2].shape[1])
        if l == 0:
            d["xsrc"] = lambda k, t: di["xT"][128 * k:128 * k + 128, t * TT:(t + 1) * TT]
        else:
            d["xsrc"] = x_gathered
        build_p1a(kb, S, l, d)
        build_p1b(kb, S, d)
        for c in range(nyc):
            kb.coll("AllGather", ygc[c], yTc[c], GROUPS)
        d2 = {"ysrc": ysrc, "onehot": di["onehot"], "Wgate": di["Wgate%d" % l], "Wb": di["Wb"][l], "Wout": di["Wout"][l],
              "PC2": di["PC2_%d" % l], "moe_g": di["moe_g"][l], "moe_u": di["moe_u"][l], "moe_d": di["moe_d"][l],
              "masks": di["masks"], "x1T": scratch["x1T"], "mT": scratch["mT"],
              "P2off": p2[l].off, "NP2": p2a[l].shape[1]}
        if l == 0:
            d2["xsrc2"] = lambda k, t: di["xq"][128 * k:128 * k + 128, t * TT:(t + 1) * TT]
            d2["odst"] = lambda j, t: x2_local(j, t)
        else:
            d2["xsrc2"] = x2_local
            d2["odst"] = lambda j, t: oT[128 * j:128 * j + 128, t * TT:(t + 1) * TT]
        build_p2(kb, T, l, d2)
        if l == 0:
            for t_ in range(ntq):
                for h_ in range(2):
                    kb.coll("AllGather", xgc[t_][h_], x2c[t_][h_], GROUPS)
    kb.flush()
    kb.close()
    res = run_bass_kernel_spmd(nc, in_maps, core_ids=list(range(len(in_maps))))
    out = np.empty((B, S, 1024), np.float32)
    for b in range(B):
        for q in range(4):
            out[b, q * T:(q + 1) * T, :] = res.results[b * 4 + q]["oT"].T
    return out
```

```python
import contextlib
import math
import numpy as np
import ml_dtypes
import concourse.bass as bass
import concourse.mybir as mybir
from concourse.bass_utils import run_bass_kernel_spmd

F32 = mybir.dt.float32
BF16 = mybir.dt.bfloat16
I32 = mybir.dt.int32
ALU = mybir.AluOpType
AF = mybir.ActivationFunctionType
AX = mybir.AxisListType

D_MODEL = 1024
IN_COLS = 9912
TT = 512
NKC = 8


class Dep:
    __slots__ = ("w", "r", "wd")

    def __init__(self):
        self.w = None
        self.r = []
        self.wd = []


class V:
    __slots__ = ("ap", "deps")

    def __init__(self, ap, deps):
        self.ap = ap
        self.deps = deps

    def __getitem__(self, k):
        return V(self.ap[k], self.deps)

    def v(self, ap):
        return V(ap, self.deps)


class Eng:
    def __init__(self, name):
        self.name = name
        self.count = 0
        self.ops = []
        self.waited = {}


class KB:
    def __init__(self, nc):
        self.nc = nc
        self.stack = contextlib.ExitStack()
        self.E = {n: Eng(n) for n in ("pe", "act", "dve", "pool", "sp")}
        import os as _os2
        self.ndma = int(_os2.environ.get("DBG_NDMA", "32"))
        self.dma_state = [0] * self.ndma
        self.dma_next = 0
        self.n_ops = 0
        self.out_tokens = []
        import os as _os
        self.limit = int(_os.environ["DBG_LIMIT"]) if _os.environ.get("DBG_LIMIT") else None
        self.dump = int(_os.environ["DBG_DUMP"]) if _os.environ.get("DBG_DUMP") else None

    def _nm(self, name):
        self._uid = getattr(self, "_uid", 0) + 1
        return "%s_u%d" % (name, self._uid)

    def sbuf(self, name, shape, dt, stack=None):
        t = (stack or self.stack).enter_context(self.nc.sbuf_tensor(self._nm(name), list(shape), dt))
        return V(t[:] if False else t, [Dep()])

    def psum(self, name, shape, dt=F32, stack=None):
        t = (stack or self.stack).enter_context(self.nc.psum_tensor(self._nm(name), list(shape), dt))
        return V(t, [Dep()])

    def dram(self, name, shape, dt, kind="Internal"):
        t = self.nc.dram_tensor(name, list(shape), dt, kind=kind)
        return V(t.ap(), [Dep()])

    @staticmethod
    def _deps(reads, writes):
        out = []
        for d in reads:
            if d.w is not None:
                out.append(d.w)
            out.extend(d.wd)
        for d in writes:
            if d.w is not None:
                out.append(d.w)
            out.extend(d.wd)
            out.extend(d.r)
        return out

    def _waits(self, e, reads, writes, skip):
        need = {}
        for (pn, pv) in self._deps(reads, writes):
            if pn == skip:
                continue
            if need.get(pn, 0) < pv:
                need[pn] = pv
        waits = []
        for pn, pv in need.items():
            if e.waited.get(pn, 0) < pv:
                e.waited[pn] = pv
                waits.append((pn, pv))
        return waits

    @staticmethod
    def _mark(tok, reads, writes):
        for d in reads:
            d.r.append(tok)
            if len(d.r) > 64:
                last = {}
                for (n, v) in d.r:
                    if last.get(n, 0) < v:
                        last[n] = v
                d.r = list(last.items())
        is_async = tok[0].startswith("dma") or tok[0] == "coll"
        for d in writes:
            if is_async:
                d.wd.append(tok)
                if len(d.wd) > 48:
                    last = {}
                    for (n, v) in d.wd:
                        if last.get(n, 0) < v:
                            last[n] = v
                    d.wd = list(last.items())
            else:
                d.w = tok
                d.wd = []
            d.r = []

    def op(self, eng, fn, reads=(), writes=()):
        if self.limit is not None and self.n_ops >= self.limit:
            return None
        if self.dump is not None and abs(self.n_ops - self.dump) <= 3:
            import sys as _s
            f = _s._getframe(1)
            chain = []
            while f is not None and len(chain) < 4:
                chain.append("%s:%d" % (f.f_code.co_name, f.f_lineno))
                f = f.f_back
            print("OP", self.n_ops, eng, chain)
        rd = [d for v in reads for d in v.deps]
        wd = [d for v in writes for d in v.deps]
        e = self.E[eng]
        skip = "pe" if eng == "pe" else None
        waits = self._waits(e, rd, wd, skip)
        e.count += 1
        tok = (eng, e.count)
        e.ops.append((waits, fn, None))
        self._mark(tok, rd, wd)
        self.n_ops += 1
        return tok

    def dma(self, q, out, in_, **kw):
        if self.limit is not None and self.n_ops >= self.limit:
            return None
        rd = list(in_.deps)
        wd = list(out.deps)
        e = self.E[q]
        waits = self._waits(e, rd, wd, None)
        s = self.dma_next
        self.dma_next = (s + 1) % self.ndma
        prev = self.dma_state[s]
        name = "dma%d" % s
        if prev > 0 and e.waited.get(name, 0) < prev:
            e.waited[name] = prev
            waits.append((name, prev))
        self.dma_state[s] = prev + 16
        tok = (name, prev + 16)
        oa, ia = out.ap, in_.ap
        e.ops.append((waits, lambda h: h.dma_start(out=oa, in_=ia, **kw), name))
        self._mark(tok, rd, wd)
        self.n_ops += 1
        return tok

    def coll(self, kind, out, in_, groups):
        import os as _os
        if _os.environ.get("DBG_NOCOLL"):
            return None
        self._cidx = getattr(self, "_cidx", -1) + 1
        if _os.environ.get("DBG_COLLMASK") and not (int(_os.environ["DBG_COLLMASK"]) >> self._cidx) & 1:
            return None
        rd = list(in_.deps)
        wd = list(out.deps)
        e = self.E["pool"]
        waits = self._waits(e, rd, wd, None)
        self.ncoll = getattr(self, "ncoll", 0) + 1
        tok = ("coll", self.ncoll)
        oa, ia = out.ap, in_.ap
        e.ops.append((waits, lambda h: h.collective_compute(kind, ALU.bypass, replica_groups=groups, ins=[ia], outs=[oa]), "coll"))
        self._mark(tok, rd, wd)
        self.n_ops += 1
        return tok

    def _ensure_sems(self):
        if getattr(self, "sem", None) is None:
            self.sem = {}
            for n in ("pe", "act", "dve", "pool"):
                self.sem[n] = self.stack.enter_context(self.nc.semaphore("s_" + n))
            for i in range(self.ndma):
                self.sem["dma%d" % i] = self.stack.enter_context(self.nc.semaphore("s_dma%d" % i))
            self.sem["coll"] = self.stack.enter_context(self.nc.semaphore("s_coll"))

    def flush(self):
        self._ensure_sems()
        sem = self.sem
        import os as _os3
        if _os3.environ.get("DBG_FLUSHLOG"):
            print("FLUSH at n_ops", self.n_ops)
        toks = [(n, self.E[n].count) for n in ("pe", "act", "dve", "pool") if self.E[n].count > 0]
        toks += [("dma%d" % i, v) for i, v in enumerate(self.dma_state) if v > 0]
        if getattr(self, "ncoll", 0) > 0:
            toks.append(("coll", self.ncoll))
        for n, e in self.E.items():
            waits = []
            for (pn, pv) in toks:
                if pn == n:
                    continue
                if e.waited.get(pn, 0) < pv:
                    e.waited[pn] = pv
                    waits.append((pn, pv))
            e.ops.append((waits, None, None))
        with self.nc.Block() as block:
            def replay(e, own):
                ops = e.ops

                def body(h):
                    for waits, fn, dsem in ops:
                        for (pn, pv) in waits:
                            h.wait_ge(sem[pn], pv)
                        if fn is None:
                            continue
                        ins = fn(h)
                        if dsem == "coll":
                            ins.then_inc(sem[dsem], 1)
                        elif dsem is not None:
                            ins.then_inc(sem[dsem], 16)
                        elif own is not None:
                            ins.then_inc(sem[own], 1)
                return body
            block.sync(replay(self.E["sp"], None))
            block.tensor(replay(self.E["pe"], "pe"))
            block.scalar(replay(self.E["act"], "act"))
            block.vector(replay(self.E["dve"], "dve"))
            block.gpsimd(replay(self.E["pool"], "pool"))
        for e in self.E.values():
            e.ops = []

    def emit(self):
        self.flush()

    def close(self):
        self.stack.close()

    def mm(self, out, lhsT, rhs, start=True, stop=True):
        oa, la, ra = out.ap, lhsT.ap, rhs.ap
        return self.op("pe", lambda h: h.matmul(oa, la, ra, start=start, stop=stop),
                       reads=[lhsT, rhs], writes=[out])

    def act(self, out, in_, func, scale=1.0, bias=None, eng="act"):
        oa, ia = out.ap, in_.ap
        reads = [in_]
        kw = {}
        if isinstance(bias, V):
            reads.append(bias)
            kw["bias"] = bias.ap
        elif bias is not None:
            kw["bias"] = bias
        if isinstance(scale, V):
            reads.append(scale)
            kw["scale"] = scale.ap
        else:
            kw["scale"] = scale
        return self.op("act", lambda h: h.activation(oa, ia, func, **kw), reads=reads, writes=[out])

    def tt(self, eng, out, in0, in1, op):
        oa, a, b = out.ap, in0.ap, in1.ap
        return self.op(eng, lambda h: h.tensor_tensor(oa, a, b, op), reads=[in0, in1], writes=[out])

    def ts(self, eng, out, in0, s1, op0, s2=None, op1=None):
        oa, a = out.ap, in0.ap
        reads = [in0]
        if isinstance(s1, V):
            reads.append(s1)
            s1 = s1.ap
        if isinstance(s2, V):
            reads.append(s2)
            s2 = s2.ap
        if op1 is None:
            return self.op(eng, lambda h: h.tensor_scalar(oa, a, s1, None, op0), reads=reads, writes=[out])
        return self.op(eng, lambda h: h.tensor_scalar(oa, a, s1, s2, op0, op1), reads=reads, writes=[out])

    def stt(self, out, in0, scalar, in1, op0, op1):
        oa, a, b = out.ap, in0.ap, in1.ap
        reads = [in0, in1]
        if isinstance(scalar, V):
            reads.append(scalar)
            scalar = scalar.ap
        return self.op("dve", lambda h: h.scalar_tensor_tensor(oa, a, scalar, b, op0, op1),
                       reads=reads, writes=[out])

    def copy(self, eng, out, in_):
        oa, ia = out.ap, in_.ap
        if eng == "act":
            return self.op("act", lambda h: h.activation(oa, ia, AF.Copy), reads=[in_], writes=[out])
        return self.op(eng, lambda h: h.tensor_copy(oa, ia), reads=[in_], writes=[out])

    def memset(self, eng, out, val):
        oa = out.ap
        return self.op(eng, lambda h: h.memset(oa, val), writes=[out])

    def scan(self, out, d0, d1, init, op0, op1):
        oa, a, b = out.ap, d0.ap, d1.ap
        return self.op("dve", lambda h: h.tensor_tensor_scan(oa, a, b, init, op0, op1),
                       reads=[d0, d1], writes=[out])

    def recip(self, out, in_):
        oa, ia = out.ap, in_.ap
        return self.op("dve", lambda h: h.reciprocal(oa, ia), reads=[in_], writes=[out])


class Pool:
    def __init__(self, kb, name, n, shape, dt, space="sbuf", stack=None):
        mk = kb.sbuf if space == "sbuf" else kb.psum
        self.t = [mk("%s%d" % (name, i), shape, dt, stack=stack) for i in range(n)]
        self.i = 0

    def get(self):
        t = self.t[self.i]
        self.i = (self.i + 1) % len(self.t)
        return t


C_W = math.exp(-0.5)
MLA_SCALE = 96 ** -0.5
LN_EPS = 1e-5
NORM_EPS = 1e-6
RWKV_LN_EPS = 64e-5
ALPHA = (2.0 * 2) ** 0.25

_IW = (256, 160, 256, 256, 512, 16, 512, 1536, 4, 4, 512, 1792, 4096)
_IO = np.concatenate([[0], np.cumsum(_IW)]).tolist()
(O_QLAT, O_KVLAT, O_GQ, O_GK, O_GV, O_GGATE, O_GOG, O_DQKV, O_DBETA, O_DA, O_DZ, O_RW, O_GATES) = _IO[:13]


class Packer:
    def __init__(self, rows):
        self.rows = rows
        self.blocks = []
        self.off = {}
        self.n = 0

    def add(self, name, arr):
        arr = np.asarray(arr, dtype=np.float32)
        if arr.ndim == 1:
            arr = arr[:, None]
        if arr.shape[0] < self.rows:
            pad = np.zeros((self.rows - arr.shape[0], arr.shape[1]), np.float32)
            arr = np.concatenate([arr, pad], 0)
        self.off[name] = (self.n, arr.shape[1])
        self.blocks.append(arr)
        self.n += arr.shape[1]

    def build(self):
        return np.ascontiguousarray(np.concatenate(self.blocks, 1))


def const_masks():
    i = np.arange(128)
    same = (i[:, None] // 64) == (i[None, :] // 64)
    m = {}
    m["U_incl"] = (same & (i[:, None] <= i[None, :])).astype(np.float32)
    m["U_strict"] = (same & (i[:, None] < i[None, :])).astype(np.float32)
    m["L_incl"] = m["U_incl"].T.copy()
    m["L_strict"] = m["U_strict"].T.copy()
    m["ident"] = np.eye(128, dtype=np.float32)
    m["ones"] = np.ones((128, 128), np.float32)
    m["ones_blk"] = same.astype(np.float32)
    m["istack"] = np.concatenate([np.eye(64), np.eye(64)], 0).astype(np.float32)
    return m


def pack_p1a(inp, l, hg):
    w_in = inp["w_in"][l]
    W = Packer(1024)
    W.add("qlat", w_in[:, O_QLAT:O_QLAT + 256])
    W.add("ckv", w_in[:, O_KVLAT:O_KVLAT + 128])
    kr = w_in[:, O_KVLAT + 128:O_KVLAT + 160]
    W.add("krope", kr)
    W.add("krope_sw", np.concatenate([kr[:, 16:32], kr[:, 0:16]], 1))
    W.add("gq", w_in[:, O_GQ + 64 * hg:O_GQ + 64 * hg + 64])
    W.add("gk", w_in[:, O_GK + 64 * hg:O_GK + 64 * hg + 64])
    W.add("gv", w_in[:, O_GV + 128 * hg:O_GV + 128 * hg + 128])
    W.add("ggate", w_in[:, O_GGATE:O_GGATE + 16])
    W.add("gog", w_in[:, O_GOG + 128 * hg:O_GOG + 128 * hg + 128])
    W.add("dq", w_in[:, O_DQKV + 128 * hg:O_DQKV + 128 * hg + 128])
    W.add("dk", w_in[:, O_DQKV + 512 + 128 * hg:O_DQKV + 512 + 128 * hg + 128])
    W.add("dv", w_in[:, O_DQKV + 1024 + 128 * hg:O_DQKV + 1024 + 128 * hg + 128])
    W.add("dbeta", np.repeat(w_in[:, O_DBETA + hg:O_DBETA + hg + 1], 128, 1))
    W.add("da", np.repeat(w_in[:, O_DA + hg:O_DA + hg + 1], 128, 1))
    W.add("dz", w_in[:, O_DZ + 128 * hg:O_DZ + 128 * hg + 128])
    for i, nm in enumerate(("rr", "rk", "rv")):
        W.add(nm, w_in[:, O_RW + 512 * i + 128 * hg:O_RW + 512 * i + 128 * hg + 128])
    W.add("rwl", w_in[:, O_RW + 1536:O_RW + 1600])
    W.add("ral", w_in[:, O_RW + 1600:O_RW + 1664])
    W.add("rgl", w_in[:, O_RW + 1664:O_RW + 1792])
    if l > 0:
        W.add("rvr", inp["w_in_vres"][l - 1])
    P = Packer(128)
    ch = slice(128 * hg, 128 * hg + 128)
    mu = inp["rwkv_mu"][l]
    P.add("mu_r", mu[0:512][ch]); P.add("mu_k", mu[512:1024][ch]); P.add("mu_v", mu[1024:1536][ch])
    P.add("mu_wl", mu[1536:1600]); P.add("mu_al", mu[1600:1664]); P.add("mu_gl", mu[1664:1792])
    P.add("w0", inp["rwkv_w0"][l][ch]); P.add("a0", inp["rwkv_a0"][l][ch])
    P.add("k_k", inp["rwkv_k_k"][l][ch]); P.add("k_a", inp["rwkv_k_a"][l][ch])
    P.add("r_k", inp["rwkv_r_k"][l].reshape(-1)[ch])
    P.add("ln_g", inp["rwkv_ln_g"][l][ch]); P.add("ln_b", inp["rwkv_ln_b"][l][ch])
    if l > 0:
        P.add("mu_vr", inp["rwkv_vres_mu"][l - 1]); P.add("v0", inp["rwkv_v0"][l - 1][ch])
    P.add("gla_norm", inp["gla_norm"][l]); P.add("gdn_norm", inp["gdn_norm"][l])
    cw = inp["gdn_conv"][l]
    for nm, o in (("cq", 0), ("ck", 512), ("cv", 1024)):
        P.add("conv_" + nm, cw[:, o + 128 * hg:o + 128 * hg + 128].T)
    P.add("a_log", np.full(128, inp["gdn_a_log"][l][hg])); P.add("dt_bias", np.full(128, inp["gdn_dt_bias"][l][hg]))
    P.add("q_norm", inp["mla_q_norm"][l].reshape(2, 128).T)
    P.add("kv_norm", inp["mla_kv_norm"][l])
    invf = (10000.0 ** (-np.arange(16, dtype=np.float32) / 16)).astype(np.float32)
    P.add("invf", np.concatenate([np.zeros(64, np.float32), invf, invf]))
    P.add("rsign", np.concatenate([np.zeros(64, np.float32), -np.ones(16, np.float32), np.ones(16, np.float32)]))
    M = Packer(128)
    M.add("gup", np.concatenate([inp["gla_w_gate_up"][l][:, 64 * hg:64 * hg + 64],
                                 inp["gla_b_gate"][l][None, 64 * hg:64 * hg + 64]], 0))
    M.add("w_up", inp["rwkv_w_up"][l][:, ch]); M.add("a_up", inp["rwkv_a_up"][l][:, ch])
    M.add("g_up", inp["rwkv_g_up"][l][:, ch])
    if l > 0:
        M.add("v_up", inp["rwkv_v_up"][l - 1][:, ch])
    uq = inp["mla_w_uq"][l]
    ukv = inp["mla_w_ukv"][l]
    for hh in range(2):
        h = 2 * hg + hh
        q = uq[:, 96 * h:96 * h + 96]
        for kc in range(2):
            M.add("uq%d_%d" % (hh, kc), q[128 * kc:128 * kc + 128])
            sw = np.concatenate([np.zeros((128, 64), np.float32), q[128 * kc:128 * kc + 128, 80:96],
                                 q[128 * kc:128 * kc + 128, 64:80]], 1)
            M.add("uqs%d_%d" % (hh, kc), sw)
        M.add("ukn%d" % hh, ukv[:, 128 * h:128 * h + 64])
    M.add("ukv_v", np.concatenate([ukv[:, 128 * (2 * hg) + 64:128 * (2 * hg) + 128],
                                   ukv[:, 128 * (2 * hg + 1) + 64:128 * (2 * hg + 1) + 128]], 1))
    return W, P, M


class Ctx:
    pass


def setup_common(kb, cx, masks_d, stack):
    mk = const_masks()
    names = ["U_strict", "U_incl", "L_incl", "L_strict", "ident", "ones", "ones_blk"]
    cx.mf = kb.sbuf("maskf", [128, 7 * 128 + 64], F32, stack)
    cx.mb = kb.sbuf("maskb", [128, 7 * 128 + 64], BF16, stack)
    kb.dma("sp", cx.mf[:, :], masks_d)
    kb.copy("pool", cx.mb[:, :], cx.mf[:, :])
    cx.M = {}
    cx.Mf = {}
    for i, n in enumerate(names):
        cx.M[n] = cx.mb[:, 128 * i:128 * i + 128]
        cx.Mf[n] = cx.mf[:, 128 * i:128 * i + 128]
    cx.M["istack"] = cx.mb[:, 896:960]
    cx.Mf["istack"] = cx.mf[:, 896:960]
    cx.M["pairU"] = cx.mb[:, 0:256]
    banks = [kb.psum("pb%d" % i, [128, 512], F32, stack) for i in range(8)]
    cx.banks = banks

    class RR:
        def __init__(self, items):
            self.items = items
            self.i = 0

        def get(self):
            x = self.items[self.i]
            self.i = (self.i + 1) % len(self.items)
            return x
    cx.RR = RR
    cx.PB = RR([banks[i][:, :] for i in range(2)])
    cx.PH = RR([banks[2][:, 256 * j:256 * j + 256] for j in range(2)])
    cx.PQ = RR([banks[3 + i][:, 128 * j:128 * j + 128] for j in range(4) for i in range(3)])
    cx.PD = [banks[6 + i][:, 128 * j:128 * j + 128] for i in range(2) for j in range(4)]


def masks_host():
    mk = const_masks()
    names = ["U_strict", "U_incl", "L_incl", "L_strict", "ident", "ones", "ones_blk"]
    return np.ascontiguousarray(np.concatenate([mk[n] for n in names] + [mk["istack"]], 1).astype(np.float32))


def scanmask_host():
    m = np.ones((128, TT), np.float32)
    m[:, ::64] = 0.0
    return m


def rsqrt_act(kb, out, in_, scale, eps, tmp):
    kb.act(tmp, in_, AF.Ln, scale=scale, bias=eps)
    kb.act(out, tmp, AF.Exp, scale=-0.5)


def sigmoid_into(kb, out, in_, tmp, scale=1.0, bias=None, eng="dve"):
    kb.act(tmp, in_, AF.Exp, scale=-scale, bias=bias)
    kb.ts(eng, tmp, tmp, 1.0, ALU.add)
    kb.recip(out, tmp)


def neumann(kb, cx, PA, PB_, pool_bf, ident_b):
    TT_ = pool_bf.get()
    kb.tt("dve", TT_[:, :], PB_, ident_b, ALU.add)
    A, B = PA, PB_
    for k in range(1, 6):
        psA = cx.PQ.get()
        kb.mm(psA, B, A)
        nA = pool_bf.get()
        kb.copy("act", nA[:, :], psA)
        if k < 5:
            psB = cx.PQ.get()
            kb.mm(psB, A, B)
            nB = pool_bf.get()
            kb.copy("pool" if False else "dve", nB[:, :], psB)
        psT = cx.PQ.get()
        kb.mm(psT, nA[:, :], TT_[:, :])
        nT = pool_bf.get()
        kb.tt("dve", nT[:, :], TT_[:, :], psT, ALU.add)
        TT_ = nT
        A = nA[:, :]
        if k < 5:
            B = nB[:, :]
    return TT_[:, :]


def build_p1a(kb, S, l, d, branches=("gla", "gdn", "rwkv", "mla")):
    import os as _os
    if "build_p1a" in _os.environ.get("DBG_SKIP", "").split(","):
        return None

    st = contextlib.ExitStack()
    cx = Ctx()
    setup_common(kb, cx, d["masks"], st)
    Wo, Po, Mo = d["Woff"], d["Poff"], d["Moff"]
    NW, NP, NM = d["NW"], d["NP"], d["NM"]
    nt = S // TT
    M, Mf = cx.M, cx.Mf

    PC = kb.sbuf("PC", [128, NP], F32, st)
    kb.dma("sp", PC[:, :], d["PC"])
    SMf = kb.sbuf("SMf", [128, NM], F32, st)
    kb.dma("sp", SMf[:, :], d["SM"])
    SMb = kb.sbuf("SMb", [128, NM], BF16, st)
    kb.copy("pool", SMb[:, :], SMf[:, :])
    scm = kb.sbuf("scm", [128, TT], F32, st)
    kb.dma("sp", scm[:, :], d["scanmask"])
    Wbf = kb.sbuf("Wbf", [128, NKC, NW], BF16, st)
    wst = Pool(kb, "wst", 2, [128, 512], F32, stack=st)
    for k in range(NKC):
        for c0 in range(0, NW, 512):
            cw_ = min(512, NW - c0)
            s_ = wst.get()
            kb.dma("sp", s_[:, 0:cw_], d["W"][128 * k:128 * k + 128, c0:c0 + cw_])
            kb.copy("pool", Wbf[:, k, c0:c0 + cw_], s_[:, 0:cw_])

    def pc(name, rows=128, j=0):
        o, w = Po[name]
        return PC[0:rows, o + j:o + j + 1]

    def sm(name, rows, cols=None):
        o, w = Mo[name]
        return SMb[0:rows, o:o + (cols or w)]

    def wcol(name, k, c0=0, cn=None):
        o, w = Wo[name]
        return Wbf[:, k, o + c0:o + c0 + (cn or w)]

    cst = kb.sbuf("cst", [128, 8], F32, st)
    kb.memset("pool", cst[:, 0:1], NORM_EPS)
    kb.memset("pool", cst[:, 1:2], RWKV_LN_EPS)
    kb.memset("pool", cst[:, 2:3], 1.0)
    kb.memset("pool", cst[:, 3:4], 0.0)
    EPS_N, EPS_R, ONE_C, ZERO_C = cst[:, 0:1], cst[:, 1:2], cst[:, 2:3], cst[:, 3:4]
    ncol = kb.sbuf("ncol", [128, 8], F32, st)
    kb.ts("pool", ncol[:, 0:1], pc("w0"), -1.0, ALU.mult)
    kb.ts("pool", ncol[:, 1:2], pc("a0"), -1.0, ALU.mult)
    if l > 0:
        kb.ts("pool", ncol[:, 2:3], pc("v0"), -1.0, ALU.mult)
    kb.act(ncol[:, 3:4], pc("a_log"), AF.Exp)

    F5 = Pool(kb, "f5_", 10, [128, TT], F32, stack=st)
    L5 = Pool(kb, "l5_", 18, [128, TT], F32, stack=st)
    B5 = Pool(kb, "b5_", 10, [128, TT], BF16, stack=st)
    BQ = Pool(kb, "bq_", 28, [128, 128], BF16, stack=st)
    FQ = Pool(kb, "fq_", 16, [128, 128], F32, stack=st)
    kb_rhs = Pool(kb, "rhs_", 4, [128, 256], BF16, stack=st)
    BL = Pool(kb, "bl_", 8, [128, 128], BF16, stack=st)
    xst = Pool(kb, "xst", 3, [128, TT], F32, stack=st)
    xbfs = [kb.sbuf("xbf%d" % i, [128, NKC, TT], BF16, st) for i in range(2)]

    def proj_f(name, xb, rows=None, c0=0, out=None):
        o, w = Wo[name]
        rows = rows or w
        ps = out if out is not None else cx.PB.get()[0:rows, :]
        for k in range(NKC):
            kb.mm(ps, wcol(name, k, c0, rows), xb[:, k, :], start=(k == 0), stop=(k == NKC - 1))
        return ps

    def shift_mix(ps, rows, mu, halo, t):
        raw = halo[t % 2]
        kb.copy("act", raw[0:rows, 1:TT + 1], ps)
        if t == 0:
            kb.memset("pool", raw[0:rows, 0:1], 0.0)
        kb.copy("pool", halo[(t + 1) % 2][0:rows, 0:1], raw[0:rows, TT:TT + 1])
        dd = F5.get()
        kb.tt("dve", dd[0:rows, :], raw[0:rows, 0:TT], raw[0:rows, 1:TT + 1], ALU.subtract)
        out = F5.get()
        kb.stt(out[0:rows, :], dd[0:rows, :], mu, raw[0:rows, 1:TT + 1], ALU.mult, ALU.add)
        return out[0:rows, :]

    if "gla" in branches:
        gl_H32 = kb.sbuf("gl_H32", [64, 128], F32, st)
        gl_Hbf = kb.sbuf("gl_Hbf", [64, 128], BF16, st)
        kb.memset("pool", gl_H32[:, :], 0.0)
        kb.memset("pool", gl_Hbf[:, :], 0.0)
        gl_ext = kb.sbuf("gl_ext", [32, TT], BF16, st)
        kb.memset("pool", gl_ext[:, :], 1.0)
    if "gdn" in branches:
        gd_H32 = kb.sbuf("gd_H32", [128, 128], F32, st)
        gd_Hbf = kb.sbuf("gd_Hbf", [128, 128], BF16, st)
        kb.memset("pool", gd_H32[:, :], 0.0)
        kb.memset("pool", gd_Hbf[:, :], 0.0)
        gd_halo = {n: [kb.sbuf("gdh_%s" % n, [128, TT + 3], F32, st), kb.sbuf("gdc_%s" % n, [128, 4], F32, st)] for n in ("q", "k", "v")}
    if "rwkv" in branches:
        rw_H32 = kb.sbuf("rw_H32", [128, 64], F32, st)
        rw_Hbf = kb.sbuf("rw_Hbf", [128, 64], BF16, st)
        kb.memset("pool", rw_H32[:, :], 0.0)
        kb.memset("pool", rw_Hbf[:, :], 0.0)
        rw_halo = {n: [kb.sbuf("rwh_%s" % n, [128, TT + 1], F32, st), kb.sbuf("rwc_%s" % n, [128, 2], F32, st)]
                   for n in ("r", "k", "v", "wl", "al", "gl", "vr")}
        rw_AR = kb.sbuf("rw_AR", [128, 2 * TT], BF16, st)
        rw_MT = kb.sbuf("rw_MT", [128, 128], F32, st)
        kb.memset("pool", rw_MT[:, :], 0.0)
    if "mla" in branches:
        mla_posi = kb.sbuf("mla_posi", [96, TT], I32, st)
        mla_vt = [kb.sbuf("mla_vt%d" % i, [128, 2, 65], BF16, st) for i in range(2)]
        for i in range(2):
            kb.memset("pool", mla_vt[i][:, :, :], 1.0)
        mla_ni = kb.sbuf("mla_ni", [96, TT], I32, st)
        uqg = kb.sbuf("uqg", [128, 2, 2, 2, 96], BF16, st)
        for hh in range(2):
            for kc in range(2):
                for sw, nm in enumerate(("uq", "uqs")):
                    o, w = Mo["%s%d_%d" % (nm, hh, kc)]
                    kb.ts("pool", uqg[:, hh, kc, sw, :], SMf[:, o:o + 96], pc("q_norm", 128, kc), ALU.mult,
                          MLA_SCALE, ALU.mult)
        ukg = kb.sbuf("ukg", [128, 2, 64], BF16, st)
        for hh in range(2):
            o, w = Mo["ukn%d" % hh]
            kb.ts("pool", ukg[:, hh, :], SMf[:, o:o + 64], pc("kv_norm"), ALU.mult)
        uvg = kb.sbuf("uvg", [128, 128], BF16, st)
        o, w = Mo["ukv_v"]
        kb.ts("pool", uvg[:, :], SMf[:, o:o + 128], pc("kv_norm"), ALU.mult)

    def silu_of(src):
        sg = L5.get()
        tmp2 = F5.get()
        sigmoid_into(kb, sg[:, :], src, tmp2[:, :])
        kb.tt("dve", sg[:, :], sg[:, :], src, ALU.mult)
        return sg

    def post_norm_gate(oT, gate_sb, gcol, row0):
        osq = B5.get()
        kb.act(osq[:, :], oT[:, :], AF.Square)
        ps_s = cx.PB.get()
        kb.mm(ps_s, M["ones"], osq[:, :])
        tmp = F5.get()
        rstd = F5.get()
        rsqrt_act(kb, rstd[:, :], ps_s, 1.0 / 128, EPS_N, tmp[:, :])
        sg = silu_of(gate_sb)
        kb.stt(oT[:, :], oT[:, :], gcol, rstd[:, :], ALU.mult, ALU.mult)
        yb = B5.get()
        kb.tt("dve", yb[:, :], oT[:, :], sg[:, :], ALU.mult)
        kb.dma("sp", d["ydst"](row0, 128, tsl_box[0].start // TT), yb[:, :])

    def l2norm_f(x, ones_m, scale):
        sq = B5.get()
        kb.act(sq[:, :], x, AF.Square)
        ps_s = cx.PB.get()
        kb.mm(ps_s, ones_m, sq[:, :])
        tmp = F5.get()
        rs = F5.get()
        rsqrt_act(kb, rs[:, :], ps_s, 1.0, EPS_N, tmp[:, :])
        out = L5.get()
        kb.stt(out[:, :], x, scale, rs[:, :], ALU.mult, ALU.mult)
        return out

    def transpose_bf(src, dst_pool=None):
        ps_t = cx.PQ.get()
        kb.mm(ps_t, src, M["ident"])
        o = BL.get()
        kb.copy("act", o[:, :], ps_t)
        return o

    tsl_box = [None]
    youts = {}

    for t in range(nt):
        tsl = slice(t * TT, (t + 1) * TT)
        tsl_box[0] = tsl
        xb = xbfs[t % 2]
        for k in range(NKC):
            s_ = xst.get()
            kb.dma("sp", s_[:, :], d["xsrc"](k, t))
            kb.copy("pool", xb[:, k, :], s_[:, :])

        if "gla" in branches:
            L5.i = 0
            ps_q = proj_f("gq", xb)
            qraw = L5.get()
            kb.copy("act", qraw[0:64, :], ps_q)
            ps_k = proj_f("gk", xb)
            kraw = L5.get()
            kb.copy("act", kraw[0:64, :], ps_k)
            ps_g = proj_f("ggate", xb)
            kb.copy("act", gl_ext[0:16, :], ps_g)
            ps_og = proj_f("gog", xb)
            og = L5.get()
            kb.copy("act", og[:, :], ps_og)
            ps_z = cx.PB.get()[0:64, :]
            kb.mm(ps_z, sm("gup", 17), gl_ext[0:17, :])
            e1 = F5.get()
            kb.act(e1[0:64, :], ps_z, AF.Exp, scale=-1.0)
            sp = F5.get()
            kb.act(sp[0:64, :], e1[0:64, :], AF.Ln, bias=ONE_C[0:64, :])
            bsp = L5.get()
            kb.scan(bsp[0:64, :], scm[0:64, :], sp[0:64, :], 0.0, ALU.mult, ALU.add)
            epos = L5.get()
            kb.act(epos[0:64, :], bsp[0:64, :], AF.Exp, scale=-1.0 / 16)
            eneg = F5.get()
            kb.act(eneg[0:64, :], bsp[0:64, :], AF.Exp, scale=1.0 / 16)
            b3 = bsp.ap[0:64, :].rearrange("p (c t) -> p c t", t=64)
            dl = F5.get()
            dl3 = dl.ap[0:64, :].rearrange("p (c t) -> p c t", t=64)
            bl_bc = b3[:, :, 63:64].broadcast_to([64, TT // 64, 64])
            kb.tt("dve", V(dl3, dl.deps), V(b3, bsp.deps), V(bl_bc, bsp.deps), ALU.subtract)
            eend = F5.get()
            kb.act(eend[0:64, :], dl[0:64, :], AF.Exp, scale=1.0 / 16)
            qd = B5.get()
            kb.stt(qd[0:64, :], qraw[0:64, :], 0.125, epos[0:64, :], ALU.mult, ALU.mult)
            ki = B5.get()
            kb.tt("dve", ki[0:64, :], kraw[0:64, :], eneg[0:64, :], ALU.mult)
            ke = B5.get()
            kb.tt("dve", ke[0:64, :], kraw[0:64, :], eend[0:64, :], ALU.mult)
            oT = L5.get()
            for blk in range(4):
                bs = slice(blk * 128, blk * 128 + 128)
                ps_v = cx.PQ.get()
                for k in range(NKC):
                    kb.mm(ps_v, xb[:, k, bs], wcol("gv", k), start=(k == 0), stop=(k == NKC - 1))
                vt = BQ.get()
                kb.copy("act", vt[:, :], ps_v)
                ps_t = cx.PQ.get()[:, 0:64]
                kb.mm(ps_t, ke[0:64, bs], M["ident"][0:64, 0:64])
                ket = BQ.get()
                kb.copy("dve", ket[:, 0:64], ps_t)
                ps_a = cx.PQ.get()
                kb.mm(ps_a, ki[0:64, bs], qd[0:64, bs])
                am = BQ.get()
                kb.tt("dve", am[:, :], ps_a, Mf["U_incl"], ALU.mult)
                ps_o = cx.PD[0]
                kb.mm(ps_o, vt[:, :], am[:, :], start=True, stop=False)
                for c in range(2):
                    cs = slice(blk * 128 + c * 64, blk * 128 + c * 64 + 64)
                    rs = slice(c * 64, c * 64 + 64)
                    kb.mm(ps_o[:, rs], gl_Hbf[:, :], qd[0:64, cs], start=False, stop=(c == 1))
                    ps_n = cx.PD[4][0:64, :]
                    kb.mm(ps_n, ket[rs, 0:64], vt[rs, :])
                    gc = blk * 2 + c
                    acol = epos[0:64, 64 * (t * 0 + gc) + 63:64 * gc + 64]
                    kb.stt(gl_H32[:, :], gl_H32[:, :], acol, ps_n, ALU.mult, ALU.add)
                    kb.copy("act", gl_Hbf[:, :], gl_H32[:, :])
                kb.copy("act", oT[:, bs], ps_o)
            post_norm_gate(oT, og[:, :], pc("gla_norm"), 128)

        if "gdn" in branches:
            L5.i = 0
            conv = {}
            for nm, cn in (("q", "conv_cq"), ("k", "conv_ck"), ("v", "conv_cv")):
                ps = proj_f("d" + nm, xb)
                raw = gd_halo[nm][0]
                hcol = gd_halo[nm][1]
                if t == 0:
                    kb.memset("pool", raw[:, 0:3], 0.0)
                else:
                    kb.copy("pool", raw[:, 0:3], hcol[:, 0:3])
                kb.copy("act", raw[:, 3:TT + 3], ps)
                kb.copy("pool", hcol[:, 0:3], raw[:, TT:TT + 3])
                acc = F5.get()
                kb.ts("dve", acc[:, :], raw[:, 0:TT], pc(cn, 128, 0), ALU.mult)
                for j in range(1, 4):
                    kb.stt(acc[:, :], raw[:, j:j + TT], pc(cn, 128, j), acc[:, :], ALU.mult, ALU.add)
                conv[nm] = silu_of(acc[:, :])
            qn = l2norm_f(conv["q"][:, :], M["ones"], 128 ** -0.5)
            kn32 = l2norm_f(conv["k"][:, :], M["ones"], 1.0)
            kn = B5.get()
            kb.copy("pool", kn[:, :], kn32[:, :])
            qnb = B5.get()
            kb.copy("pool", qnb[:, :], qn[:, :])
            ps_b = proj_f("dbeta", xb)
            beta = L5.get()
            tb = F5.get()
            sigmoid_into(kb, beta[:, :], ps_b, tb[:, :])
            nbeta = L5.get()
            kb.ts("pool", nbeta[:, :], beta[:, :], -1.0, ALU.mult)
            ps_a = proj_f("da", xb)
            e1 = F5.get()
            kb.act(e1[:, :], ps_a, AF.Exp, bias=pc("dt_bias"))
            spl = F5.get()
            kb.act(spl[:, :], e1[:, :], AF.Ln, bias=ONE_C)
            g = F5.get()
            kb.ts("dve", g[:, :], spl[:, :], ncol[:, 3:4], ALU.mult, -1.0, ALU.mult)
            gc = L5.get()
            kb.scan(gc[:, :], scm[:, :], g[:, :], 0.0, ALU.mult, ALU.add)
            egc = L5.get()
            kb.act(egc[:, :], gc[:, :], AF.Exp)
            g3 = gc.ap[:, :].rearrange("p (c t) -> p c t", t=64)
            dl = F5.get()
            dl3 = dl.ap[:, :].rearrange("p (c t) -> p c t", t=64)
            gl_bc = g3[:, :, 63:64].broadcast_to([128, TT // 64, 64])
            kb.tt("dve", V(dl3, dl.deps), V(gl_bc, gc.deps), V(g3, gc.deps), ALU.subtract)
            eend = F5.get()
            kb.act(eend[:, :], dl[:, :], AF.Exp)
            qdec = L5.get()
            kb.tt("dve", qdec[:, :], qn[:, :], egc[:, :], ALU.mult)
            kbg = B5.get()
            tk = F5.get()
            kb.tt("dve", tk[:, :], kn32[:, :], beta[:, :], ALU.mult)
            kb.tt("dve", kbg[:, :], tk[:, :], egc[:, :], ALU.mult)
            vb = B5.get()
            kb.tt("dve", vb[:, :], conv["v"][:, :], beta[:, :], ALU.mult)
            kend = B5.get()
            kb.tt("dve", kend[:, :], kn32[:, :], eend[:, :], ALU.mult)
            ps_z = proj_f("dz", xb)
            zs = L5.get()
            kb.copy("act", zs[:, :], ps_z)
            oT = L5.get()
            for blk in range(4):
                bs = slice(blk * 128, blk * 128 + 128)
                BL.i = 0
                Rm = gc[:, bs]
                t1 = FQ.get()
                kb.tt("dve", t1[:, :], Rm, Mf["ident"], ALU.mult)
                cc = FQ.get()
                kb.op("dve", (lambda o_, i_: (lambda h: h.tensor_reduce(o_, i_, AX.X, ALU.add)))(cc.ap[:, 0:1], t1.ap[:, :]),
                      reads=[t1], writes=[cc])
                kb.tt("dve", t1[:, :], nbeta[:, bs], Mf["ident"], ALU.mult)
                kb.op("dve", (lambda o_, i_: (lambda h: h.tensor_reduce(o_, i_, AX.X, ALU.add)))(cc.ap[:, 1:2], t1.ap[:, :]),
                      reads=[t1], writes=[cc])
                ccol, nbcol = cc[:, 0:1], cc[:, 1:2]
                tB = FQ.get()
                kb.ts("dve", tB[:, :], Rm, ccol, ALU.subtract, 0.0, ALU.min)
                eB = FQ.get()
                kb.act(eB[:, :], tB[:, :], AF.Exp)
                DBi = FQ.get()
                kb.tt("dve", DBi[:, :], eB[:, :], Mf["U_incl"], ALU.mult)
                DBs = FQ.get()
                kb.tt("pool", DBs[:, :], eB[:, :], Mf["U_strict"], ALU.mult)
                kb.tt("pool", DBs[:, :], DBs[:, :], nbeta[:, bs], ALU.mult)
                tA = FQ.get()
                kb.ts("dve", tA[:, :], Rm, ccol, ALU.subtract, 0.0, ALU.max)
                eA = FQ.get()
                kb.act(eA[:, :], tA[:, :], AF.Exp, scale=-1.0)
                kb.tt("pool", eA[:, :], eA[:, :], Mf["L_strict"], ALU.mult)
                ps_kk = cx.PQ.get()
                kb.mm(ps_kk, kn[:, bs], kn[:, bs])
                PA = BQ.get()
                kb.stt(PA[:, :], eA[:, :], nbcol, ps_kk, ALU.mult, ALU.mult)
                PB_ = BQ.get()
                kb.tt("dve", PB_[:, :], ps_kk, DBs[:, :], ALU.mult)
                TTm = neumann(kb, cx, PA[:, :], PB_[:, :], BQ, M["ident"])
                rhs = kb_rhs.get()
                ps_t = cx.PQ.get()
                kb.mm(ps_t, vb[:, bs], M["ident"])
                kb.copy("act", rhs[:, 0:128], ps_t)
                ps_t = cx.PQ.get()
                kb.mm(ps_t, kbg[:, bs], M["ident"])
                kb.copy("act", rhs[:, 128:256], ps_t)
                kendt = transpose_bf(kend[:, bs])
                ps_uw = cx.PH.get()
                kb.mm(ps_uw, TTm, rhs[:, :])
                UW = kb_rhs.get()
                kb.copy("act", UW[:, :], ps_uw)
                ps_qk = cx.PQ.get()
                kb.mm(ps_qk, kn[:, bs], qnb[:, bs])
                attB = BL.get()
                kb.tt("dve", attB[:, :], ps_qk, DBi[:, :], ALU.mult)
                ps_aw = cx.PQ.get()
                kb.mm(ps_aw, UW[:, 128:256], attB[:, :])
                qeff = BL.get()
                kb.tt("dve", qeff[:, :], qdec[:, bs], ps_aw, ALU.subtract)
                ps_o = cx.PD[1]
                kb.mm(ps_o, UW[:, 0:128], attB[:, :], start=True, stop=False)
                for c in range(2):
                    rs = slice(c * 64, c * 64 + 64)
                    gcn = blk * 2 + c
                    kb.mm(ps_o[:, rs], gd_Hbf[:, :], qeff[:, rs], start=False, stop=(c == 1))
                    ps_m = cx.PD[5]
                    kb.mm(ps_m, UW[rs, 128:256], kendt[rs, :])
                    MT = FQ.get()
                    kb.stt(MT[:, :], Mf["ident"], egc[:, 64 * gcn + 63:64 * gcn + 64], ps_m, ALU.mult, ALU.subtract)
                    ps_h = cx.PD[6]
                    kb.mm(ps_h, MT[:, :], gd_H32[:, :], start=True, stop=False)
                    kb.mm(ps_h, kendt[rs, :], UW[rs, 0:128], start=False, stop=True)
                    kb.copy("act", gd_Hbf[:, :], ps_h)
                    kb.copy("act", gd_H32[:, :], ps_h)
                kb.copy("act", oT[:, bs], ps_o)
            post_norm_gate(oT, zs[:, :], pc("gdn_norm"), 256)

        if "rwkv" in branches:
            L5.i = 0

            def shiftmix(pname, rows, mu_name, hname):
                ps = proj_f(pname, xb)
                raw, hcol = rw_halo[hname]
                if t == 0:
                    kb.memset("pool", raw[0:rows, 0:1], 0.0)
                else:
                    kb.copy("pool", raw[0:rows, 0:1], hcol[0:rows, 0:1])
                kb.copy("act", raw[0:rows, 1:TT + 1], ps)
                kb.copy("pool", hcol[0:rows, 0:1], raw[0:rows, TT:TT + 1])
                dd = F5.get()
                kb.tt("dve", dd[0:rows, :], raw[0:rows, 0:TT], raw[0:rows, 1:TT + 1], ALU.subtract)
                out = L5.get()
                kb.stt(out[0:rows, :], dd[0:rows, :], pc(mu_name, rows), raw[0:rows, 1:TT + 1], ALU.mult, ALU.add)
                return out
            r_ = shiftmix("rr", 128, "mu_r", "r")
            k_ = shiftmix("rk", 128, "mu_k", "k")
            v_ = shiftmix("rv", 128, "mu_v", "v")
            wl = shiftmix("rwl", 64, "mu_wl", "wl")
            al = shiftmix("ral", 64, "mu_al", "al")
            gl = shiftmix("rgl", 128, "mu_gl", "gl")
            tmp = F5.get()
            sgt = F5.get()
            sigmoid_into(kb, sgt[0:64, :], wl[0:64, :], tmp[0:64, :], scale=2.0)
            th = B5.get()
            kb.ts("dve", th[0:64, :], sgt[0:64, :], 2.0, ALU.mult, -1.0, ALU.add)
            ps_z = cx.PB.get()
            kb.mm(ps_z, sm("w_up", 64), th[0:64, :])
            sgw = L5.get()
            tmp = F5.get()
            sigmoid_into(kb, sgw[:, :], ps_z, tmp[:, :], bias=ncol[:, 0:1])
            cs = L5.get()
            kb.scan(cs[:, :], scm[:, :], sgw[:, :], 0.0, ALU.mult, ALU.add)
            albf = B5.get()
            kb.copy("pool", albf[0:64, :], al[0:64, :])
            ps_a = cx.PB.get()
            kb.mm(ps_a, sm("a_up", 64), albf[0:64, :])
            a_ = L5.get()
            tmp = F5.get()
            sigmoid_into(kb, a_[:, :], ps_a, tmp[:, :], bias=ncol[:, 1:2])
            sgg = F5.get()
            tmp = F5.get()
            sigmoid_into(kb, sgg[:, :], gl[:, :], tmp[:, :])
            sggb = B5.get()
            kb.copy("pool", sggb[:, :], sgg[:, :])
            ps_g = cx.PB.get()
            kb.mm(ps_g, sm("g_up", 128), sggb[:, :])
            gte = L5.get()
            kb.copy("act", gte[:, :], ps_g)
            if l > 0:
                ps = proj_f("rvr", xb)
                raw, hcol = rw_halo["vr"]
                if t == 0:
                    kb.memset("pool", raw[0:32, 0:1], 0.0)
                else:
                    kb.copy("pool", raw[0:32, 0:1], hcol[0:32, 0:1])
                kb.copy("act", raw[0:32, 1:TT + 1], ps)
                kb.copy("pool", hcol[0:32, 0:1], raw[0:32, TT:TT + 1])
                dd = F5.get()
                kb.tt("dve", dd[0:32, :], raw[0:32, 0:TT], raw[0:32, 1:TT + 1], ALU.subtract)
                vrl = B5.get()
                kb.stt(vrl[0:32, :], dd[0:32, :], pc("mu_vr", 32), raw[0:32, 1:TT + 1], ALU.mult, ALU.add)
                ps_v = cx.PB.get()
                kb.mm(ps_v, sm("v_up", 32), vrl[0:32, :])
                vg = F5.get()
                tmp = F5.get()
                sigmoid_into(kb, vg[:, :], ps_v, tmp[:, :], bias=ncol[:, 2:3])
                vf = F5.get()
                kb.dma("sp", vf[:, :], d["vfirst"][:, tsl])
                kb.tt("dve", vf[:, :], vf[:, :], v_[:, :], ALU.subtract)
                kb.tt("dve", vf[:, :], vf[:, :], vg[:, :], ALU.mult)
                kb.tt("dve", v_[:, :], v_[:, :], vf[:, :], ALU.add)
            else:
                kb.dma("sp", d["vfirst"][:, tsl], v_[:, :])
            kx = F5.get()
            kb.ts("dve", kx[:, :], k_[:, :], pc("k_k"), ALU.mult)
            kk = l2norm_f(kx[:, :], M["ones_blk"], 1.0)
            tk = F5.get()
            kb.ts("dve", tk[:, :], a_[:, :], -1.0, ALU.add, pc("k_a"), ALU.mult)
            kmod = L5.get()
            kb.stt(kmod[:, :], tk[:, :], 1.0, k_[:, :], ALU.add, ALU.mult)
            bb = L5.get()
            kb.tt("dve", bb[:, :], kk[:, :], a_[:, :], ALU.mult)
            rk = B5.get()
            kb.stt(rk[:, :], r_[:, :], pc("r_k"), kmod[:, :], ALU.mult, ALU.mult)
            ps_bn = cx.PB.get()
            kb.mm(ps_bn, M["ones_blk"], rk[:, :])
            bonus = L5.get()
            kb.tt("dve", bonus[:, :], ps_bn, v_[:, :], ALU.mult)
            ecw = F5.get()
            kb.act(ecw[:, :], cs[:, :], AF.Exp, scale=-C_W)
            encw = L5.get()
            kb.act(encw[:, :], cs[:, :], AF.Exp, scale=C_W)
            dx = F5.get()
            kb.tt("dve", dx[:, :], cs[:, :], sgw[:, :], ALU.subtract)
            eprev = F5.get()
            kb.act(eprev[:, :], dx[:, :], AF.Exp, scale=-C_W)
            c3 = cs.ap[:, :].rearrange("p (c t) -> p c t", t=64)
            dl = F5.get()
            dl3 = dl.ap[:, :].rearrange("p (c t) -> p c t", t=64)
            cl_bc = c3[:, :, 63:64].broadcast_to([128, TT // 64, 64])
            kb.tt("dve", V(dl3, dl.deps), V(cl_bc, cs.deps), V(c3, cs.deps), ALU.subtract)
            eend = F5.get()
            kb.act(eend[:, :], dl[:, :], AF.Exp, scale=-C_W)
            gam = L5.get()
            kb.copy("pool", gam[:, :], ecw[:, :])
            AR = rw_AR
            AR4 = AR.ap[:, :].rearrange("p (b two i) -> p b two i", two=2, i=128)
            e4 = eprev.ap[:, :].rearrange("p (b i) -> p b i", i=128)
            k4 = kk.ap[:, :].rearrange("p (b i) -> p b i", i=128)
            kb.stt(V(AR4[:, :, 0, :], AR.deps), V(k4, kk.deps), -1.0, V(e4, eprev.deps), ALU.mult, ALU.mult)
            r4 = r_.ap[:, :].rearrange("p (b i) -> p b i", i=128)
            g4 = ecw.ap[:, :].rearrange("p (b i) -> p b i", i=128)
            kb.tt("dve", V(AR4[:, :, 1, :], AR.deps), V(r4, r_.deps), V(g4, ecw.deps), ALU.mult)
            bt = B5.get()
            kb.tt("dve", bt[:, :], bb[:, :], encw[:, :], ALU.mult)
            kt = B5.get()
            kb.tt("dve", kt[:, :], kmod[:, :], encw[:, :], ALU.mult)
            bendT = B5.get()
            kb.tt("dve", bendT[:, :], bb[:, :], eend[:, :], ALU.mult)
            kendT = B5.get()
            kb.tt("dve", kendT[:, :], kmod[:, :], eend[:, :], ALU.mult)
            vbf = B5.get()
            kb.copy("pool", vbf[:, :], v_[:, :])
            yT_ = L5.get()
            for blk in range(4):
                bs = slice(blk * 128, blk * 128 + 128)
                ARb = AR[:, blk * 256:blk * 256 + 256]
                BL.i = 0
                At_tok = transpose_bf(ARb[:, 0:128])
                V_tok = transpose_bf(vbf[:, bs])
                Bend = transpose_bf(bendT[:, bs])
                Kend = transpose_bf(kendT[:, bs])
                psQ = cx.PD[2]
                psY = cx.PD[3]
                WUs = []
                for h in range(2):
                    hp = slice(64 * h, 64 * h + 64)
                    ps1 = cx.PH.get()
                    kb.mm(ps1, bt[hp, bs], ARb[hp, :])
                    SB1 = kb_rhs.get()
                    kb.tt("dve", SB1[:, :], ps1, cx.mf[:, 0:256], ALU.mult)
                    ps2 = cx.PH.get()
                    kb.mm(ps2, kt[hp, bs], ARb[hp, :])
                    SB2 = kb_rhs.get()
                    kb.tt("dve", SB2[:, :], ps2, cx.mf[:, 0:256], ALU.mult)
                    psA = cx.PQ.get()
                    kb.mm(psA, ARb[hp, 0:128], bt[hp, bs])
                    PA = BQ.get()
                    kb.tt("dve", PA[:, :], psA, Mf["L_strict"], ALU.mult)
                    TTm = neumann(kb, cx, PA[:, :], SB1[:, 0:128], BQ, M["ident"])
                    psX = cx.PQ.get()
                    kb.mm(psX[:, 0:64], SB2[:, 0:128], V_tok[:, hp])
                    Xb = BQ.get()
                    kb.copy("act", Xb[:, 0:64], psX[:, 0:64])
                    psWU = cx.PQ.get()
                    kb.mm(psWU[:, 0:64], TTm, At_tok[:, hp])
                    kb.mm(psWU[:, 64:128], TTm, Xb[:, 0:64])
                    WU = BL.get()
                    kb.copy("act", WU[:, :], psWU)
                    WUs.append(WU)
                    kb.mm(psQ[hp, :], WU[:, 0:64], SB1[:, 128:256])
                    kb.mm(psY[hp, :], WU[:, 64:128], SB1[:, 128:256], start=True, stop=False)
                    kb.mm(psY[hp, :], V_tok[:, hp], SB2[:, 128:256], start=False, stop=False)
                qeff = BL.get()
                kb.tt("dve", qeff[:, :], ARb[:, 128:256], psQ, ALU.add)
                for c in range(2):
                    rs = slice(c * 64, c * 64 + 64)
                    gcn = blk * 2 + c
                    for h in range(2):
                        hp = slice(64 * h, 64 * h + 64)
                        kb.mm(psY[hp, rs], rw_Hbf[hp, :], qeff[hp, rs], start=False, stop=(c == 1))
                    psM = cx.PD[5]
                    for h in range(2):
                        hp = slice(64 * h, 64 * h + 64)
                        kb.mm(psM[hp, 0:64], WUs[h][rs, 0:64], Bend[rs, hp])
                    MT = rw_MT
                    for h in range(2):
                        hp = slice(64 * h, 64 * h + 64)
                        kb.stt(MT[hp, 64 * h:64 * h + 64], Mf["istack"][hp, :], gam[hp, 64 * gcn + 63:64 * gcn + 64],
                               psM[hp, 0:64], ALU.mult, ALU.add)
                    psH = cx.PD[6]
                    kb.mm(psH[:, 0:64], MT[:, :], rw_H32[:, :], start=True, stop=False)
                    for h in range(2):
                        hp = slice(64 * h, 64 * h + 64)
                        kb.mm(psH[hp, 0:64], Bend[rs, hp], WUs[h][rs, 64:128], start=False, stop=False)
                        kb.mm(psH[hp, 0:64], Kend[rs, hp], V_tok[rs, hp], start=False, stop=True)
                    kb.copy("act", rw_Hbf[:, :], psH[:, 0:64])
                    kb.copy("act", rw_H32[:, :], psH[:, 0:64])
                kb.copy("act", yT_[:, bs], psY)
            ps_m = cx.PB.get()
            kb.mm(ps_m, Mf["ones_blk"], yT_[:, :])
            yc = F5.get()
            kb.stt(yc[:, :], ps_m, -1.0 / 64, yT_[:, :], ALU.mult, ALU.add)
            ysq = F5.get()
            kb.act(ysq[:, :], yc[:, :], AF.Square)
            ps_v2 = cx.PB.get()
            kb.mm(ps_v2, Mf["ones_blk"], ysq[:, :])
            tmp = F5.get()
            rstd = F5.get()
            rsqrt_act(kb, rstd[:, :], ps_v2, 1.0 / 64, EPS_R, tmp[:, :])
            kb.stt(yc[:, :], yc[:, :], pc("ln_g"), rstd[:, :], ALU.mult, ALU.mult)
            kb.stt(yc[:, :], yc[:, :], pc("ln_b"), bonus[:, :], ALU.add, ALU.add)
            yo = B5.get()
            kb.tt("dve", yo[:, :], yc[:, :], gte[:, :], ALU.mult)
            kb.dma("sp", d["ydst"](384, 128, t), yo[:, :])

        if "mla" in branches:
            L5.i = 0
            posi = mla_posi
            kb.dma("sp", posi[:, :], d["pos"][:, tsl])
            posf = L5.get()
            kb.copy("dve", posf[0:96, :], posi[:, :])
            ang = L5.get()
            kb.ts("dve", ang[0:96, :], posf[0:96, :], pc("invf", 96), ALU.mult)
            tabs = []
            for phase in (math.pi / 2, 0.0):
                y = F5.get()
                kb.ts("dve", y[0:96, :], ang[0:96, :], phase, ALU.add, 1.0 / (2 * math.pi), ALU.mult)
                ni = mla_ni
                kb.copy("dve", ni[:, :], y[0:96, :])
                nf = F5.get()
                kb.copy("dve", nf[0:96, :], ni[:, :])
                r = F5.get()
                kb.stt(r[0:96, :], nf[0:96, :], -2 * math.pi, ang[0:96, :], ALU.mult, ALU.add)
                if phase != 0.0:
                    kb.ts("dve", r[0:96, :], r[0:96, :], phase, ALU.add)
                m1 = F5.get()
                kb.ts("dve", m1[0:96, :], r[0:96, :], math.pi, ALU.is_gt, -2 * math.pi, ALU.mult)
                kb.tt("dve", r[0:96, :], r[0:96, :], m1[0:96, :], ALU.add)
                kb.ts("dve", m1[0:96, :], r[0:96, :], -math.pi, ALU.is_lt, 2 * math.pi, ALU.mult)
                kb.tt("dve", r[0:96, :], r[0:96, :], m1[0:96, :], ALU.add)
                tab = L5.get()
                kb.act(tab[0:96, :], r[0:96, :], AF.Sin)
                tabs.append(tab)
            C96, S96 = tabs
            kb.ts("dve", S96[0:96, :], S96[0:96, :], pc("rsign", 96), ALU.mult)
            qn = []
            qr = []
            sqs = []
            for kc in range(2):
                ps = proj_f("qlat", xb, rows=128, c0=128 * kc)
                rr = L5.get()
                kb.copy("act", rr[:, :], ps)
                sq = B5.get()
                kb.act(sq[:, :], rr[:, :], AF.Square)
                qr.append(rr)
                sqs.append(sq)
            ps_s = cx.PB.get()
            kb.mm(ps_s, M["ones"], sqs[0][:, :], start=True, stop=False)
            kb.mm(ps_s, M["ones"], sqs[1][:, :], start=False, stop=True)
            tmp = F5.get()
            rstd = F5.get()
            rsqrt_act(kb, rstd[:, :], ps_s, 1.0 / 256, EPS_N, tmp[:, :])
            for kc in range(2):
                q_ = B5.get()
                kb.tt("dve", q_[:, :], qr[kc][:, :], rstd[:, :], ALU.mult)
                qn.append(q_)
            ps = proj_f("ckv", xb)
            cr = L5.get()
            kb.copy("act", cr[:, :], ps)
            sq = B5.get()
            kb.act(sq[:, :], cr[:, :], AF.Square)
            ps_s = cx.PB.get()
            kb.mm(ps_s, M["ones"], sq[:, :])
            tmp = F5.get()
            rstd = F5.get()
            rsqrt_act(kb, rstd[:, :], ps_s, 1.0 / 128, EPS_N, tmp[:, :])
            cn = B5.get()
            kb.tt("dve", cn[:, :], cr[:, :], rstd[:, :], ALU.mult)
            psK = cx.PB.get()
            proj_f("krope", xb, rows=32, out=psK[64:96, :])
            t1 = F5.get()
            kb.tt("dve", t1[64:96, :], psK[64:96, :], C96[64:96, :], ALU.mult)
            psK2 = cx.PB.get()
            proj_f("krope_sw", xb, rows=32, out=psK2[64:96, :])
            t2 = F5.get()
            kb.tt("dve", t2[64:96, :], psK2[64:96, :], S96[64:96, :], ALU.mult)
            krot = B5.get()
            kb.tt("dve", krot[64:96, :], t1[64:96, :], t2[64:96, :], ALU.add)
            for hh in range(2):
                kb.dma("sp", d["KT"][hh, 64:96, tsl], krot[64:96, :])
                psn = cx.PB.get()
                kb.mm(psn[0:64, :], ukg[:, hh, :], cn[:, :])
                kn_ = B5.get()
                kb.copy("act", kn_[0:64, :], psn[0:64, :])
                kb.dma("sp", d["KT"][hh, 0:64, tsl], kn_[0:64, :])
                psP = cx.PB.get()
                kb.mm(psP[0:96, :], uqg[:, hh, 0, 0, :], qn[0][:, :], start=True, stop=False)
                kb.mm(psP[0:96, :], uqg[:, hh, 1, 0, :], qn[1][:, :], start=False, stop=True)
                t1 = F5.get()
                kb.tt("dve", t1[0:96, :], psP[0:96, :], C96[0:96, :], ALU.mult)
                psS = cx.PB.get()
                kb.mm(psS[0:96, :], uqg[:, hh, 0, 1, :], qn[0][:, :], start=True, stop=False)
                kb.mm(psS[0:96, :], uqg[:, hh, 1, 1, :], qn[1][:, :], start=False, stop=True)
                t2 = F5.get()
                kb.tt("dve", t2[0:96, :], psS[0:96, :], S96[0:96, :], ALU.mult)
                qb_ = B5.get()
                kb.tt("dve", qb_[0:96, :], t1[0:96, :], t2[0:96, :], ALU.add)
                kb.dma("sp", d["QT"][hh, :, tsl], qb_[0:96, :])
            for blk in range(4):
                bs = slice(blk * 128, blk * 128 + 128)
                psV = cx.PQ.get()
                kb.mm(psV, cn[:, bs], uvg[:, :])
                vt = mla_vt[blk % 2]
                kb.op("act", (lambda o_, i_: (lambda h: h.activation(o_, i_, AF.Copy)))(
                    vt.ap[:, :, 0:64], psV.ap.rearrange("p (h e) -> p h e", e=64)), reads=[psV], writes=[vt])
                kb.dma("sp", d["VT"][:, :, t * 4 + blk, :], vt[:, :, :])
    kb.flush()
    st.close()
    return cx


def build_p1b(kb, S, d):
    import os as _os
    if "build_p1b" in _os.environ.get("DBG_SKIP", "").split(","):
        return None

    st = contextlib.ExitStack()
    nt = S // TT
    nb = S // 128
    banks = [kb.psum("ab%d" % i, [128, 512], F32, st) for i in range(8)]
    KTs = kb.sbuf("a_KT", [96, S], BF16, st)
    Vx = kb.sbuf("a_Vx", [128, nb, 65], BF16, st)
    onesf = kb.sbuf("a_ones", [128, 64], F32, st)
    kb.memset("pool", onesf[:, :], 1.0)
    dms = []
    for dd in range(4):
        m = kb.sbuf("a_dm%d" % dd, [128, TT], BF16, st)
        kb.memset("pool", m[:, :], 1.0)
        ma = m.ap[:, :]
        kb.op("pool", (lambda a, b: (lambda h: h.affine_select(a, a, pattern=[[1, TT]], compare_op=ALU.is_ge,
                                                               fill=0.0, base=b, channel_multiplier=-1)))(ma, -128 * dd),
              reads=[m], writes=[m])
        dms.append(m)
    Qts = [kb.sbuf("a_Q%d" % i, [96, TT], BF16, st) for i in range(2)]
    Ps = Pool(kb, "a_P", 4, [128, TT], BF16, stack=st)
    rsb = kb.sbuf("a_rs", [65, TT], F32, st)
    rbs = Pool(kb, "a_rb", 2, [64, TT], F32, stack=st)
    yos = Pool(kb, "a_yo", 2, [64, TT], BF16, stack=st)
    si = 0
    for hh in range(2):
        for c0 in range(0, S, 2048):
            c1 = min(S, c0 + 2048)
            kb.dma("sp", KTs[:, c0:c1], d["KT"][hh, :, c0:c1])
        for b0 in range(0, nb, 32):
            b1 = min(nb, b0 + 32)
            kb.dma("sp", Vx[:, b0:b1, :], d["VT"][:, hh, b0:b1, :])
        for t in range(nt):
            tsl = slice(t * TT, (t + 1) * TT)
            Qt = Qts[t % 2]
            kb.dma("sp", Qt[:, :], d["QT"][hh, :, tsl])
            psO = banks[4 + (t % 2)]
            nkb = 4 * (t + 1)

            def scores(kb_):
                nonlocal si
                dd = kb_ - 4 * t
                c0 = 128 * dd if dd > 0 else 0
                psS = banks[si % 3]
                si += 1
                kb.mm(psS[:, c0:TT], KTs[:, kb_ * 128:kb_ * 128 + 128], Qt[:, c0:TT])
                return psS, c0, dd
            nxt = scores(0)
            for kb_ in range(nkb):
                psS, c0, dd = nxt
                if kb_ + 1 < nkb:
                    nxt = scores(kb_ + 1)
                P = Ps.get()
                kb.act(P[:, c0:TT], psS[:, c0:TT], AF.Exp)
                if dd >= 0:
                    kb.tt("pool", P[:, c0:TT], P[:, c0:TT], dms[dd][:, c0:TT], ALU.mult)
                kb.mm(psO[0:65, c0:TT], Vx[:, kb_, :], P[:, c0:TT], start=(kb_ == 0), stop=(kb_ == nkb - 1))
            kb.copy("act", rsb[64:65, :], psO[64:65, :])
            psB = banks[6]
            kb.mm(psB[0:64, :], onesf[64:65, 0:64], rsb[64:65, :])
            rb = rbs.get()
            kb.recip(rb[:, :], psB[0:64, :])
            yo = yos.get()
            kb.tt("dve", yo[:, :], psO[0:64, :], rb[:, :], ALU.mult)
            kb.dma("sp", d["ydst"](hh * 64, 64, t), yo[:, :])
    kb.flush()
    st.close()


def pack_p2(inp, l):
    P = Packer(128)
    for nm in ("ln1_g", "ln1_b", "ln2_g", "ln2_b"):
        P.add(nm, inp[nm][l].reshape(8, 128).T)
    P.add("rbias", np.repeat(inp["router_bias"][l][None, :], 128, 0))
    P.add("rw", inp["router_w"].reshape(8, 128, 16).transpose(1, 0, 2).reshape(128, 128))
    sel = np.zeros((16, 16, 128), np.float32)
    for e in range(16):
        sel[e, e, :] = 1.0
    P.add("sel", np.concatenate([sel.reshape(16, 2048), np.zeros((112, 2048), np.float32)], 0))
    return P


def ln_fmajor(kb, cx, h32, gname, bname, pcs, F5, EPS, out_fn):
    Mf = cx.Mf
    ps_m = cx.PB.get()
    for j in range(8):
        kb.mm(ps_m, Mf["ones"], h32[:, j, :], start=(j == 0), stop=(j == 7))
    mean = F5.get()
    kb.act(mean[:, :], ps_m, AF.Copy, scale=1.0 / 1024)
    for j in range(8):
        kb.tt("pool", h32[:, j, :], h32[:, j, :], mean[:, :], ALU.subtract)
    ps_v = cx.PB.get()
    for j in range(8):
        sq = F5.get()
        kb.act(sq[:, :], h32[:, j, :], AF.Square)
        kb.mm(ps_v, Mf["ones"], sq[:, :], start=(j == 0), stop=(j == 7))
    tmp = F5.get()
    rstd = F5.get()
    rsqrt_act(kb, rstd[:, :], ps_v, 1.0 / 1024, EPS, tmp[:, :])
    for j in range(8):
        o = F5.get()
        kb.tt("dve", o[:, :], h32[:, j, :], rstd[:, :], ALU.mult)
        kb.ts("dve", o[:, :], o[:, :], pcs(gname, j), ALU.mult, pcs(bname, j), ALU.add)
        out_fn(j, o)


def build_p2(kb, T, l, d):
    import os as _os
    if "build_p2" in _os.environ.get("DBG_SKIP", "").split(","):
        return None

    nt = T // TT
    Po = d["P2off"]

    st = contextlib.ExitStack()
    cx = Ctx()
    setup_common(kb, cx, d["masks"], st)
    Wg = kb.sbuf("Wg", [128, 8, 4096], BF16, st)
    for k in range(8):
        for c0 in range(0, 4096, 1024):
            kb.dma("pool", Wg[:, k, c0:c0 + 1024], d["Wgate"][128 * k:128 * k + 128, c0:c0 + 1024])
    Wb = kb.sbuf("Wb", [128, 4, 4, 1024], BF16, st)
    for n in range(4):
        for kc in range(4):
            kb.dma("pool", Wb[:, n, kc, :], d["Wb"][n, 128 * kc:128 * kc + 128, :])
    F5 = Pool(kb, "p2f", 8, [128, TT], F32, stack=st)
    yst = Pool(kb, "p2ys", 12, [128, TT], BF16, stack=st)
    OH = kb.sbuf("p2oh", [128, 4], F32, st)
    kb.dma("sp", OH[:, :], d["onehot"])
    xbfs = [kb.sbuf("p2x%d" % i, [128, 8, TT], BF16, st) for i in range(2)]
    ybfs = [kb.sbuf("p2y%d" % i, [128, 16, TT], BF16, st) for i in range(2)]
    m32 = kb.sbuf("p2m", [128, 8, TT], F32, st)
    mbf = kb.sbuf("p2mb", [128, 8, TT], BF16, st)

    def load_x(t):
        for k in range(8):
            kb.dma("pool", xbfs[t % 2][:, k, :], d["xsrc2"](k, t))
    load_x(0)
    for t in range(nt):
        tsl = slice(t * TT, (t + 1) * TT)
        if t + 1 < nt:
            load_x(t + 1)
        xbf = xbfs[t % 2]
        ybf = ybfs[t % 2]
        for c in range(16):
            n_, hg_ = c // 4, c % 4
            r0 = hg_ * 512 + n_ * 128
            for jq in range(4):
                yq = yst.get()
                kb.dma("sp", yq[:, :], d["ysrc"](r0, jq, t))
                if jq == 0:
                    kb.ts("dve", ybf[:, c, :], yq[:, :], OH[:, 0:1], ALU.mult)
                else:
                    kb.stt(ybf[:, c, :], yq[:, :], OH[:, jq:jq + 1], ybf[:, c, :], ALU.mult, ALU.add)
        for n in range(4):
            for j in range(8):
                ps_g = cx.PB.get()
                for k in range(8):
                    kb.mm(ps_g, Wg[:, k, n * 1024 + 128 * j:n * 1024 + 128 * j + 128], xbf[:, k, :],
                          start=(k == 0), stop=(k == 7))
                sg = F5.get()
                kb.act(sg[:, :], ps_g, AF.Sigmoid)
                ps_p = cx.banks[2 + (j % 2)][:, :]
                for kc in range(4):
                    kb.mm(ps_p, Wb[:, n, kc, 128 * j:128 * j + 128], ybf[:, 4 * n + kc, :],
                          start=(kc == 0), stop=(kc == 3))
                if n == 0:
                    kb.tt("dve", m32[:, j, :], sg[:, :], ps_p, ALU.mult)
                else:
                    tq = F5.get()
                    kb.tt("dve", tq[:, :], sg[:, :], ps_p, ALU.mult)
                    kb.tt("pool", m32[:, j, :], m32[:, j, :], tq[:, :], ALU.add)
        for j in range(8):
            kb.copy("act", mbf[:, j, :], m32[:, j, :])
            kb.dma("sp", d["mT"][128 * j:128 * j + 128, tsl], mbf[:, j, :])
    kb.flush()
    st.close()

    st = contextlib.ExitStack()
    cx = Ctx()
    setup_common(kb, cx, d["masks"], st)
    PC = kb.sbuf("PC2", [128, d["NP2"]], F32, st)
    kb.dma("sp", PC[:, :], d["PC2"])

    def pcs(name, j):
        o, w = Po[name]
        return PC[:, o + j:o + j + 1]
    cst = kb.sbuf("cst2", [128, 2], F32, st)
    kb.memset("pool", cst[:, 0:1], LN_EPS)
    EPS = cst[:, 0:1]
    wst = Pool(kb, "wst3", 3, [128, 512], F32, stack=st)
    Wo = kb.sbuf("Wo", [128, 8, 1024], BF16, st)
    for k in range(8):
        for c0 in range(0, 1024, 512):
            s_ = wst.get()
            kb.dma("sp", s_[:, :], d["Wout"][128 * k:128 * k + 128, c0:c0 + 512])
            kb.copy("pool", Wo[:, k, c0:c0 + 512], s_[:, :])
    F5 = Pool(kb, "p2g", 10, [128, TT], F32, stack=st)
    mbf = kb.sbuf("p2mb2", [128, 8, TT], BF16, st)
    x32 = kb.sbuf("p2x32", [128, 8, TT], F32, st)
    h32 = kb.sbuf("p2h32", [128, 8, TT], F32, st)
    for t in range(nt):
        tsl = slice(t * TT, (t + 1) * TT)
        for k in range(8):
            kb.dma("sp", mbf[:, k, :], d["mT"][128 * k:128 * k + 128, tsl])
            kb.dma("sp", x32[:, k, :], d["xsrc2"](k, t))
        for j in range(8):
            ps = cx.PB.get()
            for k in range(8):
                kb.mm(ps, Wo[:, k, 128 * j:128 * j + 128], mbf[:, k, :], start=(k == 0), stop=(k == 7))
            kb.stt(h32[:, j, :], x32[:, j, :], ALPHA, ps, ALU.mult, ALU.add)

        def out1(j, o, tsl=tsl):
            kb.dma("sp", d["x1T"][128 * j:128 * j + 128, tsl], o[:, :])
        ln_fmajor(kb, cx, h32, "ln1_g", "ln1_b", pcs, F5, EPS, out1)
    kb.flush()
    st.close()

    HT = min(T, 2048)
    for half in range(T // HT):
        st = contextlib.ExitStack()
        cx = Ctx()
        setup_common(kb, cx, d["masks"], st)
        Mf = cx.Mf
        PC = kb.sbuf("PC2b", [128, d["NP2"]], F32, st)
        kb.dma("sp", PC[:, :], d["PC2"])
        PCb = kb.sbuf("PC2bb", [16, 2048], BF16, st)
        o_sel = Po["sel"][0]
        kb.copy("pool", PCb[:, :], PC[0:16, o_sel:o_sel + 2048])

        def pcs(name, j):
            o, w = Po[name]
            return PC[:, o + j:o + j + 1]
        cst = kb.sbuf("cst3", [128, 2], F32, st)
        kb.memset("pool", cst[:, 0:1], LN_EPS)
        EPS = cst[:, 0:1]
        o_rw = Po["rw"][0]
        o_rb = Po["rbias"][0]
        wst = Pool(kb, "wst4", 4, [128, 512], F32, stack=st)
        F5 = Pool(kb, "p2h", 8, [128, TT], F32, stack=st)
        S16 = Pool(kb, "s16", 24, [128, 16], F32, stack=st)
        x1bf = kb.sbuf("x1bf", [128, 8, HT], BF16, st)
        acc = kb.sbuf("acc", [128, 8, HT], F32, st)
        cTb = kb.sbuf("cTb", [16, HT], BF16, st)
        nth = HT // TT
        for t in range(nth):
            g0 = half * HT + t * TT
            rps = [cx.banks[b][:, 0:16] for b in range(4)]
            for k in range(8):
                s_ = wst.get()
                kb.dma("sp", s_[:, :], d["x1T"][128 * k:128 * k + 128, g0:g0 + TT])
                kb.copy("pool", x1bf[:, k, t * TT:(t + 1) * TT], s_[:, :])
                for blk in range(4):
                    kb.mm(rps[blk], s_[:, blk * 128:blk * 128 + 128], PC[:, o_rw + 16 * k:o_rw + 16 * k + 16],
                          start=(k == 0), stop=(k == 7))
            for blk in range(4):
                e = S16.get()
                kb.act(e[:, :], rps[blk], AF.Exp, scale=-1.0)
                kb.ts("dve", e[:, :], e[:, :], 1.0, ALU.add)
                sc = S16.get()
                kb.recip(sc[:, :], e[:, :])
                bi = S16.get()
                kb.tt("dve", bi[:, :], sc[:, :], PC[:, o_rb:o_rb + 16], ALU.add)
                b4 = bi.ap[:, :].rearrange("p (g e) -> p g e", e=4)
                p6 = S16.get()
                p6v = p6.ap[:, :].rearrange("p (g s) -> p g s", s=4)
                p6b = S16.get()
                p6bv = p6b.ap[:, :].rearrange("p (g s) -> p g s", s=4)
                pairs = [(0, 1), (0, 2), (0, 3), (1, 2), (1, 3), (2, 3)]
                for pi, (i_, j_) in enumerate(pairs):
                    dst = p6v[:, :, pi:pi + 1] if pi < 4 else p6bv[:, :, pi - 4:pi - 3]
                    dd = p6 if pi < 4 else p6b
                    kb.tt("dve", V(dst, dd.deps), V(b4[:, :, i_:i_ + 1], bi.deps), V(b4[:, :, j_:j_ + 1], bi.deps), ALU.add)
                gs = S16.get()
                gsv = gs.ap[:, 0:4].rearrange("p (g o) -> p g o", o=1)
                kb.tt("dve", V(gsv, gs.deps), V(p6v[:, :, 0:1], p6.deps), V(p6v[:, :, 1:2], p6.deps), ALU.max)
                kb.tt("dve", V(gsv, gs.deps), V(gsv, gs.deps), V(p6v[:, :, 2:3], p6.deps), ALU.max)
                kb.tt("dve", V(gsv, gs.deps), V(gsv, gs.deps), V(p6v[:, :, 3:4], p6.deps), ALU.max)
                kb.tt("dve", V(gsv, gs.deps), V(gsv, gs.deps), V(p6bv[:, :, 0:1], p6b.deps), ALU.max)
                kb.tt("dve", V(gsv, gs.deps), V(gsv, gs.deps), V(p6bv[:, :, 1:2], p6b.deps), ALU.max)
                gm = S16.get()
                kb.op("dve", (lambda o_, i_: (lambda h: h.tensor_reduce(o_, i_, AX.X, ALU.max)))(gm.ap[:, 0:1], gs.ap[:, 0:4]),
                      reads=[gs], writes=[gm])
                ing = S16.get()
                kb.ts("dve", ing[:, 0:4], gs[:, 0:4], gm[:, 0:1], ALU.is_ge)
                off = S16.get()
                kb.ts("dve", off[:, 0:4], ing[:, 0:4], 1e30, ALU.mult, -1e30, ALU.add)
                mk = S16.get()
                mk4 = mk.ap[:, :].rearrange("p (g e) -> p g e", e=4)
                ing_bc = ing.ap[:, 0:4].rearrange("p (g o) -> p g o", o=1).broadcast_to([128, 4, 4])
                off_bc = off.ap[:, 0:4].rearrange("p (g o) -> p g o", o=1).broadcast_to([128, 4, 4])
                kb.tt("dve", V(mk4, mk.deps), V(b4, bi.deps), V(ing_bc, ing.deps), ALU.mult)
                kb.tt("dve", V(mk4, mk.deps), V(mk4, mk.deps), V(off_bc, off.deps), ALU.add)
                t8 = S16.get()
                kb.op("dve", (lambda o_, i_: (lambda h: h.max(o_, i_)))(t8.ap[:, 0:8], mk.ap[:, :]), reads=[mk], writes=[t8])
                sel = S16.get()
                kb.ts("dve", sel[:, :], mk[:, :], t8[:, 1:2], ALU.is_ge)
                ws = S16.get()
                kb.tt("dve", ws[:, :], sel[:, :], sc[:, :], ALU.mult)
                den = S16.get()
                kb.op("dve", (lambda o_, i_: (lambda h: h.tensor_reduce(o_, i_, AX.X, ALU.add)))(den.ap[:, 0:1], ws.ap[:, :]),
                      reads=[ws], writes=[den])
                kb.recip(den[:, 1:2], den[:, 0:1])
                cmb = S16.get()
                kb.ts("dve", cmb[:, :], ws[:, :], den[:, 1:2], ALU.mult)
                ps_t = cx.banks[4][0:16, 0:128]
                kb.mm(ps_t, cmb[:, :], Mf["ident"])
                kb.copy("act", cTb[:, t * TT + blk * 128:t * TT + blk * 128 + 128], ps_t)
        st2 = contextlib.ExitStack()
        Wgs = [kb.sbuf("Wge%d" % i, [128, 8, 512], BF16, st2) for i in range(2)]
        Wus = [kb.sbuf("Wue%d" % i, [128, 8, 512], BF16, st2) for i in range(2)]
        Wds = [kb.sbuf("Wde%d" % i, [128, 4, 1024], BF16, st2) for i in range(2)]
        cbcs = Pool(kb, "cbc", 2, [128, TT], BF16, stack=st2)
        acts = Pool(kb, "actb", 8, [128, TT], BF16, stack=st2)
        gi = 0

        def load_expert(e):
            Wg_, Wu_, Wd_ = Wgs[e % 2], Wus[e % 2], Wds[e % 2]
            for k in range(8):
                kb.dma("pool", Wg_[:, k, :], d["moe_g"][e, 128 * k:128 * k + 128, :])
                kb.dma("pool", Wu_[:, k, :], d["moe_u"][e, 128 * k:128 * k + 128, :])
            for fc in range(4):
                kb.dma("pool", Wd_[:, fc, :], d["moe_d"][e, 128 * fc:128 * fc + 128, :])
        load_expert(0)
        for e in range(16):
            Wg_, Wu_, Wd_ = Wgs[e % 2], Wus[e % 2], Wds[e % 2]
            if e + 1 < 16:
                load_expert(e + 1)
            for t in range(nth):
                tl = slice(t * TT, (t + 1) * TT)
                ps_c = cx.banks[6][:, :]
                kb.mm(ps_c, PCb[0:16, 128 * e:128 * e + 128], cTb[:, tl])
                cbc = cbcs.get()
                kb.copy("act", cbc[:, :], ps_c)
                at = []
                for fc in range(4):
                    ps_g = cx.banks[gi % 2][:, :]
                    ps_u = cx.banks[2 + gi % 2][:, :]
                    gi += 1
                    for k in range(8):
                        kb.mm(ps_g, Wg_[:, k, 128 * fc:128 * fc + 128], x1bf[:, k, tl], start=(k == 0), stop=(k == 7))
                    for k in range(8):
                        kb.mm(ps_u, Wu_[:, k, 128 * fc:128 * fc + 128], x1bf[:, k, tl], start=(k == 0), stop=(k == 7))
                    r_ = F5.get()
                    kb.act(r_[:, :], ps_g, AF.Silu)
                    kb.tt("dve", r_[:, :], r_[:, :], ps_u, ALU.mult)
                    a_ = acts.get()
                    kb.tt("dve", a_[:, :], r_[:, :], cbc[:, :], ALU.mult)
                    at.append(a_)
                for j in range(8):
                    ps_d = cx.banks[4 + j % 2][:, :]
                    for fc in range(4):
                        kb.mm(ps_d, Wd_[:, fc, 128 * j:128 * j + 128], at[fc][:, :], start=(fc == 0), stop=(fc == 3))
                    if e == 0:
                        kb.copy("dve", acc[:, j, tl], ps_d)
                    else:
                        kb.tt("dve", acc[:, j, tl], acc[:, j, tl], ps_d, ALU.add)
        kb.flush()
        st2.close()
        x32 = kb.sbuf("b_x32", [128, 8, TT], F32, st)
        for t in range(nth):
            g0 = half * HT + t * TT
            tl = slice(t * TT, (t + 1) * TT)
            for k in range(8):
                kb.dma("sp", x32[:, k, :], d["x1T"][128 * k:128 * k + 128, g0:g0 + TT])
                kb.stt(x32[:, k, :], x32[:, k, :], ALPHA, acc[:, k, tl], ALU.mult, ALU.add)

            def out2(j, o, g0=g0):
                kb.dma("sp", d["odst"](j, g0 // TT), o[:, :])
            ln_fmajor(kb, cx, x32, "ln2_g", "ln2_b", pcs, F5, EPS, out2)
        kb.flush()
        st.close()


def _din(nc, name, arr):
    dt = I32 if arr.dtype == np.int32 else F32
    t = nc.dram_tensor(name, list(arr.shape), dt, kind="ExternalInput")
    return V(t.ap(), [Dep()])


def _dout(nc, name, shape, dt=F32):
    t = nc.dram_tensor(name, list(shape), dt, kind="ExternalOutput")
    return V(t.ap(), [Dep()])


def _dint(nc, name, shape, dt):
    t = nc.dram_tensor(name, list(shape), dt)
    return V(t.ap(), [Dep()])


GROUPS = [[0, 1, 2, 3], [4, 5, 6, 7]]


def kernel(**inputs):
    inp = {k: np.asarray(v) for k, v in inputs.items()}
    x = inp["x"]
    B, S = x.shape[0], x.shape[1]
    T = S // 4
    xT = [np.ascontiguousarray(x[b].T) for b in range(B)]
    masks = masks_host()
    scm = scanmask_host()
    packs = [[pack_p1a(inp, l, hg) for hg in range(4)] for l in range(2)]
    arrs = [[(W.build(), P.build(), M.build()) for (W, P, M) in packs[l]] for l in range(2)]
    p2 = [pack_p2(inp, l) for l in range(2)]
    p2a = [p.build() for p in p2]
    posrep = [np.ascontiguousarray(np.repeat(inp["positions"][b][None, :], 96, 0)).astype(np.int32) for b in range(B)]
    common = {"Wgate0": np.ascontiguousarray(inp["w_in"][0][:, O_GATES:O_GATES + 4096]),
              "Wgate1": np.ascontiguousarray(inp["w_in"][1][:, O_GATES:O_GATES + 4096]),
              "Wb": np.ascontiguousarray(inp["w_branch"]), "Wout": np.ascontiguousarray(inp["w_out"]),
              "PC2_0": p2a[0], "PC2_1": p2a[1], "moe_g": np.ascontiguousarray(inp["moe_w_gate"]),
              "moe_u": np.ascontiguousarray(inp["moe_w_up"]), "moe_d": np.ascontiguousarray(inp["moe_w_down"]),
              "masks": masks, "scanmask": scm}
    in_maps = []
    for b in range(B):
        for r in range(4):
            oh = np.zeros((128, 4), np.float32)
            oh[:, r] = 1.0
            m = {"xT": xT[b], "xq": np.ascontiguousarray(xT[b][:, r * T:(r + 1) * T]), "pos": posrep[b], "onehot": oh}
            for l in range(2):
                m["W%d" % l], m["PC%d" % l], m["SM%d" % l] = arrs[l][r]
            m.update(common)
            in_maps.append({"in_" + k: v for k, v in m.items()})

    nc = bass.Bass("TRN2", target_bir_lowering=False)
    kb = KB(nc)
    di = {k[3:]: _din(nc, k, v) for k, v in in_maps[0].items()}
    CW = 1024
    nyc = S // CW
    yTc = _dint(nc, "yTc", [nyc, 512, CW], BF16)
    ygc = _dint(nc, "ygc", [nyc, 2048, CW], BF16)
    ntq = T // TT
    x2c = _dint(nc, "x2c", [ntq, 2, 512, TT], F32)
    xgc = _dint(nc, "xgc", [ntq, 2, 2048, TT], F32)
    vfirst = _dint(nc, "vfirst_s", [128, S], F32)
    oT = _dout(nc, "oT", [1024, T])
    scratch = {"QT": _dint(nc, "QT", [2, 96, S], BF16), "KT": _dint(nc, "KT", [2, 96, S], BF16),
               "VT": _dint(nc, "VT", [128, 2, S // 128, 65], BF16),
               "x1T": _dint(nc, "x1T", [1024, T], F32), "mT": _dint(nc, "mT", [1024, T], BF16)}

    def ydst(row0, nrows, t):
        c, o = divmod(t * TT, CW)
        return yTc[c][row0:row0 + nrows, o:o + TT]

    def ysrc(r0, jq, t):
        c, o = divmod(jq * T + t * TT, CW)
        return ygc[c][r0:r0 + 128, o:o + TT]

    def x2_local(k, t):
        return x2c[t][k // 4][(k % 4) * 128:(k % 4) * 128 + 128, :]

    def x_gathered(k, tt):
        q, tl = divmod(tt, ntq)
        return xgc[tl][k // 4][q * 512 + (k % 4) * 128:q * 512 + (k % 4) * 128 + 128, :]

    for l in range(2):
        W0, P0, M0 = packs[l][0]
        d = {"pos": di["pos"], "W": di["W%d" % l], "PC": di["PC%d" % l], "SM": di["SM%d" % l],
             "masks": di["masks"], "scanmask": di["scanmask"], "ydst": ydst, "vfirst": vfirst}
        d.update(scratch)
        d.update(Woff=W0.off, Poff=P0.off, Moff=M0.off, NW=arrs[l][0][0].shape[1], NP=arrs[l][0][1].shape[1],
                 NM=arrs[l][0][2].shape[1])
        if l == 0:
            d["xsrc"] = lambda k, t: di["xT"][128 * k:128 * k + 128, t * TT:(t + 1) * TT]
        else:
            d["xsrc"] = x_gathered
        build_p1a(kb, S, l, d)
        build_p1b(kb, S, d)
        for c in range(nyc):
            kb.coll("AllGather", ygc[c], yTc[c], GROUPS)
        d2 = {"ysrc": ysrc, "onehot": di["onehot"], "Wgate": di["Wgate%d" % l], "Wb": di["Wb"][l], "Wout": di["Wout"][l],
              "PC2": di["PC2_%d" % l], "moe_g": di["moe_g"][l], "moe_u": di["moe_u"][l], "moe_d": di["moe_d"][l],
              "masks": di["masks"], "x1T": scratch["x1T"], "mT": scratch["mT"],
              "P2off": p2[l].off, "NP2": p2a[l].shape[1]}
        if l == 0:
            d2["xsrc2"] = lambda k, t: di["xq"][128 * k:128 * k + 128, t * TT:(t + 1) * TT]
            d2["odst"] = lambda j, t: x2_local(j, t)
        else:
            d2["xsrc2"] = x2_local
            d2["odst"] = lambda j, t: oT[128 * j:128 * j + 128, t * TT:(t + 1) * TT]
        build_p2(kb, T, l, d2)
        if l == 0:
            for t_ in range(ntq):
                for h_ in range(2):
                    kb.coll("AllGather", xgc[t_][h_], x2c[t_][h_], GROUPS)
    kb.flush()
    kb.close()
    res = run_bass_kernel_spmd(nc, in_maps, core_ids=list(range(len(in_maps))))
    out = np.empty((B, S, 1024), np.float32)
    for b in range(B):
        for q in range(4):
            out[b, q * T:(q + 1) * T, :] = res.results[b * 4 + q]["oT"].T
    return out
```

```python
import contextlib
import math
import numpy as np
import ml_dtypes
import concourse.bass as bass
import concourse.mybir as mybir
from concourse.bass_utils import run_bass_kernel_spmd

F32 = mybir.dt.float32
BF16 = mybir.dt.bfloat16
I32 = mybir.dt.int32
ALU = mybir.AluOpType
AF = mybir.ActivationFunctionType
AX = mybir.AxisListType

D_MODEL = 1024
IN_COLS = 9912
TT = 512
NKC = 8


class Dep:
    __slots__ = ("w", "r", "wd")

    def __init__(self):
        self.w = None
        self.r = []
        self.wd = []


class V:
    __slots__ = ("ap", "deps")

    def __init__(self, ap, deps):
        self.ap = ap
        self.deps = deps

    def __getitem__(self, k):
        return V(self.ap[k], self.deps)

    def v(self, ap):
        return V(ap, self.deps)


class Eng:
    def __init__(self, name):
        self.name = name
        self.count = 0
        self.ops = []
        self.waited = {}


class KB:
    def __init__(self, nc):
        self.nc = nc
        self.stack = contextlib.ExitStack()
        self.E = {n: Eng(n) for n in ("pe", "act", "dve", "pool", "sp")}
        import os as _os2
        self.ndma = int(_os2.environ.get("DBG_NDMA", "32"))
        self.dma_state = [0] * self.ndma
        self.dma_next = 0
        self.n_ops = 0
        self.out_tokens = []
        import os as _os
        self.limit = int(_os.environ["DBG_LIMIT"]) if _os.environ.get("DBG_LIMIT") else None
        self.dump = int(_os.environ["DBG_DUMP"]) if _os.environ.get("DBG_DUMP") else None
        self.nosame = set(_os.environ.get("DBG_NOSAME", "").split(","))

    def _nm(self, name):
        self._uid = getattr(self, "_uid", 0) + 1
        return "%s_u%d" % (name, self._uid)

    def sbuf(self, name, shape, dt, stack=None):
        t = (stack or self.stack).enter_context(self.nc.sbuf_tensor(self._nm(name), list(shape), dt))
        return V(t[:] if False else t, [Dep()])

    def psum(self, name, shape, dt=F32, stack=None):
        t = (stack or self.stack).enter_context(self.nc.psum_tensor(self._nm(name), list(shape), dt))
        return V(t, [Dep()])

    def dram(self, name, shape, dt, kind="Internal"):
        t = self.nc.dram_tensor(name, list(shape), dt, kind=kind)
        return V(t.ap(), [Dep()])

    @staticmethod
    def _deps(reads, writes):
        out = []
        for d in reads:
            if d.w is not None:
                out.append(d.w)
            out.extend(d.wd)
        for d in writes:
            if d.w is not None:
                out.append(d.w)
            out.extend(d.wd)
            out.extend(d.r)
        return out

    def _waits(self, e, reads, writes, skip):
        need = {}
        for (pn, pv) in self._deps(reads, writes):
            if pn == skip:
                continue
            if need.get(pn, 0) < pv:
                need[pn] = pv
        waits = []
        for pn, pv in need.items():
            if e.waited.get(pn, 0) < pv:
                e.waited[pn] = pv
                waits.append((pn, pv))
        return waits

    @staticmethod
    def _mark(tok, reads, writes):
        for d in reads:
            d.r.append(tok)
            if len(d.r) > 64:
                last = {}
                for (n, v) in d.r:
                    if last.get(n, 0) < v:
                        last[n] = v
                d.r = list(last.items())
        is_async = tok[0].startswith("dma") or tok[0] == "coll"
        for d in writes:
            if is_async:
                d.wd.append(tok)
                if len(d.wd) > 48:
                    last = {}
                    for (n, v) in d.wd:
                        if last.get(n, 0) < v:
                            last[n] = v
                    d.wd = list(last.items())
            else:
                d.w = tok
                d.wd = []
            d.r = []

    def op(self, eng, fn, reads=(), writes=()):
        if self.limit is not None and self.n_ops >= self.limit:
            return None
        if self.dump is not None and abs(self.n_ops - self.dump) <= 3:
            import sys as _s
            f = _s._getframe(1)
            chain = []
            while f is not None and len(chain) < 4:
                chain.append("%s:%d" % (f.f_code.co_name, f.f_lineno))
                f = f.f_back
            print("OP", self.n_ops, eng, chain)
        rd = [d for v in reads for d in v.deps]
        wd = [d for v in writes for d in v.deps]
        e = self.E[eng]
        skip = eng if (eng == "pe" or eng in self.nosame) else None
        waits = self._waits(e, rd, wd, skip)
        e.count += 1
        tok = (eng, e.count)
        e.ops.append((waits, fn, None))
        self._mark(tok, rd, wd)
        self.n_ops += 1
        return tok

    def dma(self, q, out, in_, **kw):
        if self.limit is not None and self.n_ops >= self.limit:
            return None
        rd = list(in_.deps)
        wd = list(out.deps)
        e = self.E[q]
        waits = self._waits(e, rd, wd, None)
        s = self.dma_next
        self.dma_next = (s + 1) % self.ndma
        prev = self.dma_state[s]
        name = "dma%d" % s
        if prev > 0 and e.waited.get(name, 0) < prev:
            e.waited[name] = prev
            waits.append((name, prev))
        self.dma_state[s] = prev + 16
        tok = (name, prev + 16)
        oa, ia = out.ap, in_.ap
        e.ops.append((waits, lambda h: h.dma_start(out=oa, in_=ia, **kw), name))
        self._mark(tok, rd, wd)
        self.n_ops += 1
        return tok

    def coll(self, kind, out, in_, groups):
        import os as _os
        if _os.environ.get("DBG_NOCOLL"):
            return None
        self._cidx = getattr(self, "_cidx", -1) + 1
        if _os.environ.get("DBG_COLLMASK") and not (int(_os.environ["DBG_COLLMASK"]) >> self._cidx) & 1:
            return None
        rd = list(in_.deps)
        wd = list(out.deps)
        e = self.E["pool"]
        waits = self._waits(e, rd, wd, None)
        self.ncoll = getattr(self, "ncoll", 0) + 1
        tok = ("coll", self.ncoll)
        oa, ia = out.ap, in_.ap
        e.ops.append((waits, lambda h: h.collective_compute(kind, ALU.bypass, replica_groups=groups, ins=[ia], outs=[oa]), "coll"))
        self._mark(tok, rd, wd)
        self.n_ops += 1
        return tok

    def _ensure_sems(self):
        if getattr(self, "sem", None) is None:
            self.sem = {}
            for n in ("pe", "act", "dve", "pool"):
                self.sem[n] = self.stack.enter_context(self.nc.semaphore("s_" + n))
            for i in range(self.ndma):
                self.sem["dma%d" % i] = self.stack.enter_context(self.nc.semaphore("s_dma%d" % i))
            self.sem["coll"] = self.stack.enter_context(self.nc.semaphore("s_coll"))

    def flush(self):
        self._ensure_sems()
        sem = self.sem
        import os as _os3
        if _os3.environ.get("DBG_FLUSHLOG"):
            print("FLUSH at n_ops", self.n_ops)
        toks = [(n, self.E[n].count) for n in ("pe", "act", "dve", "pool") if self.E[n].count > 0]
        toks += [("dma%d" % i, v) for i, v in enumerate(self.dma_state) if v > 0]
        if getattr(self, "ncoll", 0) > 0:
            toks.append(("coll", self.ncoll))
        for n, e in self.E.items():
            waits = []
            for (pn, pv) in toks:
                if pn == n:
                    continue
                if e.waited.get(pn, 0) < pv:
                    e.waited[pn] = pv
                    waits.append((pn, pv))
            e.ops.append((waits, None, None))
        with self.nc.Block() as block:
            def replay(e, own):
                ops = e.ops

                def body(h):
                    for waits, fn, dsem in ops:
                        for (pn, pv) in waits:
                            h.wait_ge(sem[pn], pv)
                        if fn is None:
                            continue
                        ins = fn(h)
                        if dsem == "coll":
                            ins.then_inc(sem[dsem], 1)
                        elif dsem is not None:
                            ins.then_inc(sem[dsem], 16)
                        elif own is not None:
                            ins.then_inc(sem[own], 1)
                return body
            block.sync(replay(self.E["sp"], None))
            block.tensor(replay(self.E["pe"], "pe"))
            block.scalar(replay(self.E["act"], "act"))
            block.vector(replay(self.E["dve"], "dve"))
            block.gpsimd(replay(self.E["pool"], "pool"))
        for e in self.E.values():
            e.ops = []

    def emit(self):
        self.flush()

    def close(self):
        self.stack.close()

    def mm(self, out, lhsT, rhs, start=True, stop=True):
        oa, la, ra = out.ap, lhsT.ap, rhs.ap
        return self.op("pe", lambda h: h.matmul(oa, la, ra, start=start, stop=stop),
                       reads=[lhsT, rhs], writes=[out])

    def act(self, out, in_, func, scale=1.0, bias=None, eng="act"):
        oa, ia = out.ap, in_.ap
        reads = [in_]
        kw = {}
        if isinstance(bias, V):
            reads.append(bias)
            kw["bias"] = bias.ap
        elif bias is not None:
            kw["bias"] = bias
        if isinstance(scale, V):
            reads.append(scale)
            kw["scale"] = scale.ap
        else:
            kw["scale"] = scale
        return self.op("act", lambda h: h.activation(oa, ia, func, **kw), reads=reads, writes=[out])

    def tt(self, eng, out, in0, in1, op):
        oa, a, b = out.ap, in0.ap, in1.ap
        return self.op(eng, lambda h: h.tensor_tensor(oa, a, b, op), reads=[in0, in1], writes=[out])

    def ts(self, eng, out, in0, s1, op0, s2=None, op1=None):
        oa, a = out.ap, in0.ap
        reads = [in0]
        if isinstance(s1, V):
            reads.append(s1)
            s1 = s1.ap
        if isinstance(s2, V):
            reads.append(s2)
            s2 = s2.ap
        if op1 is None:
            return self.op(eng, lambda h: h.tensor_scalar(oa, a, s1, None, op0), reads=reads, writes=[out])
        return self.op(eng, lambda h: h.tensor_scalar(oa, a, s1, s2, op0, op1), reads=reads, writes=[out])

    def stt(self, out, in0, scalar, in1, op0, op1):
        oa, a, b = out.ap, in0.ap, in1.ap
        reads = [in0, in1]
        if isinstance(scalar, V):
            reads.append(scalar)
            scalar = scalar.ap
        return self.op("dve", lambda h: h.scalar_tensor_tensor(oa, a, scalar, b, op0, op1),
                       reads=reads, writes=[out])

    def copy(self, eng, out, in_):
        oa, ia = out.ap, in_.ap
        if eng == "act":
            return self.op("act", lambda h: h.activation(oa, ia, AF.Copy), reads=[in_], writes=[out])
        return self.op(eng, lambda h: h.tensor_copy(oa, ia), reads=[in_], writes=[out])

    def memset(self, eng, out, val):
        oa = out.ap
        return self.op(eng, lambda h: h.memset(oa, val), writes=[out])

    def scan(self, out, d0, d1, init, op0, op1):
        oa, a, b = out.ap, d0.ap, d1.ap
        return self.op("dve", lambda h: h.tensor_tensor_scan(oa, a, b, init, op0, op1),
                       reads=[d0, d1], writes=[out])

    def recip(self, out, in_):
        oa, ia = out.ap, in_.ap
        return self.op("dve", lambda h: h.reciprocal(oa, ia), reads=[in_], writes=[out])


class Pool:
    def __init__(self, kb, name, n, shape, dt, space="sbuf", stack=None):
        mk = kb.sbuf if space == "sbuf" else kb.psum
        self.t = [mk("%s%d" % (name, i), shape, dt, stack=stack) for i in range(n)]
        self.i = 0

    def get(self):
        t = self.t[self.i]
        self.i = (self.i + 1) % len(self.t)
        return t


C_W = math.exp(-0.5)
MLA_SCALE = 96 ** -0.5
LN_EPS = 1e-5
NORM_EPS = 1e-6
RWKV_LN_EPS = 64e-5
ALPHA = (2.0 * 2) ** 0.25

_IW = (256, 160, 256, 256, 512, 16, 512, 1536, 4, 4, 512, 1792, 4096)
_IO = np.concatenate([[0], np.cumsum(_IW)]).tolist()
(O_QLAT, O_KVLAT, O_GQ, O_GK, O_GV, O_GGATE, O_GOG, O_DQKV, O_DBETA, O_DA, O_DZ, O_RW, O_GATES) = _IO[:13]


class Packer:
    def __init__(self, rows):
        self.rows = rows
        self.blocks = []
        self.off = {}
        self.n = 0

    def add(self, name, arr):
        arr = np.asarray(arr, dtype=np.float32)
        if arr.ndim == 1:
            arr = arr[:, None]
        if arr.shape[0] < self.rows:
            pad = np.zeros((self.rows - arr.shape[0], arr.shape[1]), np.float32)
            arr = np.concatenate([arr, pad], 0)
        self.off[name] = (self.n, arr.shape[1])
        self.blocks.append(arr)
        self.n += arr.shape[1]

    def build(self):
        return np.ascontiguousarray(np.concatenate(self.blocks, 1))


def const_masks():
    i = np.arange(128)
    same = (i[:, None] // 64) == (i[None, :] // 64)
    m = {}
    m["U_incl"] = (same & (i[:, None] <= i[None, :])).astype(np.float32)
    m["U_strict"] = (same & (i[:, None] < i[None, :])).astype(np.float32)
    m["L_incl"] = m["U_incl"].T.copy()
    m["L_strict"] = m["U_strict"].T.copy()
    m["ident"] = np.eye(128, dtype=np.float32)
    m["ones"] = np.ones((128, 128), np.float32)
    m["ones_blk"] = same.astype(np.float32)
    m["istack"] = np.concatenate([np.eye(64), np.eye(64)], 0).astype(np.float32)
    return m


def pack_p1a(inp, l, hg):
    w_in = inp["w_in"][l]
    W = Packer(1024)
    W.add("qlat", w_in[:, O_QLAT:O_QLAT + 256])
    W.add("ckv", w_in[:, O_KVLAT:O_KVLAT + 128])
    kr = w_in[:, O_KVLAT + 128:O_KVLAT + 160]
    W.add("krope", kr)
    W.add("krope_sw", np.concatenate([kr[:, 16:32], kr[:, 0:16]], 1))
    W.add("gq", w_in[:, O_GQ + 64 * hg:O_GQ + 64 * hg + 64])
    W.add("gk", w_in[:, O_GK + 64 * hg:O_GK + 64 * hg + 64])
    W.add("gv", w_in[:, O_GV + 128 * hg:O_GV + 128 * hg + 128])
    W.add("ggate", w_in[:, O_GGATE:O_GGATE + 16])
    W.add("gog", w_in[:, O_GOG + 128 * hg:O_GOG + 128 * hg + 128])
    W.add("dq", w_in[:, O_DQKV + 128 * hg:O_DQKV + 128 * hg + 128])
    W.add("dk", w_in[:, O_DQKV + 512 + 128 * hg:O_DQKV + 512 + 128 * hg + 128])
    W.add("dv", w_in[:, O_DQKV + 1024 + 128 * hg:O_DQKV + 1024 + 128 * hg + 128])
    W.add("dbeta", np.repeat(w_in[:, O_DBETA + hg:O_DBETA + hg + 1], 128, 1))
    W.add("da", np.repeat(w_in[:, O_DA + hg:O_DA + hg + 1], 128, 1))
    W.add("dz", w_in[:, O_DZ + 128 * hg:O_DZ + 128 * hg + 128])
    for i, nm in enumerate(("rr", "rk", "rv")):
        W.add(nm, w_in[:, O_RW + 512 * i + 128 * hg:O_RW + 512 * i + 128 * hg + 128])
    W.add("rwl", w_in[:, O_RW + 1536:O_RW + 1600])
    W.add("ral", w_in[:, O_RW + 1600:O_RW + 1664])
    W.add("rgl", w_in[:, O_RW + 1664:O_RW + 1792])
    if l > 0:
        W.add("rvr", inp["w_in_vres"][l - 1])
    P = Packer(128)
    ch = slice(128 * hg, 128 * hg + 128)
    mu = inp["rwkv_mu"][l]
    P.add("mu_r", mu[0:512][ch]); P.add("mu_k", mu[512:1024][ch]); P.add("mu_v", mu[1024:1536][ch])
    P.add("mu_wl", mu[1536:1600]); P.add("mu_al", mu[1600:1664]); P.add("mu_gl", mu[1664:1792])
    P.add("w0", inp["rwkv_w0"][l][ch]); P.add("a0", inp["rwkv_a0"][l][ch])
    P.add("k_k", inp["rwkv_k_k"][l][ch]); P.add("k_a", inp["rwkv_k_a"][l][ch])
    P.add("r_k", inp["rwkv_r_k"][l].reshape(-1)[ch])
    P.add("ln_g", inp["rwkv_ln_g"][l][ch]); P.add("ln_b", inp["rwkv_ln_b"][l][ch])
    if l > 0:
        P.add("mu_vr", inp["rwkv_vres_mu"][l - 1]); P.add("v0", inp["rwkv_v0"][l - 1][ch])
    P.add("gla_norm", inp["gla_norm"][l]); P.add("gdn_norm", inp["gdn_norm"][l])
    cw = inp["gdn_conv"][l]
    for nm, o in (("cq", 0), ("ck", 512), ("cv", 1024)):
        P.add("conv_" + nm, cw[:, o + 128 * hg:o + 128 * hg + 128].T)
    P.add("a_log", np.full(128, inp["gdn_a_log"][l][hg])); P.add("dt_bias", np.full(128, inp["gdn_dt_bias"][l][hg]))
    P.add("q_norm", inp["mla_q_norm"][l].reshape(2, 128).T)
    P.add("kv_norm", inp["mla_kv_norm"][l])
    invf = (10000.0 ** (-np.arange(16, dtype=np.float32) / 16)).astype(np.float32)
    P.add("invf", np.concatenate([np.zeros(64, np.float32), invf, invf]))
    P.add("rsign", np.concatenate([np.zeros(64, np.float32), -np.ones(16, np.float32), np.ones(16, np.float32)]))
    M = Packer(128)
    M.add("gup", np.concatenate([inp["gla_w_gate_up"][l][:, 64 * hg:64 * hg + 64],
                                 inp["gla_b_gate"][l][None, 64 * hg:64 * hg + 64]], 0))
    M.add("w_up", inp["rwkv_w_up"][l][:, ch]); M.add("a_up", inp["rwkv_a_up"][l][:, ch])
    M.add("g_up", inp["rwkv_g_up"][l][:, ch])
    if l > 0:
        M.add("v_up", inp["rwkv_v_up"][l - 1][:, ch])
    uq = inp["mla_w_uq"][l]
    ukv = inp["mla_w_ukv"][l]
    for hh in range(2):
        h = 2 * hg + hh
        q = uq[:, 96 * h:96 * h + 96]
        for kc in range(2):
            M.add("uq%d_%d" % (hh, kc), q[128 * kc:128 * kc + 128])
            sw = np.concatenate([np.zeros((128, 64), np.float32), q[128 * kc:128 * kc + 128, 80:96],
                                 q[128 * kc:128 * kc + 128, 64:80]], 1)
            M.add("uqs%d_%d" % (hh, kc), sw)
        M.add("ukn%d" % hh, ukv[:, 128 * h:128 * h + 64])
    M.add("ukv_v", np.concatenate([ukv[:, 128 * (2 * hg) + 64:128 * (2 * hg) + 128],
                                   ukv[:, 128 * (2 * hg + 1) + 64:128 * (2 * hg + 1) + 128]], 1))
    return W, P, M


class Ctx:
    pass


def setup_common(kb, cx, masks_d, stack):
    mk = const_masks()
    names = ["U_strict", "U_incl", "L_incl", "L_strict", "ident", "ones", "ones_blk"]
    cx.mf = kb.sbuf("maskf", [128, 7 * 128 + 64], F32, stack)
    cx.mb = kb.sbuf("maskb", [128, 7 * 128 + 64], BF16, stack)
    kb.dma("sp", cx.mf[:, :], masks_d)
    kb.copy("pool", cx.mb[:, :], cx.mf[:, :])
    cx.M = {}
    cx.Mf = {}
    for i, n in enumerate(names):
        cx.M[n] = cx.mb[:, 128 * i:128 * i + 128]
        cx.Mf[n] = cx.mf[:, 128 * i:128 * i + 128]
    cx.M["istack"] = cx.mb[:, 896:960]
    cx.Mf["istack"] = cx.mf[:, 896:960]
    cx.M["pairU"] = cx.mb[:, 0:256]
    banks = [kb.psum("pb%d" % i, [128, 512], F32, stack) for i in range(8)]
    cx.banks = banks

    class RR:
        def __init__(self, items):
            self.items = items
            self.i = 0

        def get(self):
            x = self.items[self.i]
            self.i = (self.i + 1) % len(self.items)
            return x
    cx.RR = RR
    cx.PB = RR([banks[i][:, :] for i in range(2)])
    cx.PH = RR([banks[2][:, 256 * j:256 * j + 256] for j in range(2)])
    cx.PQ = RR([banks[3 + i][:, 128 * j:128 * j + 128] for j in range(4) for i in range(3)])
    cx.PD = [banks[6 + i][:, 128 * j:128 * j + 128] for i in range(2) for j in range(4)]


def masks_host():
    mk = const_masks()
    names = ["U_strict", "U_incl", "L_incl", "L_strict", "ident", "ones", "ones_blk"]
    return np.ascontiguousarray(np.concatenate([mk[n] for n in names] + [mk["istack"]], 1).astype(np.float32))


def scanmask_host():
    m = np.ones((128, TT), np.float32)
    m[:, ::64] = 0.0
    return m


def rsqrt_act(kb, out, in_, scale, eps, tmp):
    kb.act(tmp, in_, AF.Ln, scale=scale, bias=eps)
    kb.act(out, tmp, AF.Exp, scale=-0.5)


def sigmoid_into(kb, out, in_, tmp, scale=1.0, bias=None, eng="dve"):
    kb.act(tmp, in_, AF.Exp, scale=-scale, bias=bias)
    kb.ts(eng, tmp, tmp, 1.0, ALU.add)
    kb.recip(out, tmp)


def neumann_multi(kb, cx, PAs, PBs, pool, ident):
    n = len(PAs)
    TTs = []
    for i in range(n):
        T0 = pool.get()
        kb.tt("dve", T0[:, :], PBs[i], ident, ALU.add)
        TTs.append(T0[:, :])
    A = list(PAs)
    B = list(PBs)
    for k in range(1, 6):
        pss = []
        for i in range(n):
            ps = cx.PQ.get()
            kb.mm(ps, B[i], A[i])
            pss.append(ps)
        nAs = []
        for i in range(n):
            nA = pool.get()
            kb.copy("act", nA[:, :], pss[i])
            nAs.append(nA[:, :])
        nBs = B
        if k < 5:
            pss = []
            for i in range(n):
                ps = cx.PQ.get()
                kb.mm(ps, A[i], B[i])
                pss.append(ps)
            nBs = []
            for i in range(n):
                nB = pool.get()
                kb.copy("dve", nB[:, :], pss[i])
                nBs.append(nB[:, :])
        pss = []
        for i in range(n):
            ps = cx.PQ.get()
            kb.mm(ps, nAs[i], TTs[i])
            pss.append(ps)
        for i in range(n):
            nT = pool.get()
            kb.tt("dve", nT[:, :], TTs[i], pss[i], ALU.add)
            TTs[i] = nT[:, :]
        A, B = nAs, nBs
    return TTs


def neumann(kb, cx, PA, PB_, pool_bf, ident_b):
    TT_ = pool_bf.get()
    kb.tt("dve", TT_[:, :], PB_, ident_b, ALU.add)
    A, B = PA, PB_
    for k in range(1, 6):
        psA = cx.PQ.get()
        kb.mm(psA, B, A)
        nA = pool_bf.get()
        kb.copy("act", nA[:, :], psA)
        if k < 5:
            psB = cx.PQ.get()
            kb.mm(psB, A, B)
            nB = pool_bf.get()
            kb.copy("pool" if False else "dve", nB[:, :], psB)
        psT = cx.PQ.get()
        kb.mm(psT, nA[:, :], TT_[:, :])
        nT = pool_bf.get()
        kb.tt("dve", nT[:, :], TT_[:, :], psT, ALU.add)
        TT_ = nT
        A = nA[:, :]
        if k < 5:
            B = nB[:, :]
    return TT_[:, :]


def build_p1a(kb, S, l, d, branches=("gla", "gdn", "rwkv", "mla")):
    import os as _os
    if "build_p1a" in _os.environ.get("DBG_SKIP", "").split(","):
        return None

    st = contextlib.ExitStack()
    cx = Ctx()
    setup_common(kb, cx, d["masks"], st)
    Wo, Po, Mo = d["Woff"], d["Poff"], d["Moff"]
    NW, NP, NM = d["NW"], d["NP"], d["NM"]
    nt = S // TT
    M, Mf = cx.M, cx.Mf

    PC = kb.sbuf("PC", [128, NP], F32, st)
    kb.dma("sp", PC[:, :], d["PC"])
    SMf = kb.sbuf("SMf", [128, NM], F32, st)
    kb.dma("sp", SMf[:, :], d["SM"])
    SMb = kb.sbuf("SMb", [128, NM], BF16, st)
    kb.copy("pool", SMb[:, :], SMf[:, :])
    scm = kb.sbuf("scm", [128, TT], F32, st)
    kb.dma("sp", scm[:, :], d["scanmask"])
    Wbf = kb.sbuf("Wbf", [128, NKC, NW], BF16, st)
    wst = Pool(kb, "wst", 2, [128, 512], F32, stack=st)
    for k in range(NKC):
        for c0 in range(0, NW, 512):
            cw_ = min(512, NW - c0)
            s_ = wst.get()
            kb.dma("sp", s_[:, 0:cw_], d["W"][128 * k:128 * k + 128, c0:c0 + cw_])
            kb.copy("pool", Wbf[:, k, c0:c0 + cw_], s_[:, 0:cw_])

    def pc(name, rows=128, j=0):
        o, w = Po[name]
        return PC[0:rows, o + j:o + j + 1]

    def sm(name, rows, cols=None):
        o, w = Mo[name]
        return SMb[0:rows, o:o + (cols or w)]

    def wcol(name, k, c0=0, cn=None):
        o, w = Wo[name]
        return Wbf[:, k, o + c0:o + c0 + (cn or w)]

    cst = kb.sbuf("cst", [128, 8], F32, st)
    kb.memset("pool", cst[:, 0:1], NORM_EPS)
    kb.memset("pool", cst[:, 1:2], RWKV_LN_EPS)
    kb.memset("pool", cst[:, 2:3], 1.0)
    kb.memset("pool", cst[:, 3:4], 0.0)
    EPS_N, EPS_R, ONE_C, ZERO_C = cst[:, 0:1], cst[:, 1:2], cst[:, 2:3], cst[:, 3:4]
    ncol = kb.sbuf("ncol", [128, 8], F32, st)
    kb.ts("pool", ncol[:, 0:1], pc("w0"), -1.0, ALU.mult)
    kb.ts("pool", ncol[:, 1:2], pc("a0"), -1.0, ALU.mult)
    if l > 0:
        kb.ts("pool", ncol[:, 2:3], pc("v0"), -1.0, ALU.mult)
    kb.act(ncol[:, 3:4], pc("a_log"), AF.Exp)

    F5 = Pool(kb, "f5_", 10, [128, TT], F32, stack=st)
    L5 = Pool(kb, "l5_", 18, [128, TT], F32, stack=st)
    B5 = Pool(kb, "b5_", 10, [128, TT], BF16, stack=st)
    BQ = Pool(kb, "bq_", 40, [128, 128], BF16, stack=st)
    FQ = Pool(kb, "fq_", 16, [128, 128], F32, stack=st)
    kb_rhs = Pool(kb, "rhs_", 4, [128, 256], BF16, stack=st)
    BL = Pool(kb, "bl_", 8, [128, 128], BF16, stack=st)
    xst = Pool(kb, "xst", 3, [128, TT], F32, stack=st)
    xbfs = [kb.sbuf("xbf%d" % i, [128, NKC, TT], BF16, st) for i in range(2)]

    def proj_f(name, xb, rows=None, c0=0, out=None):
        o, w = Wo[name]
        rows = rows or w
        ps = out if out is not None else cx.PB.get()[0:rows, :]
        for k in range(NKC):
            kb.mm(ps, wcol(name, k, c0, rows), xb[:, k, :], start=(k == 0), stop=(k == NKC - 1))
        return ps

    def shift_mix(ps, rows, mu, halo, t):
        raw = halo[t % 2]
        kb.copy("act", raw[0:rows, 1:TT + 1], ps)
        if t == 0:
            kb.memset("pool", raw[0:rows, 0:1], 0.0)
        kb.copy("pool", halo[(t + 1) % 2][0:rows, 0:1], raw[0:rows, TT:TT + 1])
        dd = F5.get()
        kb.tt("dve", dd[0:rows, :], raw[0:rows, 0:TT], raw[0:rows, 1:TT + 1], ALU.subtract)
        out = F5.get()
        kb.stt(out[0:rows, :], dd[0:rows, :], mu, raw[0:rows, 1:TT + 1], ALU.mult, ALU.add)
        return out[0:rows, :]

    if "gla" in branches:
        gl_H32 = kb.sbuf("gl_H32", [64, 128], F32, st)
        gl_Hbf = kb.sbuf("gl_Hbf", [64, 128], BF16, st)
        kb.memset("pool", gl_H32[:, :], 0.0)
        kb.memset("pool", gl_Hbf[:, :], 0.0)
        gl_ext = kb.sbuf("gl_ext", [32, TT], BF16, st)
        kb.memset("pool", gl_ext[:, :], 1.0)
    if "gdn" in branches:
        gd_H32 = kb.sbuf("gd_H32", [128, 128], F32, st)
        gd_Hbf = kb.sbuf("gd_Hbf", [128, 128], BF16, st)
        kb.memset("pool", gd_H32[:, :], 0.0)
        kb.memset("pool", gd_Hbf[:, :], 0.0)
        gd_halo = {n: [kb.sbuf("gdh_%s" % n, [128, TT + 3], F32, st), kb.sbuf("gdc_%s" % n, [128, 4], F32, st)] for n in ("q", "k", "v")}
    if "rwkv" in branches:
        rw_H32 = kb.sbuf("rw_H32", [128, 64], F32, st)
        rw_Hbf = kb.sbuf("rw_Hbf", [128, 64], BF16, st)
        kb.memset("pool", rw_H32[:, :], 0.0)
        kb.memset("pool", rw_Hbf[:, :], 0.0)
        rw_halo = {n: [kb.sbuf("rwh_%s" % n, [128, TT + 1], F32, st), kb.sbuf("rwc_%s" % n, [128, 2], F32, st)]
                   for n in ("r", "k", "v", "wl", "al", "gl", "vr")}
        rw_AR = kb.sbuf("rw_AR", [128, 2 * TT], BF16, st)
        rw_MT = kb.sbuf("rw_MT", [128, 128], F32, st)
        kb.memset("pool", rw_MT[:, :], 0.0)
    if "mla" in branches:
        mla_posi = kb.sbuf("mla_posi", [96, TT], I32, st)
        mla_vt = [kb.sbuf("mla_vt%d" % i, [128, 2, 65], BF16, st) for i in range(2)]
        for i in range(2):
            kb.memset("pool", mla_vt[i][:, :, :], 1.0)
        mla_ni = kb.sbuf("mla_ni", [96, TT], I32, st)
        uqg = kb.sbuf("uqg", [128, 2, 2, 2, 96], BF16, st)
        for hh in range(2):
            for kc in range(2):
                for sw, nm in enumerate(("uq", "uqs")):
                    o, w = Mo["%s%d_%d" % (nm, hh, kc)]
                    kb.ts("pool", uqg[:, hh, kc, sw, :], SMf[:, o:o + 96], pc("q_norm", 128, kc), ALU.mult,
                          MLA_SCALE, ALU.mult)
        ukg = kb.sbuf("ukg", [128, 2, 64], BF16, st)
        for hh in range(2):
            o, w = Mo["ukn%d" % hh]
            kb.ts("pool", ukg[:, hh, :], SMf[:, o:o + 64], pc("kv_norm"), ALU.mult)
        uvg = kb.sbuf("uvg", [128, 128], BF16, st)
        o, w = Mo["ukv_v"]
        kb.ts("pool", uvg[:, :], SMf[:, o:o + 128], pc("kv_norm"), ALU.mult)

    def silu_of(src):
        sg = L5.get()
        tmp2 = F5.get()
        sigmoid_into(kb, sg[:, :], src, tmp2[:, :])
        kb.tt("dve", sg[:, :], sg[:, :], src, ALU.mult)
        return sg

    def post_norm_gate(oT, gate_sb, gcol, row0):
        osq = B5.get()
        kb.act(osq[:, :], oT[:, :], AF.Square)
        ps_s = cx.PB.get()
        kb.mm(ps_s, M["ones"], osq[:, :])
        tmp = F5.get()
        rstd = F5.get()
        rsqrt_act(kb, rstd[:, :], ps_s, 1.0 / 128, EPS_N, tmp[:, :])
        sg = silu_of(gate_sb)
        kb.stt(oT[:, :], oT[:, :], gcol, rstd[:, :], ALU.mult, ALU.mult)
        yb = B5.get()
        kb.tt("dve", yb[:, :], oT[:, :], sg[:, :], ALU.mult)
        kb.dma("sp", d["ydst"](row0, 128, tsl_box[0].start // TT), yb[:, :])

    def l2norm_f(x, ones_m, scale):
        sq = B5.get()
        kb.act(sq[:, :], x, AF.Square)
        ps_s = cx.PB.get()
        kb.mm(ps_s, ones_m, sq[:, :])
        tmp = F5.get()
        rs = F5.get()
        rsqrt_act(kb, rs[:, :], ps_s, 1.0, EPS_N, tmp[:, :])
        out = L5.get()
        kb.stt(out[:, :], x, scale, rs[:, :], ALU.mult, ALU.mult)
        return out

    def transpose_bf(src, dst_pool=None):
        ps_t = cx.PQ.get()
        kb.mm(ps_t, src, M["ident"])
        o = BL.get()
        kb.copy("act", o[:, :], ps_t)
        return o

    tsl_box = [None]
    youts = {}

    def load_x(t_):
        for k in range(NKC):
            kb.dma("pool", xbfs[t_ % 2][:, k, :], d["xsrc"](k, t_))
    load_x(0)
    for t in range(nt):
        tsl = slice(t * TT, (t + 1) * TT)
        tsl_box[0] = tsl
        xb = xbfs[t % 2]
        if t + 1 < nt:
            load_x(t + 1)

        if "gla" in branches:
            L5.i = 0
            ps_q = proj_f("gq", xb)
            qraw = L5.get()
            kb.copy("act", qraw[0:64, :], ps_q)
            ps_k = proj_f("gk", xb)
            kraw = L5.get()
            kb.copy("act", kraw[0:64, :], ps_k)
            ps_g = proj_f("ggate", xb)
            kb.copy("act", gl_ext[0:16, :], ps_g)
            ps_og = proj_f("gog", xb)
            og = L5.get()
            kb.copy("act", og[:, :], ps_og)
            ps_z = cx.PB.get()[0:64, :]
            kb.mm(ps_z, sm("gup", 17), gl_ext[0:17, :])
            e1 = F5.get()
            kb.act(e1[0:64, :], ps_z, AF.Exp, scale=-1.0)
            sp = F5.get()
            kb.act(sp[0:64, :], e1[0:64, :], AF.Ln, bias=ONE_C[0:64, :])
            bsp = L5.get()
            kb.scan(bsp[0:64, :], scm[0:64, :], sp[0:64, :], 0.0, ALU.mult, ALU.add)
            epos = L5.get()
            kb.act(epos[0:64, :], bsp[0:64, :], AF.Exp, scale=-1.0 / 16)
            eneg = F5.get()
            kb.act(eneg[0:64, :], bsp[0:64, :], AF.Exp, scale=1.0 / 16)
            b3 = bsp.ap[0:64, :].rearrange("p (c t) -> p c t", t=64)
            dl = F5.get()
            dl3 = dl.ap[0:64, :].rearrange("p (c t) -> p c t", t=64)
            bl_bc = b3[:, :, 63:64].broadcast_to([64, TT // 64, 64])
            kb.tt("dve", V(dl3, dl.deps), V(b3, bsp.deps), V(bl_bc, bsp.deps), ALU.subtract)
            eend = F5.get()
            kb.act(eend[0:64, :], dl[0:64, :], AF.Exp, scale=1.0 / 16)
            qd = B5.get()
            kb.stt(qd[0:64, :], qraw[0:64, :], 0.125, epos[0:64, :], ALU.mult, ALU.mult)
            ki = B5.get()
            kb.tt("dve", ki[0:64, :], kraw[0:64, :], eneg[0:64, :], ALU.mult)
            ke = B5.get()
            kb.tt("dve", ke[0:64, :], kraw[0:64, :], eend[0:64, :], ALU.mult)
            oT = L5.get()
            for blk in range(4):
                bs = slice(blk * 128, blk * 128 + 128)
                ps_v = cx.PQ.get()
                for k in range(NKC):
                    kb.mm(ps_v, xb[:, k, bs], wcol("gv", k), start=(k == 0), stop=(k == NKC - 1))
                vt = BQ.get()
                kb.copy("act", vt[:, :], ps_v)
                ps_t = cx.PQ.get()[:, 0:64]
                kb.mm(ps_t, ke[0:64, bs], M["ident"][0:64, 0:64])
                ket = BQ.get()
                kb.copy("dve", ket[:, 0:64], ps_t)
                ps_a = cx.PQ.get()
                kb.mm(ps_a, ki[0:64, bs], qd[0:64, bs])
                am = BQ.get()
                kb.tt("dve", am[:, :], ps_a, Mf["U_incl"], ALU.mult)
                ps_o = cx.PD[0]
                kb.mm(ps_o, vt[:, :], am[:, :], start=True, stop=False)
                for c in range(2):
                    cs = slice(blk * 128 + c * 64, blk * 128 + c * 64 + 64)
                    rs = slice(c * 64, c * 64 + 64)
                    kb.mm(ps_o[:, rs], gl_Hbf[:, :], qd[0:64, cs], start=False, stop=(c == 1))
                    ps_n = cx.PD[4][0:64, :]
                    kb.mm(ps_n, ket[rs, 0:64], vt[rs, :])
                    gc = blk * 2 + c
                    acol = epos[0:64, 64 * (t * 0 + gc) + 63:64 * gc + 64]
                    kb.stt(gl_H32[:, :], gl_H32[:, :], acol, ps_n, ALU.mult, ALU.add)
                    kb.copy("act", gl_Hbf[:, :], gl_H32[:, :])
                kb.copy("act", oT[:, bs], ps_o)
            post_norm_gate(oT, og[:, :], pc("gla_norm"), 128)

        if "gdn" in branches:
            L5.i = 0
            conv = {}
            for nm, cn in (("q", "conv_cq"), ("k", "conv_ck"), ("v", "conv_cv")):
                ps = proj_f("d" + nm, xb)
                raw = gd_halo[nm][0]
                hcol = gd_halo[nm][1]
                if t == 0:
                    kb.memset("pool", raw[:, 0:3], 0.0)
                else:
                    kb.copy("pool", raw[:, 0:3], hcol[:, 0:3])
                kb.copy("act", raw[:, 3:TT + 3], ps)
                kb.copy("pool", hcol[:, 0:3], raw[:, TT:TT + 3])
                acc = F5.get()
                kb.ts("dve", acc[:, :], raw[:, 0:TT], pc(cn, 128, 0), ALU.mult)
                for j in range(1, 4):
                    kb.stt(acc[:, :], raw[:, j:j + TT], pc(cn, 128, j), acc[:, :], ALU.mult, ALU.add)
                conv[nm] = silu_of(acc[:, :])
            qn = l2norm_f(conv["q"][:, :], M["ones"], 128 ** -0.5)
            kn32 = l2norm_f(conv["k"][:, :], M["ones"], 1.0)
            kn = B5.get()
            kb.copy("pool", kn[:, :], kn32[:, :])
            qnb = B5.get()
            kb.copy("pool", qnb[:, :], qn[:, :])
            ps_b = proj_f("dbeta", xb)
            beta = L5.get()
            tb = F5.get()
            sigmoid_into(kb, beta[:, :], ps_b, tb[:, :])
            nbeta = L5.get()
            kb.ts("pool", nbeta[:, :], beta[:, :], -1.0, ALU.mult)
            ps_a = proj_f("da", xb)
            e1 = F5.get()
            kb.act(e1[:, :], ps_a, AF.Exp, bias=pc("dt_bias"))
            spl = F5.get()
            kb.act(spl[:, :], e1[:, :], AF.Ln, bias=ONE_C)
            g = F5.get()
            kb.ts("dve", g[:, :], spl[:, :], ncol[:, 3:4], ALU.mult, -1.0, ALU.mult)
            gc = L5.get()
            kb.scan(gc[:, :], scm[:, :], g[:, :], 0.0, ALU.mult, ALU.add)
            egc = L5.get()
            kb.act(egc[:, :], gc[:, :], AF.Exp)
            g3 = gc.ap[:, :].rearrange("p (c t) -> p c t", t=64)
            dl = F5.get()
            dl3 = dl.ap[:, :].rearrange("p (c t) -> p c t", t=64)
            gl_bc = g3[:, :, 63:64].broadcast_to([128, TT // 64, 64])
            kb.tt("dve", V(dl3, dl.deps), V(gl_bc, gc.deps), V(g3, gc.deps), ALU.subtract)
            eend = F5.get()
            kb.act(eend[:, :], dl[:, :], AF.Exp)
            qdec = L5.get()
            kb.tt("dve", qdec[:, :], qn[:, :], egc[:, :], ALU.mult)
            kbg = B5.get()
            tk = F5.get()
            kb.tt("dve", tk[:, :], kn32[:, :], beta[:, :], ALU.mult)
            kb.tt("dve", kbg[:, :], tk[:, :], egc[:, :], ALU.mult)
            vb = B5.get()
            kb.tt("dve", vb[:, :], conv["v"][:, :], beta[:, :], ALU.mult)
            kend = B5.get()
            kb.tt("dve", kend[:, :], kn32[:, :], eend[:, :], ALU.mult)
            ps_z = proj_f("dz", xb)
            zs = L5.get()
            kb.copy("act", zs[:, :], ps_z)
            oT = L5.get()
            for blk in range(4):
                bs = slice(blk * 128, blk * 128 + 128)
                BL.i = 0
                Rm = gc[:, bs]
                t1 = FQ.get()
                kb.tt("dve", t1[:, :], Rm, Mf["ident"], ALU.mult)
                cc = FQ.get()
                kb.op("dve", (lambda o_, i_: (lambda h: h.tensor_reduce(o_, i_, AX.X, ALU.add)))(cc.ap[:, 0:1], t1.ap[:, :]),
                      reads=[t1], writes=[cc])
                kb.tt("dve", t1[:, :], nbeta[:, bs], Mf["ident"], ALU.mult)
                kb.op("dve", (lambda o_, i_: (lambda h: h.tensor_reduce(o_, i_, AX.X, ALU.add)))(cc.ap[:, 1:2], t1.ap[:, :]),
                      reads=[t1], writes=[cc])
                ccol, nbcol = cc[:, 0:1], cc[:, 1:2]
                tB = FQ.get()
                kb.ts("dve", tB[:, :], Rm, ccol, ALU.subtract, 0.0, ALU.min)
                eB = FQ.get()
                kb.act(eB[:, :], tB[:, :], AF.Exp)
                DBi = FQ.get()
                kb.tt("dve", DBi[:, :], eB[:, :], Mf["U_incl"], ALU.mult)
                DBs = FQ.get()
                kb.tt("pool", DBs[:, :], eB[:, :], Mf["U_strict"], ALU.mult)
                kb.tt("pool", DBs[:, :], DBs[:, :], nbeta[:, bs], ALU.mult)
                tA = FQ.get()
                kb.ts("dve", tA[:, :], Rm, ccol, ALU.subtract, 0.0, ALU.max)
                eA = FQ.get()
                kb.act(eA[:, :], tA[:, :], AF.Exp, scale=-1.0)
                kb.tt("pool", eA[:, :], eA[:, :], Mf["L_strict"], ALU.mult)
                ps_kk = cx.PQ.get()
                kb.mm(ps_kk, kn[:, bs], kn[:, bs])
                PA = BQ.get()
                kb.stt(PA[:, :], eA[:, :], nbcol, ps_kk, ALU.mult, ALU.mult)
                PB_ = BQ.get()
                kb.tt("dve", PB_[:, :], ps_kk, DBs[:, :], ALU.mult)
                TTm = neumann(kb, cx, PA[:, :], PB_[:, :], BQ, M["ident"])
                rhs = kb_rhs.get()
                ps_t = cx.PQ.get()
                kb.mm(ps_t, vb[:, bs], M["ident"])
                kb.copy("act", rhs[:, 0:128], ps_t)
                ps_t = cx.PQ.get()
                kb.mm(ps_t, kbg[:, bs], M["ident"])
                kb.copy("act", rhs[:, 128:256], ps_t)
                kendt = transpose_bf(kend[:, bs])
                ps_uw = cx.PH.get()
                kb.mm(ps_uw, TTm, rhs[:, :])
                UW = kb_rhs.get()
                kb.copy("act", UW[:, :], ps_uw)
                ps_qk = cx.PQ.get()
                kb.mm(ps_qk, kn[:, bs], qnb[:, bs])
                attB = BL.get()
                kb.tt("dve", attB[:, :], ps_qk, DBi[:, :], ALU.mult)
                ps_aw = cx.PQ.get()
                kb.mm(ps_aw, UW[:, 128:256], attB[:, :])
                qeff = BL.get()
                kb.tt("dve", qeff[:, :], qdec[:, bs], ps_aw, ALU.subtract)
                ps_o = cx.PD[1]
                kb.mm(ps_o, UW[:, 0:128], attB[:, :], start=True, stop=False)
                for c in range(2):
                    rs = slice(c * 64, c * 64 + 64)
                    gcn = blk * 2 + c
                    kb.mm(ps_o[:, rs], gd_Hbf[:, :], qeff[:, rs], start=False, stop=(c == 1))
                    ps_m = cx.PD[5]
                    kb.mm(ps_m, UW[rs, 128:256], kendt[rs, :])
                    MT = FQ.get()
                    kb.stt(MT[:, :], Mf["ident"], egc[:, 64 * gcn + 63:64 * gcn + 64], ps_m, ALU.mult, ALU.subtract)
                    ps_h = cx.PD[6]
                    kb.mm(ps_h, MT[:, :], gd_H32[:, :], start=True, stop=False)
                    kb.mm(ps_h, kendt[rs, :], UW[rs, 0:128], start=False, stop=True)
                    kb.copy("act", gd_Hbf[:, :], ps_h)
                    kb.copy("act", gd_H32[:, :], ps_h)
                kb.copy("act", oT[:, bs], ps_o)
            post_norm_gate(oT, zs[:, :], pc("gdn_norm"), 256)

        if "rwkv" in branches:
            L5.i = 0

            def shiftmix(pname, rows, mu_name, hname):
                ps = proj_f(pname, xb)
                raw, hcol = rw_halo[hname]
                if t == 0:
                    kb.memset("pool", raw[0:rows, 0:1], 0.0)
                else:
                    kb.copy("pool", raw[0:rows, 0:1], hcol[0:rows, 0:1])
                kb.copy("act", raw[0:rows, 1:TT + 1], ps)
                kb.copy("pool", hcol[0:rows, 0:1], raw[0:rows, TT:TT + 1])
                dd = F5.get()
                kb.tt("dve", dd[0:rows, :], raw[0:rows, 0:TT], raw[0:rows, 1:TT + 1], ALU.subtract)
                out = L5.get()
                kb.stt(out[0:rows, :], dd[0:rows, :], pc(mu_name, rows), raw[0:rows, 1:TT + 1], ALU.mult, ALU.add)
                return out
            r_ = shiftmix("rr", 128, "mu_r", "r")
            k_ = shiftmix("rk", 128, "mu_k", "k")
            v_ = shiftmix("rv", 128, "mu_v", "v")
            wl = shiftmix("rwl", 64, "mu_wl", "wl")
            al = shiftmix("ral", 64, "mu_al", "al")
            gl = shiftmix("rgl", 128, "mu_gl", "gl")
            tmp = F5.get()
            sgt = F5.get()
            sigmoid_into(kb, sgt[0:64, :], wl[0:64, :], tmp[0:64, :], scale=2.0)
            th = B5.get()
            kb.ts("dve", th[0:64, :], sgt[0:64, :], 2.0, ALU.mult, -1.0, ALU.add)
            ps_z = cx.PB.get()
            kb.mm(ps_z, sm("w_up", 64), th[0:64, :])
            sgw = L5.get()
            tmp = F5.get()
            sigmoid_into(kb, sgw[:, :], ps_z, tmp[:, :], bias=ncol[:, 0:1])
            cs = L5.get()
            kb.scan(cs[:, :], scm[:, :], sgw[:, :], 0.0, ALU.mult, ALU.add)
            albf = B5.get()
            kb.copy("pool", albf[0:64, :], al[0:64, :])
            ps_a = cx.PB.get()
            kb.mm(ps_a, sm("a_up", 64), albf[0:64, :])
            a_ = L5.get()
            tmp = F5.get()
            sigmoid_into(kb, a_[:, :], ps_a, tmp[:, :], bias=ncol[:, 1:2])
            sgg = F5.get()
            tmp = F5.get()
            sigmoid_into(kb, sgg[:, :], gl[:, :], tmp[:, :])
            sggb = B5.get()
            kb.copy("pool", sggb[:, :], sgg[:, :])
            ps_g = cx.PB.get()
            kb.mm(ps_g, sm("g_up", 128), sggb[:, :])
            gte = L5.get()
            kb.copy("act", gte[:, :], ps_g)
            if l > 0:
                ps = proj_f("rvr", xb)
                raw, hcol = rw_halo["vr"]
                if t == 0:
                    kb.memset("pool", raw[0:32, 0:1], 0.0)
                else:
                    kb.copy("pool", raw[0:32, 0:1], hcol[0:32, 0:1])
                kb.copy("act", raw[0:32, 1:TT + 1], ps)
                kb.copy("pool", hcol[0:32, 0:1], raw[0:32, TT:TT + 1])
                dd = F5.get()
                kb.tt("dve", dd[0:32, :], raw[0:32, 0:TT], raw[0:32, 1:TT + 1], ALU.subtract)
                vrl = B5.get()
                kb.stt(vrl[0:32, :], dd[0:32, :], pc("mu_vr", 32), raw[0:32, 1:TT + 1], ALU.mult, ALU.add)
                ps_v = cx.PB.get()
                kb.mm(ps_v, sm("v_up", 32), vrl[0:32, :])
                vg = F5.get()
                tmp = F5.get()
                sigmoid_into(kb, vg[:, :], ps_v, tmp[:, :], bias=ncol[:, 2:3])
                vf = F5.get()
                kb.dma("sp", vf[:, :], d["vfirst"][:, tsl])
                kb.tt("dve", vf[:, :], vf[:, :], v_[:, :], ALU.subtract)
                kb.tt("dve", vf[:, :], vf[:, :], vg[:, :], ALU.mult)
                kb.tt("dve", v_[:, :], v_[:, :], vf[:, :], ALU.add)
            else:
                kb.dma("sp", d["vfirst"][:, tsl], v_[:, :])
            kx = F5.get()
            kb.ts("dve", kx[:, :], k_[:, :], pc("k_k"), ALU.mult)
            kk = l2norm_f(kx[:, :], M["ones_blk"], 1.0)
            tk = F5.get()
            kb.ts("dve", tk[:, :], a_[:, :], -1.0, ALU.add, pc("k_a"), ALU.mult)
            kmod = L5.get()
            kb.stt(kmod[:, :], tk[:, :], 1.0, k_[:, :], ALU.add, ALU.mult)
            bb = L5.get()
            kb.tt("dve", bb[:, :], kk[:, :], a_[:, :], ALU.mult)
            rk = B5.get()
            kb.stt(rk[:, :], r_[:, :], pc("r_k"), kmod[:, :], ALU.mult, ALU.mult)
            ps_bn = cx.PB.get()
            kb.mm(ps_bn, M["ones_blk"], rk[:, :])
            bonus = L5.get()
            kb.tt("dve", bonus[:, :], ps_bn, v_[:, :], ALU.mult)
            ecw = F5.get()
            kb.act(ecw[:, :], cs[:, :], AF.Exp, scale=-C_W)
            encw = L5.get()
            kb.act(encw[:, :], cs[:, :], AF.Exp, scale=C_W)
            dx = F5.get()
            kb.tt("dve", dx[:, :], cs[:, :], sgw[:, :], ALU.subtract)
            eprev = F5.get()
            kb.act(eprev[:, :], dx[:, :], AF.Exp, scale=-C_W)
            c3 = cs.ap[:, :].rearrange("p (c t) -> p c t", t=64)
            dl = F5.get()
            dl3 = dl.ap[:, :].rearrange("p (c t) -> p c t", t=64)
            cl_bc = c3[:, :, 63:64].broadcast_to([128, TT // 64, 64])
            kb.tt("dve", V(dl3, dl.deps), V(cl_bc, cs.deps), V(c3, cs.deps), ALU.subtract)
            eend = F5.get()
            kb.act(eend[:, :], dl[:, :], AF.Exp, scale=-C_W)
            gam = L5.get()
            kb.copy("pool", gam[:, :], ecw[:, :])
            AR = rw_AR
            AR4 = AR.ap[:, :].rearrange("p (b two i) -> p b two i", two=2, i=128)
            e4 = eprev.ap[:, :].rearrange("p (b i) -> p b i", i=128)
            k4 = kk.ap[:, :].rearrange("p (b i) -> p b i", i=128)
            kb.stt(V(AR4[:, :, 0, :], AR.deps), V(k4, kk.deps), -1.0, V(e4, eprev.deps), ALU.mult, ALU.mult)
            r4 = r_.ap[:, :].rearrange("p (b i) -> p b i", i=128)
            g4 = ecw.ap[:, :].rearrange("p (b i) -> p b i", i=128)
            kb.tt("dve", V(AR4[:, :, 1, :], AR.deps), V(r4, r_.deps), V(g4, ecw.deps), ALU.mult)
            bt = B5.get()
            kb.tt("dve", bt[:, :], bb[:, :], encw[:, :], ALU.mult)
            kt = B5.get()
            kb.tt("dve", kt[:, :], kmod[:, :], encw[:, :], ALU.mult)
            bendT = B5.get()
            kb.tt("dve", bendT[:, :], bb[:, :], eend[:, :], ALU.mult)
            kendT = B5.get()
            kb.tt("dve", kendT[:, :], kmod[:, :], eend[:, :], ALU.mult)
            vbf = B5.get()
            kb.copy("pool", vbf[:, :], v_[:, :])
            yT_ = L5.get()
            for blk in range(4):
                bs = slice(blk * 128, blk * 128 + 128)
                ARb = AR[:, blk * 256:blk * 256 + 256]
                BL.i = 0
                At_tok = transpose_bf(ARb[:, 0:128])
                V_tok = transpose_bf(vbf[:, bs])
                Bend = transpose_bf(bendT[:, bs])
                Kend = transpose_bf(kendT[:, bs])
                psQ = cx.PD[2]
                psY = cx.PD[3]
                WUs = []
                r_SB1, r_SB2, r_PA = [], [], []
                for h in range(2):
                    hp = slice(64 * h, 64 * h + 64)
                    ps1 = cx.PH.get()
                    kb.mm(ps1, bt[hp, bs], ARb[hp, :])
                    SB1 = kb_rhs.get()
                    kb.tt("dve", SB1[:, :], ps1, cx.mf[:, 0:256], ALU.mult)
                    ps2 = cx.PH.get()
                    kb.mm(ps2, kt[hp, bs], ARb[hp, :])
                    SB2 = kb_rhs.get()
                    kb.tt("dve", SB2[:, :], ps2, cx.mf[:, 0:256], ALU.mult)
                    psA = cx.PQ.get()
                    kb.mm(psA, ARb[hp, 0:128], bt[hp, bs])
                    PA = BQ.get()
                    kb.tt("dve", PA[:, :], psA, Mf["L_strict"], ALU.mult)
                    r_SB1.append(SB1)
                    r_SB2.append(SB2)
                    r_PA.append(PA[:, :])
                r_TT = neumann_multi(kb, cx, r_PA, [r_SB1[0][:, 0:128], r_SB1[1][:, 0:128]], BQ, M["ident"])
                for h in range(2):
                    hp = slice(64 * h, 64 * h + 64)
                    SB1, SB2, TTm = r_SB1[h], r_SB2[h], r_TT[h]
                    psX = cx.PQ.get()
                    kb.mm(psX[:, 0:64], SB2[:, 0:128], V_tok[:, hp])
                    Xb = BQ.get()
                    kb.copy("act", Xb[:, 0:64], psX[:, 0:64])
                    psWU = cx.PQ.get()
                    kb.mm(psWU[:, 0:64], TTm, At_tok[:, hp])
                    kb.mm(psWU[:, 64:128], TTm, Xb[:, 0:64])
                    WU = BL.get()
                    kb.copy("act", WU[:, :], psWU)
                    WUs.append(WU)
                    kb.mm(psQ[hp, :], WU[:, 0:64], SB1[:, 128:256])
                    kb.mm(psY[hp, :], WU[:, 64:128], SB1[:, 128:256], start=True, stop=False)
                    kb.mm(psY[hp, :], V_tok[:, hp], SB2[:, 128:256], start=False, stop=False)
                qeff = BL.get()
                kb.tt("dve", qeff[:, :], ARb[:, 128:256], psQ, ALU.add)
                for c in range(2):
                    rs = slice(c * 64, c * 64 + 64)
                    gcn = blk * 2 + c
                    for h in range(2):
                        hp = slice(64 * h, 64 * h + 64)
                        kb.mm(psY[hp, rs], rw_Hbf[hp, :], qeff[hp, rs], start=False, stop=(c == 1))
                    psM = cx.PD[5]
                    for h in range(2):
                        hp = slice(64 * h, 64 * h + 64)
                        kb.mm(psM[hp, 0:64], WUs[h][rs, 0:64], Bend[rs, hp])
                    MT = rw_MT
                    for h in range(2):
                        hp = slice(64 * h, 64 * h + 64)
                        kb.stt(MT[hp, 64 * h:64 * h + 64], Mf["istack"][hp, :], gam[hp, 64 * gcn + 63:64 * gcn + 64],
                               psM[hp, 0:64], ALU.mult, ALU.add)
                    psH = cx.PD[6]
                    kb.mm(psH[:, 0:64], MT[:, :], rw_H32[:, :], start=True, stop=False)
                    for h in range(2):
                        hp = slice(64 * h, 64 * h + 64)
                        kb.mm(psH[hp, 0:64], Bend[rs, hp], WUs[h][rs, 64:128], start=False, stop=False)
                        kb.mm(psH[hp, 0:64], Kend[rs, hp], V_tok[rs, hp], start=False, stop=True)
                    kb.copy("act", rw_Hbf[:, :], psH[:, 0:64])
                    kb.copy("act", rw_H32[:, :], psH[:, 0:64])
                kb.copy("act", yT_[:, bs], psY)
            ps_m = cx.PB.get()
            kb.mm(ps_m, Mf["ones_blk"], yT_[:, :])
            yc = F5.get()
            kb.stt(yc[:, :], ps_m, -1.0 / 64, yT_[:, :], ALU.mult, ALU.add)
            ysq = F5.get()
            kb.act(ysq[:, :], yc[:, :], AF.Square)
            ps_v2 = cx.PB.get()
            kb.mm(ps_v2, Mf["ones_blk"], ysq[:, :])
            tmp = F5.get()
            rstd = F5.get()
            rsqrt_act(kb, rstd[:, :], ps_v2, 1.0 / 64, EPS_R, tmp[:, :])
            kb.stt(yc[:, :], yc[:, :], pc("ln_g"), rstd[:, :], ALU.mult, ALU.mult)
            kb.stt(yc[:, :], yc[:, :], pc("ln_b"), bonus[:, :], ALU.add, ALU.add)
            yo = B5.get()
            kb.tt("dve", yo[:, :], yc[:, :], gte[:, :], ALU.mult)
            kb.dma("sp", d["ydst"](384, 128, t), yo[:, :])

        if "mla" in branches:
            L5.i = 0
            posi = mla_posi
            kb.dma("sp", posi[:, :], d["pos"][:, tsl])
            posf = L5.get()
            kb.copy("dve", posf[0:96, :], posi[:, :])
            ang = L5.get()
            kb.ts("dve", ang[0:96, :], posf[0:96, :], pc("invf", 96), ALU.mult)
            tabs = []
            for phase in (math.pi / 2, 0.0):
                y = F5.get()
                kb.ts("dve", y[0:96, :], ang[0:96, :], phase, ALU.add, 1.0 / (2 * math.pi), ALU.mult)
                ni = mla_ni
                kb.copy("dve", ni[:, :], y[0:96, :])
                nf = F5.get()
                kb.copy("dve", nf[0:96, :], ni[:, :])
                r = F5.get()
                kb.stt(r[0:96, :], nf[0:96, :], -2 * math.pi, ang[0:96, :], ALU.mult, ALU.add)
                if phase != 0.0:
                    kb.ts("dve", r[0:96, :], r[0:96, :], phase, ALU.add)
                m1 = F5.get()
                kb.ts("dve", m1[0:96, :], r[0:96, :], math.pi, ALU.is_gt, -2 * math.pi, ALU.mult)
                kb.tt("dve", r[0:96, :], r[0:96, :], m1[0:96, :], ALU.add)
                kb.ts("dve", m1[0:96, :], r[0:96, :], -math.pi, ALU.is_lt, 2 * math.pi, ALU.mult)
                kb.tt("dve", r[0:96, :], r[0:96, :], m1[0:96, :], ALU.add)
                tab = L5.get()
                kb.act(tab[0:96, :], r[0:96, :], AF.Sin)
                tabs.append(tab)
            C96, S96 = tabs
            kb.ts("dve", S96[0:96, :], S96[0:96, :], pc("rsign", 96), ALU.mult)
            qn = []
            qr = []
            sqs = []
            for kc in range(2):
                ps = proj_f("qlat", xb, rows=128, c0=128 * kc)
                rr = L5.get()
                kb.copy("act", rr[:, :], ps)
                sq = B5.get()
                kb.act(sq[:, :], rr[:, :], AF.Square)
                qr.append(rr)
                sqs.append(sq)
            ps_s = cx.PB.get()
            kb.mm(ps_s, M["ones"], sqs[0][:, :], start=True, stop=False)
            kb.mm(ps_s, M["ones"], sqs[1][:, :], start=False, stop=True)
            tmp = F5.get()
            rstd = F5.get()
            rsqrt_act(kb, rstd[:, :], ps_s, 1.0 / 256, EPS_N, tmp[:, :])
            for kc in range(2):
                q_ = B5.get()
                kb.tt("dve", q_[:, :], qr[kc][:, :], rstd[:, :], ALU.mult)
                qn.append(q_)
            ps = proj_f("ckv", xb)
            cr = L5.get()
            kb.copy("act", cr[:, :], ps)
            sq = B5.get()
            kb.act(sq[:, :], cr[:, :], AF.Square)
            ps_s = cx.PB.get()
            kb.mm(ps_s, M["ones"], sq[:, :])
            tmp = F5.get()
            rstd = F5.get()
            rsqrt_act(kb, rstd[:, :], ps_s, 1.0 / 128, EPS_N, tmp[:, :])
            cn = B5.get()
            kb.tt("dve", cn[:, :], cr[:, :], rstd[:, :], ALU.mult)
            psK = cx.PB.get()
            proj_f("krope", xb, rows=32, out=psK[64:96, :])
            t1 = F5.get()
            kb.tt("dve", t1[64:96, :], psK[64:96, :], C96[64:96, :], ALU.mult)
            psK2 = cx.PB.get()
            proj_f("krope_sw", xb, rows=32, out=psK2[64:96, :])
            t2 = F5.get()
            kb.tt("dve", t2[64:96, :], psK2[64:96, :], S96[64:96, :], ALU.mult)
            krot = B5.get()
            kb.tt("dve", krot[64:96, :], t1[64:96, :], t2[64:96, :], ALU.add)
            for hh in range(2):
                kb.dma("sp", d["KT"][hh, 64:96, tsl], krot[64:96, :])
                psn = cx.PB.get()
                kb.mm(psn[0:64, :], ukg[:, hh, :], cn[:, :])
                kn_ = B5.get()
                kb.copy("act", kn_[0:64, :], psn[0:64, :])
                kb.dma("sp", d["KT"][hh, 0:64, tsl], kn_[0:64, :])
                psP = cx.PB.get()
                kb.mm(psP[0:96, :], uqg[:, hh, 0, 0, :], qn[0][:, :], start=True, stop=False)
                kb.mm(psP[0:96, :], uqg[:, hh, 1, 0, :], qn[1][:, :], start=False, stop=True)
                t1 = F5.get()
                kb.tt("dve", t1[0:96, :], psP[0:96, :], C96[0:96, :], ALU.mult)
                psS = cx.PB.get()
                kb.mm(psS[0:96, :], uqg[:, hh, 0, 1, :], qn[0][:, :], start=True, stop=False)
                kb.mm(psS[0:96, :], uqg[:, hh, 1, 1, :], qn[1][:, :], start=False, stop=True)
                t2 = F5.get()
                kb.tt("dve", t2[0:96, :], psS[0:96, :], S96[0:96, :], ALU.mult)
                qb_ = B5.get()
                kb.tt("dve", qb_[0:96, :], t1[0:96, :], t2[0:96, :], ALU.add)
                kb.dma("sp", d["QT"][hh, :, tsl], qb_[0:96, :])
            for blk in range(4):
                bs = slice(blk * 128, blk * 128 + 128)
                psV = cx.PQ.get()
                kb.mm(psV, cn[:, bs], uvg[:, :])
                vt = mla_vt[blk % 2]
                kb.op("act", (lambda o_, i_: (lambda h: h.activation(o_, i_, AF.Copy)))(
                    vt.ap[:, :, 0:64], psV.ap.rearrange("p (h e) -> p h e", e=64)), reads=[psV], writes=[vt])
                kb.dma("sp", d["VT"][:, :, t * 4 + blk, :], vt[:, :, :])
    kb.flush()
    st.close()
    return cx


def build_p1b(kb, S, d):
    import os as _os
    if "build_p1b" in _os.environ.get("DBG_SKIP", "").split(","):
        return None

    st = contextlib.ExitStack()
    nt = S // TT
    nb = S // 128
    banks = [kb.psum("ab%d" % i, [128, 512], F32, st) for i in range(8)]
    KTs = kb.sbuf("a_KT", [96, S], BF16, st)
    Vx = kb.sbuf("a_Vx", [128, nb, 65], BF16, st)
    onesf = kb.sbuf("a_ones", [128, 64], F32, st)
    kb.memset("pool", onesf[:, :], 1.0)
    dms = []
    for dd in range(4):
        m = kb.sbuf("a_dm%d" % dd, [128, TT], BF16, st)
        kb.memset("pool", m[:, :], 1.0)
        ma = m.ap[:, :]
        kb.op("pool", (lambda a, b: (lambda h: h.affine_select(a, a, pattern=[[1, TT]], compare_op=ALU.is_ge,
                                                               fill=0.0, base=b, channel_multiplier=-1)))(ma, -128 * dd),
              reads=[m], writes=[m])
        dms.append(m)
    Qts = [kb.sbuf("a_Q%d" % i, [96, TT], BF16, st) for i in range(2)]
    Ps = Pool(kb, "a_P", 4, [128, TT], BF16, stack=st)
    rsb = kb.sbuf("a_rs", [65, TT], F32, st)
    rbs = Pool(kb, "a_rb", 2, [64, TT], F32, stack=st)
    yos = Pool(kb, "a_yo", 2, [64, TT], BF16, stack=st)
    si = 0
    for hh in range(2):
        for c0 in range(0, S, 2048):
            c1 = min(S, c0 + 2048)
            kb.dma("sp", KTs[:, c0:c1], d["KT"][hh, :, c0:c1])
        for b0 in range(0, nb, 32):
            b1 = min(nb, b0 + 32)
            kb.dma("sp", Vx[:, b0:b1, :], d["VT"][:, hh, b0:b1, :])
        for t in range(nt):
            tsl = slice(t * TT, (t + 1) * TT)
            Qt = Qts[t % 2]
            kb.dma("sp", Qt[:, :], d["QT"][hh, :, tsl])
            psO = banks[4 + (t % 2)]
            nkb = 4 * (t + 1)

            def scores(kb_):
                nonlocal si
                dd = kb_ - 4 * t
                c0 = 128 * dd if dd > 0 else 0
                psS = banks[si % 3]
                si += 1
                kb.mm(psS[:, c0:TT], KTs[:, kb_ * 128:kb_ * 128 + 128], Qt[:, c0:TT])
                return psS, c0, dd
            nxt = scores(0)
            for kb_ in range(nkb):
                psS, c0, dd = nxt
                if kb_ + 1 < nkb:
                    nxt = scores(kb_ + 1)
                P = Ps.get()
                kb.act(P[:, c0:TT], psS[:, c0:TT], AF.Exp)
                if dd >= 0:
                    kb.tt("dve", P[:, c0:TT], P[:, c0:TT], dms[dd][:, c0:TT], ALU.mult)
                kb.mm(psO[0:65, c0:TT], Vx[:, kb_, :], P[:, c0:TT], start=(kb_ == 0), stop=(kb_ == nkb - 1))
            kb.copy("act", rsb[64:65, :], psO[64:65, :])
            psB = banks[6]
            kb.mm(psB[0:64, :], onesf[64:65, 0:64], rsb[64:65, :])
            rb = rbs.get()
            kb.recip(rb[:, :], psB[0:64, :])
            yo = yos.get()
            kb.tt("dve", yo[:, :], psO[0:64, :], rb[:, :], ALU.mult)
            kb.dma("sp", d["ydst"](hh * 64, 64, t), yo[:, :])
    kb.flush()
    st.close()


def pack_p2(inp, l):
    P = Packer(128)
    for nm in ("ln1_g", "ln1_b", "ln2_g", "ln2_b"):
        P.add(nm, inp[nm][l].reshape(8, 128).T)
    P.add("rbias", np.repeat(inp["router_bias"][l][None, :], 128, 0))
    P.add("rw", inp["router_w"].reshape(8, 128, 16).transpose(1, 0, 2).reshape(128, 128))
    sel = np.zeros((16, 16, 128), np.float32)
    for e in range(16):
        sel[e, e, :] = 1.0
    P.add("sel", np.concatenate([sel.reshape(16, 2048), np.zeros((112, 2048), np.float32)], 0))
    return P


def ln_fmajor(kb, cx, h32, gname, bname, pcs, F5, EPS, out_fn):
    Mf = cx.Mf
    ps_m = cx.PB.get()
    for j in range(8):
        kb.mm(ps_m, Mf["ones"], h32[:, j, :], start=(j == 0), stop=(j == 7))
    mean = F5.get()
    kb.act(mean[:, :], ps_m, AF.Copy, scale=1.0 / 1024)
    for j in range(8):
        kb.tt("pool", h32[:, j, :], h32[:, j, :], mean[:, :], ALU.subtract)
    ps_v = cx.PB.get()
    for j in range(8):
        sq = F5.get()
        kb.act(sq[:, :], h32[:, j, :], AF.Square)
        kb.mm(ps_v, Mf["ones"], sq[:, :], start=(j == 0), stop=(j == 7))
    tmp = F5.get()
    rstd = F5.get()
    rsqrt_act(kb, rstd[:, :], ps_v, 1.0 / 1024, EPS, tmp[:, :])
    for j in range(8):
        o = F5.get()
        kb.tt("dve", o[:, :], h32[:, j, :], rstd[:, :], ALU.mult)
        kb.ts("dve", o[:, :], o[:, :], pcs(gname, j), ALU.mult, pcs(bname, j), ALU.add)
        out_fn(j, o)


def build_p2(kb, T, l, d):
    import os as _os
    if "build_p2" in _os.environ.get("DBG_SKIP", "").split(","):
        return None

    nt = T // TT
    Po = d["P2off"]

    st = contextlib.ExitStack()
    cx = Ctx()
    setup_common(kb, cx, d["masks"], st)
    Wg = kb.sbuf("Wg", [128, 8, 4096], BF16, st)
    for k in range(8):
        for c0 in range(0, 4096, 1024):
            kb.dma("pool", Wg[:, k, c0:c0 + 1024], d["Wgate"][128 * k:128 * k + 128, c0:c0 + 1024])
    Wb = kb.sbuf("Wb", [128, 4, 4, 1024], BF16, st)
    for n in range(4):
        for kc in range(4):
            kb.dma("pool", Wb[:, n, kc, :], d["Wb"][n, 128 * kc:128 * kc + 128, :])
    F5 = Pool(kb, "p2f", 8, [128, TT], F32, stack=st)
    yst = Pool(kb, "p2ys", 12, [128, TT], BF16, stack=st)
    OH = kb.sbuf("p2oh", [128, 4], F32, st)
    kb.dma("sp", OH[:, :], d["onehot"])
    xbfs = [kb.sbuf("p2x%d" % i, [128, 8, TT], BF16, st) for i in range(2)]
    ybfs = [kb.sbuf("p2y%d" % i, [128, 16, TT], BF16, st) for i in range(2)]
    m32 = kb.sbuf("p2m", [128, 8, TT], F32, st)
    mbf = kb.sbuf("p2mb", [128, 8, TT], BF16, st)

    def load_x(t):
        for k in range(8):
            kb.dma("pool", xbfs[t % 2][:, k, :], d["xsrc2"](k, t))
    load_x(0)
    for t in range(nt):
        tsl = slice(t * TT, (t + 1) * TT)
        if t + 1 < nt:
            load_x(t + 1)
        xbf = xbfs[t % 2]
        ybf = ybfs[t % 2]
        for c in range(16):
            n_, hg_ = c // 4, c % 4
            r0 = hg_ * 512 + n_ * 128
            for jq in range(4):
                yq = yst.get()
                kb.dma("sp", yq[:, :], d["ysrc"](r0, jq, t))
                if jq == 0:
                    kb.ts("dve", ybf[:, c, :], yq[:, :], OH[:, 0:1], ALU.mult)
                else:
                    kb.stt(ybf[:, c, :], yq[:, :], OH[:, jq:jq + 1], ybf[:, c, :], ALU.mult, ALU.add)
        for n in range(4):
            for j in range(8):
                ps_g = cx.PB.get()
                for k in range(8):
                    kb.mm(ps_g, Wg[:, k, n * 1024 + 128 * j:n * 1024 + 128 * j + 128], xbf[:, k, :],
                          start=(k == 0), stop=(k == 7))
                sg = F5.get()
                kb.act(sg[:, :], ps_g, AF.Sigmoid)
                ps_p = cx.banks[2 + (j % 2)][:, :]
                for kc in range(4):
                    kb.mm(ps_p, Wb[:, n, kc, 128 * j:128 * j + 128], ybf[:, 4 * n + kc, :],
                          start=(kc == 0), stop=(kc == 3))
                if n == 0:
                    kb.tt("dve", m32[:, j, :], sg[:, :], ps_p, ALU.mult)
                else:
                    tq = F5.get()
                    kb.tt("dve", tq[:, :], sg[:, :], ps_p, ALU.mult)
                    kb.tt("pool", m32[:, j, :], m32[:, j, :], tq[:, :], ALU.add)
        for j in range(8):
            kb.copy("act", mbf[:, j, :], m32[:, j, :])
            kb.dma("sp", d["mT"][128 * j:128 * j + 128, tsl], mbf[:, j, :])
    kb.flush()
    st.close()

    st = contextlib.ExitStack()
    cx = Ctx()
    setup_common(kb, cx, d["masks"], st)
    PC = kb.sbuf("PC2", [128, d["NP2"]], F32, st)
    kb.dma("sp", PC[:, :], d["PC2"])

    def pcs(name, j):
        o, w = Po[name]
        return PC[:, o + j:o + j + 1]
    cst = kb.sbuf("cst2", [128, 2], F32, st)
    kb.memset("pool", cst[:, 0:1], LN_EPS)
    EPS = cst[:, 0:1]
    wst = Pool(kb, "wst3", 3, [128, 512], F32, stack=st)
    Wo = kb.sbuf("Wo", [128, 8, 1024], BF16, st)
    for k in range(8):
        for c0 in range(0, 1024, 512):
            s_ = wst.get()
            kb.dma("sp", s_[:, :], d["Wout"][128 * k:128 * k + 128, c0:c0 + 512])
            kb.copy("pool", Wo[:, k, c0:c0 + 512], s_[:, :])
    F5 = Pool(kb, "p2g", 10, [128, TT], F32, stack=st)
    mbf = kb.sbuf("p2mb2", [128, 8, TT], BF16, st)
    x32 = kb.sbuf("p2x32", [128, 8, TT], F32, st)
    h32 = kb.sbuf("p2h32", [128, 8, TT], F32, st)
    for t in range(nt):
        tsl = slice(t * TT, (t + 1) * TT)
        for k in range(8):
            kb.dma("sp", mbf[:, k, :], d["mT"][128 * k:128 * k + 128, tsl])
            kb.dma("sp", x32[:, k, :], d["xsrc2"](k, t))
        for j in range(8):
            ps = cx.PB.get()
            for k in range(8):
                kb.mm(ps, Wo[:, k, 128 * j:128 * j + 128], mbf[:, k, :], start=(k == 0), stop=(k == 7))
            kb.stt(h32[:, j, :], x32[:, j, :], ALPHA, ps, ALU.mult, ALU.add)

        def out1(j, o, tsl=tsl):
            kb.dma("sp", d["x1T"][128 * j:128 * j + 128, tsl], o[:, :])
        ln_fmajor(kb, cx, h32, "ln1_g", "ln1_b", pcs, F5, EPS, out1)
    kb.flush()
    st.close()

    HT = min(T, 2048)
    for half in range(T // HT):
        st = contextlib.ExitStack()
        cx = Ctx()
        setup_common(kb, cx, d["masks"], st)
        Mf = cx.Mf
        PC = kb.sbuf("PC2b", [128, d["NP2"]], F32, st)
        kb.dma("sp", PC[:, :], d["PC2"])
        PCb = kb.sbuf("PC2bb", [16, 2048], BF16, st)
        o_sel = Po["sel"][0]
        kb.copy("pool", PCb[:, :], PC[0:16, o_sel:o_sel + 2048])

        def pcs(name, j):
            o, w = Po[name]
            return PC[:, o + j:o + j + 1]
        cst = kb.sbuf("cst3", [128, 2], F32, st)
        kb.memset("pool", cst[:, 0:1], LN_EPS)
        EPS = cst[:, 0:1]
        o_rw = Po["rw"][0]
        o_rb = Po["rbias"][0]
        wst = Pool(kb, "wst4", 4, [128, 512], F32, stack=st)
        F5 = Pool(kb, "p2h", 8, [128, TT], F32, stack=st)
        S16 = Pool(kb, "s16", 24, [128, 16], F32, stack=st)
        x1bf = kb.sbuf("x1bf", [128, 8, HT], BF16, st)
        acc = kb.sbuf("acc", [128, 8, HT], F32, st)
        cTb = kb.sbuf("cTb", [16, HT], BF16, st)
        nth = HT // TT
        for t in range(nth):
            g0 = half * HT + t * TT
            rps = [cx.banks[b][:, 0:16] for b in range(4)]
            for k in range(8):
                s_ = wst.get()
                kb.dma("sp", s_[:, :], d["x1T"][128 * k:128 * k + 128, g0:g0 + TT])
                kb.copy("pool", x1bf[:, k, t * TT:(t + 1) * TT], s_[:, :])
                for blk in range(4):
                    kb.mm(rps[blk], s_[:, blk * 128:blk * 128 + 128], PC[:, o_rw + 16 * k:o_rw + 16 * k + 16],
                          start=(k == 0), stop=(k == 7))
            for blk in range(4):
                e = S16.get()
                kb.act(e[:, :], rps[blk], AF.Exp, scale=-1.0)
                kb.ts("dve", e[:, :], e[:, :], 1.0, ALU.add)
                sc = S16.get()
                kb.recip(sc[:, :], e[:, :])
                bi = S16.get()
                kb.tt("dve", bi[:, :], sc[:, :], PC[:, o_rb:o_rb + 16], ALU.add)
                b4 = bi.ap[:, :].rearrange("p (g e) -> p g e", e=4)
                p6 = S16.get()
                p6v = p6.ap[:, :].rearrange("p (g s) -> p g s", s=4)
                p6b = S16.get()
                p6bv = p6b.ap[:, :].rearrange("p (g s) -> p g s", s=4)
                pairs = [(0, 1), (0, 2), (0, 3), (1, 2), (1, 3), (2, 3)]
                for pi, (i_, j_) in enumerate(pairs):
                    dst = p6v[:, :, pi:pi + 1] if pi < 4 else p6bv[:, :, pi - 4:pi - 3]
                    dd = p6 if pi < 4 else p6b
                    kb.tt("dve", V(dst, dd.deps), V(b4[:, :, i_:i_ + 1], bi.deps), V(b4[:, :, j_:j_ + 1], bi.deps), ALU.add)
                gs = S16.get()
                gsv = gs.ap[:, 0:4].rearrange("p (g o) -> p g o", o=1)
                kb.tt("dve", V(gsv, gs.deps), V(p6v[:, :, 0:1], p6.deps), V(p6v[:, :, 1:2], p6.deps), ALU.max)
                kb.tt("dve", V(gsv, gs.deps), V(gsv, gs.deps), V(p6v[:, :, 2:3], p6.deps), ALU.max)
                kb.tt("dve", V(gsv, gs.deps), V(gsv, gs.deps), V(p6v[:, :, 3:4], p6.deps), ALU.max)
                kb.tt("dve", V(gsv, gs.deps), V(gsv, gs.deps), V(p6bv[:, :, 0:1], p6b.deps), ALU.max)
                kb.tt("dve", V(gsv, gs.deps), V(gsv, gs.deps), V(p6bv[:, :, 1:2], p6b.deps), ALU.max)
                gm = S16.get()
                kb.op("dve", (lambda o_, i_: (lambda h: h.tensor_reduce(o_, i_, AX.X, ALU.max)))(gm.ap[:, 0:1], gs.ap[:, 0:4]),
                      reads=[gs], writes=[gm])
                ing = S16.get()
                kb.ts("dve", ing[:, 0:4], gs[:, 0:4], gm[:, 0:1], ALU.is_ge)
                off = S16.get()
                kb.ts("dve", off[:, 0:4], ing[:, 0:4], 1e30, ALU.mult, -1e30, ALU.add)
                mk = S16.get()
                mk4 = mk.ap[:, :].rearrange("p (g e) -> p g e", e=4)
                ing_bc = ing.ap[:, 0:4].rearrange("p (g o) -> p g o", o=1).broadcast_to([128, 4, 4])
                off_bc = off.ap[:, 0:4].rearrange("p (g o) -> p g o", o=1).broadcast_to([128, 4, 4])
                kb.tt("dve", V(mk4, mk.deps), V(b4, bi.deps), V(ing_bc, ing.deps), ALU.mult)
                kb.tt("dve", V(mk4, mk.deps), V(mk4, mk.deps), V(off_bc, off.deps), ALU.add)
                t8 = S16.get()
                kb.op("dve", (lambda o_, i_: (lambda h: h.max(o_, i_)))(t8.ap[:, 0:8], mk.ap[:, :]), reads=[mk], writes=[t8])
                sel = S16.get()
                kb.ts("dve", sel[:, :], mk[:, :], t8[:, 1:2], ALU.is_ge)
                ws = S16.get()
                kb.tt("dve", ws[:, :], sel[:, :], sc[:, :], ALU.mult)
                den = S16.get()
                kb.op("dve", (lambda o_, i_: (lambda h: h.tensor_reduce(o_, i_, AX.X, ALU.add)))(den.ap[:, 0:1], ws.ap[:, :]),
                      reads=[ws], writes=[den])
                kb.recip(den[:, 1:2], den[:, 0:1])
                cmb = S16.get()
                kb.ts("dve", cmb[:, :], ws[:, :], den[:, 1:2], ALU.mult)
                ps_t = cx.banks[4][0:16, 0:128]
                kb.mm(ps_t, cmb[:, :], Mf["ident"])
                kb.copy("act", cTb[:, t * TT + blk * 128:t * TT + blk * 128 + 128], ps_t)
        st2 = contextlib.ExitStack()
        Wgs = [kb.sbuf("Wge%d" % i, [128, 8, 512], BF16, st2) for i in range(2)]
        Wus = [kb.sbuf("Wue%d" % i, [128, 8, 512], BF16, st2) for i in range(2)]
        Wds = [kb.sbuf("Wde%d" % i, [128, 4, 1024], BF16, st2) for i in range(2)]
        cbcs = Pool(kb, "cbc", 2, [128, TT], BF16, stack=st2)
        acts = Pool(kb, "actb", 8, [128, TT], BF16, stack=st2)
        gi = 0

        def load_expert(e):
            Wg_, Wu_, Wd_ = Wgs[e % 2], Wus[e % 2], Wds[e % 2]
            for k in range(8):
                kb.dma("pool", Wg_[:, k, :], d["moe_g"][e, 128 * k:128 * k + 128, :])
                kb.dma("pool", Wu_[:, k, :], d["moe_u"][e, 128 * k:128 * k + 128, :])
            for fc in range(4):
                kb.dma("pool", Wd_[:, fc, :], d["moe_d"][e, 128 * fc:128 * fc + 128, :])
        load_expert(0)
        for e in range(16):
            Wg_, Wu_, Wd_ = Wgs[e % 2], Wus[e % 2], Wds[e % 2]
            if e + 1 < 16:
                load_expert(e + 1)
            for t in range(nth):
                tl = slice(t * TT, (t + 1) * TT)
                ps_c = cx.banks[6][:, :]
                kb.mm(ps_c, PCb[0:16, 128 * e:128 * e + 128], cTb[:, tl])
                cbc = cbcs.get()
                kb.copy("act", cbc[:, :], ps_c)
                at = []
                for fc in range(4):
                    ps_g = cx.banks[gi % 2][:, :]
                    ps_u = cx.banks[2 + gi % 2][:, :]
                    gi += 1
                    for k in range(8):
                        kb.mm(ps_g, Wg_[:, k, 128 * fc:128 * fc + 128], x1bf[:, k, tl], start=(k == 0), stop=(k == 7))
                    for k in range(8):
                        kb.mm(ps_u, Wu_[:, k, 128 * fc:128 * fc + 128], x1bf[:, k, tl], start=(k == 0), stop=(k == 7))
                    r_ = F5.get()
                    kb.act(r_[:, :], ps_g, AF.Silu)
                    kb.tt("dve", r_[:, :], r_[:, :], ps_u, ALU.mult)
                    a_ = acts.get()
                    kb.tt("dve", a_[:, :], r_[:, :], cbc[:, :], ALU.mult)
                    at.append(a_)
                for j in range(8):
                    ps_d = cx.banks[4 + j % 2][:, :]
                    for fc in range(4):
                        kb.mm(ps_d, Wd_[:, fc, 128 * j:128 * j + 128], at[fc][:, :], start=(fc == 0), stop=(fc == 3))
                    if e == 0:
                        kb.copy("dve", acc[:, j, tl], ps_d)
                    else:
                        kb.tt("dve", acc[:, j, tl], acc[:, j, tl], ps_d, ALU.add)
        kb.flush()
        st2.close()
        x32 = kb.sbuf("b_x32", [128, 8, TT], F32, st)
        for t in range(nth):
            g0 = half * HT + t * TT
            tl = slice(t * TT, (t + 1) * TT)
            for k in range(8):
                kb.dma("sp", x32[:, k, :], d["x1T"][128 * k:128 * k + 128, g0:g0 + TT])
                kb.stt(x32[:, k, :], x32[:, k, :], ALPHA, acc[:, k, tl], ALU.mult, ALU.add)

            def out2(j, o, g0=g0):
                kb.dma("sp", d["odst"](j, g0 // TT), o[:, :])
            ln_fmajor(kb, cx, x32, "ln2_g", "ln2_b", pcs, F5, EPS, out2)
        kb.flush()
        st.close()


def _din(nc, name, arr):
    dt = I32 if arr.dtype == np.int32 else F32
    t = nc.dram_tensor(name, list(arr.shape), dt, kind="ExternalInput")
    return V(t.ap(), [Dep()])


def _dout(nc, name, shape, dt=F32):
    t = nc.dram_tensor(name, list(shape), dt, kind="ExternalOutput")
    return V(t.ap(), [Dep()])


def _dint(nc, name, shape, dt):
    t = nc.dram_tensor(name, list(shape), dt)
    return V(t.ap(), [Dep()])


GROUPS = [[0, 1, 2, 3], [4, 5, 6, 7]]


def kernel(**inputs):
    inp = {k: np.asarray(v) for k, v in inputs.items()}
    x = inp["x"]
    B, S = x.shape[0], x.shape[1]
    T = S // 4
    xT = [np.ascontiguousarray(x[b].T) for b in range(B)]
    masks = masks_host()
    scm = scanmask_host()
    packs = [[pack_p1a(inp, l, hg) for hg in range(4)] for l in range(2)]
    arrs = [[(W.build(), P.build(), M.build()) for (W, P, M) in packs[l]] for l in range(2)]
    p2 = [pack_p2(inp, l) for l in range(2)]
    p2a = [p.build() for p in p2]
    posrep = [np.ascontiguousarray(np.repeat(inp["positions"][b][None, :], 96, 0)).astype(np.int32) for b in range(B)]
    common = {"Wgate0": np.ascontiguousarray(inp["w_in"][0][:, O_GATES:O_GATES + 4096]),
              "Wgate1": np.ascontiguousarray(inp["w_in"][1][:, O_GATES:O_GATES + 4096]),
              "Wb": np.ascontiguousarray(inp["w_branch"]), "Wout": np.ascontiguousarray(inp["w_out"]),
              "PC2_0": p2a[0], "PC2_1": p2a[1], "moe_g": np.ascontiguousarray(inp["moe_w_gate"]),
              "moe_u": np.ascontiguousarray(inp["moe_w_up"]), "moe_d": np.ascontiguousarray(inp["moe_w_down"]),
              "masks": masks, "scanmask": scm}
    in_maps = []
    for b in range(B):
        for r in range(4):
            oh = np.zeros((128, 4), np.float32)
            oh[:, r] = 1.0
            m = {"xT": xT[b], "xq": np.ascontiguousarray(xT[b][:, r * T:(r + 1) * T]), "pos": posrep[b], "onehot": oh}
            for l in range(2):
                m["W%d" % l], m["PC%d" % l], m["SM%d" % l] = arrs[l][r]
            m.update(common)
            in_maps.append({"in_" + k: v for k, v in m.items()})

    nc = bass.Bass("TRN2", target_bir_lowering=False)
    kb = KB(nc)
    di = {k[3:]: _din(nc, k, v) for k, v in in_maps[0].items()}
    CW = 1024
    nyc = S // CW
    yTc = _dint(nc, "yTc", [nyc, 512, CW], BF16)
    ygc = _dint(nc, "ygc", [nyc, 2048, CW], BF16)
    ntq = T // TT
    x2c = _dint(nc, "x2c", [ntq, 2, 512, TT], F32)
    xgc = _dint(nc, "xgc", [ntq, 2, 2048, TT], F32)
    vfirst = _dint(nc, "vfirst_s", [128, S], F32)
    oT = _dout(nc, "oT", [1024, T])
    scratch = {"QT": _dint(nc, "QT", [2, 96, S], BF16), "KT": _dint(nc, "KT", [2, 96, S], BF16),
               "VT": _dint(nc, "VT", [128, 2, S // 128, 65], BF16),
               "x1T": _dint(nc, "x1T", [1024, T], F32), "mT": _dint(nc, "mT", [1024, T], BF16)}

    def ydst(row0, nrows, t):
        c, o = divmod(t * TT, CW)
        return yTc[c][row0:row0 + nrows, o:o + TT]

    def ysrc(r0, jq, t):
        c, o = divmod(jq * T + t * TT, CW)
        return ygc[c][r0:r0 + 128, o:o + TT]

    def x2_local(k, t):
        return x2c[t][k // 4][(k % 4) * 128:(k % 4) * 128 + 128, :]

    def x_gathered(k, tt):
        q, tl = divmod(tt, ntq)
        return xgc[tl][k // 4][q * 512 + (k % 4) * 128:q * 512 + (k % 4) * 128 + 128, :]

    for l in range(2):
        W0, P0, M0 = packs[l][0]
        d = {"pos": di["pos"], "W": di["W%d" % l], "PC": di["PC%d" % l], "SM": di["SM%d" % l],
             "masks": di["masks"], "scanmask": di["scanmask"], "ydst": ydst, "vfirst": vfirst}
        d.update(scratch)
        d.update(Woff=W0.off, Poff=P0.off, Moff=M0.off, NW=arrs[l][0][0].shape[1], NP=arrs[l][0][1].shape[1],
                 NM=arrs[l][0][2].shape[1])
        if l == 0:
            d["xsrc"] = lambda k, t: di["xT"][128 * k:128 * k + 128, t * TT:(t + 1) * TT]
        else:
            d["xsrc"] = x_gathered
        build_p1a(kb, S, l, d)
        build_p1b(kb, S, d)
        for c in range(nyc):
            kb.coll("AllGather", ygc[c], yTc[c], GROUPS)
        d2 = {"ysrc": ysrc, "onehot": di["onehot"], "Wgate": di["Wgate%d" % l], "Wb": di["Wb"][l], "Wout": di["Wout"][l],
              "PC2": di["PC2_%d" % l], "moe_g": di["moe_g"][l], "moe_u": di["moe_u"][l], "moe_d": di["moe_d"][l],
              "masks": di["masks"], "x1T": scratch["x1T"], "mT": scratch["mT"],
              "P2off": p2[l].off, "NP2": p2a[l].shape[1]}
        if l == 0:
            d2["xsrc2"] = lambda k, t: di["xq"][128 * k:128 * k + 128, t * TT:(t + 1) * TT]
            d2["odst"] = lambda j, t: x2_local(j, t)
        else:
            d2["xsrc2"] = x2_local
            d2["odst"] = lambda j, t: oT[128 * j:128 * j + 128, t * TT:(t + 1) * TT]
        build_p2(kb, T, l, d2)
        if l == 0:
            for t_ in range(ntq):
                for h_ in range(2):
                    kb.coll("AllGather", xgc[t_][h_], x2c[t_][h_], GROUPS)
    kb.flush()
    kb.close()
    res = run_bass_kernel_spmd(nc, in_maps, core_ids=list(range(len(in_maps))))
    out = np.empty((B, S, 1024), np.float32)
    for b in range(B):
        for q in range(4):
            out[b, q * T:(q + 1) * T, :] = res.results[b * 4 + q]["oT"].T
    return out
```

```python
import contextlib
import math
import numpy as np
import ml_dtypes
import concourse.bass as bass
import concourse.mybir as mybir
from concourse.bass_utils import run_bass_kernel_spmd

F32 = mybir.dt.float32
BF16 = mybir.dt.bfloat16
I32 = mybir.dt.int32
ALU = mybir.AluOpType
AF = mybir.ActivationFunctionType
AX = mybir.AxisListType

D_MODEL = 1024
IN_COLS = 9912
TT = 512
NKC = 8


class Dep:
    __slots__ = ("w", "r", "wd")

    def __init__(self):
        self.w = None
        self.r = []
        self.wd = []


class V:
    __slots__ = ("ap", "deps")

    def __init__(self, ap, deps):
        self.ap = ap
        self.deps = deps

    def __getitem__(self, k):
        return V(self.ap[k], self.deps)

    def v(self, ap):
        return V(ap, self.deps)


class Eng:
    def __init__(self, name):
        self.name = name
        self.count = 0
        self.ops = []
        self.waited = {}


class KB:
    def __init__(self, nc):
        self.nc = nc
        self.stack = contextlib.ExitStack()
        self.E = {n: Eng(n) for n in ("pe", "act", "dve", "pool", "sp")}
        import os as _os2
        self.ndma = int(_os2.environ.get("DBG_NDMA", "32"))
        self.dma_state = [0] * self.ndma
        self.dma_next = 0
        self.n_ops = 0
        self.out_tokens = []
        import os as _os
        self.limit = int(_os.environ["DBG_LIMIT"]) if _os.environ.get("DBG_LIMIT") else None
        self.dump = int(_os.environ["DBG_DUMP"]) if _os.environ.get("DBG_DUMP") else None
        self.nosame = set(_os.environ.get("DBG_NOSAME", "").split(","))

    def _nm(self, name):
        self._uid = getattr(self, "_uid", 0) + 1
        return "%s_u%d" % (name, self._uid)

    def sbuf(self, name, shape, dt, stack=None):
        t = (stack or self.stack).enter_context(self.nc.sbuf_tensor(self._nm(name), list(shape), dt))
        return V(t[:] if False else t, [Dep()])

    def psum(self, name, shape, dt=F32, stack=None):
        t = (stack or self.stack).enter_context(self.nc.psum_tensor(self._nm(name), list(shape), dt))
        return V(t, [Dep()])

    def dram(self, name, shape, dt, kind="Internal"):
        t = self.nc.dram_tensor(name, list(shape), dt, kind=kind)
        return V(t.ap(), [Dep()])

    @staticmethod
    def _deps(reads, writes):
        out = []
        for d in reads:
            if d.w is not None:
                out.append(d.w)
            out.extend(d.wd)
        for d in writes:
            if d.w is not None:
                out.append(d.w)
            out.extend(d.wd)
            out.extend(d.r)
        return out

    def _waits(self, e, reads, writes, skip):
        need = {}
        for (pn, pv) in self._deps(reads, writes):
            if pn == skip:
                continue
            if need.get(pn, 0) < pv:
                need[pn] = pv
        waits = []
        for pn, pv in need.items():
            if e.waited.get(pn, 0) < pv:
                e.waited[pn] = pv
                waits.append((pn, pv))
        return waits

    @staticmethod
    def _mark(tok, reads, writes):
        for d in reads:
            d.r.append(tok)
            if len(d.r) > 64:
                last = {}
                for (n, v) in d.r:
                    if last.get(n, 0) < v:
                        last[n] = v
                d.r = list(last.items())
        is_async = tok[0].startswith("dma") or tok[0] == "coll"
        for d in writes:
            if is_async:
                d.wd.append(tok)
                if len(d.wd) > 48:
                    last = {}
                    for (n, v) in d.wd:
                        if last.get(n, 0) < v:
                            last[n] = v
                    d.wd = list(last.items())
            else:
                d.w = tok
                d.wd = []
            d.r = []

    def op(self, eng, fn, reads=(), writes=()):
        if self.limit is not None and self.n_ops >= self.limit:
            return None
        if self.dump is not None and abs(self.n_ops - self.dump) <= 3:
            import sys as _s
            f = _s._getframe(1)
            chain = []
            while f is not None and len(chain) < 4:
                chain.append("%s:%d" % (f.f_code.co_name, f.f_lineno))
                f = f.f_back
            print("OP", self.n_ops, eng, chain)
        rd = [d for v in reads for d in v.deps]
        wd = [d for v in writes for d in v.deps]
        e = self.E[eng]
        skip = eng if (eng == "pe" or eng in self.nosame) else None
        waits = self._waits(e, rd, wd, skip)
        e.count += 1
        tok = (eng, e.count)
        e.ops.append((waits, fn, None))
        self._mark(tok, rd, wd)
        self.n_ops += 1
        return tok

    def dma(self, q, out, in_, **kw):
        if self.limit is not None and self.n_ops >= self.limit:
            return None
        rd = list(in_.deps)
        wd = list(out.deps)
        e = self.E[q]
        waits = self._waits(e, rd, wd, None)
        s = self.dma_next
        self.dma_next = (s + 1) % self.ndma
        prev = self.dma_state[s]
        name = "dma%d" % s
        if prev > 0 and e.waited.get(name, 0) < prev:
            e.waited[name] = prev
            waits.append((name, prev))
        self.dma_state[s] = prev + 16
        tok = (name, prev + 16)
        oa, ia = out.ap, in_.ap
        e.ops.append((waits, lambda h: h.dma_start(out=oa, in_=ia, **kw), name))
        self._mark(tok, rd, wd)
        self.n_ops += 1
        return tok

    def coll(self, kind, out, in_, groups):
        import os as _os
        if _os.environ.get("DBG_NOCOLL"):
            return None
        self._cidx = getattr(self, "_cidx", -1) + 1
        if _os.environ.get("DBG_COLLMASK") and not (int(_os.environ["DBG_COLLMASK"]) >> self._cidx) & 1:
            return None
        rd = list(in_.deps)
        wd = list(out.deps)
        e = self.E["pool"]
        waits = self._waits(e, rd, wd, None)
        self.ncoll = getattr(self, "ncoll", 0) + 1
        tok = ("coll", self.ncoll)
        oa, ia = out.ap, in_.ap
        e.ops.append((waits, lambda h: h.collective_compute(kind, ALU.bypass, replica_groups=groups, ins=[ia], outs=[oa]), "coll"))
        self._mark(tok, rd, wd)
        self.n_ops += 1
        return tok

    def _ensure_sems(self):
        if getattr(self, "sem", None) is None:
            self.sem = {}
            for n in ("pe", "act", "dve", "pool"):
                self.sem[n] = self.stack.enter_context(self.nc.semaphore("s_" + n))
            for i in range(self.ndma):
                self.sem["dma%d" % i] = self.stack.enter_context(self.nc.semaphore("s_dma%d" % i))
            self.sem["coll"] = self.stack.enter_context(self.nc.semaphore("s_coll"))

    def flush(self):
        self._ensure_sems()
        sem = self.sem
        import os as _os3
        if _os3.environ.get("DBG_FLUSHLOG"):
            print("FLUSH at n_ops", self.n_ops)
        toks = [(n, self.E[n].count) for n in ("pe", "act", "dve", "pool") if self.E[n].count > 0]
        toks += [("dma%d" % i, v) for i, v in enumerate(self.dma_state) if v > 0]
        if getattr(self, "ncoll", 0) > 0:
            toks.append(("coll", self.ncoll))
        for n, e in self.E.items():
            waits = []
            for (pn, pv) in toks:
                if pn == n:
                    continue
                if e.waited.get(pn, 0) < pv:
                    e.waited[pn] = pv
                    waits.append((pn, pv))
            e.ops.append((waits, None, None))
        with self.nc.Block() as block:
            def replay(e, own):
                ops = e.ops

                def body(h):
                    for waits, fn, dsem in ops:
                        for (pn, pv) in waits:
                            h.wait_ge(sem[pn], pv)
                        if fn is None:
                            continue
                        ins = fn(h)
                        if dsem == "coll":
                            ins.then_inc(sem[dsem], 1)
                        elif dsem is not None:
                            ins.then_inc(sem[dsem], 16)
                        elif own is not None:
                            ins.then_inc(sem[own], 1)
                return body
            block.sync(replay(self.E["sp"], None))
            block.tensor(replay(self.E["pe"], "pe"))
            block.scalar(replay(self.E["act"], "act"))
            block.vector(replay(self.E["dve"], "dve"))
            block.gpsimd(replay(self.E["pool"], "pool"))
        for e in self.E.values():
            e.ops = []

    def emit(self):
        self.flush()

    def close(self):
        self.stack.close()

    def mm(self, out, lhsT, rhs, start=True, stop=True):
        oa, la, ra = out.ap, lhsT.ap, rhs.ap
        return self.op("pe", lambda h: h.matmul(oa, la, ra, start=start, stop=stop),
                       reads=[lhsT, rhs], writes=[out])

    def act(self, out, in_, func, scale=1.0, bias=None, eng="act"):
        oa, ia = out.ap, in_.ap
        reads = [in_]
        kw = {}
        if isinstance(bias, V):
            reads.append(bias)
            kw["bias"] = bias.ap
        elif bias is not None:
            kw["bias"] = bias
        if isinstance(scale, V):
            reads.append(scale)
            kw["scale"] = scale.ap
        else:
            kw["scale"] = scale
        return self.op("act", lambda h: h.activation(oa, ia, func, **kw), reads=reads, writes=[out])

    def tt(self, eng, out, in0, in1, op):
        oa, a, b = out.ap, in0.ap, in1.ap
        return self.op(eng, lambda h: h.tensor_tensor(oa, a, b, op), reads=[in0, in1], writes=[out])

    def ts(self, eng, out, in0, s1, op0, s2=None, op1=None):
        oa, a = out.ap, in0.ap
        reads = [in0]
        if isinstance(s1, V):
            reads.append(s1)
            s1 = s1.ap
        if isinstance(s2, V):
            reads.append(s2)
            s2 = s2.ap
        if op1 is None:
            return self.op(eng, lambda h: h.tensor_scalar(oa, a, s1, None, op0), reads=reads, writes=[out])
        return self.op(eng, lambda h: h.tensor_scalar(oa, a, s1, s2, op0, op1), reads=reads, writes=[out])

    def stt(self, out, in0, scalar, in1, op0, op1):
        oa, a, b = out.ap, in0.ap, in1.ap
        reads = [in0, in1]
        if isinstance(scalar, V):
            reads.append(scalar)
            scalar = scalar.ap
        return self.op("dve", lambda h: h.scalar_tensor_tensor(oa, a, scalar, b, op0, op1),
                       reads=reads, writes=[out])

    def copy(self, eng, out, in_):
        oa, ia = out.ap, in_.ap
        if eng == "act":
            return self.op("act", lambda h: h.activation(oa, ia, AF.Copy), reads=[in_], writes=[out])
        return self.op(eng, lambda h: h.tensor_copy(oa, ia), reads=[in_], writes=[out])

    def memset(self, eng, out, val):
        oa = out.ap
        return self.op(eng, lambda h: h.memset(oa, val), writes=[out])

    def scan(self, out, d0, d1, init, op0, op1):
        oa, a, b = out.ap, d0.ap, d1.ap
        return self.op("dve", lambda h: h.tensor_tensor_scan(oa, a, b, init, op0, op1),
                       reads=[d0, d1], writes=[out])

    def recip(self, out, in_):
        oa, ia = out.ap, in_.ap
        return self.op("dve", lambda h: h.reciprocal(oa, ia), reads=[in_], writes=[out])


class Pool:
    def __init__(self, kb, name, n, shape, dt, space="sbuf", stack=None):
        mk = kb.sbuf if space == "sbuf" else kb.psum
        self.t = [mk("%s%d" % (name, i), shape, dt, stack=stack) for i in range(n)]
        self.i = 0

    def get(self):
        t = self.t[self.i]
        self.i = (self.i + 1) % len(self.t)
        return t


C_W = math.exp(-0.5)
MLA_SCALE = 96 ** -0.5
LN_EPS = 1e-5
NORM_EPS = 1e-6
RWKV_LN_EPS = 64e-5
ALPHA = (2.0 * 2) ** 0.25

_IW = (256, 160, 256, 256, 512, 16, 512, 1536, 4, 4, 512, 1792, 4096)
_IO = np.concatenate([[0], np.cumsum(_IW)]).tolist()
(O_QLAT, O_KVLAT, O_GQ, O_GK, O_GV, O_GGATE, O_GOG, O_DQKV, O_DBETA, O_DA, O_DZ, O_RW, O_GATES) = _IO[:13]


class Packer:
    def __init__(self, rows):
        self.rows = rows
        self.blocks = []
        self.off = {}
        self.n = 0

    def add(self, name, arr):
        arr = np.asarray(arr, dtype=np.float32)
        if arr.ndim == 1:
            arr = arr[:, None]
        if arr.shape[0] < self.rows:
            pad = np.zeros((self.rows - arr.shape[0], arr.shape[1]), np.float32)
            arr = np.concatenate([arr, pad], 0)
        self.off[name] = (self.n, arr.shape[1])
        self.blocks.append(arr)
        self.n += arr.shape[1]

    def build(self):
        return np.ascontiguousarray(np.concatenate(self.blocks, 1))


def const_masks():
    i = np.arange(128)
    same = (i[:, None] // 64) == (i[None, :] // 64)
    m = {}
    m["U_incl"] = (same & (i[:, None] <= i[None, :])).astype(np.float32)
    m["U_strict"] = (same & (i[:, None] < i[None, :])).astype(np.float32)
    m["L_incl"] = m["U_incl"].T.copy()
    m["L_strict"] = m["U_strict"].T.copy()
    m["ident"] = np.eye(128, dtype=np.float32)
    m["ones"] = np.ones((128, 128), np.float32)
    m["ones_blk"] = same.astype(np.float32)
    m["istack"] = np.concatenate([np.eye(64), np.eye(64)], 0).astype(np.float32)
    return m


def pack_p1a(inp, l, hg):
    w_in = inp["w_in"][l]
    W = Packer(1024)
    W.add("qlat", w_in[:, O_QLAT:O_QLAT + 256])
    W.add("ckv", w_in[:, O_KVLAT:O_KVLAT + 128])
    kr = w_in[:, O_KVLAT + 128:O_KVLAT + 160]
    W.add("krope", kr)
    W.add("krope_sw", np.concatenate([kr[:, 16:32], kr[:, 0:16]], 1))
    W.add("gq", w_in[:, O_GQ + 64 * hg:O_GQ + 64 * hg + 64])
    W.add("gk", w_in[:, O_GK + 64 * hg:O_GK + 64 * hg + 64])
    W.add("gv", w_in[:, O_GV + 128 * hg:O_GV + 128 * hg + 128])
    W.add("ggate", w_in[:, O_GGATE:O_GGATE + 16])
    W.add("gog", w_in[:, O_GOG + 128 * hg:O_GOG + 128 * hg + 128])
    W.add("dq", w_in[:, O_DQKV + 128 * hg:O_DQKV + 128 * hg + 128])
    W.add("dk", w_in[:, O_DQKV + 512 + 128 * hg:O_DQKV + 512 + 128 * hg + 128])
    W.add("dv", w_in[:, O_DQKV + 1024 + 128 * hg:O_DQKV + 1024 + 128 * hg + 128])
    W.add("dbeta", np.repeat(w_in[:, O_DBETA + hg:O_DBETA + hg + 1], 128, 1))
    W.add("da", np.repeat(w_in[:, O_DA + hg:O_DA + hg + 1], 128, 1))
    W.add("dz", w_in[:, O_DZ + 128 * hg:O_DZ + 128 * hg + 128])
    for i, nm in enumerate(("rr", "rk", "rv")):
        W.add(nm, w_in[:, O_RW + 512 * i + 128 * hg:O_RW + 512 * i + 128 * hg + 128])
    W.add("rwl", w_in[:, O_RW + 1536:O_RW + 1600])
    W.add("ral", w_in[:, O_RW + 1600:O_RW + 1664])
    W.add("rgl", w_in[:, O_RW + 1664:O_RW + 1792])
    if l > 0:
        W.add("rvr", inp["w_in_vres"][l - 1])
    P = Packer(128)
    ch = slice(128 * hg, 128 * hg + 128)
    mu = inp["rwkv_mu"][l]
    P.add("mu_r", mu[0:512][ch]); P.add("mu_k", mu[512:1024][ch]); P.add("mu_v", mu[1024:1536][ch])
    P.add("mu_wl", mu[1536:1600]); P.add("mu_al", mu[1600:1664]); P.add("mu_gl", mu[1664:1792])
    P.add("w0", inp["rwkv_w0"][l][ch]); P.add("a0", inp["rwkv_a0"][l][ch])
    P.add("k_k", inp["rwkv_k_k"][l][ch]); P.add("k_a", inp["rwkv_k_a"][l][ch])
    P.add("r_k", inp["rwkv_r_k"][l].reshape(-1)[ch])
    P.add("ln_g", inp["rwkv_ln_g"][l][ch]); P.add("ln_b", inp["rwkv_ln_b"][l][ch])
    if l > 0:
        P.add("mu_vr", inp["rwkv_vres_mu"][l - 1]); P.add("v0", inp["rwkv_v0"][l - 1][ch])
    P.add("gla_norm", inp["gla_norm"][l]); P.add("gdn_norm", inp["gdn_norm"][l])
    cw = inp["gdn_conv"][l]
    for nm, o in (("cq", 0), ("ck", 512), ("cv", 1024)):
        P.add("conv_" + nm, cw[:, o + 128 * hg:o + 128 * hg + 128].T)
    P.add("a_log", np.full(128, inp["gdn_a_log"][l][hg])); P.add("dt_bias", np.full(128, inp["gdn_dt_bias"][l][hg]))
    P.add("q_norm", inp["mla_q_norm"][l].reshape(2, 128).T)
    P.add("kv_norm", inp["mla_kv_norm"][l])
    invf = (10000.0 ** (-np.arange(16, dtype=np.float32) / 16)).astype(np.float32)
    P.add("invf", np.concatenate([np.zeros(64, np.float32), invf, invf]))
    P.add("rsign", np.concatenate([np.zeros(64, np.float32), -np.ones(16, np.float32), np.ones(16, np.float32)]))
    M = Packer(128)
    M.add("gup", np.concatenate([inp["gla_w_gate_up"][l][:, 64 * hg:64 * hg + 64],
                                 inp["gla_b_gate"][l][None, 64 * hg:64 * hg + 64]], 0))
    M.add("w_up", inp["rwkv_w_up"][l][:, ch]); M.add("a_up", inp["rwkv_a_up"][l][:, ch])
    M.add("g_up", inp["rwkv_g_up"][l][:, ch])
    if l > 0:
        M.add("v_up", inp["rwkv_v_up"][l - 1][:, ch])
    uq = inp["mla_w_uq"][l]
    ukv = inp["mla_w_ukv"][l]
    for hh in range(2):
        h = 2 * hg + hh
        q = uq[:, 96 * h:96 * h + 96]
        for kc in range(2):
            M.add("uq%d_%d" % (hh, kc), q[128 * kc:128 * kc + 128])
            sw = np.concatenate([np.zeros((128, 64), np.float32), q[128 * kc:128 * kc + 128, 80:96],
                                 q[128 * kc:128 * kc + 128, 64:80]], 1)
            M.add("uqs%d_%d" % (hh, kc), sw)
        M.add("ukn%d" % hh, ukv[:, 128 * h:128 * h + 64])
    M.add("ukv_v", np.concatenate([ukv[:, 128 * (2 * hg) + 64:128 * (2 * hg) + 128],
                                   ukv[:, 128 * (2 * hg + 1) + 64:128 * (2 * hg + 1) + 128]], 1))
    return W, P, M


class Ctx:
    pass


def setup_common(kb, cx, masks_d, stack):
    mk = const_masks()
    names = ["U_strict", "U_incl", "L_incl", "L_strict", "ident", "ones", "ones_blk"]
    cx.mf = kb.sbuf("maskf", [128, 7 * 128 + 64], F32, stack)
    cx.mb = kb.sbuf("maskb", [128, 7 * 128 + 64], BF16, stack)
    kb.dma("sp", cx.mf[:, :], masks_d)
    kb.copy("pool", cx.mb[:, :], cx.mf[:, :])
    cx.M = {}
    cx.Mf = {}
    for i, n in enumerate(names):
        cx.M[n] = cx.mb[:, 128 * i:128 * i + 128]
        cx.Mf[n] = cx.mf[:, 128 * i:128 * i + 128]
    cx.M["istack"] = cx.mb[:, 896:960]
    cx.Mf["istack"] = cx.mf[:, 896:960]
    cx.M["pairU"] = cx.mb[:, 0:256]
    banks = [kb.psum("pb%d" % i, [128, 512], F32, stack) for i in range(8)]
    cx.banks = banks

    class RR:
        def __init__(self, items):
            self.items = items
            self.i = 0

        def get(self):
            x = self.items[self.i]
            self.i = (self.i + 1) % len(self.items)
            return x
    cx.RR = RR
    cx.PB = RR([banks[i][:, :] for i in range(2)])
    cx.PH = RR([banks[2][:, 256 * j:256 * j + 256] for j in range(2)])
    cx.PQ = RR([banks[3 + i][:, 128 * j:128 * j + 128] for j in range(4) for i in range(3)])
    cx.PD = [banks[6 + i][:, 128 * j:128 * j + 128] for i in range(2) for j in range(4)]


def masks_host():
    mk = const_masks()
    names = ["U_strict", "U_incl", "L_incl", "L_strict", "ident", "ones", "ones_blk"]
    return np.ascontiguousarray(np.concatenate([mk[n] for n in names] + [mk["istack"]], 1).astype(np.float32))


def scanmask_host():
    m = np.ones((128, TT), np.float32)
    m[:, ::64] = 0.0
    return m


def rsqrt_act(kb, out, in_, scale, eps, tmp):
    kb.act(tmp, in_, AF.Ln, scale=scale, bias=eps)
    kb.act(out, tmp, AF.Exp, scale=-0.5)


def sigmoid_into(kb, out, in_, tmp, scale=1.0, bias=None, eng="dve"):
    kb.act(tmp, in_, AF.Exp, scale=-scale, bias=bias)
    kb.ts(eng, tmp, tmp, 1.0, ALU.add)
    kb.recip(out, tmp)


def neumann_multi(kb, cx, PAs, PBs, pool, ident):
    n = len(PAs)
    TTs = []
    for i in range(n):
        T0 = pool.get()
        kb.tt("dve", T0[:, :], PBs[i], ident, ALU.add)
        TTs.append(T0[:, :])
    A = list(PAs)
    B = list(PBs)
    for k in range(1, 6):
        pss = []
        for i in range(n):
            ps = cx.PQ.get()
            kb.mm(ps, B[i], A[i])
            pss.append(ps)
        nAs = []
        for i in range(n):
            nA = pool.get()
            kb.copy("act", nA[:, :], pss[i])
            nAs.append(nA[:, :])
        nBs = B
        if k < 5:
            pss = []
            for i in range(n):
                ps = cx.PQ.get()
                kb.mm(ps, A[i], B[i])
                pss.append(ps)
            nBs = []
            for i in range(n):
                nB = pool.get()
                kb.copy("dve", nB[:, :], pss[i])
                nBs.append(nB[:, :])
        pss = []
        for i in range(n):
            ps = cx.PQ.get()
            kb.mm(ps, nAs[i], TTs[i])
            pss.append(ps)
        for i in range(n):
            nT = pool.get()
            kb.tt("dve", nT[:, :], TTs[i], pss[i], ALU.add)
            TTs[i] = nT[:, :]
        A, B = nAs, nBs
    return TTs


def neumann(kb, cx, PA, PB_, pool_bf, ident_b):
    TT_ = pool_bf.get()
    kb.tt("dve", TT_[:, :], PB_, ident_b, ALU.add)
    A, B = PA, PB_
    for k in range(1, 6):
        psA = cx.PQ.get()
        kb.mm(psA, B, A)
        nA = pool_bf.get()
        kb.copy("act", nA[:, :], psA)
        if k < 5:
            psB = cx.PQ.get()
            kb.mm(psB, A, B)
            nB = pool_bf.get()
            kb.copy("pool" if False else "dve", nB[:, :], psB)
        psT = cx.PQ.get()
        kb.mm(psT, nA[:, :], TT_[:, :])
        nT = pool_bf.get()
        kb.tt("dve", nT[:, :], TT_[:, :], psT, ALU.add)
        TT_ = nT
        A = nA[:, :]
        if k < 5:
            B = nB[:, :]
    return TT_[:, :]


def build_p1a(kb, S, l, d, branches=("gla", "gdn", "rwkv", "mla")):
    import os as _os
    if "build_p1a" in _os.environ.get("DBG_SKIP", "").split(","):
        return None

    st = contextlib.ExitStack()
    cx = Ctx()
    setup_common(kb, cx, d["masks"], st)
    Wo, Po, Mo = d["Woff"], d["Poff"], d["Moff"]
    NW, NP, NM = d["NW"], d["NP"], d["NM"]
    nt = S // TT
    M, Mf = cx.M, cx.Mf

    PC = kb.sbuf("PC", [128, NP], F32, st)
    kb.dma("sp", PC[:, :], d["PC"])
    SMf = kb.sbuf("SMf", [128, NM], F32, st)
    kb.dma("sp", SMf[:, :], d["SM"])
    SMb = kb.sbuf("SMb", [128, NM], BF16, st)
    kb.copy("pool", SMb[:, :], SMf[:, :])
    scm = kb.sbuf("scm", [128, TT], F32, st)
    kb.dma("sp", scm[:, :], d["scanmask"])
    Wbf = kb.sbuf("Wbf", [128, NKC, NW], BF16, st)
    wst = Pool(kb, "wst", 2, [128, 512], F32, stack=st)
    for k in range(NKC):
        for c0 in range(0, NW, 512):
            cw_ = min(512, NW - c0)
            s_ = wst.get()
            kb.dma("sp", s_[:, 0:cw_], d["W"][128 * k:128 * k + 128, c0:c0 + cw_])
            kb.copy("pool", Wbf[:, k, c0:c0 + cw_], s_[:, 0:cw_])

    def pc(name, rows=128, j=0):
        o, w = Po[name]
        return PC[0:rows, o + j:o + j + 1]

    def sm(name, rows, cols=None):
        o, w = Mo[name]
        return SMb[0:rows, o:o + (cols or w)]

    def wcol(name, k, c0=0, cn=None):
        o, w = Wo[name]
        return Wbf[:, k, o + c0:o + c0 + (cn or w)]

    cst = kb.sbuf("cst", [128, 8], F32, st)
    kb.memset("pool", cst[:, 0:1], NORM_EPS)
    kb.memset("pool", cst[:, 1:2], RWKV_LN_EPS)
    kb.memset("pool", cst[:, 2:3], 1.0)
    kb.memset("pool", cst[:, 3:4], 0.0)
    EPS_N, EPS_R, ONE_C, ZERO_C = cst[:, 0:1], cst[:, 1:2], cst[:, 2:3], cst[:, 3:4]
    ncol = kb.sbuf("ncol", [128, 8], F32, st)
    kb.ts("pool", ncol[:, 0:1], pc("w0"), -1.0, ALU.mult)
    kb.ts("pool", ncol[:, 1:2], pc("a0"), -1.0, ALU.mult)
    if l > 0:
        kb.ts("pool", ncol[:, 2:3], pc("v0"), -1.0, ALU.mult)
    kb.act(ncol[:, 3:4], pc("a_log"), AF.Exp)

    F5 = Pool(kb, "f5_", 10, [128, TT], F32, stack=st)
    L5 = Pool(kb, "l5_", 18, [128, TT], F32, stack=st)
    B5 = Pool(kb, "b5_", 10, [128, TT], BF16, stack=st)
    BQ = Pool(kb, "bq_", 40, [128, 128], BF16, stack=st)
    FQ = Pool(kb, "fq_", 16, [128, 128], F32, stack=st)
    kb_rhs = Pool(kb, "rhs_", 4, [128, 256], BF16, stack=st)
    BL = Pool(kb, "bl_", 8, [128, 128], BF16, stack=st)
    xst = Pool(kb, "xst", 3, [128, TT], F32, stack=st)
    xbfs = [kb.sbuf("xbf%d" % i, [128, NKC, TT], BF16, st) for i in range(2)]

    def proj_f(name, xb, rows=None, c0=0, out=None):
        o, w = Wo[name]
        rows = rows or w
        ps = out if out is not None else cx.PB.get()[0:rows, :]
        for k in range(NKC):
            kb.mm(ps, wcol(name, k, c0, rows), xb[:, k, :], start=(k == 0), stop=(k == NKC - 1))
        return ps

    def shift_mix(ps, rows, mu, halo, t):
        raw = halo[t % 2]
        kb.copy("act", raw[0:rows, 1:TT + 1], ps)
        if t == 0:
            kb.memset("pool", raw[0:rows, 0:1], 0.0)
        kb.copy("pool", halo[(t + 1) % 2][0:rows, 0:1], raw[0:rows, TT:TT + 1])
        dd = F5.get()
        kb.tt("dve", dd[0:rows, :], raw[0:rows, 0:TT], raw[0:rows, 1:TT + 1], ALU.subtract)
        out = F5.get()
        kb.stt(out[0:rows, :], dd[0:rows, :], mu, raw[0:rows, 1:TT + 1], ALU.mult, ALU.add)
        return out[0:rows, :]

    if "gla" in branches:
        gl_H32 = kb.sbuf("gl_H32", [64, 128], F32, st)
        gl_Hbf = kb.sbuf("gl_Hbf", [64, 128], BF16, st)
        kb.memset("pool", gl_H32[:, :], 0.0)
        kb.memset("pool", gl_Hbf[:, :], 0.0)
        gl_ext = kb.sbuf("gl_ext", [32, TT], BF16, st)
        kb.memset("pool", gl_ext[:, :], 1.0)
    if "gdn" in branches:
        gd_H32 = kb.sbuf("gd_H32", [128, 128], F32, st)
        gd_Hbf = kb.sbuf("gd_Hbf", [128, 128], BF16, st)
        kb.memset("pool", gd_H32[:, :], 0.0)
        kb.memset("pool", gd_Hbf[:, :], 0.0)
        gd_halo = {n: [kb.sbuf("gdh_%s" % n, [128, TT + 3], F32, st), kb.sbuf("gdc_%s" % n, [128, 4], F32, st)] for n in ("q", "k", "v")}
    if "rwkv" in branches:
        rw_H32 = kb.sbuf("rw_H32", [128, 64], F32, st)
        rw_Hbf = kb.sbuf("rw_Hbf", [128, 64], BF16, st)
        kb.memset("pool", rw_H32[:, :], 0.0)
        kb.memset("pool", rw_Hbf[:, :], 0.0)
        rw_halo = {n: [kb.sbuf("rwh_%s" % n, [128, TT + 1], F32, st), kb.sbuf("rwc_%s" % n, [128, 2], F32, st)]
                   for n in ("r", "k", "v", "wl", "al", "gl", "vr")}
        rw_AR = kb.sbuf("rw_AR", [128, 2 * TT], BF16, st)
        rw_MT = kb.sbuf("rw_MT", [128, 128], F32, st)
        kb.memset("pool", rw_MT[:, :], 0.0)
    if "mla" in branches:
        mla_posi = kb.sbuf("mla_posi", [96, TT], I32, st)
        mla_vt = [kb.sbuf("mla_vt%d" % i, [128, 2, 65], BF16, st) for i in range(2)]
        for i in range(2):
            kb.memset("pool", mla_vt[i][:, :, :], 1.0)
        mla_ni = kb.sbuf("mla_ni", [96, TT], I32, st)
        uqg = kb.sbuf("uqg", [128, 2, 2, 2, 96], BF16, st)
        for hh in range(2):
            for kc in range(2):
                for sw, nm in enumerate(("uq", "uqs")):
                    o, w = Mo["%s%d_%d" % (nm, hh, kc)]
                    kb.ts("pool", uqg[:, hh, kc, sw, :], SMf[:, o:o + 96], pc("q_norm", 128, kc), ALU.mult,
                          MLA_SCALE, ALU.mult)
        ukg = kb.sbuf("ukg", [128, 2, 64], BF16, st)
        for hh in range(2):
            o, w = Mo["ukn%d" % hh]
            kb.ts("pool", ukg[:, hh, :], SMf[:, o:o + 64], pc("kv_norm"), ALU.mult)
        uvg = kb.sbuf("uvg", [128, 128], BF16, st)
        o, w = Mo["ukv_v"]
        kb.ts("pool", uvg[:, :], SMf[:, o:o + 128], pc("kv_norm"), ALU.mult)

    def silu_of(src):
        sg = L5.get()
        tmp2 = F5.get()
        sigmoid_into(kb, sg[:, :], src, tmp2[:, :])
        kb.tt("dve", sg[:, :], sg[:, :], src, ALU.mult)
        return sg

    def post_norm_gate(oT, gate_sb, gcol, row0):
        osq = B5.get()
        kb.act(osq[:, :], oT[:, :], AF.Square)
        ps_s = cx.PB.get()
        kb.mm(ps_s, M["ones"], osq[:, :])
        tmp = F5.get()
        rstd = F5.get()
        rsqrt_act(kb, rstd[:, :], ps_s, 1.0 / 128, EPS_N, tmp[:, :])
        sg = silu_of(gate_sb)
        kb.stt(oT[:, :], oT[:, :], gcol, rstd[:, :], ALU.mult, ALU.mult)
        yb = B5.get()
        kb.tt("dve", yb[:, :], oT[:, :], sg[:, :], ALU.mult)
        kb.dma("sp", d["ydst"](row0, 128, tsl_box[0].start // TT), yb[:, :])

    def l2norm_f(x, ones_m, scale):
        sq = B5.get()
        kb.act(sq[:, :], x, AF.Square)
        ps_s = cx.PB.get()
        kb.mm(ps_s, ones_m, sq[:, :])
        tmp = F5.get()
        rs = F5.get()
        rsqrt_act(kb, rs[:, :], ps_s, 1.0, EPS_N, tmp[:, :])
        out = L5.get()
        kb.stt(out[:, :], x, scale, rs[:, :], ALU.mult, ALU.mult)
        return out

    def transpose_bf(src, dst_pool=None):
        ps_t = cx.PQ.get()
        kb.mm(ps_t, src, M["ident"])
        o = BL.get()
        kb.copy("act", o[:, :], ps_t)
        return o

    tsl_box = [None]
    youts = {}

    def load_x(t_):
        for k in range(NKC):
            kb.dma("pool", xbfs[t_ % 2][:, k, :], d["xsrc"](k, t_))
    load_x(0)
    for t in range(nt):
        tsl = slice(t * TT, (t + 1) * TT)
        tsl_box[0] = tsl
        xb = xbfs[t % 2]
        if t + 1 < nt:
            load_x(t + 1)

        if "gla" in branches:
            L5.i = 0
            ps_q = proj_f("gq", xb)
            qraw = L5.get()
            kb.copy("act", qraw[0:64, :], ps_q)
            ps_k = proj_f("gk", xb)
            kraw = L5.get()
            kb.copy("act", kraw[0:64, :], ps_k)
            ps_g = proj_f("ggate", xb)
            kb.copy("act", gl_ext[0:16, :], ps_g)
            ps_og = proj_f("gog", xb)
            og = L5.get()
            kb.copy("act", og[:, :], ps_og)
            ps_z = cx.PB.get()[0:64, :]
            kb.mm(ps_z, sm("gup", 17), gl_ext[0:17, :])
            e1 = F5.get()
            kb.act(e1[0:64, :], ps_z, AF.Exp, scale=-1.0)
            sp = F5.get()
            kb.act(sp[0:64, :], e1[0:64, :], AF.Ln, bias=ONE_C[0:64, :])
            bsp = L5.get()
            kb.scan(bsp[0:64, :], scm[0:64, :], sp[0:64, :], 0.0, ALU.mult, ALU.add)
            epos = L5.get()
            kb.act(epos[0:64, :], bsp[0:64, :], AF.Exp, scale=-1.0 / 16)
            eneg = F5.get()
            kb.act(eneg[0:64, :], bsp[0:64, :], AF.Exp, scale=1.0 / 16)
            b3 = bsp.ap[0:64, :].rearrange("p (c t) -> p c t", t=64)
            dl = F5.get()
            dl3 = dl.ap[0:64, :].rearrange("p (c t) -> p c t", t=64)
            bl_bc = b3[:, :, 63:64].broadcast_to([64, TT // 64, 64])
            kb.tt("dve", V(dl3, dl.deps), V(b3, bsp.deps), V(bl_bc, bsp.deps), ALU.subtract)
            eend = F5.get()
            kb.act(eend[0:64, :], dl[0:64, :], AF.Exp, scale=1.0 / 16)
            qd = B5.get()
            kb.stt(qd[0:64, :], qraw[0:64, :], 0.125, epos[0:64, :], ALU.mult, ALU.mult)
            ki = B5.get()
            kb.tt("dve", ki[0:64, :], kraw[0:64, :], eneg[0:64, :], ALU.mult)
            ke = B5.get()
            kb.tt("dve", ke[0:64, :], kraw[0:64, :], eend[0:64, :], ALU.mult)
            oT = L5.get()
            for blk in range(4):
                bs = slice(blk * 128, blk * 128 + 128)
                ps_v = cx.PQ.get()
                for k in range(NKC):
                    kb.mm(ps_v, xb[:, k, bs], wcol("gv", k), start=(k == 0), stop=(k == NKC - 1))
                vt = BQ.get()
                kb.copy("act", vt[:, :], ps_v)
                ps_t = cx.PQ.get()[:, 0:64]
                kb.mm(ps_t, ke[0:64, bs], M["ident"][0:64, 0:64])
                ket = BQ.get()
                kb.copy("dve", ket[:, 0:64], ps_t)
                ps_a = cx.PQ.get()
                kb.mm(ps_a, ki[0:64, bs], qd[0:64, bs])
                am = BQ.get()
                kb.tt("dve", am[:, :], ps_a, Mf["U_incl"], ALU.mult)
                ps_o = cx.PD[0]
                kb.mm(ps_o, vt[:, :], am[:, :], start=True, stop=False)
                for c in range(2):
                    cs = slice(blk * 128 + c * 64, blk * 128 + c * 64 + 64)
                    rs = slice(c * 64, c * 64 + 64)
                    kb.mm(ps_o[:, rs], gl_Hbf[:, :], qd[0:64, cs], start=False, stop=(c == 1))
                    ps_n = cx.PD[4][0:64, :]
                    kb.mm(ps_n, ket[rs, 0:64], vt[rs, :])
                    gc = blk * 2 + c
                    acol = epos[0:64, 64 * (t * 0 + gc) + 63:64 * gc + 64]
                    kb.stt(gl_H32[:, :], gl_H32[:, :], acol, ps_n, ALU.mult, ALU.add)
                    kb.copy("act", gl_Hbf[:, :], gl_H32[:, :])
                kb.copy("act", oT[:, bs], ps_o)
            post_norm_gate(oT, og[:, :], pc("gla_norm"), 128)

        if "gdn" in branches:
            L5.i = 0
            conv = {}
            for nm, cn in (("q", "conv_cq"), ("k", "conv_ck"), ("v", "conv_cv")):
                ps = proj_f("d" + nm, xb)
                raw = gd_halo[nm][0]
                hcol = gd_halo[nm][1]
                if t == 0:
                    kb.memset("pool", raw[:, 0:3], 0.0)
                else:
                    kb.copy("pool", raw[:, 0:3], hcol[:, 0:3])
                kb.copy("act", raw[:, 3:TT + 3], ps)
                kb.copy("pool", hcol[:, 0:3], raw[:, TT:TT + 3])
                acc = F5.get()
                kb.ts("dve", acc[:, :], raw[:, 0:TT], pc(cn, 128, 0), ALU.mult)
                for j in range(1, 4):
                    kb.stt(acc[:, :], raw[:, j:j + TT], pc(cn, 128, j), acc[:, :], ALU.mult, ALU.add)
                conv[nm] = silu_of(acc[:, :])
            qn = l2norm_f(conv["q"][:, :], M["ones"], 128 ** -0.5)
            kn32 = l2norm_f(conv["k"][:, :], M["ones"], 1.0)
            kn = B5.get()
            kb.copy("pool", kn[:, :], kn32[:, :])
            qnb = B5.get()
            kb.copy("pool", qnb[:, :], qn[:, :])
            ps_b = proj_f("dbeta", xb)
            beta = L5.get()
            tb = F5.get()
            sigmoid_into(kb, beta[:, :], ps_b, tb[:, :])
            nbeta = L5.get()
            kb.ts("pool", nbeta[:, :], beta[:, :], -1.0, ALU.mult)
            ps_a = proj_f("da", xb)
            e1 = F5.get()
            kb.act(e1[:, :], ps_a, AF.Exp, bias=pc("dt_bias"))
            spl = F5.get()
            kb.act(spl[:, :], e1[:, :], AF.Ln, bias=ONE_C)
            g = F5.get()
            kb.ts("dve", g[:, :], spl[:, :], ncol[:, 3:4], ALU.mult, -1.0, ALU.mult)
            gc = L5.get()
            kb.scan(gc[:, :], scm[:, :], g[:, :], 0.0, ALU.mult, ALU.add)
            egc = L5.get()
            kb.act(egc[:, :], gc[:, :], AF.Exp)
            g3 = gc.ap[:, :].rearrange("p (c t) -> p c t", t=64)
            dl = F5.get()
            dl3 = dl.ap[:, :].rearrange("p (c t) -> p c t", t=64)
            gl_bc = g3[:, :, 63:64].broadcast_to([128, TT // 64, 64])
            kb.tt("dve", V(dl3, dl.deps), V(gl_bc, gc.deps), V(g3, gc.deps), ALU.subtract)
            eend = F5.get()
            kb.act(eend[:, :], dl[:, :], AF.Exp)
            qdec = L5.get()
            kb.tt("dve", qdec[:, :], qn[:, :], egc[:, :], ALU.mult)
            kbg = B5.get()
            tk = F5.get()
            kb.tt("dve", tk[:, :], kn32[:, :], beta[:, :], ALU.mult)
            kb.tt("dve", kbg[:, :], tk[:, :], egc[:, :], ALU.mult)
            vb = B5.get()
            kb.tt("dve", vb[:, :], conv["v"][:, :], beta[:, :], ALU.mult)
            kend = B5.get()
            kb.tt("dve", kend[:, :], kn32[:, :], eend[:, :], ALU.mult)
            ps_z = proj_f("dz", xb)
            zs = L5.get()
            kb.copy("act", zs[:, :], ps_z)
            oT = L5.get()
            for blk in range(4):
                bs = slice(blk * 128, blk * 128 + 128)
                BL.i = 0
                Rm = gc[:, bs]
                t1 = FQ.get()
                kb.tt("dve", t1[:, :], Rm, Mf["ident"], ALU.mult)
                cc = FQ.get()
                kb.op("dve", (lambda o_, i_: (lambda h: h.tensor_reduce(o_, i_, AX.X, ALU.add)))(cc.ap[:, 0:1], t1.ap[:, :]),
                      reads=[t1], writes=[cc])
                kb.tt("dve", t1[:, :], nbeta[:, bs], Mf["ident"], ALU.mult)
                kb.op("dve", (lambda o_, i_: (lambda h: h.tensor_reduce(o_, i_, AX.X, ALU.add)))(cc.ap[:, 1:2], t1.ap[:, :]),
                      reads=[t1], writes=[cc])
                ccol, nbcol = cc[:, 0:1], cc[:, 1:2]
                tB = FQ.get()
                kb.ts("dve", tB[:, :], Rm, ccol, ALU.subtract, 0.0, ALU.min)
                eB = FQ.get()
                kb.act(eB[:, :], tB[:, :], AF.Exp)
                DBi = FQ.get()
                kb.tt("dve", DBi[:, :], eB[:, :], Mf["U_incl"], ALU.mult)
                DBs = FQ.get()
                kb.tt("pool", DBs[:, :], eB[:, :], Mf["U_strict"], ALU.mult)
                kb.tt("pool", DBs[:, :], DBs[:, :], nbeta[:, bs], ALU.mult)
                tA = FQ.get()
                kb.ts("dve", tA[:, :], Rm, ccol, ALU.subtract, 0.0, ALU.max)
                eA = FQ.get()
                kb.act(eA[:, :], tA[:, :], AF.Exp, scale=-1.0)
                kb.tt("pool", eA[:, :], eA[:, :], Mf["L_strict"], ALU.mult)
                ps_kk = cx.PQ.get()
                kb.mm(ps_kk, kn[:, bs], kn[:, bs])
                PA = BQ.get()
                kb.stt(PA[:, :], eA[:, :], nbcol, ps_kk, ALU.mult, ALU.mult)
                PB_ = BQ.get()
                kb.tt("dve", PB_[:, :], ps_kk, DBs[:, :], ALU.mult)
                TTm = neumann(kb, cx, PA[:, :], PB_[:, :], BQ, M["ident"])
                rhs = kb_rhs.get()
                ps_t = cx.PQ.get()
                kb.mm(ps_t, vb[:, bs], M["ident"])
                kb.copy("act", rhs[:, 0:128], ps_t)
                ps_t = cx.PQ.get()
                kb.mm(ps_t, kbg[:, bs], M["ident"])
                kb.copy("act", rhs[:, 128:256], ps_t)
                kendt = transpose_bf(kend[:, bs])
                ps_uw = cx.PH.get()
                kb.mm(ps_uw, TTm, rhs[:, :])
                UW = kb_rhs.get()
                kb.copy("act", UW[:, :], ps_uw)
                ps_qk = cx.PQ.get()
                kb.mm(ps_qk, kn[:, bs], qnb[:, bs])
                attB = BL.get()
                kb.tt("dve", attB[:, :], ps_qk, DBi[:, :], ALU.mult)
                ps_aw = cx.PQ.get()
                kb.mm(ps_aw, UW[:, 128:256], attB[:, :])
                qeff = BL.get()
                kb.tt("dve", qeff[:, :], qdec[:, bs], ps_aw, ALU.subtract)
                ps_o = cx.PD[1]
                kb.mm(ps_o, UW[:, 0:128], attB[:, :], start=True, stop=False)
                for c in range(2):
                    rs = slice(c * 64, c * 64 + 64)
                    gcn = blk * 2 + c
                    kb.mm(ps_o[:, rs], gd_Hbf[:, :], qeff[:, rs], start=False, stop=(c == 1))
                    ps_m = cx.PD[5]
                    kb.mm(ps_m, UW[rs, 128:256], kendt[rs, :])
                    MT = FQ.get()
                    kb.stt(MT[:, :], Mf["ident"], egc[:, 64 * gcn + 63:64 * gcn + 64], ps_m, ALU.mult, ALU.subtract)
                    ps_h = cx.PD[6]
                    kb.mm(ps_h, MT[:, :], gd_H32[:, :], start=True, stop=False)
                    kb.mm(ps_h, kendt[rs, :], UW[rs, 0:128], start=False, stop=True)
                    kb.copy("act", gd_Hbf[:, :], ps_h)
                    kb.copy("act", gd_H32[:, :], ps_h)
                kb.copy("act", oT[:, bs], ps_o)
            post_norm_gate(oT, zs[:, :], pc("gdn_norm"), 256)

        if "rwkv" in branches:
            L5.i = 0

            def shiftmix(pname, rows, mu_name, hname):
                ps = proj_f(pname, xb)
                raw, hcol = rw_halo[hname]
                if t == 0:
                    kb.memset("pool", raw[0:rows, 0:1], 0.0)
                else:
                    kb.copy("pool", raw[0:rows, 0:1], hcol[0:rows, 0:1])
                kb.copy("act", raw[0:rows, 1:TT + 1], ps)
                kb.copy("pool", hcol[0:rows, 0:1], raw[0:rows, TT:TT + 1])
                dd = F5.get()
                kb.tt("dve", dd[0:rows, :], raw[0:rows, 0:TT], raw[0:rows, 1:TT + 1], ALU.subtract)
                out = L5.get()
                kb.stt(out[0:rows, :], dd[0:rows, :], pc(mu_name, rows), raw[0:rows, 1:TT + 1], ALU.mult, ALU.add)
                return out
            r_ = shiftmix("rr", 128, "mu_r", "r")
            k_ = shiftmix("rk", 128, "mu_k", "k")
            v_ = shiftmix("rv", 128, "mu_v", "v")
            wl = shiftmix("rwl", 64, "mu_wl", "wl")
            al = shiftmix("ral", 64, "mu_al", "al")
            gl = shiftmix("rgl", 128, "mu_gl", "gl")
            tmp = F5.get()
            sgt = F5.get()
            sigmoid_into(kb, sgt[0:64, :], wl[0:64, :], tmp[0:64, :], scale=2.0)
            th = B5.get()
            kb.ts("dve", th[0:64, :], sgt[0:64, :], 2.0, ALU.mult, -1.0, ALU.add)
            ps_z = cx.PB.get()
            kb.mm(ps_z, sm("w_up", 64), th[0:64, :])
            sgw = L5.get()
            tmp = F5.get()
            sigmoid_into(kb, sgw[:, :], ps_z, tmp[:, :], bias=ncol[:, 0:1])
            cs = L5.get()
            kb.scan(cs[:, :], scm[:, :], sgw[:, :], 0.0, ALU.mult, ALU.add)
            albf = B5.get()
            kb.copy("pool", albf[0:64, :], al[0:64, :])
            ps_a = cx.PB.get()
            kb.mm(ps_a, sm("a_up", 64), albf[0:64, :])
            a_ = L5.get()
            tmp = F5.get()
            sigmoid_into(kb, a_[:, :], ps_a, tmp[:, :], bias=ncol[:, 1:2])
            sgg = F5.get()
            tmp = F5.get()
            sigmoid_into(kb, sgg[:, :], gl[:, :], tmp[:, :])
            sggb = B5.get()
            kb.copy("pool", sggb[:, :], sgg[:, :])
            ps_g = cx.PB.get()
            kb.mm(ps_g, sm("g_up", 128), sggb[:, :])
            gte = L5.get()
            kb.copy("act", gte[:, :], ps_g)
            if l > 0:
                ps = proj_f("rvr", xb)
                raw, hcol = rw_halo["vr"]
                if t == 0:
                    kb.memset("pool", raw[0:32, 0:1], 0.0)
                else:
                    kb.copy("pool", raw[0:32, 0:1], hcol[0:32, 0:1])
                kb.copy("act", raw[0:32, 1:TT + 1], ps)
                kb.copy("pool", hcol[0:32, 0:1], raw[0:32, TT:TT + 1])
                dd = F5.get()
                kb.tt("dve", dd[0:32, :], raw[0:32, 0:TT], raw[0:32, 1:TT + 1], ALU.subtract)
                vrl = B5.get()
                kb.stt(vrl[0:32, :], dd[0:32, :], pc("mu_vr", 32), raw[0:32, 1:TT + 1], ALU.mult, ALU.add)
                ps_v = cx.PB.get()
                kb.mm(ps_v, sm("v_up", 32), vrl[0:32, :])
                vg = F5.get()
                tmp = F5.get()
                sigmoid_into(kb, vg[:, :], ps_v, tmp[:, :], bias=ncol[:, 2:3])
                vf = F5.get()
                kb.dma("sp", vf[:, :], d["vfirst"][:, tsl])
                kb.tt("dve", vf[:, :], vf[:, :], v_[:, :], ALU.subtract)
                kb.tt("dve", vf[:, :], vf[:, :], vg[:, :], ALU.mult)
                kb.tt("dve", v_[:, :], v_[:, :], vf[:, :], ALU.add)
            else:
                kb.dma("sp", d["vfirst"][:, tsl], v_[:, :])
            kx = F5.get()
            kb.ts("dve", kx[:, :], k_[:, :], pc("k_k"), ALU.mult)
            kk = l2norm_f(kx[:, :], M["ones_blk"], 1.0)
            tk = F5.get()
            kb.ts("dve", tk[:, :], a_[:, :], -1.0, ALU.add, pc("k_a"), ALU.mult)
            kmod = L5.get()
            kb.stt(kmod[:, :], tk[:, :], 1.0, k_[:, :], ALU.add, ALU.mult)
            bb = L5.get()
            kb.tt("dve", bb[:, :], kk[:, :], a_[:, :], ALU.mult)
            rk = B5.get()
            kb.stt(rk[:, :], r_[:, :], pc("r_k"), kmod[:, :], ALU.mult, ALU.mult)
            ps_bn = cx.PB.get()
            kb.mm(ps_bn, M["ones_blk"], rk[:, :])
            bonus = L5.get()
            kb.tt("dve", bonus[:, :], ps_bn, v_[:, :], ALU.mult)
            ecw = F5.get()
            kb.act(ecw[:, :], cs[:, :], AF.Exp, scale=-C_W)
            encw = L5.get()
            kb.act(encw[:, :], cs[:, :], AF.Exp, scale=C_W)
            dx = F5.get()
            kb.tt("dve", dx[:, :], cs[:, :], sgw[:, :], ALU.subtract)
            eprev = F5.get()
            kb.act(eprev[:, :], dx[:, :], AF.Exp, scale=-C_W)
            c3 = cs.ap[:, :].rearrange("p (c t) -> p c t", t=64)
            dl = F5.get()
            dl3 = dl.ap[:, :].rearrange("p (c t) -> p c t", t=64)
            cl_bc = c3[:, :, 63:64].broadcast_to([128, TT // 64, 64])
            kb.tt("dve", V(dl3, dl.deps), V(cl_bc, cs.deps), V(c3, cs.deps), ALU.subtract)
            eend = F5.get()
            kb.act(eend[:, :], dl[:, :], AF.Exp, scale=-C_W)
            gam = L5.get()
            kb.copy("pool", gam[:, :], ecw[:, :])
            AR = rw_AR
            AR4 = AR.ap[:, :].rearrange("p (b two i) -> p b two i", two=2, i=128)
            e4 = eprev.ap[:, :].rearrange("p (b i) -> p b i", i=128)
            k4 = kk.ap[:, :].rearrange("p (b i) -> p b i", i=128)
            kb.stt(V(AR4[:, :, 0, :], AR.deps), V(k4, kk.deps), -1.0, V(e4, eprev.deps), ALU.mult, ALU.mult)
            r4 = r_.ap[:, :].rearrange("p (b i) -> p b i", i=128)
            g4 = ecw.ap[:, :].rearrange("p (b i) -> p b i", i=128)
            kb.tt("dve", V(AR4[:, :, 1, :], AR.deps), V(r4, r_.deps), V(g4, ecw.deps), ALU.mult)
            bt = B5.get()
            kb.tt("dve", bt[:, :], bb[:, :], encw[:, :], ALU.mult)
            kt = B5.get()
            kb.tt("dve", kt[:, :], kmod[:, :], encw[:, :], ALU.mult)
            bendT = B5.get()
            kb.tt("dve", bendT[:, :], bb[:, :], eend[:, :], ALU.mult)
            kendT = B5.get()
            kb.tt("dve", kendT[:, :], kmod[:, :], eend[:, :], ALU.mult)
            vbf = B5.get()
            kb.copy("pool", vbf[:, :], v_[:, :])
            yT_ = L5.get()
            for blk in range(4):
                bs = slice(blk * 128, blk * 128 + 128)
                ARb = AR[:, blk * 256:blk * 256 + 256]
                BL.i = 0
                At_tok = transpose_bf(ARb[:, 0:128])
                V_tok = transpose_bf(vbf[:, bs])
                Bend = transpose_bf(bendT[:, bs])
                Kend = transpose_bf(kendT[:, bs])
                psQ = cx.PD[2]
                psY = cx.PD[3]
                WUs = []
                r_SB1, r_SB2, r_PA = [], [], []
                for h in range(2):
                    hp = slice(64 * h, 64 * h + 64)
                    ps1 = cx.PH.get()
                    kb.mm(ps1, bt[hp, bs], ARb[hp, :])
                    SB1 = kb_rhs.get()
                    kb.tt("dve", SB1[:, :], ps1, cx.mf[:, 0:256], ALU.mult)
                    ps2 = cx.PH.get()
                    kb.mm(ps2, kt[hp, bs], ARb[hp, :])
                    SB2 = kb_rhs.get()
                    kb.tt("dve", SB2[:, :], ps2, cx.mf[:, 0:256], ALU.mult)
                    psA = cx.PQ.get()
                    kb.mm(psA, ARb[hp, 0:128], bt[hp, bs])
                    PA = BQ.get()
                    kb.tt("dve", PA[:, :], psA, Mf["L_strict"], ALU.mult)
                    r_SB1.append(SB1)
                    r_SB2.append(SB2)
                    r_PA.append(PA[:, :])
                r_TT = neumann_multi(kb, cx, r_PA, [r_SB1[0][:, 0:128], r_SB1[1][:, 0:128]], BQ, M["ident"])
                for h in range(2):
                    hp = slice(64 * h, 64 * h + 64)
                    SB1, SB2, TTm = r_SB1[h], r_SB2[h], r_TT[h]
                    psX = cx.PQ.get()
                    kb.mm(psX[:, 0:64], SB2[:, 0:128], V_tok[:, hp])
                    Xb = BQ.get()
                    kb.copy("act", Xb[:, 0:64], psX[:, 0:64])
                    psWU = cx.PQ.get()
                    kb.mm(psWU[:, 0:64], TTm, At_tok[:, hp])
                    kb.mm(psWU[:, 64:128], TTm, Xb[:, 0:64])
                    WU = BL.get()
                    kb.copy("act", WU[:, :], psWU)
                    WUs.append(WU)
                    kb.mm(psQ[hp, :], WU[:, 0:64], SB1[:, 128:256])
                    kb.mm(psY[hp, :], WU[:, 64:128], SB1[:, 128:256], start=True, stop=False)
                    kb.mm(psY[hp, :], V_tok[:, hp], SB2[:, 128:256], start=False, stop=False)
                qeff = BL.get()
                kb.tt("dve", qeff[:, :], ARb[:, 128:256], psQ, ALU.add)
                for c in range(2):
                    rs = slice(c * 64, c * 64 + 64)
                    gcn = blk * 2 + c
                    for h in range(2):
                        hp = slice(64 * h, 64 * h + 64)
                        kb.mm(psY[hp, rs], rw_Hbf[hp, :], qeff[hp, rs], start=False, stop=(c == 1))
                    psM = cx.PD[5]
                    for h in range(2):
                        hp = slice(64 * h, 64 * h + 64)
                        kb.mm(psM[hp, 0:64], WUs[h][rs, 0:64], Bend[rs, hp])
                    MT = rw_MT
                    for h in range(2):
                        hp = slice(64 * h, 64 * h + 64)
                        kb.stt(MT[hp, 64 * h:64 * h + 64], Mf["istack"][hp, :], gam[hp, 64 * gcn + 63:64 * gcn + 64],
                               psM[hp, 0:64], ALU.mult, ALU.add)
                    psH = cx.PD[6]
                    kb.mm(psH[:, 0:64], MT[:, :], rw_H32[:, :], start=True, stop=False)
                    for h in range(2):
                        hp = slice(64 * h, 64 * h + 64)
                        kb.mm(psH[hp, 0:64], Bend[rs, hp], WUs[h][rs, 64:128], start=False, stop=False)
                        kb.mm(psH[hp, 0:64], Kend[rs, hp], V_tok[rs, hp], start=False, stop=True)
                    kb.copy("act", rw_Hbf[:, :], psH[:, 0:64])
                    kb.copy("act", rw_H32[:, :], psH[:, 0:64])
                kb.copy("act", yT_[:, bs], psY)
            ps_m = cx.PB.get()
            kb.mm(ps_m, Mf["ones_blk"], yT_[:, :])
            yc = F5.get()
            kb.stt(yc[:, :], ps_m, -1.0 / 64, yT_[:, :], ALU.mult, ALU.add)
            ysq = F5.get()
            kb.act(ysq[:, :], yc[:, :], AF.Square)
            ps_v2 = cx.PB.get()
            kb.mm(ps_v2, Mf["ones_blk"], ysq[:, :])
            tmp = F5.get()
            rstd = F5.get()
            rsqrt_act(kb, rstd[:, :], ps_v2, 1.0 / 64, EPS_R, tmp[:, :])
            kb.stt(yc[:, :], yc[:, :], pc("ln_g"), rstd[:, :], ALU.mult, ALU.mult)
            kb.stt(yc[:, :], yc[:, :], pc("ln_b"), bonus[:, :], ALU.add, ALU.add)
            yo = B5.get()
            kb.tt("dve", yo[:, :], yc[:, :], gte[:, :], ALU.mult)
            kb.dma("sp", d["ydst"](384, 128, t), yo[:, :])

        if "mla" in branches:
            L5.i = 0
            posi = mla_posi
            kb.dma("sp", posi[:, :], d["pos"][:, tsl])
            posf = L5.get()
            kb.copy("dve", posf[0:96, :], posi[:, :])
            ang = L5.get()
            kb.ts("dve", ang[0:96, :], posf[0:96, :], pc("invf", 96), ALU.mult)
            tabs = []
            for phase in (math.pi / 2, 0.0):
                y = F5.get()
                kb.ts("dve", y[0:96, :], ang[0:96, :], phase, ALU.add, 1.0 / (2 * math.pi), ALU.mult)
                ni = mla_ni
                kb.copy("dve", ni[:, :], y[0:96, :])
                nf = F5.get()
                kb.copy("dve", nf[0:96, :], ni[:, :])
                r = F5.get()
                kb.stt(r[0:96, :], nf[0:96, :], -2 * math.pi, ang[0:96, :], ALU.mult, ALU.add)
                if phase != 0.0:
                    kb.ts("dve", r[0:96, :], r[0:96, :], phase, ALU.add)
                m1 = F5.get()
                kb.ts("dve", m1[0:96, :], r[0:96, :], math.pi, ALU.is_gt, -2 * math.pi, ALU.mult)
                kb.tt("dve", r[0:96, :], r[0:96, :], m1[0:96, :], ALU.add)
                kb.ts("dve", m1[0:96, :], r[0:96, :], -math.pi, ALU.is_lt, 2 * math.pi, ALU.mult)
                kb.tt("dve", r[0:96, :], r[0:96, :], m1[0:96, :], ALU.add)
                tab = L5.get()
                kb.act(tab[0:96, :], r[0:96, :], AF.Sin)
                tabs.append(tab)
            C96, S96 = tabs
            kb.ts("dve", S96[0:96, :], S96[0:96, :], pc("rsign", 96), ALU.mult)
            qn = []
            qr = []
            sqs = []
            for kc in range(2):
                ps = proj_f("qlat", xb, rows=128, c0=128 * kc)
                rr = L5.get()
                kb.copy("act", rr[:, :], ps)
                sq = B5.get()
                kb.act(sq[:, :], rr[:, :], AF.Square)
                qr.append(rr)
                sqs.append(sq)
            ps_s = cx.PB.get()
            kb.mm(ps_s, M["ones"], sqs[0][:, :], start=True, stop=False)
            kb.mm(ps_s, M["ones"], sqs[1][:, :], start=False, stop=True)
            tmp = F5.get()
            rstd = F5.get()
            rsqrt_act(kb, rstd[:, :], ps_s, 1.0 / 256, EPS_N, tmp[:, :])
            for kc in range(2):
                q_ = B5.get()
                kb.tt("dve", q_[:, :], qr[kc][:, :], rstd[:, :], ALU.mult)
                qn.append(q_)
            ps = proj_f("ckv", xb)
            cr = L5.get()
            kb.copy("act", cr[:, :], ps)
            sq = B5.get()
            kb.act(sq[:, :], cr[:, :], AF.Square)
            ps_s = cx.PB.get()
            kb.mm(ps_s, M["ones"], sq[:, :])
            tmp = F5.get()
            rstd = F5.get()
            rsqrt_act(kb, rstd[:, :], ps_s, 1.0 / 128, EPS_N, tmp[:, :])
            cn = B5.get()
            kb.tt("dve", cn[:, :], cr[:, :], rstd[:, :], ALU.mult)
            psK = cx.PB.get()
            proj_f("krope", xb, rows=32, out=psK[64:96, :])
            t1 = F5.get()
            kb.tt("dve", t1[64:96, :], psK[64:96, :], C96[64:96, :], ALU.mult)
            psK2 = cx.PB.get()
            proj_f("krope_sw", xb, rows=32, out=psK2[64:96, :])
            t2 = F5.get()
            kb.tt("dve", t2[64:96, :], psK2[64:96, :], S96[64:96, :], ALU.mult)
            krot = B5.get()
            kb.tt("dve", krot[64:96, :], t1[64:96, :], t2[64:96, :], ALU.add)
            for hh in range(2):
                kb.dma("sp", d["KT"][hh, 64:96, tsl], krot[64:96, :])
                psn = cx.PB.get()
                kb.mm(psn[0:64, :], ukg[:, hh, :], cn[:, :])
                kn_ = B5.get()
                kb.copy("act", kn_[0:64, :], psn[0:64, :])
                kb.dma("sp", d["KT"][hh, 0:64, tsl], kn_[0:64, :])
                psP = cx.PB.get()
                kb.mm(psP[0:96, :], uqg[:, hh, 0, 0, :], qn[0][:, :], start=True, stop=False)
                kb.mm(psP[0:96, :], uqg[:, hh, 1, 0, :], qn[1][:, :], start=False, stop=True)
                t1 = F5.get()
                kb.tt("dve", t1[0:96, :], psP[0:96, :], C96[0:96, :], ALU.mult)
                psS = cx.PB.get()
                kb.mm(psS[0:96, :], uqg[:, hh, 0, 1, :], qn[0][:, :], start=True, stop=False)
                kb.mm(psS[0:96, :], uqg[:, hh, 1, 1, :], qn[1][:, :], start=False, stop=True)
                t2 = F5.get()
                kb.tt("dve", t2[0:96, :], psS[0:96, :], S96[0:96, :], ALU.mult)
                qb_ = B5.get()
                kb.tt("dve", qb_[0:96, :], t1[0:96, :], t2[0:96, :], ALU.add)
                kb.dma("sp", d["QT"][hh, :, tsl], qb_[0:96, :])
            for blk in range(4):
                bs = slice(blk * 128, blk * 128 + 128)
                psV = cx.PQ.get()
                kb.mm(psV, cn[:, bs], uvg[:, :])
                vt = mla_vt[blk % 2]
                kb.op("act", (lambda o_, i_: (lambda h: h.activation(o_, i_, AF.Copy)))(
                    vt.ap[:, :, 0:64], psV.ap.rearrange("p (h e) -> p h e", e=64)), reads=[psV], writes=[vt])
                kb.dma("sp", d["VT"][:, :, t * 4 + blk, :], vt[:, :, :])
    kb.flush()
    st.close()
    return cx


def build_p1b(kb, S, d):
    import os as _os
    if "build_p1b" in _os.environ.get("DBG_SKIP", "").split(","):
        return None

    st = contextlib.ExitStack()
    nt = S // TT
    nb = S // 128
    banks = [kb.psum("ab%d" % i, [128, 512], F32, st) for i in range(8)]
    KTs = kb.sbuf("a_KT", [96, S], BF16, st)
    Vx = kb.sbuf("a_Vx", [128, nb, 65], BF16, st)
    onesf = kb.sbuf("a_ones", [128, 64], F32, st)
    kb.memset("pool", onesf[:, :], 1.0)
    dms = []
    for dd in range(4):
        m = kb.sbuf("a_dm%d" % dd, [128, TT], BF16, st)
        kb.memset("pool", m[:, :], 1.0)
        ma = m.ap[:, :]
        kb.op("pool", (lambda a, b: (lambda h: h.affine_select(a, a, pattern=[[1, TT]], compare_op=ALU.is_ge,
                                                               fill=0.0, base=b, channel_multiplier=-1)))(ma, -128 * dd),
              reads=[m], writes=[m])
        dms.append(m)
    Qts = [kb.sbuf("a_Q%d" % i, [96, TT], BF16, st) for i in range(2)]
    Ps = Pool(kb, "a_P", 4, [128, TT], BF16, stack=st)
    rsb = kb.sbuf("a_rs", [65, TT], F32, st)
    rbs = Pool(kb, "a_rb", 2, [64, TT], F32, stack=st)
    yos = Pool(kb, "a_yo", 2, [64, TT], BF16, stack=st)
    si = 0
    for hh in range(2):
        for c0 in range(0, S, 2048):
            c1 = min(S, c0 + 2048)
            kb.dma("sp", KTs[:, c0:c1], d["KT"][hh, :, c0:c1])
        for b0 in range(0, nb, 32):
            b1 = min(nb, b0 + 32)
            kb.dma("sp", Vx[:, b0:b1, :], d["VT"][:, hh, b0:b1, :])
        for t in range(nt):
            tsl = slice(t * TT, (t + 1) * TT)
            Qt = Qts[t % 2]
            kb.dma("sp", Qt[:, :], d["QT"][hh, :, tsl])
            psO = banks[4 + (t % 2)]
            nkb = 4 * (t + 1)

            def scores(kb_):
                nonlocal si
                dd = kb_ - 4 * t
                c0 = 128 * dd if dd > 0 else 0
                psS = banks[si % 3]
                si += 1
                kb.mm(psS[:, c0:TT], KTs[:, kb_ * 128:kb_ * 128 + 128], Qt[:, c0:TT])
                return psS, c0, dd
            nxt = scores(0)
            for kb_ in range(nkb):
                psS, c0, dd = nxt
                if kb_ + 1 < nkb:
                    nxt = scores(kb_ + 1)
                P = Ps.get()
                kb.act(P[:, c0:TT], psS[:, c0:TT], AF.Exp)
                if dd >= 0:
                    kb.tt("dve", P[:, c0:TT], P[:, c0:TT], dms[dd][:, c0:TT], ALU.mult)
                kb.mm(psO[0:65, c0:TT], Vx[:, kb_, :], P[:, c0:TT], start=(kb_ == 0), stop=(kb_ == nkb - 1))
            kb.copy("act", rsb[64:65, :], psO[64:65, :])
            psB = banks[6]
            kb.mm(psB[0:64, :], onesf[64:65, 0:64], rsb[64:65, :])
            rb = rbs.get()
            kb.recip(rb[:, :], psB[0:64, :])
            yo = yos.get()
            kb.tt("dve", yo[:, :], psO[0:64, :], rb[:, :], ALU.mult)
            kb.dma("sp", d["ydst"](hh * 64, 64, t), yo[:, :])
    kb.flush()
    st.close()


def pack_p2(inp, l):
    P = Packer(128)
    for nm in ("ln1_g", "ln1_b", "ln2_g", "ln2_b"):
        P.add(nm, inp[nm][l].reshape(8, 128).T)
    P.add("rbias", np.repeat(inp["router_bias"][l][None, :], 128, 0))
    P.add("rw", inp["router_w"].reshape(8, 128, 16).transpose(1, 0, 2).reshape(128, 128))
    sel = np.zeros((16, 16, 128), np.float32)
    for e in range(16):
        sel[e, e, :] = 1.0
    P.add("sel", np.concatenate([sel.reshape(16, 2048), np.zeros((112, 2048), np.float32)], 0))
    return P


def ln_fmajor(kb, cx, h32, gname, bname, pcs, F5, EPS, out_fn):
    Mf = cx.Mf
    ps_m = cx.PB.get()
    for j in range(8):
        kb.mm(ps_m, Mf["ones"], h32[:, j, :], start=(j == 0), stop=(j == 7))
    mean = F5.get()
    kb.act(mean[:, :], ps_m, AF.Copy, scale=1.0 / 1024)
    for j in range(8):
        kb.tt("pool", h32[:, j, :], h32[:, j, :], mean[:, :], ALU.subtract)
    ps_v = cx.PB.get()
    for j in range(8):
        sq = F5.get()
        kb.act(sq[:, :], h32[:, j, :], AF.Square)
        kb.mm(ps_v, Mf["ones"], sq[:, :], start=(j == 0), stop=(j == 7))
    tmp = F5.get()
    rstd = F5.get()
    rsqrt_act(kb, rstd[:, :], ps_v, 1.0 / 1024, EPS, tmp[:, :])
    for j in range(8):
        o = F5.get()
        kb.tt("dve", o[:, :], h32[:, j, :], rstd[:, :], ALU.mult)
        kb.ts("dve", o[:, :], o[:, :], pcs(gname, j), ALU.mult, pcs(bname, j), ALU.add)
        out_fn(j, o)


def build_p2(kb, T, l, d):
    import os as _os
    if "build_p2" in _os.environ.get("DBG_SKIP", "").split(","):
        return None

    nt = T // TT
    Po = d["P2off"]

    st = contextlib.ExitStack()
    cx = Ctx()
    setup_common(kb, cx, d["masks"], st)
    Wg = kb.sbuf("Wg", [128, 8, 4096], BF16, st)
    for k in range(8):
        for c0 in range(0, 4096, 1024):
            kb.dma("pool", Wg[:, k, c0:c0 + 1024], d["Wgate"][128 * k:128 * k + 128, c0:c0 + 1024])
    Wb = kb.sbuf("Wb", [128, 4, 4, 1024], BF16, st)
    for n in range(4):
        for kc in range(4):
            kb.dma("pool", Wb[:, n, kc, :], d["Wb"][n, 128 * kc:128 * kc + 128, :])
    F5 = Pool(kb, "p2f", 8, [128, TT], F32, stack=st)
    yst = Pool(kb, "p2ys", 12, [128, TT], BF16, stack=st)
    OH = kb.sbuf("p2oh", [128, 4], F32, st)
    kb.dma("sp", OH[:, :], d["onehot"])
    xbfs = [kb.sbuf("p2x%d" % i, [128, 8, TT], BF16, st) for i in range(2)]
    ybfs = [kb.sbuf("p2y%d" % i, [128, 16, TT], BF16, st) for i in range(2)]
    m32 = kb.sbuf("p2m", [128, 8, TT], F32, st)
    mbf = kb.sbuf("p2mb", [128, 8, TT], BF16, st)

    def load_x(t):
        for k in range(8):
            kb.dma("pool", xbfs[t % 2][:, k, :], d["xsrc2"](k, t))
    load_x(0)
    for t in range(nt):
        tsl = slice(t * TT, (t + 1) * TT)
        if t + 1 < nt:
            load_x(t + 1)
        xbf = xbfs[t % 2]
        ybf = ybfs[t % 2]
        for c in range(16):
            n_, hg_ = c // 4, c % 4
            r0 = hg_ * 512 + n_ * 128
            for jq in range(4):
                yq = yst.get()
                kb.dma("sp", yq[:, :], d["ysrc"](r0, jq, t))
                if jq == 0:
                    kb.ts("dve", ybf[:, c, :], yq[:, :], OH[:, 0:1], ALU.mult)
                else:
                    kb.stt(ybf[:, c, :], yq[:, :], OH[:, jq:jq + 1], ybf[:, c, :], ALU.mult, ALU.add)
        for n in range(4):
            for j in range(8):
                ps_g = cx.PB.get()
                for k in range(8):
                    kb.mm(ps_g, Wg[:, k, n * 1024 + 128 * j:n * 1024 + 128 * j + 128], xbf[:, k, :],
                          start=(k == 0), stop=(k == 7))
                sg = F5.get()
                kb.act(sg[:, :], ps_g, AF.Sigmoid)
                ps_p = cx.banks[2 + (j % 2)][:, :]
                for kc in range(4):
                    kb.mm(ps_p, Wb[:, n, kc, 128 * j:128 * j + 128], ybf[:, 4 * n + kc, :],
                          start=(kc == 0), stop=(kc == 3))
                if n == 0:
                    kb.tt("dve", m32[:, j, :], sg[:, :], ps_p, ALU.mult)
                else:
                    tq = F5.get()
                    kb.tt("dve", tq[:, :], sg[:, :], ps_p, ALU.mult)
                    kb.tt("pool", m32[:, j, :], m32[:, j, :], tq[:, :], ALU.add)
        for j in range(8):
            kb.copy("act", mbf[:, j, :], m32[:, j, :])
            kb.dma("sp", d["mT"][128 * j:128 * j + 128, tsl], mbf[:, j, :])
    kb.flush()
    st.close()

    st = contextlib.ExitStack()
    cx = Ctx()
    setup_common(kb, cx, d["masks"], st)
    PC = kb.sbuf("PC2", [128, d["NP2"]], F32, st)
    kb.dma("sp", PC[:, :], d["PC2"])

    def pcs(name, j):
        o, w = Po[name]
        return PC[:, o + j:o + j + 1]
    cst = kb.sbuf("cst2", [128, 2], F32, st)
    kb.memset("pool", cst[:, 0:1], LN_EPS)
    EPS = cst[:, 0:1]
    wst = Pool(kb, "wst3", 3, [128, 512], F32, stack=st)
    Wo = kb.sbuf("Wo", [128, 8, 1024], BF16, st)
    for k in range(8):
        for c0 in range(0, 1024, 512):
            s_ = wst.get()
            kb.dma("sp", s_[:, :], d["Wout"][128 * k:128 * k + 128, c0:c0 + 512])
            kb.copy("pool", Wo[:, k, c0:c0 + 512], s_[:, :])
    F5 = Pool(kb, "p2g", 10, [128, TT], F32, stack=st)
    mbf = kb.sbuf("p2mb2", [128, 8, TT], BF16, st)
    x32 = kb.sbuf("p2x32", [128, 8, TT], F32, st)
    h32 = kb.sbuf("p2h32", [128, 8, TT], F32, st)
    for t in range(nt):
        tsl = slice(t * TT, (t + 1) * TT)
        for k in range(8):
            kb.dma("sp", mbf[:, k, :], d["mT"][128 * k:128 * k + 128, tsl])
            kb.dma("sp", x32[:, k, :], d["xsrc2"](k, t))
        for j in range(8):
            ps = cx.PB.get()
            for k in range(8):
                kb.mm(ps, Wo[:, k, 128 * j:128 * j + 128], mbf[:, k, :], start=(k == 0), stop=(k == 7))
            kb.stt(h32[:, j, :], x32[:, j, :], ALPHA, ps, ALU.mult, ALU.add)

        def out1(j, o, tsl=tsl):
            kb.dma("sp", d["x1T"][128 * j:128 * j + 128, tsl], o[:, :])
        ln_fmajor(kb, cx, h32, "ln1_g", "ln1_b", pcs, F5, EPS, out1)
    kb.flush()
    st.close()

    HT = min(T, 2048)
    for half in range(T // HT):
        st = contextlib.ExitStack()
        cx = Ctx()
        setup_common(kb, cx, d["masks"], st)
        Mf = cx.Mf
        PC = kb.sbuf("PC2b", [128, d["NP2"]], F32, st)
        kb.dma("sp", PC[:, :], d["PC2"])
        PCb = kb.sbuf("PC2bb", [16, 2048], BF16, st)
        o_sel = Po["sel"][0]
        kb.copy("pool", PCb[:, :], PC[0:16, o_sel:o_sel + 2048])

        def pcs(name, j):
            o, w = Po[name]
            return PC[:, o + j:o + j + 1]
        cst = kb.sbuf("cst3", [128, 2], F32, st)
        kb.memset("pool", cst[:, 0:1], LN_EPS)
        EPS = cst[:, 0:1]
        o_rw = Po["rw"][0]
        o_rb = Po["rbias"][0]
        wst = Pool(kb, "wst4", 4, [128, 512], F32, stack=st)
        F5 = Pool(kb, "p2h", 8, [128, TT], F32, stack=st)
        S16 = Pool(kb, "s16", 24, [128, 16], F32, stack=st)
        x1bf = kb.sbuf("x1bf", [128, 8, HT], BF16, st)
        acc = kb.sbuf("acc", [128, 8, HT], F32, st)
        cTb = kb.sbuf("cTb", [16, HT], BF16, st)
        nth = HT // TT
        for t in range(nth):
            g0 = half * HT + t * TT
            rps = [cx.banks[b][:, 0:16] for b in range(4)]
            for k in range(8):
                s_ = wst.get()
                kb.dma("sp", s_[:, :], d["x1T"][128 * k:128 * k + 128, g0:g0 + TT])
                kb.copy("pool", x1bf[:, k, t * TT:(t + 1) * TT], s_[:, :])
                for blk in range(4):
                    kb.mm(rps[blk], s_[:, blk * 128:blk * 128 + 128], PC[:, o_rw + 16 * k:o_rw + 16 * k + 16],
                          start=(k == 0), stop=(k == 7))
            for blk in range(4):
                e = S16.get()
                kb.act(e[:, :], rps[blk], AF.Exp, scale=-1.0)
                kb.ts("dve", e[:, :], e[:, :], 1.0, ALU.add)
                sc = S16.get()
                kb.recip(sc[:, :], e[:, :])
                bi = S16.get()
                kb.tt("dve", bi[:, :], sc[:, :], PC[:, o_rb:o_rb + 16], ALU.add)
                b4 = bi.ap[:, :].rearrange("p (g e) -> p g e", e=4)
                p6 = S16.get()
                p6v = p6.ap[:, :].rearrange("p (g s) -> p g s", s=4)
                p6b = S16.get()
                p6bv = p6b.ap[:, :].rearrange("p (g s) -> p g s", s=4)
                pairs = [(0, 1), (0, 2), (0, 3), (1, 2), (1, 3), (2, 3)]
                for pi, (i_, j_) in enumerate(pairs):
                    dst = p6v[:, :, pi:pi + 1] if pi < 4 else p6bv[:, :, pi - 4:pi - 3]
                    dd = p6 if pi < 4 else p6b
                    kb.tt("dve", V(dst, dd.deps), V(b4[:, :, i_:i_ + 1], bi.deps), V(b4[:, :, j_:j_ + 1], bi.deps), ALU.add)
                gs = S16.get()
                gsv = gs.ap[:, 0:4].rearrange("p (g o) -> p g o", o=1)
                kb.tt("dve", V(gsv, gs.deps), V(p6v[:, :, 0:1], p6.deps), V(p6v[:, :, 1:2], p6.deps), ALU.max)
                kb.tt("dve", V(gsv, gs.deps), V(gsv, gs.deps), V(p6v[:, :, 2:3], p6.deps), ALU.max)
                kb.tt("dve", V(gsv, gs.deps), V(gsv, gs.deps), V(p6v[:, :, 3:4], p6.deps), ALU.max)
                kb.tt("dve", V(gsv, gs.deps), V(gsv, gs.deps), V(p6bv[:, :, 0:1], p6b.deps), ALU.max)
                kb.tt("dve", V(gsv, gs.deps), V(gsv, gs.deps), V(p6bv[:, :, 1:2], p6b.deps), ALU.max)
                gm = S16.get()
                kb.op("dve", (lambda o_, i_: (lambda h: h.tensor_reduce(o_, i_, AX.X, ALU.max)))(gm.ap[:, 0:1], gs.ap[:, 0:4]),
                      reads=[gs], writes=[gm])
                ing = S16.get()
                kb.ts("dve", ing[:, 0:4], gs[:, 0:4], gm[:, 0:1], ALU.is_ge)
                off = S16.get()
                kb.ts("dve", off[:, 0:4], ing[:, 0:4], 1e30, ALU.mult, -1e30, ALU.add)
                mk = S16.get()
                mk4 = mk.ap[:, :].rearrange("p (g e) -> p g e", e=4)
                ing_bc = ing.ap[:, 0:4].rearrange("p (g o) -> p g o", o=1).broadcast_to([128, 4, 4])
                off_bc = off.ap[:, 0:4].rearrange("p (g o) -> p g o", o=1).broadcast_to([128, 4, 4])
                kb.tt("dve", V(mk4, mk.deps), V(b4, bi.deps), V(ing_bc, ing.deps), ALU.mult)
                kb.tt("dve", V(mk4, mk.deps), V(mk4, mk.deps), V(off_bc, off.deps), ALU.add)
                t8 = S16.get()
                kb.op("dve", (lambda o_, i_: (lambda h: h.max(o_, i_)))(t8.ap[:, 0:8], mk.ap[:, :]), reads=[mk], writes=[t8])
                sel = S16.get()
                kb.ts("dve", sel[:, :], mk[:, :], t8[:, 1:2], ALU.is_ge)
                ws = S16.get()
                kb.tt("dve", ws[:, :], sel[:, :], sc[:, :], ALU.mult)
                den = S16.get()
                kb.op("dve", (lambda o_, i_: (lambda h: h.tensor_reduce(o_, i_, AX.X, ALU.add)))(den.ap[:, 0:1], ws.ap[:, :]),
                      reads=[ws], writes=[den])
                kb.recip(den[:, 1:2], den[:, 0:1])
                cmb = S16.get()
                kb.ts("dve", cmb[:, :], ws[:, :], den[:, 1:2], ALU.mult)
                ps_t = cx.banks[4][0:16, 0:128]
                kb.mm(ps_t, cmb[:, :], Mf["ident"])
                kb.copy("act", cTb[:, t * TT + blk * 128:t * TT + blk * 128 + 128], ps_t)
        st2 = contextlib.ExitStack()
        Wgs = [kb.sbuf("Wge%d" % i, [128, 8, 512], BF16, st2) for i in range(2)]
        Wus = [kb.sbuf("Wue%d" % i, [128, 8, 512], BF16, st2) for i in range(2)]
        Wds = [kb.sbuf("Wde%d" % i, [128, 4, 1024], BF16, st2) for i in range(2)]
        cbcs = Pool(kb, "cbc", 2, [128, TT], BF16, stack=st2)
        acts = Pool(kb, "actb", 8, [128, TT], BF16, stack=st2)
        gi = 0

        def load_expert(e):
            Wg_, Wu_, Wd_ = Wgs[e % 2], Wus[e % 2], Wds[e % 2]
            for k in range(8):
                kb.dma("pool", Wg_[:, k, :], d["moe_g"][e, 128 * k:128 * k + 128, :])
                kb.dma("pool", Wu_[:, k, :], d["moe_u"][e, 128 * k:128 * k + 128, :])
            for fc in range(4):
                kb.dma("pool", Wd_[:, fc, :], d["moe_d"][e, 128 * fc:128 * fc + 128, :])
        load_expert(0)
        for e in range(16):
            Wg_, Wu_, Wd_ = Wgs[e % 2], Wus[e % 2], Wds[e % 2]
            if e + 1 < 16:
                load_expert(e + 1)
            for t in range(nth):
                tl = slice(t * TT, (t + 1) * TT)
                ps_c = cx.banks[6][:, :]
                kb.mm(ps_c, PCb[0:16, 128 * e:128 * e + 128], cTb[:, tl])
                cbc = cbcs.get()
                kb.copy("act", cbc[:, :], ps_c)
                at = []
                for fc in range(4):
                    ps_g = cx.banks[gi % 2][:, :]
                    ps_u = cx.banks[2 + gi % 2][:, :]
                    gi += 1
                    for k in range(8):
                        kb.mm(ps_g, Wg_[:, k, 128 * fc:128 * fc + 128], x1bf[:, k, tl], start=(k == 0), stop=(k == 7))
                    for k in range(8):
                        kb.mm(ps_u, Wu_[:, k, 128 * fc:128 * fc + 128], x1bf[:, k, tl], start=(k == 0), stop=(k == 7))
                    r_ = F5.get()
                    kb.act(r_[:, :], ps_g, AF.Silu)
                    kb.tt("dve", r_[:, :], r_[:, :], ps_u, ALU.mult)
                    a_ = acts.get()
                    kb.tt("dve", a_[:, :], r_[:, :], cbc[:, :], ALU.mult)
                    at.append(a_)
                for j in range(8):
                    ps_d = cx.banks[4 + j % 2][:, :]
                    for fc in range(4):
                        kb.mm(ps_d, Wd_[:, fc, 128 * j:128 * j + 128], at[fc][:, :], start=(fc == 0), stop=(fc == 3))
                    if e == 0:
                        kb.copy("dve", acc[:, j, tl], ps_d)
                    else:
                        kb.tt("dve", acc[:, j, tl], acc[:, j, tl], ps_d, ALU.add)
        kb.flush()
        st2.close()
        x32 = kb.sbuf("b_x32", [128, 8, TT], F32, st)
        for t in range(nth):
            g0 = half * HT + t * TT
            tl = slice(t * TT, (t + 1) * TT)
            for k in range(8):
                kb.dma("sp", x32[:, k, :], d["x1T"][128 * k:128 * k + 128, g0:g0 + TT])
                kb.stt(x32[:, k, :], x32[:, k, :], ALPHA, acc[:, k, tl], ALU.mult, ALU.add)

            def out2(j, o, g0=g0):
                kb.dma("sp", d["odst"](j, g0 // TT), o[:, :])
            ln_fmajor(kb, cx, x32, "ln2_g", "ln2_b", pcs, F5, EPS, out2)
        kb.flush()
        st.close()


def _din(nc, name, arr):
    dt = I32 if arr.dtype == np.int32 else F32
    t = nc.dram_tensor(name, list(arr.shape), dt, kind="ExternalInput")
    return V(t.ap(), [Dep()])


def _dout(nc, name, shape, dt=F32):
    t = nc.dram_tensor(name, list(shape), dt, kind="ExternalOutput")
    return V(t.ap(), [Dep()])


def _dint(nc, name, shape, dt):
    t = nc.dram_tensor(name, list(shape), dt)
    return V(t.ap(), [Dep()])


GROUPS = [[0, 1, 2, 3], [4, 5, 6, 7]]


def kernel(**inputs):
    inp = {k: np.asarray(v) for k, v in inputs.items()}
    x = inp["x"]
    B, S = x.shape[0], x.shape[1]
    T = S // 4
    xT = [np.ascontiguousarray(x[b].T) for b in range(B)]
    masks = masks_host()
    scm = scanmask_host()
    packs = [[pack_p1a(inp, l, hg) for hg in range(4)] for l in range(2)]
    arrs = [[(W.build(), P.build(), M.build()) for (W, P, M) in packs[l]] for l in range(2)]
    p2 = [pack_p2(inp, l) for l in range(2)]
    p2a = [p.build() for p in p2]
    posrep = [np.ascontiguousarray(np.repeat(inp["positions"][b][None, :], 96, 0)).astype(np.int32) for b in range(B)]
    common = {"Wgate0": np.ascontiguousarray(inp["w_in"][0][:, O_GATES:O_GATES + 4096]),
              "Wgate1": np.ascontiguousarray(inp["w_in"][1][:, O_GATES:O_GATES + 4096]),
              "Wb": np.ascontiguousarray(inp["w_branch"]), "Wout": np.ascontiguousarray(inp["w_out"]),
              "PC2_0": p2a[0], "PC2_1": p2a[1], "moe_g": np.ascontiguousarray(inp["moe_w_gate"]),
              "moe_u": np.ascontiguousarray(inp["moe_w_up"]), "moe_d": np.ascontiguousarray(inp["moe_w_down"]),
              "masks": masks, "scanmask": scm}
    in_maps = []
    for b in range(B):
        for r in range(4):
            oh = np.zeros((128, 4), np.float32)
            oh[:, r] = 1.0
            m = {"xT": xT[b], "xq": np.ascontiguousarray(xT[b][:, r * T:(r + 1) * T]), "pos": posrep[b], "onehot": oh}
            for l in range(2):
                m["W%d" % l], m["PC%d" % l], m["SM%d" % l] = arrs[l][r]
            m.update(common)
            in_maps.append({"in_" + k: v for k, v in m.items()})

    nc = bass.Bass("TRN2", target_bir_lowering=False)
    kb = KB(nc)
    di = {k[3:]: _din(nc, k, v) for k, v in in_maps[0].items()}
    CW = 1024
    nyc = S // CW
    yTc = _dint(nc, "yTc", [nyc, 512, CW], BF16)
    ygc = _dint(nc, "ygc", [nyc, 2048, CW], BF16)
    ntq = T // TT
    x2c = _dint(nc, "x2c", [ntq, 2, 512, TT], F32)
    xgc = _dint(nc, "xgc", [ntq, 2, 2048, TT], F32)
    vfirst = _dint(nc, "vfirst_s", [128, S], F32)
    oT = _dout(nc, "oT", [1024, T])
    scratch = {"QT": _dint(nc, "QT", [2, 96, S], BF16), "KT": _dint(nc, "KT", [2, 96, S], BF16),
               "VT": _dint(nc, "VT", [128, 2, S // 128, 65], BF16),
               "x1T": _dint(nc, "x1T", [1024, T], F32), "mT": _dint(nc, "mT", [1024, T], BF16)}

    def ydst(row0, nrows, t):
        c, o = divmod(t * TT, CW)
        return yTc[c][row0:row0 + nrows, o:o + TT]

    ygc_c = [V(ygc.ap[c], [Dep()]) for c in range(nyc)]
    xgc_c = [[V(xgc.ap[t_][h_], [Dep()]) for h_ in range(2)] for t_ in range(ntq)]

    def ysrc(r0, jq, t):
        c, o = divmod(jq * T + t * TT, CW)
        return ygc_c[c][r0:r0 + 128, o:o + TT]

    def x2_local(k, t):
        return x2c[t][k // 4][(k % 4) * 128:(k % 4) * 128 + 128, :]

    def x_gathered(k, tt):
        q, tl = divmod(tt, ntq)
        return xgc_c[tl][k // 4][q * 512 + (k % 4) * 128:q * 512 + (k % 4) * 128 + 128, :]

    for l in range(2):
        W0, P0, M0 = packs[l][0]
        d = {"pos": di["pos"], "W": di["W%d" % l], "PC": di["PC%d" % l], "SM": di["SM%d" % l],
             "masks": di["masks"], "scanmask": di["scanmask"], "ydst": ydst, "vfirst": vfirst}
        d.update(scratch)
        d.update(Woff=W0.off, Poff=P0.off, Moff=M0.off, NW=arrs[l][0][0].shape[1], NP=arrs[l][0][1].shape[1],
                 NM=arrs[l][0][2].shape[1])
        if l == 0:
            d["xsrc"] = lambda k, t: di["xT"][128 * k:128 * k + 128, t * TT:(t + 1) * TT]
        else:
            d["xsrc"] = x_gathered
        build_p1a(kb, S, l, d)
        build_p1b(kb, S, d)
        cpq = max(1, T // CW)
        order = [jq * cpq + i for i in range(cpq) for jq in range(4)] if nyc == 4 * cpq else list(range(nyc))
        for c in order:
            kb.coll("AllGather", ygc_c[c], yTc[c], GROUPS)
        d2 = {"ysrc": ysrc, "onehot": di["onehot"], "Wgate": di["Wgate%d" % l], "Wb": di["Wb"][l], "Wout": di["Wout"][l],
              "PC2": di["PC2_%d" % l], "moe_g": di["moe_g"][l], "moe_u": di["moe_u"][l], "moe_d": di["moe_d"][l],
              "masks": di["masks"], "x1T": scratch["x1T"], "mT": scratch["mT"],
              "P2off": p2[l].off, "NP2": p2a[l].shape[1]}
        if l == 0:
            d2["xsrc2"] = lambda k, t: di["xq"][128 * k:128 * k + 128, t * TT:(t + 1) * TT]
            d2["odst"] = lambda j, t: x2_local(j, t)
        else:
            d2["xsrc2"] = x2_local
            d2["odst"] = lambda j, t: oT[128 * j:128 * j + 128, t * TT:(t + 1) * TT]
        build_p2(kb, T, l, d2)
        if l == 0:
            for t_ in range(ntq):
                for h_ in range(2):
                    kb.coll("AllGather", xgc_c[t_][h_], x2c[t_][h_], GROUPS)
    kb.flush()
    kb.close()
    res = run_bass_kernel_spmd(nc, in_maps, core_ids=list(range(len(in_maps))))
    out = np.empty((B, S, 1024), np.float32)
    for b in range(B):
        for q in range(4):
            out[b, q * T:(q + 1) * T, :] = res.results[b * 4 + q]["oT"].T
    return out
```

```python
import contextlib
import math
import numpy as np
import ml_dtypes
import concourse.bass as bass
import concourse.mybir as mybir
from concourse.bass_utils import run_bass_kernel_spmd

F32 = mybir.dt.float32
BF16 = mybir.dt.bfloat16
I32 = mybir.dt.int32
ALU = mybir.AluOpType
AF = mybir.ActivationFunctionType
AX = mybir.AxisListType

D_MODEL = 1024
IN_COLS = 9912
TT = 512
NKC = 8


class Dep:
    __slots__ = ("w", "r", "wd")

    def __init__(self):
        self.w = None
        self.r = []
        self.wd = []


class V:
    __slots__ = ("ap", "deps")

    def __init__(self, ap, deps):
        self.ap = ap
        self.deps = deps

    def __getitem__(self, k):
        return V(self.ap[k], self.deps)

    def v(self, ap):
        return V(ap, self.deps)


class Eng:
    def __init__(self, name):
        self.name = name
        self.count = 0
        self.ops = []
        self.waited = {}


class KB:
    def __init__(self, nc):
        self.nc = nc
        self.stack = contextlib.ExitStack()
        self.E = {n: Eng(n) for n in ("pe", "act", "dve", "pool", "sp")}
        import os as _os2
        self.ndma = int(_os2.environ.get("DBG_NDMA", "32"))
        self.dma_state = [0] * self.ndma
        self.dma_next = 0
        self.n_ops = 0
        self.out_tokens = []
        import os as _os
        self.limit = int(_os.environ["DBG_LIMIT"]) if _os.environ.get("DBG_LIMIT") else None
        self.dump = int(_os.environ["DBG_DUMP"]) if _os.environ.get("DBG_DUMP") else None
        self.nosame = set(_os.environ.get("DBG_NOSAME", "").split(","))

    def _nm(self, name):
        self._uid = getattr(self, "_uid", 0) + 1
        return "%s_u%d" % (name, self._uid)

    def sbuf(self, name, shape, dt, stack=None):
        t = (stack or self.stack).enter_context(self.nc.sbuf_tensor(self._nm(name), list(shape), dt))
        return V(t[:] if False else t, [Dep()])

    def psum(self, name, shape, dt=F32, stack=None):
        t = (stack or self.stack).enter_context(self.nc.psum_tensor(self._nm(name), list(shape), dt))
        return V(t, [Dep()])

    def dram(self, name, shape, dt, kind="Internal"):
        t = self.nc.dram_tensor(name, list(shape), dt, kind=kind)
        return V(t.ap(), [Dep()])

    @staticmethod
    def _deps(reads, writes):
        out = []
        for d in reads:
            if d.w is not None:
                out.append(d.w)
            out.extend(d.wd)
        for d in writes:
            if d.w is not None:
                out.append(d.w)
            out.extend(d.wd)
            out.extend(d.r)
        return out

    def _waits(self, e, reads, writes, skip):
        need = {}
        for (pn, pv) in self._deps(reads, writes):
            if pn == skip:
                continue
            if need.get(pn, 0) < pv:
                need[pn] = pv
        waits = []
        for pn, pv in need.items():
            if e.waited.get(pn, 0) < pv:
                e.waited[pn] = pv
                waits.append((pn, pv))
        return waits

    @staticmethod
    def _mark(tok, reads, writes):
        for d in reads:
            d.r.append(tok)
            if len(d.r) > 64:
                last = {}
                for (n, v) in d.r:
                    if last.get(n, 0) < v:
                        last[n] = v
                d.r = list(last.items())
        is_async = tok[0].startswith("dma") or tok[0] == "coll"
        for d in writes:
            if is_async:
                d.wd.append(tok)
                if len(d.wd) > 48:
                    last = {}
                    for (n, v) in d.wd:
                        if last.get(n, 0) < v:
                            last[n] = v
                    d.wd = list(last.items())
            else:
                d.w = tok
                d.wd = []
            d.r = []

    def op(self, eng, fn, reads=(), writes=()):
        if self.limit is not None and self.n_ops >= self.limit:
            return None
        if self.dump is not None and abs(self.n_ops - self.dump) <= 3:
            import sys as _s
            f = _s._getframe(1)
            chain = []
            while f is not None and len(chain) < 4:
                chain.append("%s:%d" % (f.f_code.co_name, f.f_lineno))
                f = f.f_back
            print("OP", self.n_ops, eng, chain)
        rd = [d for v in reads for d in v.deps]
        wd = [d for v in writes for d in v.deps]
        e = self.E[eng]
        skip = eng if (eng == "pe" or eng in self.nosame) else None
        waits = self._waits(e, rd, wd, skip)
        e.count += 1
        tok = (eng, e.count)
        e.ops.append((waits, fn, None))
        self._mark(tok, rd, wd)
        self.n_ops += 1
        return tok

    def dma(self, q, out, in_, **kw):
        if self.limit is not None and self.n_ops >= self.limit:
            return None
        rd = list(in_.deps)
        wd = list(out.deps)
        e = self.E[q]
        waits = self._waits(e, rd, wd, None)
        s = self.dma_next
        self.dma_next = (s + 1) % self.ndma
        prev = self.dma_state[s]
        name = "dma%d" % s
        if prev > 0 and e.waited.get(name, 0) < prev:
            e.waited[name] = prev
            waits.append((name, prev))
        self.dma_state[s] = prev + 16
        tok = (name, prev + 16)
        oa, ia = out.ap, in_.ap
        e.ops.append((waits, lambda h: h.dma_start(out=oa, in_=ia, **kw), name))
        self._mark(tok, rd, wd)
        self.n_ops += 1
        return tok

    def coll(self, kind, out, in_, groups):
        import os as _os
        if _os.environ.get("DBG_NOCOLL"):
            return None
        self._cidx = getattr(self, "_cidx", -1) + 1
        if _os.environ.get("DBG_COLLMASK") and not (int(_os.environ["DBG_COLLMASK"]) >> self._cidx) & 1:
            return None
        rd = list(in_.deps)
        wd = list(out.deps)
        e = self.E["pool"]
        waits = self._waits(e, rd, wd, None)
        self.ncoll = getattr(self, "ncoll", 0) + 1
        tok = ("coll", self.ncoll)
        oa, ia = out.ap, in_.ap
        e.ops.append((waits, lambda h: h.collective_compute(kind, ALU.bypass, replica_groups=groups, ins=[ia], outs=[oa]), "coll"))
        self._mark(tok, rd, wd)
        self.n_ops += 1
        return tok

    def _ensure_sems(self):
        if getattr(self, "sem", None) is None:
            self.sem = {}
            for n in ("pe", "act", "dve", "pool"):
                self.sem[n] = self.stack.enter_context(self.nc.semaphore("s_" + n))
            for i in range(self.ndma):
                self.sem["dma%d" % i] = self.stack.enter_context(self.nc.semaphore("s_dma%d" % i))
            self.sem["coll"] = self.stack.enter_context(self.nc.semaphore("s_coll"))

    def flush(self):
        self._ensure_sems()
        sem = self.sem
        import os as _os3
        if _os3.environ.get("DBG_FLUSHLOG"):
            print("FLUSH at n_ops", self.n_ops)
        toks = [(n, self.E[n].count) for n in ("pe", "act", "dve", "pool") if self.E[n].count > 0]
        toks += [("dma%d" % i, v) for i, v in enumerate(self.dma_state) if v > 0]
        if getattr(self, "ncoll", 0) > 0:
            toks.append(("coll", self.ncoll))
        for n, e in self.E.items():
            waits = []
            for (pn, pv) in toks:
                if pn == n:
                    continue
                if e.waited.get(pn, 0) < pv:
                    e.waited[pn] = pv
                    waits.append((pn, pv))
            e.ops.append((waits, None, None))
        with self.nc.Block() as block:
            def replay(e, own):
                ops = e.ops

                def body(h):
                    for waits, fn, dsem in ops:
                        for (pn, pv) in waits:
                            h.wait_ge(sem[pn], pv)
                        if fn is None:
                            continue
                        ins = fn(h)
                        if dsem == "coll":
                            ins.then_inc(sem[dsem], 1)
                        elif dsem is not None:
                            ins.then_inc(sem[dsem], 16)
                        elif own is not None:
                            ins.then_inc(sem[own], 1)
                return body
            block.sync(replay(self.E["sp"], None))
            block.tensor(replay(self.E["pe"], "pe"))
            block.scalar(replay(self.E["act"], "act"))
            block.vector(replay(self.E["dve"], "dve"))
            block.gpsimd(replay(self.E["pool"], "pool"))
        for e in self.E.values():
            e.ops = []

    def emit(self):
        self.flush()

    def close(self):
        self.stack.close()

    def mm(self, out, lhsT, rhs, start=True, stop=True):
        oa, la, ra = out.ap, lhsT.ap, rhs.ap
        return self.op("pe", lambda h: h.matmul(oa, la, ra, start=start, stop=stop),
                       reads=[lhsT, rhs], writes=[out])

    def act(self, out, in_, func, scale=1.0, bias=None, eng="act"):
        oa, ia = out.ap, in_.ap
        reads = [in_]
        kw = {}
        if isinstance(bias, V):
            reads.append(bias)
            kw["bias"] = bias.ap
        elif bias is not None:
            kw["bias"] = bias
        if isinstance(scale, V):
            reads.append(scale)
            kw["scale"] = scale.ap
        else:
            kw["scale"] = scale
        return self.op("act", lambda h: h.activation(oa, ia, func, **kw), reads=reads, writes=[out])

    def tt(self, eng, out, in0, in1, op):
        oa, a, b = out.ap, in0.ap, in1.ap
        return self.op(eng, lambda h: h.tensor_tensor(oa, a, b, op), reads=[in0, in1], writes=[out])

    def ts(self, eng, out, in0, s1, op0, s2=None, op1=None):
        oa, a = out.ap, in0.ap
        reads = [in0]
        if isinstance(s1, V):
            reads.append(s1)
            s1 = s1.ap
        if isinstance(s2, V):
            reads.append(s2)
            s2 = s2.ap
        if op1 is None:
            return self.op(eng, lambda h: h.tensor_scalar(oa, a, s1, None, op0), reads=reads, writes=[out])
        return self.op(eng, lambda h: h.tensor_scalar(oa, a, s1, s2, op0, op1), reads=reads, writes=[out])

    def stt(self, out, in0, scalar, in1, op0, op1):
        oa, a, b = out.ap, in0.ap, in1.ap
        reads = [in0, in1]
        if isinstance(scalar, V):
            reads.append(scalar)
            scalar = scalar.ap
        return self.op("dve", lambda h: h.scalar_tensor_tensor(oa, a, scalar, b, op0, op1),
                       reads=reads, writes=[out])

    def copy(self, eng, out, in_):
        oa, ia = out.ap, in_.ap
        if eng == "act":
            return self.op("act", lambda h: h.activation(oa, ia, AF.Copy), reads=[in_], writes=[out])
        return self.op(eng, lambda h: h.tensor_copy(oa, ia), reads=[in_], writes=[out])

    def memset(self, eng, out, val):
        oa = out.ap
        return self.op(eng, lambda h: h.memset(oa, val), writes=[out])

    def scan(self, out, d0, d1, init, op0, op1):
        oa, a, b = out.ap, d0.ap, d1.ap
        return self.op("dve", lambda h: h.tensor_tensor_scan(oa, a, b, init, op0, op1),
                       reads=[d0, d1], writes=[out])

    def recip(self, out, in_):
        oa, ia = out.ap, in_.ap
        return self.op("dve", lambda h: h.reciprocal(oa, ia), reads=[in_], writes=[out])


class Pool:
    def __init__(self, kb, name, n, shape, dt, space="sbuf", stack=None):
        mk = kb.sbuf if space == "sbuf" else kb.psum
        self.t = [mk("%s%d" % (name, i), shape, dt, stack=stack) for i in range(n)]
        self.i = 0

    def get(self):
        t = self.t[self.i]
        self.i = (self.i + 1) % len(self.t)
        return t


C_W = math.exp(-0.5)
MLA_SCALE = 96 ** -0.5
LN_EPS = 1e-5
NORM_EPS = 1e-6
RWKV_LN_EPS = 64e-5
ALPHA = (2.0 * 2) ** 0.25

_IW = (256, 160, 256, 256, 512, 16, 512, 1536, 4, 4, 512, 1792, 4096)
_IO = np.concatenate([[0], np.cumsum(_IW)]).tolist()
(O_QLAT, O_KVLAT, O_GQ, O_GK, O_GV, O_GGATE, O_GOG, O_DQKV, O_DBETA, O_DA, O_DZ, O_RW, O_GATES) = _IO[:13]


class Packer:
    def __init__(self, rows):
        self.rows = rows
        self.blocks = []
        self.off = {}
        self.n = 0

    def add(self, name, arr):
        arr = np.asarray(arr, dtype=np.float32)
        if arr.ndim == 1:
            arr = arr[:, None]
        if arr.shape[0] < self.rows:
            pad = np.zeros((self.rows - arr.shape[0], arr.shape[1]), np.float32)
            arr = np.concatenate([arr, pad], 0)
        self.off[name] = (self.n, arr.shape[1])
        self.blocks.append(arr)
        self.n += arr.shape[1]

    def build(self):
        return np.ascontiguousarray(np.concatenate(self.blocks, 1))


def const_masks():
    i = np.arange(128)
    same = (i[:, None] // 64) == (i[None, :] // 64)
    m = {}
    m["U_incl"] = (same & (i[:, None] <= i[None, :])).astype(np.float32)
    m["U_strict"] = (same & (i[:, None] < i[None, :])).astype(np.float32)
    m["L_incl"] = m["U_incl"].T.copy()
    m["L_strict"] = m["U_strict"].T.copy()
    m["ident"] = np.eye(128, dtype=np.float32)
    m["ones"] = np.ones((128, 128), np.float32)
    m["ones_blk"] = same.astype(np.float32)
    m["istack"] = np.concatenate([np.eye(64), np.eye(64)], 0).astype(np.float32)
    return m


def pack_p1a(inp, l, hg):
    w_in = inp["w_in"][l]
    W = Packer(1024)
    W.add("qlat", w_in[:, O_QLAT:O_QLAT + 256])
    W.add("ckv", w_in[:, O_KVLAT:O_KVLAT + 128])
    kr = w_in[:, O_KVLAT + 128:O_KVLAT + 160]
    W.add("krope", kr)
    W.add("krope_sw", np.concatenate([kr[:, 16:32], kr[:, 0:16]], 1))
    W.add("gq", w_in[:, O_GQ + 64 * hg:O_GQ + 64 * hg + 64])
    W.add("gk", w_in[:, O_GK + 64 * hg:O_GK + 64 * hg + 64])
    W.add("gv", w_in[:, O_GV + 128 * hg:O_GV + 128 * hg + 128])
    W.add("ggate", w_in[:, O_GGATE:O_GGATE + 16])
    W.add("gog", w_in[:, O_GOG + 128 * hg:O_GOG + 128 * hg + 128])
    W.add("dq", w_in[:, O_DQKV + 128 * hg:O_DQKV + 128 * hg + 128])
    W.add("dk", w_in[:, O_DQKV + 512 + 128 * hg:O_DQKV + 512 + 128 * hg + 128])
    W.add("dv", w_in[:, O_DQKV + 1024 + 128 * hg:O_DQKV + 1024 + 128 * hg + 128])
    W.add("dbeta", np.repeat(w_in[:, O_DBETA + hg:O_DBETA + hg + 1], 128, 1))
    W.add("da", np.repeat(w_in[:, O_DA + hg:O_DA + hg + 1], 128, 1))
    W.add("dz", w_in[:, O_DZ + 128 * hg:O_DZ + 128 * hg + 128])
    for i, nm in enumerate(("rr", "rk", "rv")):
        W.add(nm, w_in[:, O_RW + 512 * i + 128 * hg:O_RW + 512 * i + 128 * hg + 128])
    W.add("rwl", w_in[:, O_RW + 1536:O_RW + 1600])
    W.add("ral", w_in[:, O_RW + 1600:O_RW + 1664])
    W.add("rgl", w_in[:, O_RW + 1664:O_RW + 1792])
    if l > 0:
        W.add("rvr", inp["w_in_vres"][l - 1])
    P = Packer(128)
    ch = slice(128 * hg, 128 * hg + 128)
    mu = inp["rwkv_mu"][l]
    P.add("mu_r", mu[0:512][ch]); P.add("mu_k", mu[512:1024][ch]); P.add("mu_v", mu[1024:1536][ch])
    P.add("mu_wl", mu[1536:1600]); P.add("mu_al", mu[1600:1664]); P.add("mu_gl", mu[1664:1792])
    P.add("w0", inp["rwkv_w0"][l][ch]); P.add("a0", inp["rwkv_a0"][l][ch])
    P.add("k_k", inp["rwkv_k_k"][l][ch]); P.add("k_a", inp["rwkv_k_a"][l][ch])
    P.add("r_k", inp["rwkv_r_k"][l].reshape(-1)[ch])
    P.add("ln_g", inp["rwkv_ln_g"][l][ch]); P.add("ln_b", inp["rwkv_ln_b"][l][ch])
    if l > 0:
        P.add("mu_vr", inp["rwkv_vres_mu"][l - 1]); P.add("v0", inp["rwkv_v0"][l - 1][ch])
    P.add("gla_norm", inp["gla_norm"][l]); P.add("gdn_norm", inp["gdn_norm"][l])
    cw = inp["gdn_conv"][l]
    for nm, o in (("cq", 0), ("ck", 512), ("cv", 1024)):
        P.add("conv_" + nm, cw[:, o + 128 * hg:o + 128 * hg + 128].T)
    P.add("a_log", np.full(128, inp["gdn_a_log"][l][hg])); P.add("dt_bias", np.full(128, inp["gdn_dt_bias"][l][hg]))
    P.add("q_norm", inp["mla_q_norm"][l].reshape(2, 128).T)
    P.add("kv_norm", inp["mla_kv_norm"][l])
    invf = (10000.0 ** (-np.arange(16, dtype=np.float32) / 16)).astype(np.float32)
    P.add("invf", np.concatenate([np.zeros(64, np.float32), invf, invf]))
    P.add("rsign", np.concatenate([np.zeros(64, np.float32), -np.ones(16, np.float32), np.ones(16, np.float32)]))
    M = Packer(128)
    M.add("gup", np.concatenate([inp["gla_w_gate_up"][l][:, 64 * hg:64 * hg + 64],
                                 inp["gla_b_gate"][l][None, 64 * hg:64 * hg + 64]], 0))
    M.add("w_up", inp["rwkv_w_up"][l][:, ch]); M.add("a_up", inp["rwkv_a_up"][l][:, ch])
    M.add("g_up", inp["rwkv_g_up"][l][:, ch])
    if l > 0:
        M.add("v_up", inp["rwkv_v_up"][l - 1][:, ch])
    uq = inp["mla_w_uq"][l]
    ukv = inp["mla_w_ukv"][l]
    for hh in range(2):
        h = 2 * hg + hh
        q = uq[:, 96 * h:96 * h + 96]
        for kc in range(2):
            M.add("uq%d_%d" % (hh, kc), q[128 * kc:128 * kc + 128])
            sw = np.concatenate([np.zeros((128, 64), np.float32), q[128 * kc:128 * kc + 128, 80:96],
                                 q[128 * kc:128 * kc + 128, 64:80]], 1)
            M.add("uqs%d_%d" % (hh, kc), sw)
        M.add("ukn%d" % hh, ukv[:, 128 * h:128 * h + 64])
    M.add("ukv_v", np.concatenate([ukv[:, 128 * (2 * hg) + 64:128 * (2 * hg) + 128],
                                   ukv[:, 128 * (2 * hg + 1) + 64:128 * (2 * hg + 1) + 128]], 1))
    return W, P, M


class Ctx:
    pass


def setup_common(kb, cx, masks_d, stack):
    mk = const_masks()
    names = ["U_strict", "U_incl", "L_incl", "L_strict", "ident", "ones", "ones_blk"]
    cx.mf = kb.sbuf("maskf", [128, 7 * 128 + 64], F32, stack)
    cx.mb = kb.sbuf("maskb", [128, 7 * 128 + 64], BF16, stack)
    kb.dma("sp", cx.mf[:, :], masks_d)
    kb.copy("pool", cx.mb[:, :], cx.mf[:, :])
    cx.M = {}
    cx.Mf = {}
    for i, n in enumerate(names):
        cx.M[n] = cx.mb[:, 128 * i:128 * i + 128]
        cx.Mf[n] = cx.mf[:, 128 * i:128 * i + 128]
    cx.M["istack"] = cx.mb[:, 896:960]
    cx.Mf["istack"] = cx.mf[:, 896:960]
    cx.M["pairU"] = cx.mb[:, 0:256]
    banks = [kb.psum("pb%d" % i, [128, 512], F32, stack) for i in range(8)]
    cx.banks = banks

    class RR:
        def __init__(self, items):
            self.items = items
            self.i = 0

        def get(self):
            x = self.items[self.i]
            self.i = (self.i + 1) % len(self.items)
            return x
    cx.RR = RR
    cx.PB = RR([banks[i][:, :] for i in range(2)])
    cx.PH = RR([banks[2][:, 256 * j:256 * j + 256] for j in range(2)])
    cx.PQ = RR([banks[3 + i][:, 128 * j:128 * j + 128] for j in range(4) for i in range(3)])
    cx.PD = [banks[6 + i][:, 128 * j:128 * j + 128] for i in range(2) for j in range(4)]


def masks_host():
    mk = const_masks()
    names = ["U_strict", "U_incl", "L_incl", "L_strict", "ident", "ones", "ones_blk"]
    return np.ascontiguousarray(np.concatenate([mk[n] for n in names] + [mk["istack"]], 1).astype(np.float32))


def scanmask_host():
    m = np.ones((128, TT), np.float32)
    m[:, ::64] = 0.0
    return m


def rsqrt_act(kb, out, in_, scale, eps, tmp):
    kb.act(tmp, in_, AF.Ln, scale=scale, bias=eps)
    kb.act(out, tmp, AF.Exp, scale=-0.5)


def sigmoid_into(kb, out, in_, tmp, scale=1.0, bias=None, eng="dve"):
    kb.act(tmp, in_, AF.Exp, scale=-scale, bias=bias)
    kb.ts(eng, tmp, tmp, 1.0, ALU.add)
    kb.recip(out, tmp)


def neumann_multi(kb, cx, PAs, PBs, pool, ident):
    n = len(PAs)
    TTs = []
    for i in range(n):
        T0 = pool.get()
        kb.tt("dve", T0[:, :], PBs[i], ident, ALU.add)
        TTs.append(T0[:, :])
    A = list(PAs)
    B = list(PBs)
    for k in range(1, 6):
        pss = []
        for i in range(n):
            ps = cx.PQ.get()
            kb.mm(ps, B[i], A[i])
            pss.append(ps)
        nAs = []
        for i in range(n):
            nA = pool.get()
            kb.copy("act", nA[:, :], pss[i])
            nAs.append(nA[:, :])
        nBs = B
        if k < 5:
            pss = []
            for i in range(n):
                ps = cx.PQ.get()
                kb.mm(ps, A[i], B[i])
                pss.append(ps)
            nBs = []
            for i in range(n):
                nB = pool.get()
                kb.copy("dve", nB[:, :], pss[i])
                nBs.append(nB[:, :])
        pss = []
        for i in range(n):
            ps = cx.PQ.get()
            kb.mm(ps, nAs[i], TTs[i])
            pss.append(ps)
        for i in range(n):
            nT = pool.get()
            kb.tt("dve", nT[:, :], TTs[i], pss[i], ALU.add)
            TTs[i] = nT[:, :]
        A, B = nAs, nBs
    return TTs


def neumann(kb, cx, PA, PB_, pool_bf, ident_b):
    TT_ = pool_bf.get()
    kb.tt("dve", TT_[:, :], PB_, ident_b, ALU.add)
    A, B = PA, PB_
    for k in range(1, 6):
        psA = cx.PQ.get()
        kb.mm(psA, B, A)
        nA = pool_bf.get()
        kb.copy("act", nA[:, :], psA)
        if k < 5:
            psB = cx.PQ.get()
            kb.mm(psB, A, B)
            nB = pool_bf.get()
            kb.copy("pool" if False else "dve", nB[:, :], psB)
        psT = cx.PQ.get()
        kb.mm(psT, nA[:, :], TT_[:, :])
        nT = pool_bf.get()
        kb.tt("dve", nT[:, :], TT_[:, :], psT, ALU.add)
        TT_ = nT
        A = nA[:, :]
        if k < 5:
            B = nB[:, :]
    return TT_[:, :]


def build_p1a(kb, S, l, d, branches=("gla", "gdn", "rwkv", "mla")):
    import os as _os
    if "build_p1a" in _os.environ.get("DBG_SKIP", "").split(","):
        return None

    st = contextlib.ExitStack()
    cx = Ctx()
    setup_common(kb, cx, d["masks"], st)
    Wo, Po, Mo = d["Woff"], d["Poff"], d["Moff"]
    NW, NP, NM = d["NW"], d["NP"], d["NM"]
    nt = S // TT
    M, Mf = cx.M, cx.Mf

    PC = kb.sbuf("PC", [128, NP], F32, st)
    kb.dma("sp", PC[:, :], d["PC"])
    SMf = kb.sbuf("SMf", [128, NM], F32, st)
    kb.dma("sp", SMf[:, :], d["SM"])
    SMb = kb.sbuf("SMb", [128, NM], BF16, st)
    kb.copy("pool", SMb[:, :], SMf[:, :])
    scm = kb.sbuf("scm", [128, TT], F32, st)
    kb.dma("sp", scm[:, :], d["scanmask"])
    Wbf = kb.sbuf("Wbf", [128, NKC, NW], BF16, st)
    wst = Pool(kb, "wst", 2, [128, 512], F32, stack=st)
    for k in range(NKC):
        for c0 in range(0, NW, 512):
            cw_ = min(512, NW - c0)
            s_ = wst.get()
            kb.dma("sp", s_[:, 0:cw_], d["W"][128 * k:128 * k + 128, c0:c0 + cw_])
            kb.copy("pool", Wbf[:, k, c0:c0 + cw_], s_[:, 0:cw_])

    def pc(name, rows=128, j=0):
        o, w = Po[name]
        return PC[0:rows, o + j:o + j + 1]

    def sm(name, rows, cols=None):
        o, w = Mo[name]
        return SMb[0:rows, o:o + (cols or w)]

    def wcol(name, k, c0=0, cn=None):
        o, w = Wo[name]
        return Wbf[:, k, o + c0:o + c0 + (cn or w)]

    cst = kb.sbuf("cst", [128, 8], F32, st)
    kb.memset("pool", cst[:, 0:1], NORM_EPS)
    kb.memset("pool", cst[:, 1:2], RWKV_LN_EPS)
    kb.memset("pool", cst[:, 2:3], 1.0)
    kb.memset("pool", cst[:, 3:4], 0.0)
    EPS_N, EPS_R, ONE_C, ZERO_C = cst[:, 0:1], cst[:, 1:2], cst[:, 2:3], cst[:, 3:4]
    ncol = kb.sbuf("ncol", [128, 8], F32, st)
    kb.ts("pool", ncol[:, 0:1], pc("w0"), -1.0, ALU.mult)
    kb.ts("pool", ncol[:, 1:2], pc("a0"), -1.0, ALU.mult)
    if l > 0:
        kb.ts("pool", ncol[:, 2:3], pc("v0"), -1.0, ALU.mult)
    kb.act(ncol[:, 3:4], pc("a_log"), AF.Exp)

    F5 = Pool(kb, "f5_", 10, [128, TT], F32, stack=st)
    L5 = Pool(kb, "l5_", 18, [128, TT], F32, stack=st)
    B5 = Pool(kb, "b5_", 10, [128, TT], BF16, stack=st)
    BQ = Pool(kb, "bq_", 40, [128, 128], BF16, stack=st)
    FQ = Pool(kb, "fq_", 16, [128, 128], F32, stack=st)
    kb_rhs = Pool(kb, "rhs_", 4, [128, 256], BF16, stack=st)
    BL = Pool(kb, "bl_", 8, [128, 128], BF16, stack=st)
    xst = Pool(kb, "xst", 3, [128, TT], F32, stack=st)
    xbfs = [kb.sbuf("xbf%d" % i, [128, NKC, TT], BF16, st) for i in range(2)]

    def proj_f(name, xb, rows=None, c0=0, out=None):
        o, w = Wo[name]
        rows = rows or w
        ps = out if out is not None else cx.PB.get()[0:rows, :]
        for k in range(NKC):
            kb.mm(ps, wcol(name, k, c0, rows), xb[:, k, :], start=(k == 0), stop=(k == NKC - 1))
        return ps

    def shift_mix(ps, rows, mu, halo, t):
        raw = halo[t % 2]
        kb.copy("act", raw[0:rows, 1:TT + 1], ps)
        if t == 0:
            kb.memset("pool", raw[0:rows, 0:1], 0.0)
        kb.copy("pool", halo[(t + 1) % 2][0:rows, 0:1], raw[0:rows, TT:TT + 1])
        dd = F5.get()
        kb.tt("dve", dd[0:rows, :], raw[0:rows, 0:TT], raw[0:rows, 1:TT + 1], ALU.subtract)
        out = F5.get()
        kb.stt(out[0:rows, :], dd[0:rows, :], mu, raw[0:rows, 1:TT + 1], ALU.mult, ALU.add)
        return out[0:rows, :]

    if "gla" in branches:
        gl_H32 = kb.sbuf("gl_H32", [64, 128], F32, st)
        gl_Hbf = kb.sbuf("gl_Hbf", [64, 128], BF16, st)
        kb.memset("pool", gl_H32[:, :], 0.0)
        kb.memset("pool", gl_Hbf[:, :], 0.0)
        gl_ext = kb.sbuf("gl_ext", [32, TT], BF16, st)
        kb.memset("pool", gl_ext[:, :], 1.0)
    if "gdn" in branches:
        gd_H32 = kb.sbuf("gd_H32", [128, 128], F32, st)
        gd_Hbf = kb.sbuf("gd_Hbf", [128, 128], BF16, st)
        kb.memset("pool", gd_H32[:, :], 0.0)
        kb.memset("pool", gd_Hbf[:, :], 0.0)
        gd_halo = {n: [kb.sbuf("gdh_%s" % n, [128, TT + 3], F32, st), kb.sbuf("gdc_%s" % n, [128, 4], F32, st)] for n in ("q", "k", "v")}
    if "rwkv" in branches:
        rw_H32 = kb.sbuf("rw_H32", [128, 64], F32, st)
        rw_Hbf = kb.sbuf("rw_Hbf", [128, 64], BF16, st)
        kb.memset("pool", rw_H32[:, :], 0.0)
        kb.memset("pool", rw_Hbf[:, :], 0.0)
        rw_halo = {n: [kb.sbuf("rwh_%s" % n, [128, TT + 1], F32, st), kb.sbuf("rwc_%s" % n, [128, 2], F32, st)]
                   for n in ("r", "k", "v", "wl", "al", "gl", "vr")}
        rw_AR = kb.sbuf("rw_AR", [128, 2 * TT], BF16, st)
        rw_MT = kb.sbuf("rw_MT", [128, 128], F32, st)
        kb.memset("pool", rw_MT[:, :], 0.0)
    if "mla" in branches:
        mla_posi = kb.sbuf("mla_posi", [96, TT], I32, st)
        mla_vt = [kb.sbuf("mla_vt%d" % i, [128, 2, 65], BF16, st) for i in range(2)]
        for i in range(2):
            kb.memset("pool", mla_vt[i][:, :, :], 1.0)
        mla_ni = kb.sbuf("mla_ni", [96, TT], I32, st)
        uqg = kb.sbuf("uqg", [128, 2, 2, 2, 96], BF16, st)
        for hh in range(2):
            for kc in range(2):
                for sw, nm in enumerate(("uq", "uqs")):
                    o, w = Mo["%s%d_%d" % (nm, hh, kc)]
                    kb.ts("pool", uqg[:, hh, kc, sw, :], SMf[:, o:o + 96], pc("q_norm", 128, kc), ALU.mult,
                          MLA_SCALE, ALU.mult)
        ukg = kb.sbuf("ukg", [128, 2, 64], BF16, st)
        for hh in range(2):
            o, w = Mo["ukn%d" % hh]
            kb.ts("pool", ukg[:, hh, :], SMf[:, o:o + 64], pc("kv_norm"), ALU.mult)
        uvg = kb.sbuf("uvg", [128, 128], BF16, st)
        o, w = Mo["ukv_v"]
        kb.ts("pool", uvg[:, :], SMf[:, o:o + 128], pc("kv_norm"), ALU.mult)

    def silu_of(src):
        sg = L5.get()
        tmp2 = F5.get()
        sigmoid_into(kb, sg[:, :], src, tmp2[:, :])
        kb.tt("dve", sg[:, :], sg[:, :], src, ALU.mult)
        return sg

    def post_norm_gate(oT, gate_sb, gcol, row0):
        osq = B5.get()
        kb.act(osq[:, :], oT[:, :], AF.Square)
        ps_s = cx.PB.get()
        kb.mm(ps_s, M["ones"], osq[:, :])
        tmp = F5.get()
        rstd = F5.get()
        rsqrt_act(kb, rstd[:, :], ps_s, 1.0 / 128, EPS_N, tmp[:, :])
        sg = silu_of(gate_sb)
        kb.stt(oT[:, :], oT[:, :], gcol, rstd[:, :], ALU.mult, ALU.mult)
        yb = B5.get()
        kb.tt("dve", yb[:, :], oT[:, :], sg[:, :], ALU.mult)
        kb.dma("sp", d["ydst"](row0, 128, tsl_box[0].start // TT), yb[:, :])

    def l2norm_f(x, ones_m, scale):
        sq = B5.get()
        kb.act(sq[:, :], x, AF.Square)
        ps_s = cx.PB.get()
        kb.mm(ps_s, ones_m, sq[:, :])
        tmp = F5.get()
        rs = F5.get()
        rsqrt_act(kb, rs[:, :], ps_s, 1.0, EPS_N, tmp[:, :])
        out = L5.get()
        kb.stt(out[:, :], x, scale, rs[:, :], ALU.mult, ALU.mult)
        return out

    def transpose_bf(src, dst_pool=None):
        ps_t = cx.PQ.get()
        kb.mm(ps_t, src, M["ident"])
        o = BL.get()
        kb.copy("act", o[:, :], ps_t)
        return o

    tsl_box = [None]
    youts = {}

    def load_x(t_):
        for k in range(NKC):
            kb.dma("pool", xbfs[t_ % 2][:, k, :], d["xsrc"](k, t_))
    load_x(0)
    for t in range(nt):
        tsl = slice(t * TT, (t + 1) * TT)
        tsl_box[0] = tsl
        xb = xbfs[t % 2]
        if t + 1 < nt:
            load_x(t + 1)

        if "gla" in branches:
            L5.i = 0
            ps_q = proj_f("gq", xb)
            qraw = L5.get()
            kb.copy("act", qraw[0:64, :], ps_q)
            ps_k = proj_f("gk", xb)
            kraw = L5.get()
            kb.copy("act", kraw[0:64, :], ps_k)
            ps_g = proj_f("ggate", xb)
            kb.copy("act", gl_ext[0:16, :], ps_g)
            ps_og = proj_f("gog", xb)
            og = L5.get()
            kb.copy("act", og[:, :], ps_og)
            ps_z = cx.PB.get()[0:64, :]
            kb.mm(ps_z, sm("gup", 17), gl_ext[0:17, :])
            e1 = F5.get()
            kb.act(e1[0:64, :], ps_z, AF.Exp, scale=-1.0)
            sp = F5.get()
            kb.act(sp[0:64, :], e1[0:64, :], AF.Ln, bias=ONE_C[0:64, :])
            bsp = L5.get()
            kb.scan(bsp[0:64, :], scm[0:64, :], sp[0:64, :], 0.0, ALU.mult, ALU.add)
            epos = L5.get()
            kb.act(epos[0:64, :], bsp[0:64, :], AF.Exp, scale=-1.0 / 16)
            eneg = F5.get()
            kb.act(eneg[0:64, :], bsp[0:64, :], AF.Exp, scale=1.0 / 16)
            b3 = bsp.ap[0:64, :].rearrange("p (c t) -> p c t", t=64)
            dl = F5.get()
            dl3 = dl.ap[0:64, :].rearrange("p (c t) -> p c t", t=64)
            bl_bc = b3[:, :, 63:64].broadcast_to([64, TT // 64, 64])
            kb.tt("dve", V(dl3, dl.deps), V(b3, bsp.deps), V(bl_bc, bsp.deps), ALU.subtract)
            eend = F5.get()
            kb.act(eend[0:64, :], dl[0:64, :], AF.Exp, scale=1.0 / 16)
            qd = B5.get()
            kb.stt(qd[0:64, :], qraw[0:64, :], 0.125, epos[0:64, :], ALU.mult, ALU.mult)
            ki = B5.get()
            kb.tt("dve", ki[0:64, :], kraw[0:64, :], eneg[0:64, :], ALU.mult)
            ke = B5.get()
            kb.tt("dve", ke[0:64, :], kraw[0:64, :], eend[0:64, :], ALU.mult)
            oT = L5.get()
            for blk in range(4):
                bs = slice(blk * 128, blk * 128 + 128)
                ps_v = cx.PQ.get()
                for k in range(NKC):
                    kb.mm(ps_v, xb[:, k, bs], wcol("gv", k), start=(k == 0), stop=(k == NKC - 1))
                vt = BQ.get()
                kb.copy("act", vt[:, :], ps_v)
                ps_t = cx.PQ.get()[:, 0:64]
                kb.mm(ps_t, ke[0:64, bs], M["ident"][0:64, 0:64])
                ket = BQ.get()
                kb.copy("dve", ket[:, 0:64], ps_t)
                ps_a = cx.PQ.get()
                kb.mm(ps_a, ki[0:64, bs], qd[0:64, bs])
                am = BQ.get()
                kb.tt("dve", am[:, :], ps_a, Mf["U_incl"], ALU.mult)
                ps_o = cx.PD[0]
                kb.mm(ps_o, vt[:, :], am[:, :], start=True, stop=False)
                for c in range(2):
                    cs = slice(blk * 128 + c * 64, blk * 128 + c * 64 + 64)
                    rs = slice(c * 64, c * 64 + 64)
                    kb.mm(ps_o[:, rs], gl_Hbf[:, :], qd[0:64, cs], start=False, stop=(c == 1))
                    ps_n = cx.PD[4][0:64, :]
                    kb.mm(ps_n, ket[rs, 0:64], vt[rs, :])
                    gc = blk * 2 + c
                    acol = epos[0:64, 64 * (t * 0 + gc) + 63:64 * gc + 64]
                    kb.stt(gl_H32[:, :], gl_H32[:, :], acol, ps_n, ALU.mult, ALU.add)
                    kb.copy("act", gl_Hbf[:, :], gl_H32[:, :])
                kb.copy("act", oT[:, bs], ps_o)
            post_norm_gate(oT, og[:, :], pc("gla_norm"), 128)

        if "gdn" in branches:
            L5.i = 0
            conv = {}
            for nm, cn in (("q", "conv_cq"), ("k", "conv_ck"), ("v", "conv_cv")):
                ps = proj_f("d" + nm, xb)
                raw = gd_halo[nm][0]
                hcol = gd_halo[nm][1]
                if t == 0:
                    kb.memset("pool", raw[:, 0:3], 0.0)
                else:
                    kb.copy("pool", raw[:, 0:3], hcol[:, 0:3])
                kb.copy("act", raw[:, 3:TT + 3], ps)
                kb.copy("pool", hcol[:, 0:3], raw[:, TT:TT + 3])
                acc = F5.get()
                kb.ts("dve", acc[:, :], raw[:, 0:TT], pc(cn, 128, 0), ALU.mult)
                for j in range(1, 4):
                    kb.stt(acc[:, :], raw[:, j:j + TT], pc(cn, 128, j), acc[:, :], ALU.mult, ALU.add)
                conv[nm] = silu_of(acc[:, :])
            qn = l2norm_f(conv["q"][:, :], M["ones"], 128 ** -0.5)
            kn32 = l2norm_f(conv["k"][:, :], M["ones"], 1.0)
            kn = B5.get()
            kb.copy("dve", kn[:, :], kn32[:, :])
            qnb = B5.get()
            kb.copy("dve", qnb[:, :], qn[:, :])
            ps_b = proj_f("dbeta", xb)
            beta = L5.get()
            tb = F5.get()
            sigmoid_into(kb, beta[:, :], ps_b, tb[:, :])
            nbeta = L5.get()
            kb.ts("dve", nbeta[:, :], beta[:, :], -1.0, ALU.mult)
            ps_a = proj_f("da", xb)
            e1 = F5.get()
            kb.act(e1[:, :], ps_a, AF.Exp, bias=pc("dt_bias"))
            spl = F5.get()
            kb.act(spl[:, :], e1[:, :], AF.Ln, bias=ONE_C)
            g = F5.get()
            kb.ts("dve", g[:, :], spl[:, :], ncol[:, 3:4], ALU.mult, -1.0, ALU.mult)
            gc = L5.get()
            kb.scan(gc[:, :], scm[:, :], g[:, :], 0.0, ALU.mult, ALU.add)
            egc = L5.get()
            kb.act(egc[:, :], gc[:, :], AF.Exp)
            g3 = gc.ap[:, :].rearrange("p (c t) -> p c t", t=64)
            dl = F5.get()
            dl3 = dl.ap[:, :].rearrange("p (c t) -> p c t", t=64)
            gl_bc = g3[:, :, 63:64].broadcast_to([128, TT // 64, 64])
            kb.tt("dve", V(dl3, dl.deps), V(gl_bc, gc.deps), V(g3, gc.deps), ALU.subtract)
            eend = F5.get()
            kb.act(eend[:, :], dl[:, :], AF.Exp)
            qdec = L5.get()
            kb.tt("dve", qdec[:, :], qn[:, :], egc[:, :], ALU.mult)
            kbg = B5.get()
            tk = F5.get()
            kb.tt("dve", tk[:, :], kn32[:, :], beta[:, :], ALU.mult)
            kb.tt("dve", kbg[:, :], tk[:, :], egc[:, :], ALU.mult)
            vb = B5.get()
            kb.tt("dve", vb[:, :], conv["v"][:, :], beta[:, :], ALU.mult)
            kend = B5.get()
            kb.tt("dve", kend[:, :], kn32[:, :], eend[:, :], ALU.mult)
            ps_z = proj_f("dz", xb)
            zs = L5.get()
            kb.copy("act", zs[:, :], ps_z)
            oT = L5.get()
            for blk in range(4):
                bs = slice(blk * 128, blk * 128 + 128)
                BL.i = 0
                Rm = gc[:, bs]
                t1 = FQ.get()
                kb.tt("dve", t1[:, :], Rm, Mf["ident"], ALU.mult)
                cc = FQ.get()
                kb.op("dve", (lambda o_, i_: (lambda h: h.tensor_reduce(o_, i_, AX.X, ALU.add)))(cc.ap[:, 0:1], t1.ap[:, :]),
                      reads=[t1], writes=[cc])
                kb.tt("dve", t1[:, :], nbeta[:, bs], Mf["ident"], ALU.mult)
                kb.op("dve", (lambda o_, i_: (lambda h: h.tensor_reduce(o_, i_, AX.X, ALU.add)))(cc.ap[:, 1:2], t1.ap[:, :]),
                      reads=[t1], writes=[cc])
                ccol, nbcol = cc[:, 0:1], cc[:, 1:2]
                tB = FQ.get()
                kb.ts("dve", tB[:, :], Rm, ccol, ALU.subtract, 0.0, ALU.min)
                eB = FQ.get()
                kb.act(eB[:, :], tB[:, :], AF.Exp)
                DBi = FQ.get()
                kb.tt("dve", DBi[:, :], eB[:, :], Mf["U_incl"], ALU.mult)
                DBs = FQ.get()
                kb.tt("dve", DBs[:, :], eB[:, :], Mf["U_strict"], ALU.mult)
                kb.tt("dve", DBs[:, :], DBs[:, :], nbeta[:, bs], ALU.mult)
                tA = FQ.get()
                kb.ts("dve", tA[:, :], Rm, ccol, ALU.subtract, 0.0, ALU.max)
                eA = FQ.get()
                kb.act(eA[:, :], tA[:, :], AF.Exp, scale=-1.0)
                kb.tt("dve", eA[:, :], eA[:, :], Mf["L_strict"], ALU.mult)
                ps_kk = cx.PQ.get()
                kb.mm(ps_kk, kn[:, bs], kn[:, bs])
                PA = BQ.get()
                kb.stt(PA[:, :], eA[:, :], nbcol, ps_kk, ALU.mult, ALU.mult)
                PB_ = BQ.get()
                kb.tt("dve", PB_[:, :], ps_kk, DBs[:, :], ALU.mult)
                TTm = neumann(kb, cx, PA[:, :], PB_[:, :], BQ, M["ident"])
                rhs = kb_rhs.get()
                ps_t = cx.PQ.get()
                kb.mm(ps_t, vb[:, bs], M["ident"])
                kb.copy("act", rhs[:, 0:128], ps_t)
                ps_t = cx.PQ.get()
                kb.mm(ps_t, kbg[:, bs], M["ident"])
                kb.copy("act", rhs[:, 128:256], ps_t)
                kendt = transpose_bf(kend[:, bs])
                ps_uw = cx.PH.get()
                kb.mm(ps_uw, TTm, rhs[:, :])
                UW = kb_rhs.get()
                kb.copy("act", UW[:, :], ps_uw)
                ps_qk = cx.PQ.get()
                kb.mm(ps_qk, kn[:, bs], qnb[:, bs])
                attB = BL.get()
                kb.tt("dve", attB[:, :], ps_qk, DBi[:, :], ALU.mult)
                ps_aw = cx.PQ.get()
                kb.mm(ps_aw, UW[:, 128:256], attB[:, :])
                qeff = BL.get()
                kb.tt("dve", qeff[:, :], qdec[:, bs], ps_aw, ALU.subtract)
                ps_o = cx.PD[1]
                kb.mm(ps_o, UW[:, 0:128], attB[:, :], start=True, stop=False)
                for c in range(2):
                    rs = slice(c * 64, c * 64 + 64)
                    gcn = blk * 2 + c
                    kb.mm(ps_o[:, rs], gd_Hbf[:, :], qeff[:, rs], start=False, stop=(c == 1))
                    ps_m = cx.PD[5]
                    kb.mm(ps_m, UW[rs, 128:256], kendt[rs, :])
                    MT = FQ.get()
                    kb.stt(MT[:, :], Mf["ident"], egc[:, 64 * gcn + 63:64 * gcn + 64], ps_m, ALU.mult, ALU.subtract)
                    ps_h = cx.PD[6]
                    kb.mm(ps_h, MT[:, :], gd_H32[:, :], start=True, stop=False)
                    kb.mm(ps_h, kendt[rs, :], UW[rs, 0:128], start=False, stop=True)
                    kb.copy("act", gd_Hbf[:, :], ps_h)
                    kb.copy("act", gd_H32[:, :], ps_h)
                kb.copy("act", oT[:, bs], ps_o)
            post_norm_gate(oT, zs[:, :], pc("gdn_norm"), 256)

        if "rwkv" in branches:
            L5.i = 0

            def shiftmix(pname, rows, mu_name, hname):
                ps = proj_f(pname, xb)
                raw, hcol = rw_halo[hname]
                if t == 0:
                    kb.memset("pool", raw[0:rows, 0:1], 0.0)
                else:
                    kb.copy("pool", raw[0:rows, 0:1], hcol[0:rows, 0:1])
                kb.copy("act", raw[0:rows, 1:TT + 1], ps)
                kb.copy("pool", hcol[0:rows, 0:1], raw[0:rows, TT:TT + 1])
                dd = F5.get()
                kb.tt("dve", dd[0:rows, :], raw[0:rows, 0:TT], raw[0:rows, 1:TT + 1], ALU.subtract)
                out = L5.get()
                kb.stt(out[0:rows, :], dd[0:rows, :], pc(mu_name, rows), raw[0:rows, 1:TT + 1], ALU.mult, ALU.add)
                return out
            r_ = shiftmix("rr", 128, "mu_r", "r")
            k_ = shiftmix("rk", 128, "mu_k", "k")
            v_ = shiftmix("rv", 128, "mu_v", "v")
            wl = shiftmix("rwl", 64, "mu_wl", "wl")
            al = shiftmix("ral", 64, "mu_al", "al")
            gl = shiftmix("rgl", 128, "mu_gl", "gl")
            tmp = F5.get()
            sgt = F5.get()
            sigmoid_into(kb, sgt[0:64, :], wl[0:64, :], tmp[0:64, :], scale=2.0)
            th = B5.get()
            kb.ts("dve", th[0:64, :], sgt[0:64, :], 2.0, ALU.mult, -1.0, ALU.add)
            ps_z = cx.PB.get()
            kb.mm(ps_z, sm("w_up", 64), th[0:64, :])
            sgw = L5.get()
            tmp = F5.get()
            sigmoid_into(kb, sgw[:, :], ps_z, tmp[:, :], bias=ncol[:, 0:1])
            cs = L5.get()
            kb.scan(cs[:, :], scm[:, :], sgw[:, :], 0.0, ALU.mult, ALU.add)
            albf = B5.get()
            kb.copy("dve", albf[0:64, :], al[0:64, :])
            ps_a = cx.PB.get()
            kb.mm(ps_a, sm("a_up", 64), albf[0:64, :])
            a_ = L5.get()
            tmp = F5.get()
            sigmoid_into(kb, a_[:, :], ps_a, tmp[:, :], bias=ncol[:, 1:2])
            sgg = F5.get()
            tmp = F5.get()
            sigmoid_into(kb, sgg[:, :], gl[:, :], tmp[:, :])
            sggb = B5.get()
            kb.copy("dve", sggb[:, :], sgg[:, :])
            ps_g = cx.PB.get()
            kb.mm(ps_g, sm("g_up", 128), sggb[:, :])
            gte = L5.get()
            kb.copy("act", gte[:, :], ps_g)
            if l > 0:
                ps = proj_f("rvr", xb)
                raw, hcol = rw_halo["vr"]
                if t == 0:
                    kb.memset("pool", raw[0:32, 0:1], 0.0)
                else:
                    kb.copy("pool", raw[0:32, 0:1], hcol[0:32, 0:1])
                kb.copy("act", raw[0:32, 1:TT + 1], ps)
                kb.copy("pool", hcol[0:32, 0:1], raw[0:32, TT:TT + 1])
                dd = F5.get()
                kb.tt("dve", dd[0:32, :], raw[0:32, 0:TT], raw[0:32, 1:TT + 1], ALU.subtract)
                vrl = B5.get()
                kb.stt(vrl[0:32, :], dd[0:32, :], pc("mu_vr", 32), raw[0:32, 1:TT + 1], ALU.mult, ALU.add)
                ps_v = cx.PB.get()
                kb.mm(ps_v, sm("v_up", 32), vrl[0:32, :])
                vg = F5.get()
                tmp = F5.get()
                sigmoid_into(kb, vg[:, :], ps_v, tmp[:, :], bias=ncol[:, 2:3])
                vf = F5.get()
                kb.dma("sp", vf[:, :], d["vfirst"][:, tsl])
                kb.tt("dve", vf[:, :], vf[:, :], v_[:, :], ALU.subtract)
                kb.tt("dve", vf[:, :], vf[:, :], vg[:, :], ALU.mult)
                kb.tt("dve", v_[:, :], v_[:, :], vf[:, :], ALU.add)
            else:
                kb.dma("sp", d["vfirst"][:, tsl], v_[:, :])
            kx = F5.get()
            kb.ts("dve", kx[:, :], k_[:, :], pc("k_k"), ALU.mult)
            kk = l2norm_f(kx[:, :], M["ones_blk"], 1.0)
            tk = F5.get()
            kb.ts("dve", tk[:, :], a_[:, :], -1.0, ALU.add, pc("k_a"), ALU.mult)
            kmod = L5.get()
            kb.stt(kmod[:, :], tk[:, :], 1.0, k_[:, :], ALU.add, ALU.mult)
            bb = L5.get()
            kb.tt("dve", bb[:, :], kk[:, :], a_[:, :], ALU.mult)
            rk = B5.get()
            kb.stt(rk[:, :], r_[:, :], pc("r_k"), kmod[:, :], ALU.mult, ALU.mult)
            ps_bn = cx.PB.get()
            kb.mm(ps_bn, M["ones_blk"], rk[:, :])
            bonus = L5.get()
            kb.tt("dve", bonus[:, :], ps_bn, v_[:, :], ALU.mult)
            ecw = F5.get()
            kb.act(ecw[:, :], cs[:, :], AF.Exp, scale=-C_W)
            encw = L5.get()
            kb.act(encw[:, :], cs[:, :], AF.Exp, scale=C_W)
            dx = F5.get()
            kb.tt("dve", dx[:, :], cs[:, :], sgw[:, :], ALU.subtract)
            eprev = F5.get()
            kb.act(eprev[:, :], dx[:, :], AF.Exp, scale=-C_W)
            c3 = cs.ap[:, :].rearrange("p (c t) -> p c t", t=64)
            dl = F5.get()
            dl3 = dl.ap[:, :].rearrange("p (c t) -> p c t", t=64)
            cl_bc = c3[:, :, 63:64].broadcast_to([128, TT // 64, 64])
            kb.tt("dve", V(dl3, dl.deps), V(cl_bc, cs.deps), V(c3, cs.deps), ALU.subtract)
            eend = F5.get()
            kb.act(eend[:, :], dl[:, :], AF.Exp, scale=-C_W)
            gam = L5.get()
            kb.copy("dve", gam[:, :], ecw[:, :])
            AR = rw_AR
            AR4 = AR.ap[:, :].rearrange("p (b two i) -> p b two i", two=2, i=128)
            e4 = eprev.ap[:, :].rearrange("p (b i) -> p b i", i=128)
            k4 = kk.ap[:, :].rearrange("p (b i) -> p b i", i=128)
            kb.stt(V(AR4[:, :, 0, :], AR.deps), V(k4, kk.deps), -1.0, V(e4, eprev.deps), ALU.mult, ALU.mult)
            r4 = r_.ap[:, :].rearrange("p (b i) -> p b i", i=128)
            g4 = ecw.ap[:, :].rearrange("p (b i) -> p b i", i=128)
            kb.tt("dve", V(AR4[:, :, 1, :], AR.deps), V(r4, r_.deps), V(g4, ecw.deps), ALU.mult)
            bt = B5.get()
            kb.tt("dve", bt[:, :], bb[:, :], encw[:, :], ALU.mult)
            kt = B5.get()
            kb.tt("dve", kt[:, :], kmod[:, :], encw[:, :], ALU.mult)
            bendT = B5.get()
            kb.tt("dve", bendT[:, :], bb[:, :], eend[:, :], ALU.mult)
            kendT = B5.get()
            kb.tt("dve", kendT[:, :], kmod[:, :], eend[:, :], ALU.mult)
            vbf = B5.get()
            kb.copy("dve", vbf[:, :], v_[:, :])
            yT_ = L5.get()
            for blk in range(4):
                bs = slice(blk * 128, blk * 128 + 128)
                ARb = AR[:, blk * 256:blk * 256 + 256]
                BL.i = 0
                At_tok = transpose_bf(ARb[:, 0:128])
                V_tok = transpose_bf(vbf[:, bs])
                Bend = transpose_bf(bendT[:, bs])
                Kend = transpose_bf(kendT[:, bs])
                psQ = cx.PD[2]
                psY = cx.PD[3]
                WUs = []
                r_SB1, r_SB2, r_PA = [], [], []
                for h in range(2):
                    hp = slice(64 * h, 64 * h + 64)
                    ps1 = cx.PH.get()
                    kb.mm(ps1, bt[hp, bs], ARb[hp, :])
                    SB1 = kb_rhs.get()
                    kb.tt("dve", SB1[:, :], ps1, cx.mf[:, 0:256], ALU.mult)
                    ps2 = cx.PH.get()
                    kb.mm(ps2, kt[hp, bs], ARb[hp, :])
                    SB2 = kb_rhs.get()
                    kb.tt("dve", SB2[:, :], ps2, cx.mf[:, 0:256], ALU.mult)
                    psA = cx.PQ.get()
                    kb.mm(psA, ARb[hp, 0:128], bt[hp, bs])
                    PA = BQ.get()
                    kb.tt("dve", PA[:, :], psA, Mf["L_strict"], ALU.mult)
                    r_SB1.append(SB1)
                    r_SB2.append(SB2)
                    r_PA.append(PA[:, :])
                r_TT = neumann_multi(kb, cx, r_PA, [r_SB1[0][:, 0:128], r_SB1[1][:, 0:128]], BQ, M["ident"])
                for h in range(2):
                    hp = slice(64 * h, 64 * h + 64)
                    SB1, SB2, TTm = r_SB1[h], r_SB2[h], r_TT[h]
                    psX = cx.PQ.get()
                    kb.mm(psX[:, 0:64], SB2[:, 0:128], V_tok[:, hp])
                    Xb = BQ.get()
                    kb.copy("act", Xb[:, 0:64], psX[:, 0:64])
                    psWU = cx.PQ.get()
                    kb.mm(psWU[:, 0:64], TTm, At_tok[:, hp])
                    kb.mm(psWU[:, 64:128], TTm, Xb[:, 0:64])
                    WU = BL.get()
                    kb.copy("act", WU[:, :], psWU)
                    WUs.append(WU)
                    kb.mm(psQ[hp, :], WU[:, 0:64], SB1[:, 128:256])
                    kb.mm(psY[hp, :], WU[:, 64:128], SB1[:, 128:256], start=True, stop=False)
                    kb.mm(psY[hp, :], V_tok[:, hp], SB2[:, 128:256], start=False, stop=False)
                qeff = BL.get()
                kb.tt("dve", qeff[:, :], ARb[:, 128:256], psQ, ALU.add)
                for c in range(2):
                    rs = slice(c * 64, c * 64 + 64)
                    gcn = blk * 2 + c
                    for h in range(2):
                        hp = slice(64 * h, 64 * h + 64)
                        kb.mm(psY[hp, rs], rw_Hbf[hp, :], qeff[hp, rs], start=False, stop=(c == 1))
                    psM = cx.PD[5]
                    for h in range(2):
                        hp = slice(64 * h, 64 * h + 64)
                        kb.mm(psM[hp, 0:64], WUs[h][rs, 0:64], Bend[rs, hp])
                    MT = rw_MT
                    for h in range(2):
                        hp = slice(64 * h, 64 * h + 64)
                        kb.stt(MT[hp, 64 * h:64 * h + 64], Mf["istack"][hp, :], gam[hp, 64 * gcn + 63:64 * gcn + 64],
                               psM[hp, 0:64], ALU.mult, ALU.add)
                    psH = cx.PD[6]
                    kb.mm(psH[:, 0:64], MT[:, :], rw_H32[:, :], start=True, stop=False)
                    for h in range(2):
                        hp = slice(64 * h, 64 * h + 64)
                        kb.mm(psH[hp, 0:64], Bend[rs, hp], WUs[h][rs, 64:128], start=False, stop=False)
                        kb.mm(psH[hp, 0:64], Kend[rs, hp], V_tok[rs, hp], start=False, stop=True)
                    kb.copy("act", rw_Hbf[:, :], psH[:, 0:64])
                    kb.copy("act", rw_H32[:, :], psH[:, 0:64])
                kb.copy("act", yT_[:, bs], psY)
            ps_m = cx.PB.get()
            kb.mm(ps_m, Mf["ones_blk"], yT_[:, :])
            yc = F5.get()
            kb.stt(yc[:, :], ps_m, -1.0 / 64, yT_[:, :], ALU.mult, ALU.add)
            ysq = F5.get()
            kb.act(ysq[:, :], yc[:, :], AF.Square)
            ps_v2 = cx.PB.get()
            kb.mm(ps_v2, Mf["ones_blk"], ysq[:, :])
            tmp = F5.get()
            rstd = F5.get()
            rsqrt_act(kb, rstd[:, :], ps_v2, 1.0 / 64, EPS_R, tmp[:, :])
            kb.stt(yc[:, :], yc[:, :], pc("ln_g"), rstd[:, :], ALU.mult, ALU.mult)
            kb.stt(yc[:, :], yc[:, :], pc("ln_b"), bonus[:, :], ALU.add, ALU.add)
            yo = B5.get()
            kb.tt("dve", yo[:, :], yc[:, :], gte[:, :], ALU.mult)
            kb.dma("sp", d["ydst"](384, 128, t), yo[:, :])

        if "mla" in branches:
            L5.i = 0
            posi = mla_posi
            kb.dma("sp", posi[:, :], d["pos"][:, tsl])
            posf = L5.get()
            kb.copy("dve", posf[0:96, :], posi[:, :])
            ang = L5.get()
            kb.ts("dve", ang[0:96, :], posf[0:96, :], pc("invf", 96), ALU.mult)
            tabs = []
            for phase in (math.pi / 2, 0.0):
                y = F5.get()
                kb.ts("dve", y[0:96, :], ang[0:96, :], phase, ALU.add, 1.0 / (2 * math.pi), ALU.mult)
                ni = mla_ni
                kb.copy("dve", ni[:, :], y[0:96, :])
                nf = F5.get()
                kb.copy("dve", nf[0:96, :], ni[:, :])
                r = F5.get()
                kb.stt(r[0:96, :], nf[0:96, :], -2 * math.pi, ang[0:96, :], ALU.mult, ALU.add)
                if phase != 0.0:
                    kb.ts("dve", r[0:96, :], r[0:96, :], phase, ALU.add)
                m1 = F5.get()
                kb.ts("dve", m1[0:96, :], r[0:96, :], math.pi, ALU.is_gt, -2 * math.pi, ALU.mult)
                kb.tt("dve", r[0:96, :], r[0:96, :], m1[0:96, :], ALU.add)
                kb.ts("dve", m1[0:96, :], r[0:96, :], -math.pi, ALU.is_lt, 2 * math.pi, ALU.mult)
                kb.tt("dve", r[0:96, :], r[0:96, :], m1[0:96, :], ALU.add)
                tab = L5.get()
                kb.act(tab[0:96, :], r[0:96, :], AF.Sin)
                tabs.append(tab)
            C96, S96 = tabs
            kb.ts("dve", S96[0:96, :], S96[0:96, :], pc("rsign", 96), ALU.mult)
            qn = []
            qr = []
            sqs = []
            for kc in range(2):
                ps = proj_f("qlat", xb, rows=128, c0=128 * kc)
                rr = L5.get()
                kb.copy("act", rr[:, :], ps)
                sq = B5.get()
                kb.act(sq[:, :], rr[:, :], AF.Square)
                qr.append(rr)
                sqs.append(sq)
            ps_s = cx.PB.get()
            kb.mm(ps_s, M["ones"], sqs[0][:, :], start=True, stop=False)
            kb.mm(ps_s, M["ones"], sqs[1][:, :], start=False, stop=True)
            tmp = F5.get()
            rstd = F5.get()
            rsqrt_act(kb, rstd[:, :], ps_s, 1.0 / 256, EPS_N, tmp[:, :])
            for kc in range(2):
                q_ = B5.get()
                kb.tt("dve", q_[:, :], qr[kc][:, :], rstd[:, :], ALU.mult)
                qn.append(q_)
            ps = proj_f("ckv", xb)
            cr = L5.get()
            kb.copy("act", cr[:, :], ps)
            sq = B5.get()
            kb.act(sq[:, :], cr[:, :], AF.Square)
            ps_s = cx.PB.get()
            kb.mm(ps_s, M["ones"], sq[:, :])
            tmp = F5.get()
            rstd = F5.get()
            rsqrt_act(kb, rstd[:, :], ps_s, 1.0 / 128, EPS_N, tmp[:, :])
            cn = B5.get()
            kb.tt("dve", cn[:, :], cr[:, :], rstd[:, :], ALU.mult)
            psK = cx.PB.get()
            proj_f("krope", xb, rows=32, out=psK[64:96, :])
            t1 = F5.get()
            kb.tt("dve", t1[64:96, :], psK[64:96, :], C96[64:96, :], ALU.mult)
            psK2 = cx.PB.get()
            proj_f("krope_sw", xb, rows=32, out=psK2[64:96, :])
            t2 = F5.get()
            kb.tt("dve", t2[64:96, :], psK2[64:96, :], S96[64:96, :], ALU.mult)
            krot = B5.get()
            kb.tt("dve", krot[64:96, :], t1[64:96, :], t2[64:96, :], ALU.add)
            for hh in range(2):
                kb.dma("sp", d["KT"][hh, 64:96, tsl], krot[64:96, :])
                psn = cx.PB.get()
                kb.mm(psn[0:64, :], ukg[:, hh, :], cn[:, :])
                kn_ = B5.get()
                kb.copy("act", kn_[0:64, :], psn[0:64, :])
                kb.dma("sp", d["KT"][hh, 0:64, tsl], kn_[0:64, :])
                psP = cx.PB.get()
                kb.mm(psP[0:96, :], uqg[:, hh, 0, 0, :], qn[0][:, :], start=True, stop=False)
                kb.mm(psP[0:96, :], uqg[:, hh, 1, 0, :], qn[1][:, :], start=False, stop=True)
                t1 = F5.get()
                kb.tt("dve", t1[0:96, :], psP[0:96, :], C96[0:96, :], ALU.mult)
                psS = cx.PB.get()
                kb.mm(psS[0:96, :], uqg[:, hh, 0, 1, :], qn[0][:, :], start=True, stop=False)
                kb.mm(psS[0:96, :], uqg[:, hh, 1, 1, :], qn[1][:, :], start=False, stop=True)
                t2 = F5.get()
                kb.tt("dve", t2[0:96, :], psS[0:96, :], S96[0:96, :], ALU.mult)
                qb_ = B5.get()
                kb.tt("dve", qb_[0:96, :], t1[0:96, :], t2[0:96, :], ALU.add)
                kb.dma("sp", d["QT"][hh, :, tsl], qb_[0:96, :])
            for blk in range(4):
                bs = slice(blk * 128, blk * 128 + 128)
                psV = cx.PQ.get()
                kb.mm(psV, cn[:, bs], uvg[:, :])
                vt = mla_vt[blk % 2]
                kb.op("act", (lambda o_, i_: (lambda h: h.activation(o_, i_, AF.Copy)))(
                    vt.ap[:, :, 0:64], psV.ap.rearrange("p (h e) -> p h e", e=64)), reads=[psV], writes=[vt])
                kb.dma("sp", d["VT"][:, :, t * 4 + blk, :], vt[:, :, :])
    kb.flush()
    st.close()
    return cx


def build_p1b(kb, S, d):
    import os as _os
    if "build_p1b" in _os.environ.get("DBG_SKIP", "").split(","):
        return None

    st = contextlib.ExitStack()
    nt = S // TT
    nb = S // 128
    banks = [kb.psum("ab%d" % i, [128, 512], F32, st) for i in range(8)]
    KTs = kb.sbuf("a_KT", [96, S], BF16, st)
    Vx = kb.sbuf("a_Vx", [128, nb, 65], BF16, st)
    onesf = kb.sbuf("a_ones", [128, 64], F32, st)
    kb.memset("pool", onesf[:, :], 1.0)
    dms = []
    for dd in range(4):
        m = kb.sbuf("a_dm%d" % dd, [128, TT], BF16, st)
        kb.memset("pool", m[:, :], 1.0)
        ma = m.ap[:, :]
        kb.op("pool", (lambda a, b: (lambda h: h.affine_select(a, a, pattern=[[1, TT]], compare_op=ALU.is_ge,
                                                               fill=0.0, base=b, channel_multiplier=-1)))(ma, -128 * dd),
              reads=[m], writes=[m])
        dms.append(m)
    Qts = [kb.sbuf("a_Q%d" % i, [96, TT], BF16, st) for i in range(2)]
    Ps = Pool(kb, "a_P", 4, [128, TT], BF16, stack=st)
    rsb = kb.sbuf("a_rs", [65, TT], F32, st)
    rbs = Pool(kb, "a_rb", 2, [64, TT], F32, stack=st)
    yos = Pool(kb, "a_yo", 2, [64, TT], BF16, stack=st)
    si = 0
    for hh in range(2):
        for c0 in range(0, S, 2048):
            c1 = min(S, c0 + 2048)
            kb.dma("sp", KTs[:, c0:c1], d["KT"][hh, :, c0:c1])
        for b0 in range(0, nb, 32):
            b1 = min(nb, b0 + 32)
            kb.dma("sp", Vx[:, b0:b1, :], d["VT"][:, hh, b0:b1, :])
        for t in range(nt):
            tsl = slice(t * TT, (t + 1) * TT)
            Qt = Qts[t % 2]
            kb.dma("sp", Qt[:, :], d["QT"][hh, :, tsl])
            psO = banks[4 + (t % 2)]
            nkb = 4 * (t + 1)

            def scores(kb_):
                nonlocal si
                dd = kb_ - 4 * t
                c0 = 128 * dd if dd > 0 else 0
                psS = banks[si % 3]
                si += 1
                kb.mm(psS[:, c0:TT], KTs[:, kb_ * 128:kb_ * 128 + 128], Qt[:, c0:TT])
                return psS, c0, dd
            nxt = scores(0)
            for kb_ in range(nkb):
                psS, c0, dd = nxt
                if kb_ + 1 < nkb:
                    nxt = scores(kb_ + 1)
                P = Ps.get()
                kb.act(P[:, c0:TT], psS[:, c0:TT], AF.Exp)
                if dd >= 0:
                    kb.tt("dve", P[:, c0:TT], P[:, c0:TT], dms[dd][:, c0:TT], ALU.mult)
                kb.mm(psO[0:65, c0:TT], Vx[:, kb_, :], P[:, c0:TT], start=(kb_ == 0), stop=(kb_ == nkb - 1))
            kb.copy("act", rsb[64:65, :], psO[64:65, :])
            psB = banks[6]
            kb.mm(psB[0:64, :], onesf[64:65, 0:64], rsb[64:65, :])
            rb = rbs.get()
            kb.recip(rb[:, :], psB[0:64, :])
            yo = yos.get()
            kb.tt("dve", yo[:, :], psO[0:64, :], rb[:, :], ALU.mult)
            kb.dma("sp", d["ydst"](hh * 64, 64, t), yo[:, :])
    kb.flush()
    st.close()


def pack_p2(inp, l):
    P = Packer(128)
    for nm in ("ln1_g", "ln1_b", "ln2_g", "ln2_b"):
        P.add(nm, inp[nm][l].reshape(8, 128).T)
    P.add("rbias", np.repeat(inp["router_bias"][l][None, :], 128, 0))
    P.add("rw", inp["router_w"].reshape(8, 128, 16).transpose(1, 0, 2).reshape(128, 128))
    sel = np.zeros((16, 16, 128), np.float32)
    for e in range(16):
        sel[e, e, :] = 1.0
    P.add("sel", np.concatenate([sel.reshape(16, 2048), np.zeros((112, 2048), np.float32)], 0))
    return P


def ln_fmajor(kb, cx, h32, gname, bname, pcs, F5, EPS, out_fn):
    Mf = cx.Mf
    ps_m = cx.PB.get()
    for j in range(8):
        kb.mm(ps_m, Mf["ones"], h32[:, j, :], start=(j == 0), stop=(j == 7))
    mean = F5.get()
    kb.act(mean[:, :], ps_m, AF.Copy, scale=1.0 / 1024)
    for j in range(8):
        kb.tt("pool", h32[:, j, :], h32[:, j, :], mean[:, :], ALU.subtract)
    ps_v = cx.PB.get()
    for j in range(8):
        sq = F5.get()
        kb.act(sq[:, :], h32[:, j, :], AF.Square)
        kb.mm(ps_v, Mf["ones"], sq[:, :], start=(j == 0), stop=(j == 7))
    tmp = F5.get()
    rstd = F5.get()
    rsqrt_act(kb, rstd[:, :], ps_v, 1.0 / 1024, EPS, tmp[:, :])
    for j in range(8):
        o = F5.get()
        kb.tt("dve", o[:, :], h32[:, j, :], rstd[:, :], ALU.mult)
        kb.ts("dve", o[:, :], o[:, :], pcs(gname, j), ALU.mult, pcs(bname, j), ALU.add)
        out_fn(j, o)


def build_p2(kb, T, l, d):
    import os as _os
    if "build_p2" in _os.environ.get("DBG_SKIP", "").split(","):
        return None

    nt = T // TT
    Po = d["P2off"]

    st = contextlib.ExitStack()
    cx = Ctx()
    setup_common(kb, cx, d["masks"], st)
    Wg = kb.sbuf("Wg", [128, 8, 4096], BF16, st)
    for k in range(8):
        for c0 in range(0, 4096, 1024):
            kb.dma("pool", Wg[:, k, c0:c0 + 1024], d["Wgate"][128 * k:128 * k + 128, c0:c0 + 1024])
    Wb = kb.sbuf("Wb", [128, 4, 4, 1024], BF16, st)
    for n in range(4):
        for kc in range(4):
            kb.dma("pool", Wb[:, n, kc, :], d["Wb"][n, 128 * kc:128 * kc + 128, :])
    F5 = Pool(kb, "p2f", 8, [128, TT], F32, stack=st)
    yst = Pool(kb, "p2ys", 12, [128, TT], BF16, stack=st)
    OH = kb.sbuf("p2oh", [128, 4], F32, st)
    kb.dma("sp", OH[:, :], d["onehot"])
    xbfs = [kb.sbuf("p2x%d" % i, [128, 8, TT], BF16, st) for i in range(2)]
    ybfs = [kb.sbuf("p2y%d" % i, [128, 16, TT], BF16, st) for i in range(2)]
    m32 = kb.sbuf("p2m", [128, 8, TT], F32, st)
    mbf = kb.sbuf("p2mb", [128, 8, TT], BF16, st)

    def load_x(t):
        for k in range(8):
            kb.dma("pool", xbfs[t % 2][:, k, :], d["xsrc2"](k, t))
    load_x(0)
    for t in range(nt):
        tsl = slice(t * TT, (t + 1) * TT)
        if t + 1 < nt:
            load_x(t + 1)
        xbf = xbfs[t % 2]
        ybf = ybfs[t % 2]
        for c in range(16):
            n_, hg_ = c // 4, c % 4
            r0 = hg_ * 512 + n_ * 128
            for jq in range(4):
                yq = yst.get()
                kb.dma("sp", yq[:, :], d["ysrc"](r0, jq, t))
                if jq == 0:
                    kb.ts("dve", ybf[:, c, :], yq[:, :], OH[:, 0:1], ALU.mult)
                else:
                    kb.stt(ybf[:, c, :], yq[:, :], OH[:, jq:jq + 1], ybf[:, c, :], ALU.mult, ALU.add)
        for n in range(4):
            for j in range(8):
                ps_g = cx.PB.get()
                for k in range(8):
                    kb.mm(ps_g, Wg[:, k, n * 1024 + 128 * j:n * 1024 + 128 * j + 128], xbf[:, k, :],
                          start=(k == 0), stop=(k == 7))
                sg = F5.get()
                kb.act(sg[:, :], ps_g, AF.Sigmoid)
                ps_p = cx.banks[2 + (j % 2)][:, :]
                for kc in range(4):
                    kb.mm(ps_p, Wb[:, n, kc, 128 * j:128 * j + 128], ybf[:, 4 * n + kc, :],
                          start=(kc == 0), stop=(kc == 3))
                if n == 0:
                    kb.tt("dve", m32[:, j, :], sg[:, :], ps_p, ALU.mult)
                else:
                    tq = F5.get()
                    kb.tt("dve", tq[:, :], sg[:, :], ps_p, ALU.mult)
                    kb.tt("pool", m32[:, j, :], m32[:, j, :], tq[:, :], ALU.add)
        for j in range(8):
            kb.copy("act", mbf[:, j, :], m32[:, j, :])
            kb.dma("sp", d["mT"][128 * j:128 * j + 128, tsl], mbf[:, j, :])
    kb.flush()
    st.close()

    st = contextlib.ExitStack()
    cx = Ctx()
    setup_common(kb, cx, d["masks"], st)
    PC = kb.sbuf("PC2", [128, d["NP2"]], F32, st)
    kb.dma("sp", PC[:, :], d["PC2"])

    def pcs(name, j):
        o, w = Po[name]
        return PC[:, o + j:o + j + 1]
    cst = kb.sbuf("cst2", [128, 2], F32, st)
    kb.memset("pool", cst[:, 0:1], LN_EPS)
    EPS = cst[:, 0:1]
    wst = Pool(kb, "wst3", 3, [128, 512], F32, stack=st)
    Wo = kb.sbuf("Wo", [128, 8, 1024], BF16, st)
    for k in range(8):
        for c0 in range(0, 1024, 512):
            s_ = wst.get()
            kb.dma("sp", s_[:, :], d["Wout"][128 * k:128 * k + 128, c0:c0 + 512])
            kb.copy("pool", Wo[:, k, c0:c0 + 512], s_[:, :])
    F5 = Pool(kb, "p2g", 10, [128, TT], F32, stack=st)
    mbf = kb.sbuf("p2mb2", [128, 8, TT], BF16, st)
    x32 = kb.sbuf("p2x32", [128, 8, TT], F32, st)
    h32 = kb.sbuf("p2h32", [128, 8, TT], F32, st)
    for t in range(nt):
        tsl = slice(t * TT, (t + 1) * TT)
        for k in range(8):
            kb.dma("sp", mbf[:, k, :], d["mT"][128 * k:128 * k + 128, tsl])
            kb.dma("sp", x32[:, k, :], d["xsrc2"](k, t))
        for j in range(8):
            ps = cx.PB.get()
            for k in range(8):
                kb.mm(ps, Wo[:, k, 128 * j:128 * j + 128], mbf[:, k, :], start=(k == 0), stop=(k == 7))
            kb.stt(h32[:, j, :], x32[:, j, :], ALPHA, ps, ALU.mult, ALU.add)

        def out1(j, o, tsl=tsl):
            kb.dma("sp", d["x1T"][128 * j:128 * j + 128, tsl], o[:, :])
        ln_fmajor(kb, cx, h32, "ln1_g", "ln1_b", pcs, F5, EPS, out1)
    kb.flush()
    st.close()

    HT = min(T, 2048)
    for half in range(T // HT):
        st = contextlib.ExitStack()
        cx = Ctx()
        setup_common(kb, cx, d["masks"], st)
        Mf = cx.Mf
        PC = kb.sbuf("PC2b", [128, d["NP2"]], F32, st)
        kb.dma("sp", PC[:, :], d["PC2"])
        PCb = kb.sbuf("PC2bb", [16, 2048], BF16, st)
        o_sel = Po["sel"][0]
        kb.copy("pool", PCb[:, :], PC[0:16, o_sel:o_sel + 2048])

        def pcs(name, j):
            o, w = Po[name]
            return PC[:, o + j:o + j + 1]
        cst = kb.sbuf("cst3", [128, 2], F32, st)
        kb.memset("pool", cst[:, 0:1], LN_EPS)
        EPS = cst[:, 0:1]
        o_rw = Po["rw"][0]
        o_rb = Po["rbias"][0]
        wst = Pool(kb, "wst4", 4, [128, 512], F32, stack=st)
        F5 = Pool(kb, "p2h", 8, [128, TT], F32, stack=st)
        S16 = Pool(kb, "s16", 24, [128, 16], F32, stack=st)
        x1bf = kb.sbuf("x1bf", [128, 8, HT], BF16, st)
        acc = kb.sbuf("acc", [128, 8, HT], F32, st)
        cTb = kb.sbuf("cTb", [16, HT], BF16, st)
        nth = HT // TT
        for t in range(nth):
            g0 = half * HT + t * TT
            rps = [cx.banks[b][:, 0:16] for b in range(4)]
            for k in range(8):
                s_ = wst.get()
                kb.dma("sp", s_[:, :], d["x1T"][128 * k:128 * k + 128, g0:g0 + TT])
                kb.copy("pool", x1bf[:, k, t * TT:(t + 1) * TT], s_[:, :])
                for blk in range(4):
                    kb.mm(rps[blk], s_[:, blk * 128:blk * 128 + 128], PC[:, o_rw + 16 * k:o_rw + 16 * k + 16],
                          start=(k == 0), stop=(k == 7))
            for blk in range(4):
                e = S16.get()
                kb.act(e[:, :], rps[blk], AF.Exp, scale=-1.0)
                kb.ts("dve", e[:, :], e[:, :], 1.0, ALU.add)
                sc = S16.get()
                kb.recip(sc[:, :], e[:, :])
                bi = S16.get()
                kb.tt("dve", bi[:, :], sc[:, :], PC[:, o_rb:o_rb + 16], ALU.add)
                b4 = bi.ap[:, :].rearrange("p (g e) -> p g e", e=4)
                p6 = S16.get()
                p6v = p6.ap[:, :].rearrange("p (g s) -> p g s", s=4)
                p6b = S16.get()
                p6bv = p6b.ap[:, :].rearrange("p (g s) -> p g s", s=4)
                pairs = [(0, 1), (0, 2), (0, 3), (1, 2), (1, 3), (2, 3)]
                for pi, (i_, j_) in enumerate(pairs):
                    dst = p6v[:, :, pi:pi + 1] if pi < 4 else p6bv[:, :, pi - 4:pi - 3]
                    dd = p6 if pi < 4 else p6b
                    kb.tt("dve", V(dst, dd.deps), V(b4[:, :, i_:i_ + 1], bi.deps), V(b4[:, :, j_:j_ + 1], bi.deps), ALU.add)
                gs = S16.get()
                gsv = gs.ap[:, 0:4].rearrange("p (g o) -> p g o", o=1)
                kb.tt("dve", V(gsv, gs.deps), V(p6v[:, :, 0:1], p6.deps), V(p6v[:, :, 1:2], p6.deps), ALU.max)
                kb.tt("dve", V(gsv, gs.deps), V(gsv, gs.deps), V(p6v[:, :, 2:3], p6.deps), ALU.max)
                kb.tt("dve", V(gsv, gs.deps), V(gsv, gs.deps), V(p6v[:, :, 3:4], p6.deps), ALU.max)
                kb.tt("dve", V(gsv, gs.deps), V(gsv, gs.deps), V(p6bv[:, :, 0:1], p6b.deps), ALU.max)
                kb.tt("dve", V(gsv, gs.deps), V(gsv, gs.deps), V(p6bv[:, :, 1:2], p6b.deps), ALU.max)
                gm = S16.get()
                kb.op("dve", (lambda o_, i_: (lambda h: h.tensor_reduce(o_, i_, AX.X, ALU.max)))(gm.ap[:, 0:1], gs.ap[:, 0:4]),
                      reads=[gs], writes=[gm])
                ing = S16.get()
                kb.ts("dve", ing[:, 0:4], gs[:, 0:4], gm[:, 0:1], ALU.is_ge)
                off = S16.get()
                kb.ts("dve", off[:, 0:4], ing[:, 0:4], 1e30, ALU.mult, -1e30, ALU.add)
                mk = S16.get()
                mk4 = mk.ap[:, :].rearrange("p (g e) -> p g e", e=4)
                ing_bc = ing.ap[:, 0:4].rearrange("p (g o) -> p g o", o=1).broadcast_to([128, 4, 4])
                off_bc = off.ap[:, 0:4].rearrange("p (g o) -> p g o", o=1).broadcast_to([128, 4, 4])
                kb.tt("dve", V(mk4, mk.deps), V(b4, bi.deps), V(ing_bc, ing.deps), ALU.mult)
                kb.tt("dve", V(mk4, mk.deps), V(mk4, mk.deps), V(off_bc, off.deps), ALU.add)
                t8 = S16.get()
                kb.op("dve", (lambda o_, i_: (lambda h: h.max(o_, i_)))(t8.ap[:, 0:8], mk.ap[:, :]), reads=[mk], writes=[t8])
                sel = S16.get()
                kb.ts("dve", sel[:, :], mk[:, :], t8[:, 1:2], ALU.is_ge)
                ws = S16.get()
                kb.tt("dve", ws[:, :], sel[:, :], sc[:, :], ALU.mult)
                den = S16.get()
                kb.op("dve", (lambda o_, i_: (lambda h: h.tensor_reduce(o_, i_, AX.X, ALU.add)))(den.ap[:, 0:1], ws.ap[:, :]),
                      reads=[ws], writes=[den])
                kb.recip(den[:, 1:2], den[:, 0:1])
                cmb = S16.get()
                kb.ts("dve", cmb[:, :], ws[:, :], den[:, 1:2], ALU.mult)
                ps_t = cx.banks[4][0:16, 0:128]
                kb.mm(ps_t, cmb[:, :], Mf["ident"])
                kb.copy("act", cTb[:, t * TT + blk * 128:t * TT + blk * 128 + 128], ps_t)
        st2 = contextlib.ExitStack()
        Wgs = [kb.sbuf("Wge%d" % i, [128, 8, 512], BF16, st2) for i in range(2)]
        Wus = [kb.sbuf("Wue%d" % i, [128, 8, 512], BF16, st2) for i in range(2)]
        Wds = [kb.sbuf("Wde%d" % i, [128, 4, 1024], BF16, st2) for i in range(2)]
        cbcs = Pool(kb, "cbc", 2, [128, TT], BF16, stack=st2)
        acts = Pool(kb, "actb", 8, [128, TT], BF16, stack=st2)
        gi = 0

        def load_expert(e):
            Wg_, Wu_, Wd_ = Wgs[e % 2], Wus[e % 2], Wds[e % 2]
            for k in range(8):
                kb.dma("pool", Wg_[:, k, :], d["moe_g"][e, 128 * k:128 * k + 128, :])
                kb.dma("pool", Wu_[:, k, :], d["moe_u"][e, 128 * k:128 * k + 128, :])
            for fc in range(4):
                kb.dma("pool", Wd_[:, fc, :], d["moe_d"][e, 128 * fc:128 * fc + 128, :])
        load_expert(0)
        for e in range(16):
            Wg_, Wu_, Wd_ = Wgs[e % 2], Wus[e % 2], Wds[e % 2]
            if e + 1 < 16:
                load_expert(e + 1)
            for t in range(nth):
                tl = slice(t * TT, (t + 1) * TT)
                ps_c = cx.banks[6][:, :]
                kb.mm(ps_c, PCb[0:16, 128 * e:128 * e + 128], cTb[:, tl])
                cbc = cbcs.get()
                kb.copy("act", cbc[:, :], ps_c)
                at = []
                for fc in range(4):
                    ps_g = cx.banks[gi % 2][:, :]
                    ps_u = cx.banks[2 + gi % 2][:, :]
                    gi += 1
                    for k in range(8):
                        kb.mm(ps_g, Wg_[:, k, 128 * fc:128 * fc + 128], x1bf[:, k, tl], start=(k == 0), stop=(k == 7))
                    for k in range(8):
                        kb.mm(ps_u, Wu_[:, k, 128 * fc:128 * fc + 128], x1bf[:, k, tl], start=(k == 0), stop=(k == 7))
                    r_ = F5.get()
                    kb.act(r_[:, :], ps_g, AF.Silu)
                    kb.tt("dve", r_[:, :], r_[:, :], ps_u, ALU.mult)
                    a_ = acts.get()
                    kb.tt("dve", a_[:, :], r_[:, :], cbc[:, :], ALU.mult)
                    at.append(a_)
                for j in range(8):
                    ps_d = cx.banks[4 + j % 2][:, :]
                    for fc in range(4):
                        kb.mm(ps_d, Wd_[:, fc, 128 * j:128 * j + 128], at[fc][:, :], start=(fc == 0), stop=(fc == 3))
                    if e == 0:
                        kb.copy("dve", acc[:, j, tl], ps_d)
                    else:
                        kb.tt("dve", acc[:, j, tl], acc[:, j, tl], ps_d, ALU.add)
        kb.flush()
        st2.close()
        x32 = kb.sbuf("b_x32", [128, 8, TT], F32, st)
        for t in range(nth):
            g0 = half * HT + t * TT
            tl = slice(t * TT, (t + 1) * TT)
            for k in range(8):
                kb.dma("sp", x32[:, k, :], d["x1T"][128 * k:128 * k + 128, g0:g0 + TT])
                kb.stt(x32[:, k, :], x32[:, k, :], ALPHA, acc[:, k, tl], ALU.mult, ALU.add)

            def out2(j, o, g0=g0):
                kb.dma("sp", d["odst"](j, g0 // TT), o[:, :])
            ln_fmajor(kb, cx, x32, "ln2_g", "ln2_b", pcs, F5, EPS, out2)
        kb.flush()
        st.close()


def _din(nc, name, arr):
    dt = I32 if arr.dtype == np.int32 else F32
    t = nc.dram_tensor(name, list(arr.shape), dt, kind="ExternalInput")
    return V(t.ap(), [Dep()])


def _dout(nc, name, shape, dt=F32):
    t = nc.dram_tensor(name, list(shape), dt, kind="ExternalOutput")
    return V(t.ap(), [Dep()])


def _dint(nc, name, shape, dt):
    t = nc.dram_tensor(name, list(shape), dt)
    return V(t.ap(), [Dep()])


GROUPS = [[0, 1, 2, 3], [4, 5, 6, 7]]


def kernel(**inputs):
    inp = {k: np.asarray(v) for k, v in inputs.items()}
    x = inp["x"]
    B, S = x.shape[0], x.shape[1]
    T = S // 4
    xT = [np.ascontiguousarray(x[b].T) for b in range(B)]
    masks = masks_host()
    scm = scanmask_host()
    packs = [[pack_p1a(inp, l, hg) for hg in range(4)] for l in range(2)]
    arrs = [[(W.build(), P.build(), M.build()) for (W, P, M) in packs[l]] for l in range(2)]
    p2 = [pack_p2(inp, l) for l in range(2)]
    p2a = [p.build() for p in p2]
    posrep = [np.ascontiguousarray(np.repeat(inp["positions"][b][None, :], 96, 0)).astype(np.int32) for b in range(B)]
    common = {"Wgate0": np.ascontiguousarray(inp["w_in"][0][:, O_GATES:O_GATES + 4096]),
              "Wgate1": np.ascontiguousarray(inp["w_in"][1][:, O_GATES:O_GATES + 4096]),
              "Wb": np.ascontiguousarray(inp["w_branch"]), "Wout": np.ascontiguousarray(inp["w_out"]),
              "PC2_0": p2a[0], "PC2_1": p2a[1], "moe_g": np.ascontiguousarray(inp["moe_w_gate"]),
              "moe_u": np.ascontiguousarray(inp["moe_w_up"]), "moe_d": np.ascontiguousarray(inp["moe_w_down"]),
              "masks": masks, "scanmask": scm}
    in_maps = []
    for b in range(B):
        for r in range(4):
            oh = np.zeros((128, 4), np.float32)
            oh[:, r] = 1.0
            m = {"xT": xT[b], "xq": np.ascontiguousarray(xT[b][:, r * T:(r + 1) * T]), "pos": posrep[b], "onehot": oh}
            for l in range(2):
                m["W%d" % l], m["PC%d" % l], m["SM%d" % l] = arrs[l][r]
            m.update(common)
            in_maps.append({"in_" + k: v for k, v in m.items()})

    nc = bass.Bass("TRN2", target_bir_lowering=False)
    kb = KB(nc)
    di = {k[3:]: _din(nc, k, v) for k, v in in_maps[0].items()}
    CW = 1024
    nyc = S // CW
    yTc = _dint(nc, "yTc", [nyc, 512, CW], BF16)
    ygc = _dint(nc, "ygc", [nyc, 2048, CW], BF16)
    ntq = T // TT
    x2c = _dint(nc, "x2c", [ntq, 2, 512, TT], F32)
    xgc = _dint(nc, "xgc", [ntq, 2, 2048, TT], F32)
    vfirst = _dint(nc, "vfirst_s", [128, S], F32)
    oT = _dout(nc, "oT", [1024, T])
    scratch = {"QT": _dint(nc, "QT", [2, 96, S], BF16), "KT": _dint(nc, "KT", [2, 96, S], BF16),
               "VT": _dint(nc, "VT", [128, 2, S // 128, 65], BF16),
               "x1T": _dint(nc, "x1T", [1024, T], F32), "mT": _dint(nc, "mT", [1024, T], BF16)}

    def ydst(row0, nrows, t):
        c, o = divmod(t * TT, CW)
        return yTc[c][row0:row0 + nrows, o:o + TT]

    ygc_c = [V(ygc.ap[c], [Dep()]) for c in range(nyc)]
    xgc_c = [[V(xgc.ap[t_][h_], [Dep()]) for h_ in range(2)] for t_ in range(ntq)]

    def ysrc(r0, jq, t):
        c, o = divmod(jq * T + t * TT, CW)
        return ygc_c[c][r0:r0 + 128, o:o + TT]

    def x2_local(k, t):
        return x2c[t][k // 4][(k % 4) * 128:(k % 4) * 128 + 128, :]

    def x_gathered(k, tt):
        q, tl = divmod(tt, ntq)
        return xgc_c[tl][k // 4][q * 512 + (k % 4) * 128:q * 512 + (k % 4) * 128 + 128, :]

    for l in range(2):
        W0, P0, M0 = packs[l][0]
        d = {"pos": di["pos"], "W": di["W%d" % l], "PC": di["PC%d" % l], "SM": di["SM%d" % l],
             "masks": di["masks"], "scanmask": di["scanmask"], "ydst": ydst, "vfirst": vfirst}
        d.update(scratch)
        d.update(Woff=W0.off, Poff=P0.off, Moff=M0.off, NW=arrs[l][0][0].shape[1], NP=arrs[l][0][1].shape[1],
                 NM=arrs[l][0][2].shape[1])
        if l == 0:
            d["xsrc"] = lambda k, t: di["xT"][128 * k:128 * k + 128, t * TT:(t + 1) * TT]
        else:
            d["xsrc"] = x_gathered
        build_p1a(kb, S, l, d)
        build_p1b(kb, S, d)
        cpq = max(1, T // CW)
        order = [jq * cpq + i for i in range(cpq) for jq in range(4)] if nyc == 4 * cpq else list(range(nyc))
        for c in order:
            kb.coll("AllGather", ygc_c[c], yTc[c], GROUPS)
        d2 = {"ysrc": ysrc, "onehot": di["onehot"], "Wgate": di["Wgate%d" % l], "Wb": di["Wb"][l], "Wout": di["Wout"][l],
              "PC2": di["PC2_%d" % l], "moe_g": di["moe_g"][l], "moe_u": di["moe_u"][l], "moe_d": di["moe_d"][l],
              "masks": di["masks"], "x1T": scratch["x1T"], "mT": scratch["mT"],
              "P2off": p2[l].off, "NP2": p2a[l].shape[1]}
        if l == 0:
            d2["xsrc2"] = lambda k, t: di["xq"][128 * k:128 * k + 128, t * TT:(t + 1) * TT]
            d2["odst"] = lambda j, t: x2_local(j, t)
        else:
            d2["xsrc2"] = x2_local
            d2["odst"] = lambda j, t: oT[128 * j:128 * j + 128, t * TT:(t + 1) * TT]
        build_p2(kb, T, l, d2)
        if l == 0:
            for t_ in range(ntq):
                for h_ in range(2):
                    kb.coll("AllGather", xgc_c[t_][h_], x2c[t_][h_], GROUPS)
    kb.flush()
    kb.close()
    res = run_bass_kernel_spmd(nc, in_maps, core_ids=list(range(len(in_maps))))
    out = np.empty((B, S, 1024), np.float32)
    for b in range(B):
        for q in range(4):
            out[b, q * T:(q + 1) * T, :] = res.results[b * 4 + q]["oT"].T
    return out
```
